# Optimizing a Trainium2 kernel written in Bass

```python
import jax, jax.numpy as jnp
from jax import lax
import numpy as np

D_MODEL = 2048
BATCH = 4
SEQ = 4096
DEPTH = 2

GRID_W = 64
CTX_LEN = 256
HEAD_DIM = 128
ROPE_THETA = 10000.0
Q_BLOCK = 128
EPS = 1e-6
NEG_INF = -1e30
A_HEADS = 8
A_KV_HEADS = 2
M_HEADS = 4
M_CHUNK = 128
M_CONV_W = 3
C_HEADS = 4
C_KV_HEADS = 2
WINDOW = 128
N_GROUPS = 4
EXPERTS_PER_GROUP = 4
N_EXPERTS = N_GROUPS * EXPERTS_PER_GROUP
TOP_K = 2
D_FF_EXPERT = 512

A_WIDTH = A_HEADS * HEAD_DIM
A_KV_WIDTH = A_KV_HEADS * HEAD_DIM
M_WIDTH = M_HEADS * HEAD_DIM
C_WIDTH = C_HEADS * HEAD_DIM
C_KV_WIDTH = C_KV_HEADS * HEAD_DIM
SPLIT_WIDTHS = (A_WIDTH, A_KV_WIDTH, A_KV_WIDTH,
                M_WIDTH, M_WIDTH, M_WIDTH, M_WIDTH, 4 * M_HEADS,
                C_WIDTH, C_KV_WIDTH, C_KV_WIDTH,
                3 * D_MODEL)
P_IN = sum(SPLIT_WIDTHS)
SPLIT_IDX = tuple(int(v) for v in np.cumsum(SPLIT_WIDTHS)[:-1])

kernel_name = 'hybrid_gqa_mlstm_swa_hmoe_prefix_dit'

f32 = jnp.float32


def rms_norm(x, g):
    xf = x.astype(f32)
    y = xf * lax.rsqrt(jnp.mean(xf * xf, axis=-1, keepdims=True) + EPS)
    return (y * g.astype(f32)).astype(x.dtype)


def _modulate(x, g, shift, scale):
    return rms_norm(x, g) * (1 + scale) + shift


def _heads(a, n_heads):
    B, T, _ = a.shape
    return a.reshape(B, T, n_heads, HEAD_DIM).transpose(0, 2, 1, 3)


def _merge_heads(a):
    B, H, T, d = a.shape
    return a.transpose(0, 2, 1, 3).reshape(B, T, H * d)


def axial_rope_tables(n_tokens):
    n_rows = n_tokens // GRID_W
    rows, cols = jnp.meshgrid(jnp.arange(n_rows), jnp.arange(GRID_W), indexing='ij')
    rows = rows.reshape(-1).astype(f32)
    cols = cols.reshape(-1).astype(f32)
    axis_dim = HEAD_DIM // 2
    inv_freq = ROPE_THETA ** (-jnp.arange(0, axis_dim, 2, dtype=f32) / axis_dim)
    ang = jnp.concatenate([rows[:, None] * inv_freq, cols[:, None] * inv_freq], axis=-1)
    return jnp.cos(ang), jnp.sin(ang)


def apply_axial_rope(x, cos, sin):
    *lead, T, d = x.shape
    xf = x.astype(f32).reshape(*lead, T, 2, 2, d // 4)
    c = cos.reshape(T, 2, d // 4)
    s = sin.reshape(T, 2, d // 4)
    x1, x2 = xf[..., 0, :], xf[..., 1, :]
    out = jnp.stack([x1 * c - x2 * s, x2 * c + x1 * s], axis=-2)
    return out.reshape(*lead, T, d).astype(x.dtype)


def centred_dwconv(x, w):
    K, C = w.shape
    return lax.conv_general_dilated(x, w.astype(x.dtype)[:, None, :], window_strides=(1,),
                                    padding=[(K // 2, K // 2)],
                                    dimension_numbers=('NWC', 'WIO', 'NWC'),
                                    feature_group_count=C)


def _attend_dense(q, k, v):
    s = jnp.einsum('bkgqd,bksd->bkgqs', q, k, preferred_element_type=f32) * (HEAD_DIM ** -0.5)
    p = jax.nn.softmax(s, axis=-1).astype(v.dtype)
    return jnp.einsum('bkgqs,bksd->bkgqd', p, v)


def global_gqa_latent(q, k_all, v_all):
    B, Hq, T, d = q.shape
    G = Hq // A_KV_HEADS
    nb = T // Q_BLOCK
    qb = q.reshape(B, A_KV_HEADS, G, nb, Q_BLOCK, d).transpose(3, 0, 1, 2, 4, 5)
    ob = lax.map(lambda qq: _attend_dense(qq, k_all, v_all), qb)
    return ob.transpose(1, 2, 3, 0, 4, 5).reshape(B, Hq, T, d)


def global_gqa_context(q, k, v):
    B, Hq, Tc, d = q.shape
    G = Hq // A_KV_HEADS
    o = _attend_dense(q.reshape(B, A_KV_HEADS, G, Tc, d), k, v)
    return o.reshape(B, Hq, Tc, d)


def _sink_probs(s, sink):
    m = jnp.maximum(jnp.max(s, axis=-1, keepdims=True), sink)
    p = jnp.exp(s - m)
    return p / (jnp.sum(p, axis=-1, keepdims=True) + jnp.exp(sink - m))


def window_gqa_latent(q, k, v, k_ctx, v_ctx, sink):
    B, Hq, T, d = q.shape
    Hkv = C_KV_HEADS
    G = Hq // Hkv
    nb = T // Q_BLOCK
    ns = -(-WINDOW // Q_BLOCK)
    nband = 2 * ns + 1
    L = nband * Q_BLOCK

    def band(a):
        ap = jnp.pad(a, ((0, 0), (0, 0), (ns * Q_BLOCK, ns * Q_BLOCK), (0, 0)))
        ap = ap.reshape(B, Hkv, nb + 2 * ns, Q_BLOCK, d)
        return jnp.concatenate([ap[:, :, j:j + nb] for j in range(nband)], axis=3)

    kb, vb = band(k), band(v)
    qb = q.reshape(B, Hkv, G, nb, Q_BLOCK, d)
    scale = HEAD_DIM ** -0.5
    s_loc = jnp.einsum('bkgnqd,bknsd->bkgnqs', qb, kb, preferred_element_type=f32) * scale
    qpos = jnp.arange(nb)[:, None] * Q_BLOCK + jnp.arange(Q_BLOCK)[None, :]
    kpos = jnp.arange(nb)[:, None] * Q_BLOCK + jnp.arange(L)[None, :] - ns * Q_BLOCK
    valid = ((jnp.abs(qpos[:, :, None] - kpos[:, None, :]) <= WINDOW)
             & (kpos[:, None, :] >= 0) & (kpos[:, None, :] < T))
    s_loc = jnp.where(valid, s_loc, NEG_INF)
    s_ctx = jnp.einsum('bkgnqd,bksd->bkgnqs', qb, k_ctx, preferred_element_type=f32) * scale
    s = jnp.concatenate([s_loc, s_ctx], axis=-1)
    p = _sink_probs(s, sink.astype(f32).reshape(1, Hkv, G, 1, 1, 1)).astype(v.dtype)
    o = (jnp.einsum('bkgnqs,bknsd->bkgnqd', p[..., :L], vb)
         + jnp.einsum('bkgnqs,bksd->bkgnqd', p[..., L:], v_ctx))
    return o.reshape(B, Hq, T, d)


def window_gqa_context(q, k, v, sink):
    B, Hq, Tc, d = q.shape
    G = Hq // C_KV_HEADS
    qg = q.reshape(B, C_KV_HEADS, G, Tc, d)
    s = jnp.einsum('bkgqd,bksd->bkgqs', qg, k, preferred_element_type=f32) * (HEAD_DIM ** -0.5)
    p = _sink_probs(s, sink.astype(f32).reshape(1, C_KV_HEADS, G, 1, 1)).astype(v.dtype)
    return jnp.einsum('bkgqs,bksd->bkgqd', p, v).reshape(B, Hq, Tc, d)


def _mlstm_zero_state(B):
    return (jnp.zeros((B, M_HEADS, HEAD_DIM, HEAD_DIM), f32),
            jnp.zeros((B, M_HEADS, HEAD_DIM), f32),
            jnp.full((B, M_HEADS), NEG_INF, f32))


def mlstm_chunk_scan(q, k, v, ig, lf, state):
    B, H, T, d = q.shape
    nc = T // M_CHUNK

    def chunks(a):
        return jnp.moveaxis(a.reshape(B, H, nc, M_CHUNK, *a.shape[3:]), 2, 0)

    tri = jnp.tril(jnp.ones((M_CHUNK, M_CHUNK), bool))

    def step(carry, xs):
        C, n, m = carry
        qc, kc, vc, ic, fc = xs
        b = jnp.cumsum(fc, axis=-1)
        log_d = jnp.where(tri, b[..., :, None] - b[..., None, :] + ic[..., None, :], -jnp.inf)
        log_inter = b + m[..., None]
        m_row = jnp.maximum(log_inter, jnp.max(log_d, axis=-1))
        w_intra = jnp.exp(log_d - m_row[..., None]) * jnp.einsum('bhjd,bhsd->bhjs', qc, kc)
        w_inter = jnp.exp(log_inter - m_row)
        num = (w_inter[..., None] * jnp.einsum('bhjd,bhde->bhje', qc, C)
               + jnp.einsum('bhjs,bhse->bhje', w_intra, vc))
        den = w_inter * jnp.einsum('bhjd,bhd->bhj', qc, n) + jnp.sum(w_intra, axis=-1)
        h = num / jnp.maximum(jnp.abs(den), jnp.exp(-m_row))[..., None]
        b_last = b[..., -1]
        log_w = b_last[..., None] - b + ic
        m_new = jnp.maximum(b_last + m, jnp.max(log_w, axis=-1))
        w_s = jnp.exp(log_w - m_new[..., None])
        decay = jnp.exp(b_last + m - m_new)
        C = decay[..., None, None] * C + jnp.einsum('bhs,bhsd,bhse->bhde', w_s, kc, vc)
        n = decay[..., None] * n + jnp.einsum('bhs,bhsd->bhd', w_s, kc)
        return (C, n, m_new), h

    state, hs = lax.scan(step, state, (chunks(q), chunks(k), chunks(v), chunks(ig), chunks(lf)))
    return jnp.moveaxis(hs, 0, 2).reshape(B, H, T, d), state


def _mlstm_prep(mq, mk, mv, mg, conv_w, ig_b, fg_b):
    qk = jax.nn.silu(centred_dwconv(jnp.concatenate([mq, mk], axis=-1), conv_w))
    q, k = jnp.split(qk, 2, axis=-1)
    q = _heads(q, M_HEADS).astype(f32)
    k = _heads(k, M_HEADS).astype(f32) * (HEAD_DIM ** -0.5)
    v = _heads(mv, M_HEADS).astype(f32)
    B, T, _ = mg.shape
    g = mg.astype(f32).reshape(B, T, 2, 2, M_HEADS).transpose(0, 2, 3, 4, 1)
    ig = g[:, 0] + ig_b.astype(f32)[None, :, :, None]
    lf = jax.nn.log_sigmoid(g[:, 1] + fg_b.astype(f32)[None, :, :, None])
    return q, k, v, ig, lf


def mlstm_bidir(lat, ctx):
    B = lat[0].shape[0]
    h_lat, h_ctx = 0.0, 0.0
    for direction in range(2):
        flip = (lambda a: jnp.flip(a, axis=2)) if direction else (lambda a: a)
        qc, kc, vc, ic, fc = ctx
        hc, st = mlstm_chunk_scan(flip(qc), flip(kc), flip(vc), flip(ic[:, direction]), flip(fc[:, direction]),
                                  _mlstm_zero_state(B))
        ql, kl, vl, il, fl = lat
        hl, _ = mlstm_chunk_scan(flip(ql), flip(kl), flip(vl), flip(il[:, direction]), flip(fl[:, direction]), st)
        h_lat = h_lat + flip(hl)
        h_ctx = h_ctx + flip(hc)
    return h_lat, h_ctx


def _mlstm_out(h, mo, norm_g):
    h = rms_norm(h, norm_g.reshape(M_HEADS, 1, HEAD_DIM))
    return _merge_heads(h).astype(mo.dtype) * jax.nn.sigmoid(mo)


def mixer_sublayer(h_l, h_c, w_in, a_qn_g, a_kn_g, m_conv, m_ig_b, m_fg_b, m_norm_g, c_sink,
                   w_br_a, w_br_m, w_br_c, w_out, need_ctx_out):
    T = h_l.shape[1]
    cos, sin = axial_rope_tables(T)
    (aq_l, ak_l, av_l, mq_l, mk_l, mv_l, mo_l, mg_l, cq_l, ck_l, cv_l, g_l) = jnp.split(h_l @ w_in, SPLIT_IDX, axis=-1)
    (aq_c, ak_c, av_c, mq_c, mk_c, mv_c, mo_c, mg_c, cq_c, ck_c, cv_c, g_c) = jnp.split(h_c @ w_in, SPLIT_IDX, axis=-1)

    aq = apply_axial_rope(rms_norm(_heads(aq_l, A_HEADS), a_qn_g), cos, sin)
    ak = apply_axial_rope(rms_norm(_heads(ak_l, A_KV_HEADS), a_kn_g), cos, sin)
    av = _heads(av_l, A_KV_HEADS)
    akc = rms_norm(_heads(ak_c, A_KV_HEADS), a_kn_g)
    avc = _heads(av_c, A_KV_HEADS)
    oa_l = global_gqa_latent(aq, jnp.concatenate([ak, akc], axis=2), jnp.concatenate([av, avc], axis=2))

    lat_in = _mlstm_prep(mq_l, mk_l, mv_l, mg_l, m_conv, m_ig_b, m_fg_b)
    ctx_in = _mlstm_prep(mq_c, mk_c, mv_c, mg_c, m_conv, m_ig_b, m_fg_b)
    hm_l, hm_c = mlstm_bidir(lat_in, ctx_in)
    om_l = _mlstm_out(hm_l, mo_l, m_norm_g)

    cq = apply_axial_rope(_heads(cq_l, C_HEADS), cos, sin)
    ck = apply_axial_rope(_heads(ck_l, C_KV_HEADS), cos, sin)
    cv = _heads(cv_l, C_KV_HEADS)
    ckc = _heads(ck_c, C_KV_HEADS)
    cvc = _heads(cv_c, C_KV_HEADS)
    oc_l = window_gqa_latent(cq, ck, cv, ckc, cvc, c_sink)

    def merge(oa, om, oc, g):
        ga, gm, gc = jnp.split(jax.nn.sigmoid(g), 3, axis=-1)
        y = ga * (_merge_heads(oa) @ w_br_a) + gm * (om @ w_br_m) + gc * (_merge_heads(oc) @ w_br_c)
        return y @ w_out

    y_l = merge(oa_l, om_l, oc_l, g_l)
    if not need_ctx_out:
        return y_l, None
    oa_c = global_gqa_context(rms_norm(_heads(aq_c, A_HEADS), a_qn_g), akc, avc)
    om_c = _mlstm_out(hm_c, mo_c, m_norm_g)
    oc_c = window_gqa_context(_heads(cq_c, C_HEADS), ckc, cvc, c_sink)
    return y_l, merge(oa_c, om_c, oc_c, g_c)


def hier_moe(h, w_rg, b_rg, w_re, b_re, w_gate, w_up, w_down):
    shape = h.shape
    hf = h.reshape(-1, shape[-1])
    N = hf.shape[0]
    g_logits = (hf @ w_rg).astype(f32) + b_rg.astype(f32)
    g_prob = jax.nn.softmax(g_logits, axis=-1)
    g_sel = jnp.argmax(g_logits, axis=-1)
    p_g = jnp.take_along_axis(g_prob, g_sel[:, None], axis=-1)[:, 0]
    e_logits = ((hf @ w_re).astype(f32) + b_re.astype(f32)).reshape(N, N_GROUPS, EXPERTS_PER_GROUP)
    e_in = jnp.take_along_axis(e_logits, g_sel[:, None, None], axis=1)[:, 0]
    top_p, top_i = lax.top_k(jax.nn.softmax(e_in, axis=-1), TOP_K)
    top_p = top_p / jnp.sum(top_p, axis=-1, keepdims=True)
    expert_ids = g_sel[:, None] * EXPERTS_PER_GROUP + top_i
    weights = p_g[:, None] * top_p
    combine = jnp.sum(jax.nn.one_hot(expert_ids, N_EXPERTS, dtype=f32) * weights[..., None], axis=1)
    a = jnp.einsum('nd,edf->nef', hf, w_gate)
    u = jnp.einsum('nd,edf->nef', hf, w_up)
    mid = jax.nn.silu(a) * u * combine.astype(hf.dtype)[..., None]
    return jnp.einsum('nef,efd->nd', mid, w_down).reshape(shape)


def setup_inputs(seed: int = 0) -> dict:
    key = jax.random.key(seed)
    ks = jax.random.split(key, 32)
    D = D_MODEL

    def nrm(k, shape, scale):
        return jax.random.normal(k, shape, f32) * scale

    return {
        'x': nrm(ks[0], (BATCH, SEQ, D), 1.0),
        'c': nrm(ks[1], (BATCH, D), 1.0),
        'ctx': nrm(ks[2], (BATCH, CTX_LEN, D), 1.0),
        'c_ctx': nrm(ks[3], (D,), 1.0),
        'norm1_g': 1.0 + nrm(ks[4], (DEPTH, D), 0.05),
        'norm2_g': 1.0 + nrm(ks[5], (DEPTH, D), 0.05),
        'w_mod': nrm(ks[6], (DEPTH, D, 6 * D), 0.5 * D ** -0.5),
        'b_mod': nrm(ks[7], (DEPTH, 6 * D), 0.02),
        'w_in': nrm(ks[8], (DEPTH, D, P_IN), D ** -0.5),
        'a_qn_g': 1.0 + nrm(ks[9], (DEPTH, HEAD_DIM), 0.05),
        'a_kn_g': 1.0 + nrm(ks[10], (DEPTH, HEAD_DIM), 0.05),
        'm_conv': nrm(ks[11], (DEPTH, M_CONV_W, 2 * M_WIDTH), M_CONV_W ** -0.5),
        'm_ig_b': nrm(ks[12], (DEPTH, 2, M_HEADS), 0.1),
        'm_fg_b': 3.0 + nrm(ks[13], (DEPTH, 2, M_HEADS), 0.5),
        'm_norm_g': 1.0 + nrm(ks[14], (DEPTH, M_WIDTH), 0.05),
        'c_sink': nrm(ks[15], (DEPTH, C_HEADS), 0.5),
        'w_br_a': nrm(ks[16], (DEPTH, A_WIDTH, D), A_WIDTH ** -0.5),
        'w_br_m': nrm(ks[17], (DEPTH, M_WIDTH, D), M_WIDTH ** -0.5),
        'w_br_c': nrm(ks[18], (DEPTH, C_WIDTH, D), C_WIDTH ** -0.5),
        'w_out': nrm(ks[19], (DEPTH, D, D), D ** -0.5),
        'w_rg': nrm(ks[20], (DEPTH, D, N_GROUPS), D ** -0.5),
        'b_rg': nrm(ks[21], (DEPTH, N_GROUPS), 0.01),
        'w_re': nrm(ks[22], (DEPTH, D, N_EXPERTS), D ** -0.5),
        'b_re': nrm(ks[23], (DEPTH, N_EXPERTS), 0.01),
        'w_gate': nrm(ks[24], (DEPTH, N_EXPERTS, D, D_FF_EXPERT), D ** -0.5),
        'w_up': nrm(ks[25], (DEPTH, N_EXPERTS, D, D_FF_EXPERT), D ** -0.5),
        'w_down': nrm(ks[26], (DEPTH, N_EXPERTS, D_FF_EXPERT, D), D_FF_EXPERT ** -0.5),
        'final_g': 1.0 + nrm(ks[27], (D,), 0.05),
    }


def reference(x, c, ctx, c_ctx, norm1_g, norm2_g, w_mod, b_mod, w_in, a_qn_g, a_kn_g, m_conv, m_ig_b, m_fg_b,
              m_norm_g, c_sink, w_br_a, w_br_m, w_br_c, w_out, w_rg, b_rg, w_re, b_re, w_gate, w_up, w_down,
              final_g):
    s_lat = jax.nn.silu(c)
    s_ctx = jax.nn.silu(c_ctx)
    xl, xc = x, ctx
    for l in range(DEPTH):
        need_ctx_out = l < DEPTH - 1
        mod_l = (s_lat @ w_mod[l] + b_mod[l])[:, None, :]
        mod_c = (s_ctx @ w_mod[l] + b_mod[l])[None, None, :]
        sh1, sc1, g1, sh2, sc2, g2 = jnp.split(mod_l, 6, axis=-1)
        sh1c, sc1c, g1c, sh2c, sc2c, g2c = jnp.split(mod_c, 6, axis=-1)
        hl = _modulate(xl, norm1_g[l], sh1, sc1)
        hc = _modulate(xc, norm1_g[l], sh1c, sc1c)
        y_l, y_c = mixer_sublayer(hl, hc, w_in[l], a_qn_g[l], a_kn_g[l], m_conv[l], m_ig_b[l], m_fg_b[l],
                                  m_norm_g[l], c_sink[l], w_br_a[l], w_br_m[l], w_br_c[l], w_out[l], need_ctx_out)
        xl = xl + g1 * y_l
        xl = xl + g2 * hier_moe(_modulate(xl, norm2_g[l], sh2, sc2), w_rg[l], b_rg[l], w_re[l], b_re[l],
                                w_gate[l], w_up[l], w_down[l])
        if need_ctx_out:
            xc = xc + g1c * y_c
            xc = xc + g2c * hier_moe(_modulate(xc, norm2_g[l], sh2c, sc2c), w_rg[l], b_rg[l], w_re[l], b_re[l],
                                     w_gate[l], w_up[l], w_down[l])
    return rms_norm(xl, final_g)
```

```python
from contextlib import ExitStack
import numpy as np
import concourse.bass as bass
import concourse.mybir as mybir
from concourse.bass_utils import run_bass_kernel_spmd

F32 = mybir.dt.float32
BF16 = mybir.dt.bfloat16
AF = mybir.ActivationFunctionType
ALU = mybir.AluOpType
AX = mybir.AxisListType

D = 2048
NK = D // 128
DEPTH = 2
EPS = 1e-6
NEG = -30000.0

ENGS = ("pe", "act", "dve", "pool", "sp")
RING = 12


class Buf:
    __slots__ = ("w", "r")

    def __init__(self):
        self.w = None
        self.r = {}


def bufs(n):
    return [Buf() for _ in range(n)]


class Fw:
    def __init__(self, nc):
        self.nc = nc
        self.e = dict(pe=nc.tensor, act=nc.scalar, dve=nc.vector, pool=nc.gpsimd, sp=nc.sync)
        self.semh = {}
        for k in ENGS:
            self.semh["e_" + k] = nc.alloc_semaphore("s_" + k)
        self.cnt = {k: 0 for k in ENGS}
        self.known = {k: {} for k in ENGS}
        self.rings = {}
        self.ring_n = {}
        self.ring_val = {}
        for q in ("sp", "pool", "act"):
            self.rings[q] = []
            self.ring_n[q] = 0
            for i in range(RING):
                key = "r_%s_%d" % (q, i)
                self.semh[key] = nc.alloc_semaphore(key)
                self.rings[q].append(key)
                self.ring_val[key] = 0

    def _wait(self, eng, dep):
        key, val, _ = dep
        if val <= 0 or self.known[eng].get(key, 0) >= val:
            return
        self.e[eng].wait_ge(self.semh[key], val)
        self.known[eng][key] = val

    def _sync(self, eng, issuer, reads, writes):
        for b in reads:
            if b.w is not None:
                self._wait(issuer, b.w)
        for b in writes:
            if b.w is not None and b.w[2] != eng:
                self._wait(issuer, b.w)
            for key, (val, pe) in b.r.items():
                if pe != eng:
                    self._wait(issuer, (key, val, pe))

    @staticmethod
    def _mark(dep, reads, writes):
        for b in reads:
            b.r[dep[0]] = (dep[1], dep[2])
        for b in writes:
            b.w = dep
            b.r = {}

    def op(self, eng, fn, reads=(), writes=()):
        self._sync(eng, eng, reads, writes)
        ins = fn(self.e[eng])
        self.cnt[eng] += 1
        ins.then_inc(self.semh["e_" + eng], 1)
        dep = ("e_" + eng, self.cnt[eng], eng)
        self._mark(dep, reads, writes)
        return dep

    def dma(self, q, out, in_, reads=(), writes=(), **kw):
        self._sync("dma", q, reads, writes)
        n = self.ring_n[q]
        self.ring_n[q] = n + 1
        key = self.rings[q][n % RING]
        prev = self.ring_val[key]
        self._wait(q, (key, prev, "dma"))
        self.e[q].dma_start(out=out, in_=in_, **kw).then_inc(self.semh[key], 16)
        self.ring_val[key] = prev + 16
        dep = (key, prev + 16, "dma")
        self._mark(dep, reads, writes)
        return dep

    def all_reduce(self, src, dst, groups, reads=(), writes=()):
        q = "pool"
        self._sync("dma", q, reads, writes)
        if "cc" not in self.semh:
            self.semh["cc"] = self.nc.alloc_semaphore("cc_sem")
            self.ring_val["cc"] = 0
        prev = self.ring_val["cc"]
        self._wait(q, ("cc", prev, "dma"))
        self.e[q].collective_compute("AllReduce", ALU.add, replica_groups=groups, ins=[src], outs=[dst]).then_inc(self.semh["cc"])
        self.ring_val["cc"] = prev + 1
        dep = ("cc", prev + 1, "dma")
        self._mark(dep, reads, writes)
        return dep

    def barrier(self):
        for k in ENGS:
            if k != "sp":
                self._wait("sp", ("e_" + k, self.cnt[k], k))
        for key, val in self.ring_val.items():
            self._wait("sp", (key, val, "dma"))
        self.e["sp"].sem_inc(self.semh["e_sp"], 1)
        self.cnt["sp"] += 1
        for k in ENGS:
            if k != "sp":
                self._wait(k, ("e_sp", self.cnt["sp"], "sp"))
        for k in ENGS:
            for kk in ENGS:
                self.known[k]["e_" + kk] = self.cnt[kk]
            for key, val in self.ring_val.items():
                self.known[k][key] = val


class Phase:
    def __init__(self, fw, name):
        self.fw = fw
        self.nc = fw.nc
        fw.nphase = getattr(fw, "nphase", 0) + 1
        self.name = "%s%d" % (name, fw.nphase)
        self.es = ExitStack()
        self.i = 0

    def __enter__(self):
        self.es.__enter__()
        return self

    def __exit__(self, *a):
        self.fw.barrier()
        return self.es.__exit__(*a)

    def sb(self, shape, dt):
        self.i += 1
        return self.es.enter_context(self.nc.sbuf_tensor("%s_s%d" % (self.name, self.i), list(shape), dt))

    def ps(self, shape, dt=F32):
        self.i += 1
        return self.es.enter_context(self.nc.psum_tensor("%s_p%d" % (self.name, self.i), list(shape), dt))


def make_ident(fw, t, n=128):
    b = Buf()
    fw.op("pool", lambda e: e.memset(t[:, :], 0.0), writes=[b])
    fw.op("pool", lambda e: e.affine_select(out=t[:, :], in_=t[:, :], pattern=[[-1, n]],
                                            compare_op=ALU.not_equal, fill=1.0, base=0,
                                            channel_multiplier=1), reads=[b], writes=[b])
    return b


C_AQ, C_AKV, C_MQ, C_MK, C_MV, C_MO, C_CQ, C_CKV, C_G, C_MG = 0, 1024, 1536, 2048, 2560, 3072, 3584, 4096, 4608, 10752
P_IN = 10768


class Cfg:
    def __init__(self, nt_own=16, nt_ctx=2):
        self.nt_own = nt_own
        self.nt_oth = nt_own
        self.nt_ctx = nt_ctx
        self.T_OWN = 128 * nt_own
        self.T_CTX = 128 * nt_ctx
        self.T_ALL = 2 * self.T_OWN + self.T_CTX
        self.O_OWN, self.O_OTH, self.O_CTX = 0, self.T_OWN, 2 * self.T_OWN


def phase_mods(fw, cfg, cvec, w_mod_l, bmod2_l, mods_l):
    nc = fw.nc
    with Phase(fw, "mod") as ph:
        cc = ph.sb([128, NK * 2], F32)
        S = ph.sb([128, NK * 2], BF16)
        bm = ph.sb([2, 6 * D], F32)
        out = ph.sb([2, 6 * D], F32)
        W = [ph.sb([128, NK, 512], BF16) for _ in range(2)]
        pp = [ph.ps([128, 512]) for _ in range(2)]
        b_cc, b_S, b_bm, b_out = bufs(4)
        b_W = bufs(2)
        b_pp = bufs(2)
        fw.dma("sp", cc[:, :], cvec, writes=[b_cc])
        fw.dma("sp", bm[:, :], bmod2_l, writes=[b_bm])
        fw.op("act", lambda e: e.activation(out=S[:, :], in_=cc[:, :], func=AF.Silu), reads=[b_cc], writes=[b_S])
        wv = w_mod_l.rearrange("(k p) c -> p k c", p=128)
        nblk = 6 * D // 512
        for j in range(nblk):
            s = j % 2
            fw.dma("pool", W[s][:, :, :], wv[:, :, j * 512:(j + 1) * 512], writes=[b_W[s]])

            def mm(e, s=s):
                for k in range(NK):
                    ins = e.matmul(pp[s][0:2, :], lhsT=S[:, 2 * k:2 * k + 2], rhs=W[s][:, k, :],
                                   start=(k == 0), stop=(k == NK - 1))
                return ins
            fw.op("pe", mm, reads=[b_S, b_W[s]], writes=[b_pp[s]])
            fw.op("dve", lambda e, s=s, j=j: e.tensor_tensor(out=out[:, j * 512:(j + 1) * 512], in0=pp[s][0:2, :],
                                                              in1=bm[:, j * 512:(j + 1) * 512], op=ALU.add),
                  reads=[b_pp[s], b_bm], writes=[b_out])
        fw.dma("sp", mods_l, out[:, :], reads=[b_out])


def load_bc(fw, q, dst, src_row, wb):
    return fw.dma(q, dst, src_row.partition_broadcast(128), writes=[wb])


def phase_inproj(fw, cfg, l, x_own, x_oth, xc, mods_l, g1row, w_in_l, S):
    nc = fw.nc
    T_OC = cfg.T_OWN + cfg.T_CTX
    passes = [
        ("oc", [(x_own, i, 0) for i in range(cfg.nt_own)] + [(xc, i, 1) for i in range(cfg.nt_ctx)]),
        ("oth", [(x_oth, i, 0) for i in range(cfg.nt_oth)]),
    ]
    with Phase(fw, "inp") as ph:
        ident = ph.sb([128, 128], BF16)
        b_id = make_ident(fw, ident)
        gain = [ph.sb([128, D], F32) for _ in range(2)]
        shift = [ph.sb([128, D], F32) for _ in range(2)]
        tmpg = ph.sb([128, D], F32)
        b_gain, b_shift = bufs(2), bufs(2)
        b_tmp = Buf()
        load_bc(fw, "sp", tmpg[:, :], g1row, b_tmp)
        for m in range(2):
            load_bc(fw, "sp", shift[m][:, :], mods_l[m:m + 1, 0:D], b_shift[m])
            load_bc(fw, "sp", gain[m][:, :], mods_l[m:m + 1, D:2 * D], b_gain[m])
            fw.op("dve", lambda e, m=m: e.scalar_tensor_tensor(out=gain[m][:, :], in0=gain[m][:, :], scalar=1.0,
                                                                in1=tmpg[:, :], op0=ALU.add, op1=ALU.mult),
                  reads=[b_gain[m], b_tmp], writes=[b_gain[m]])
        hT = ph.sb([128, NK, T_OC], BF16)
        xt = [ph.sb([128, D], F32)] * 2
        b_xt = [Buf()] * 2
        junk = ph.sb([128, D], BF16)
        b_junk = Buf()
        y32 = ph.sb([128, D], F32)
        b_y32 = Buf()
        hb = [ph.sb([128, D], BF16)] * 2
        b_hb = [Buf()] * 2
        ss = [ph.sb([128, 1], F32) for _ in range(2)]
        b_ss = bufs(2)
        ptr = [ph.ps([128, 512], BF16) for _ in range(2)]
        b_ptr = bufs(2)
        pmm = [ph.ps([128, 512]) for _ in range(4)]
        b_pmm = bufs(4)
        W = [ph.sb([128, NK, 512], BF16) for _ in range(2)]
        b_W = bufs(2)
        stage = [ph.sb([128, T_OC // 128, 512], BF16) for _ in range(2)]
        b_stage = bufs(2)
        stg32 = ph.sb([128, T_OC // 128, 16], F32)
        b_stg32 = Buf()
        wv = w_in_l.rearrange("(k p) c -> p k c", p=128)
        wi = 0
        ti = 0
        pi = 0
        for pname, tiles in passes:
            nt = len(tiles)
            b_hT = bufs(nt)
            for t, (src, i, m) in enumerate(tiles):
                s = ti % 2
                ti += 1
                fw.dma("sp", xt[s][:, :], src[i * 128:(i + 1) * 128, :], writes=[b_xt[s]])
                fw.op("pool", lambda e, s=s: e.memset(ss[s][:, :], 0.0), writes=[b_ss[s]])
                fw.op("act", lambda e, s=s: e.activation(out=junk[:, :], in_=xt[s][:, :], func=AF.Square,
                                                         accum_out=ss[s][:, 0:1]),
                      reads=[b_xt[s], b_ss[s]], writes=[b_junk, b_ss[s]])
                fw.op("dve", lambda e, s=s: e.tensor_scalar(out=ss[s][:, :], in0=ss[s][:, :], scalar1=1.0 / D,
                                                            scalar2=EPS, op0=ALU.mult, op1=ALU.add),
                      reads=[b_ss[s]], writes=[b_ss[s]])
                fw.op("act", lambda e, s=s: e.sqrt(out=ss[s][:, :], in_=ss[s][:, :]), reads=[b_ss[s]], writes=[b_ss[s]])
                fw.op("dve", lambda e, s=s: e.reciprocal(out=ss[s][:, :], in_=ss[s][:, :]),
                      reads=[b_ss[s]], writes=[b_ss[s]])
                fw.op("dve", lambda e, s=s, m=m: e.scalar_tensor_tensor(out=y32[:, :], in0=xt[s][:, :],
                                                                        scalar=ss[s][:, 0:1], in1=gain[m][:, :],
                                                                        op0=ALU.mult, op1=ALU.mult),
                      reads=[b_xt[s], b_ss[s], b_gain[m]], writes=[b_y32])
                fw.op("pool", lambda e, s=s, m=m: e.tensor_tensor(out=hb[s][:, :], in0=y32[:, :], in1=shift[m][:, :],
                                                                  op=ALU.add),
                      reads=[b_y32, b_shift[m]], writes=[b_hb[s]])
                for k4 in range(NK // 4):
                    p = pi % 2
                    pi += 1

                    def tr(e, s=s, k4=k4, p=p):
                        for kk in range(4):
                            k = k4 * 4 + kk
                            ins = e.transpose(ptr[p][:, kk * 128:(kk + 1) * 128], hb[s][:, k * 128:(k + 1) * 128],
                                              ident[:, :])
                        return ins
                    fw.op("pe", tr, reads=[b_hb[s], b_id], writes=[b_ptr[p]])
                    fw.op("act", lambda e, k4=k4, p=p, t=t: e.copy(
                        out=hT[:, k4 * 4:(k4 + 1) * 4, t * 128:(t + 1) * 128],
                        in_=ptr[p][:, :].rearrange("p (k t) -> p k t", k=4)),
                        reads=[b_ptr[p]], writes=[b_hT[t]])
            if pname == "oc":
                blocks = [("tm", c0, 512) for c0 in range(0, C_MQ, 512)]
                blocks += [("fm", C_MQ, 512), ("fm", C_MK, 512)]
                blocks += [("tm", c0, 512) for c0 in range(C_MV, C_MG, 512)]
                blocks += [("tm32", C_MG, 16)]
                segs = [(0, cfg.nt_own, cfg.O_OWN), (cfg.nt_own, cfg.nt_ctx, cfg.O_CTX)]
            else:
                blocks = [("tm", C_AKV, 512), ("fm", C_MQ, 512), ("fm", C_MK, 512), ("tm", C_MV, 512),
                          ("tm", C_CKV, 512), ("tm32", C_MG, 16)]
                segs = [(0, cfg.nt_oth, cfg.O_OTH)]
            for kind, c0, cw in blocks:
                s = wi % 2
                wi += 1
                fw.dma("pool", W[s][:, :, 0:cw], wv[:, :, c0:c0 + cw], writes=[b_W[s]])
                if kind in ("tm", "tm32"):
                    st = stage[s] if kind == "tm" else stg32
                    bst = b_stage[s] if kind == "tm" else b_stg32
                    for t in range(nt):
                        p = pi % 4
                        pi += 1

                        def mm(e, s=s, t=t, p=p, cw=cw):
                            for k in range(NK):
                                ins = e.matmul(pmm[p][:, 0:cw], lhsT=hT[:, k, t * 128:(t + 1) * 128],
                                               rhs=W[s][:, k, 0:cw], start=(k == 0), stop=(k == NK - 1))
                            return ins
                        fw.op("pe", mm, reads=[b_hT[t], b_W[s]], writes=[b_pmm[p]])
                        ev = "act" if t % 2 == 0 else "dve"
                        if ev == "act":
                            fw.op("act", lambda e, t=t, p=p, cw=cw, st=st: e.copy(out=st[:, t, 0:cw], in_=pmm[p][:, 0:cw]),
                                  reads=[b_pmm[p]], writes=[bst])
                        else:
                            fw.op("dve", lambda e, t=t, p=p, cw=cw, st=st: e.tensor_copy(out=st[:, t, 0:cw],
                                                                                         in_=pmm[p][:, 0:cw]),
                                  reads=[b_pmm[p]], writes=[bst])
                    dst = S.tm(c0, cw)
                    for (t0, ntl, o) in segs:
                        fw.dma("sp", dst[o:o + ntl * 128, :].rearrange("(t p) c -> p t c", p=128),
                               st[:, t0:t0 + ntl, 0:cw], reads=[bst])
                else:
                    ntok = nt * 128 if (pname == "oc" or c0 == C_MK) else min(512, nt * 128)
                    stT = stage[s][:, :, :].rearrange("p t c -> p (t c)")
                    for cc in range(4):
                        for tb in range(0, ntok, 512):
                            tw = min(512, ntok - tb)
                            p = pi % 4
                            pi += 1

                            def mm(e, s=s, cc=cc, tb=tb, tw=tw, p=p):
                                for k in range(NK):
                                    ins = e.matmul(pmm[p][:, 0:tw], lhsT=W[s][:, k, cc * 128:(cc + 1) * 128],
                                                   rhs=hT[:, k, tb:tb + tw], start=(k == 0), stop=(k == NK - 1))
                                return ins
                            fw.op("pe", mm, reads=[b_hT[tt] for tt in range(tb // 128, (tb + tw) // 128)] + [b_W[s]],
                                  writes=[b_pmm[p]])
                            if (tb // 512) % 2 == 0:
                                fw.op("act", lambda e, tb=tb, tw=tw, p=p, cc=cc: e.copy(
                                    out=stT[:, cc * ntok + tb:cc * ntok + tb + tw], in_=pmm[p][:, 0:tw]),
                                    reads=[b_pmm[p]], writes=[b_stage[s]])
                            else:
                                fw.op("dve", lambda e, tb=tb, tw=tw, p=p, cc=cc: e.tensor_copy(
                                    out=stT[:, cc * ntok + tb:cc * ntok + tb + tw], in_=pmm[p][:, 0:tw]),
                                    reads=[b_pmm[p]], writes=[b_stage[s]])
                    dstT = S.fm(c0)
                    for (t0, ntl, o) in segs:
                        a0, a1 = t0 * 128, min((t0 + ntl) * 128, ntok)
                        if a1 <= a0:
                            continue
                        fw.dma("sp", dstT[:, o:o + a1 - a0].rearrange("(cc p) t -> p cc t", p=128),
                               stT[:, 0:4 * ntok].rearrange("p (cc t) -> p cc t", cc=4)[:, :, a0:a1],
                               reads=[b_stage[s]])


class Scratch:
    def __init__(self, nc, cfg, taps=()):
        self.nc = nc
        self.cfg = cfg
        T = cfg.T_ALL

        def dt(name, shape, dtype):
            kind = "ExternalOutput" if name in taps else "Internal"
            return nc.dram_tensor(name, list(shape), dtype, kind=kind).ap()
        self.mods = [dt("mods%d" % l, [2, 6 * D], F32) for l in range(DEPTH)]
        self.P = dt("P_tm", [T, C_MG], BF16)
        self.MG = dt("MG", [T, 16], F32)
        self.MQT = dt("MQT", [512, T], BF16)
        self.MKT = dt("MKT", [512, T], BF16)
        self.HT2 = dt("HT2", [2048, cfg.T_OWN + cfg.T_CTX], BF16)
        self.COMB = dt("COMB", [cfg.T_OWN + cfg.T_CTX, 16], F32)
        self.ZT = dt("ZT", [2048, cfg.T_OWN + cfg.T_CTX], BF16)
        self.HD = dt("HD", [2, cfg.T_OWN + cfg.T_CTX, 512], F32)
        self.OT = dt("OT", [2048, cfg.T_OWN + cfg.T_CTX], BF16)

    def tm(self, c0, cw):
        if c0 == C_MG:
            return self.MG
        return self.P[:, c0:c0 + cw]

    def fm(self, c0):
        return self.MQT if c0 == C_MQ else self.MKT


def _win_perm(half):
    mg = np.arange(3584, 3600).reshape(2, 2, 4)
    if half == 1:
        mg = mg[:, ::-1, :]
    return np.concatenate([np.arange(0, 3584), np.arange(3600, 10768), mg.reshape(-1)])


def core_inputs(inp, cfg, b, half):
    To = cfg.T_OWN
    seq = np.asarray(inp["x"][b][:2 * To])
    ctx = np.asarray(inp["ctx"][b][:cfg.T_CTX])
    if half == 1:
        seq = seq[::-1]
        ctx = ctx[::-1]
    cv = np.stack([np.asarray(inp["c"][b]).reshape(NK, 128).T, np.asarray(inp["c_ctx"]).reshape(NK, 128).T], axis=2)
    return {
        "x_own": np.ascontiguousarray(seq[:To]),
        "x_oth": np.ascontiguousarray(seq[To:]),
        "xc": np.ascontiguousarray(ctx),
        "cvec": np.ascontiguousarray(cv.reshape(128, 2 * NK)),
    }


def layer_inputs(inp, l, half):
    return {
        "w_mod": np.ascontiguousarray(inp["w_mod"][l]),
        "bmod2": np.ascontiguousarray(np.stack([inp["b_mod"][l], inp["b_mod"][l]], 0)),
        "w_in": np.ascontiguousarray(np.asarray(inp["w_in"][l])[:, _win_perm(half)]),
        "g1row": np.ascontiguousarray(np.asarray(inp["norm1_g"][l])[None, :]),
    }


def _rstd(fw, ss, b_ss, n):
    fw.op("dve", lambda e: e.tensor_scalar(out=ss, in0=ss, scalar1=1.0 / n, scalar2=EPS, op0=ALU.mult, op1=ALU.add),
          reads=[b_ss], writes=[b_ss])
    fw.op("act", lambda e: e.sqrt(out=ss, in_=ss), reads=[b_ss], writes=[b_ss])
    fw.op("dve", lambda e: e.reciprocal(out=ss, in_=ss), reads=[b_ss], writes=[b_ss])


def phase_attn(fw, cfg, l, S, A, kind, need_ctx):
    nc = fw.nc
    To, Tc = cfg.T_OWN, cfg.T_CTX
    if kind == "a":
        Hq, Hkv, cq0, ckv0, orow = 8, 2, C_AQ, C_AKV, 0
    else:
        Hq, Hkv, cq0, ckv0, orow = 4, 2, C_CQ, C_CKV, 1536
    G = Hq // Hkv
    scale = 128.0 ** -0.5
    QB = min(512, To)
    nsub = QB // 128
    ktiles = [(cfg.O_OWN + i * 128, i * 128) for i in range(cfg.nt_own)]
    if kind == "a":
        ktiles += [(cfg.O_OTH + i * 128, To + i * 128) for i in range(cfg.nt_oth)]
    else:
        ktiles += [(cfg.O_OTH, To)]
    n_lat_k = len(ktiles)
    ktiles += [(cfg.O_CTX + i * 128, None) for i in range(cfg.nt_ctx)]
    nkt = len(ktiles)
    qtiles = [(cfg.O_OWN + i * 128, i * 128) for i in range(cfg.nt_own)]
    if need_ctx:
        qtiles += [(cfg.O_CTX + i * 128, None) for i in range(cfg.nt_ctx)]
    nqt = len(qtiles)
    with Phase(fw, "at" + kind) as ph:
        ident = ph.sb([128, 128], BF16)
        b_id = make_ident(fw, ident)
        KT = ph.sb([128, Hkv, nkt * 128], BF16)
        VA = ph.sb([128, nkt, Hkv, 129], BF16)
        QT = ph.sb([128, Hq, nqt * 128], BF16)
        b_KT, b_VA, b_QT = bufs(nkt), bufs(nkt), bufs(nqt)
        b_va1 = Buf()
        fw.op("pool", lambda e: e.memset(VA[:, :, :, 128:129], 1.0), writes=[b_va1])
        gq = ph.sb([128, 128], F32)
        gk = ph.sb([128, 128], F32)
        b_g = Buf()
        b_g2 = Buf()
        if kind == "a":
            load_bc(fw, "sp", gq[:, :], A["a_qn_g"][l:l + 1, :], b_g)
            load_bc(fw, "sp", gk[:, :], A["a_kn_g"][l:l + 1, :], b_g2)
            fw.op("dve", lambda e: e.tensor_scalar(out=gq[:, :], in0=gq[:, :], scalar1=scale, scalar2=None, op0=ALU.mult),
                  reads=[b_g, b_g2], writes=[b_g])
        esink = ph.sb([128, 4], F32)
        b_es = Buf()
        if kind == "c":
            load_bc(fw, "sp", esink[:, :], A["c_sink"][l:l + 1, :], b_es)
            fw.op("act", lambda e: e.activation(out=esink[:, :], in_=esink[:, :], func=AF.Exp), reads=[b_es], writes=[b_es])
        masks = {}
        b_mask = Buf()
        if kind == "c":
            mk_t = ph.sb([128, nsub + 2, QB], BF16)
            fw.op("pool", lambda e: e.memset(mk_t[:, :, :], 1.0), writes=[b_mask])
            for r in range(-1, nsub + 1):
                mv = mk_t[:, r + 1, :]
                fw.op("pool", lambda e, mv=mv, r=r: e.affine_select(out=mv, in_=mv, pattern=[[1, QB]], compare_op=ALU.is_ge,
                                                                    fill=0.0, base=128 - r * 128, channel_multiplier=-1),
                      reads=[b_mask], writes=[b_mask])
                fw.op("pool", lambda e, mv=mv, r=r: e.affine_select(out=mv, in_=mv, pattern=[[-1, QB]], compare_op=ALU.is_ge,
                                                                    fill=0.0, base=128 + r * 128, channel_multiplier=1),
                      reads=[b_mask], writes=[b_mask])
                masks[r] = mv
        HM = max(Hq, 2 * Hkv)
        ld = [ph.sb([128, HM * 128], BF16) for _ in range(2)]
        b_ld = bufs(2)
        rp = [ph.sb([128, 128], F32) for _ in range(2)]
        b_rp = bufs(2)
        x32 = ph.sb([128, Hq * 128], F32)
        sq = ph.sb([128, Hq * 128], F32)
        t1 = ph.sb([128, Hq * 64], F32)
        t2 = ph.sb([128, Hq * 64], F32)
        xr = [ph.sb([128, Hq * 128], BF16) for _ in range(2)]
        b_x32, b_sq, b_t1, b_t2 = bufs(4)
        b_xr = bufs(2)
        ssn = ph.sb([128, Hq], F32)
        b_ssn = Buf()
        ptr = [ph.ps([128, 512], BF16) for _ in range(2)]
        b_ptr = bufs(2)
        cnt = {"ld": 0, "tr": 0, "xr": 0}

        def prep(row, rrow, c0, H, gt, dests, vdest=None):
            s = cnt["ld"] % 2
            cnt["ld"] += 1
            w = H * 128 + (Hkv * 128 if vdest is not None else 0)
            fw.dma("sp", ld[s][:, 0:w], S.P[row:row + 128, c0:c0 + w], writes=[b_ld[s]])
            if rrow is not None:
                fw.dma("sp", rp[s][:, :], A["rope"][rrow:rrow + 128, :], writes=[b_rp[s]])
            if vdest is not None:
                fw.op("pool", lambda e: e.tensor_copy(out=vdest[0], in_=ld[s][:, H * 128:w].rearrange("p (h d) -> p h d", h=Hkv)),
                      reads=[b_ld[s], b_va1], writes=[vdest[1]])
            xs = x32[:, 0:H * 128]
            if gt is not None:
                fw.op("act", lambda e: e.copy(out=xs, in_=ld[s][:, 0:H * 128]), reads=[b_ld[s]], writes=[b_x32])
                fw.op("dve", lambda e: e.tensor_tensor(out=sq[:, 0:H * 128], in0=xs, in1=xs, op=ALU.mult),
                      reads=[b_x32], writes=[b_sq])
                fw.op("dve", lambda e: e.tensor_reduce(out=ssn[:, 0:H], in_=sq[:, 0:H * 128].rearrange("p (h d) -> p h d", h=H),
                                                       axis=AX.X, op=ALU.add), reads=[b_sq], writes=[b_ssn])
                _rstd(fw, ssn[:, 0:H], b_ssn, 128)
                x3 = xs.rearrange("p (h d) -> p h d", h=H)
                fw.op("dve", lambda e: e.tensor_tensor(out=x3, in0=x3, in1=ssn[:, 0:H].unsqueeze(2).to_broadcast([128, H, 128]),
                                                       op=ALU.mult), reads=[b_x32, b_ssn], writes=[b_x32])
                fw.op("dve", lambda e: e.tensor_tensor(out=x3, in0=x3, in1=gt[:, :].unsqueeze(1).to_broadcast([128, H, 128]),
                                                       op=ALU.mult), reads=[b_x32, b_g], writes=[b_x32])
            else:
                sc = scale if dests[0][2] == "q" else 1.0
                fw.op("act", lambda e: e.activation(out=xs, in_=ld[s][:, 0:H * 128], func=AF.Copy, scale=sc),
                      reads=[b_ld[s]], writes=[b_x32])
            xi = cnt["xr"] % 2
            cnt["xr"] += 1
            xo = xr[xi][:, 0:H * 128]
            if rrow is not None:
                x5 = xs.rearrange("p (h a b f) -> p h a b f", h=H, a=2, b=2)
                o5 = xo.rearrange("p (h a b f) -> p h a b f", h=H, a=2, b=2)
                cs = rp[s][:, 0:64].rearrange("p (a f) -> p a f", a=2).unsqueeze(1).to_broadcast([128, H, 2, 32])
                sn = rp[s][:, 64:128].rearrange("p (a f) -> p a f", a=2).unsqueeze(1).to_broadcast([128, H, 2, 32])
                x1, x2 = x5[:, :, :, 0, :], x5[:, :, :, 1, :]
                u1 = t1[:, 0:H * 64].rearrange("p (h a f) -> p h a f", h=H, a=2)
                u2 = t2[:, 0:H * 64].rearrange("p (h a f) -> p h a f", h=H, a=2)
                fw.op("dve", lambda e: e.tensor_tensor(out=u1, in0=x1, in1=cs, op=ALU.mult), reads=[b_x32, b_rp[s]], writes=[b_t1])
                fw.op("dve", lambda e: e.tensor_tensor(out=u2, in0=x2, in1=sn, op=ALU.mult), reads=[b_x32, b_rp[s]], writes=[b_t2])
                fw.op("dve", lambda e: e.tensor_tensor(out=o5[:, :, :, 0, :], in0=u1, in1=u2, op=ALU.subtract),
                      reads=[b_t1, b_t2], writes=[b_xr[xi]])
                fw.op("dve", lambda e: e.tensor_tensor(out=u1, in0=x2, in1=cs, op=ALU.mult), reads=[b_x32, b_rp[s]], writes=[b_t1])
                fw.op("dve", lambda e: e.tensor_tensor(out=u2, in0=x1, in1=sn, op=ALU.mult), reads=[b_x32, b_rp[s]], writes=[b_t2])
                fw.op("dve", lambda e: e.tensor_tensor(out=o5[:, :, :, 1, :], in0=u1, in1=u2, op=ALU.add),
                      reads=[b_t1, b_t2], writes=[b_xr[xi]])
            else:
                fw.op("dve", lambda e: e.tensor_copy(out=xo, in_=xs), reads=[b_x32], writes=[b_xr[xi]])
            for h0 in range(0, H, 4):
                hn = min(4, H - h0)
                p = cnt["tr"] % 2
                cnt["tr"] += 1

                def tr(e):
                    for hh in range(hn):
                        ins = e.transpose(ptr[p][:, hh * 128:(hh + 1) * 128], xo[:, (h0 + hh) * 128:(h0 + hh + 1) * 128], ident[:, :])
                    return ins
                fw.op("pe", tr, reads=[b_xr[xi], b_id], writes=[b_ptr[p]])
                fw.op("act", lambda e: e.copy(out=dests[0][0][:, h0:h0 + hn, dests[0][3]:dests[0][3] + 128],
                                              in_=ptr[p][:, 0:hn * 128].rearrange("p (h t) -> p h t", h=hn)),
                      reads=[b_ptr[p]], writes=[dests[0][1]])

        for ki, (row, rrow) in enumerate(ktiles):
            prep(row, rrow, ckv0, Hkv, gk if kind == "a" else None, [(KT, b_KT[ki], "k", ki * 128)],
                 vdest=(VA[:, ki, :, 0:128], b_VA[ki]))
        for qi, (row, rrow) in enumerate(qtiles):
            prep(row, rrow, cq0, Hq, gq if kind == "a" else None, [(QT, b_QT[qi], "q", qi * 128)])

        if getattr(S, "dbg", None) and kind in S.dbg:
            dq = nc.dram_tensor("dbgQT" + kind, [128, Hq, nqt * 128], BF16, kind="ExternalOutput").ap()
            dk = nc.dram_tensor("dbgKT" + kind, [128, Hkv, nkt * 128], BF16, kind="ExternalOutput").ap()
            dv = nc.dram_tensor("dbgVA" + kind, [128, nkt, Hkv, 129], BF16, kind="ExternalOutput").ap()
            fw.dma("sp", dq, QT[:, :, :], reads=b_QT)
            fw.dma("sp", dk, KT[:, :, :], reads=b_KT)
            fw.dma("sp", dv, VA[:, :, :, :], reads=b_VA + [b_va1])
        pst = [ph.ps([128, 512]) for _ in range(2)]
        b_pst = bufs(2)
        po = [ph.ps([128, 512]) for _ in range(4)]
        b_po = bufs(4)
        PT = [ph.sb([128, 512], BF16) for _ in range(3)]
        b_PT = bufs(3)
        rden = ph.sb([128, 4], F32)
        b_rden = Buf()
        on = [ph.sb([128, 4, 128], BF16) for _ in range(2)]
        b_on = bufs(2)
        ost = [ph.sb([128, 512], BF16) for _ in range(2)]
        b_ost = bufs(2)
        it = {"s": 0, "p": 0, "o": 0}
        qblocks = []
        for q0 in range(0, To, QB):
            i0 = q0 // 128
            if kind == "a":
                kl = [(ki, None) for ki in range(nkt)]
            else:
                kl = []
                for r in range(-1, nsub + 1):
                    kt = i0 + r
                    if 0 <= kt <= cfg.nt_own:
                        kl.append((kt, r))
                kl += [(n_lat_k + i, None) for i in range(cfg.nt_ctx)]
            qblocks.append((q0, nsub, kl, q0))
        if need_ctx:
            qblocks.append((To, cfg.nt_ctx, [(n_lat_k + i, None) for i in range(cfg.nt_ctx)], To))
        for h in range(Hq):
            g = h // G
            for (q0, ns, kl, oc0) in qblocks:
                qw = ns * 128
                for idx, (ki, r) in enumerate(kl):
                    sp_ = it["s"] % 2
                    it["s"] += 1
                    fw.op("pe", lambda e: e.matmul(pst[sp_][:, 0:qw], lhsT=KT[:, g, ki * 128:(ki + 1) * 128],
                                                   rhs=QT[:, h, q0:q0 + qw], start=True, stop=True),
                          reads=[b_KT[ki]] + [b_QT[q0 // 128 + j] for j in range(ns)], writes=[b_pst[sp_]])
                    pi_ = it["p"] % 3
                    it["p"] += 1
                    fw.op("act", lambda e: e.activation(out=PT[pi_][:, 0:qw], in_=pst[sp_][:, 0:qw], func=AF.Exp),
                          reads=[b_pst[sp_]], writes=[b_PT[pi_]])
                    if r is not None:
                        fw.op("pool", lambda e: e.tensor_tensor(out=PT[pi_][:, 0:qw], in0=PT[pi_][:, 0:qw], in1=masks[r][:, 0:qw],
                                                                op=ALU.mult), reads=[b_PT[pi_], b_mask], writes=[b_PT[pi_]])

                    def pv(e):
                        ins = None
                        for j in range(ns):
                            if r is not None and abs(r - j) > 1:
                                continue
                            first = (idx == 0) if r is None else (ki == max(0, q0 // 128 + j - 1))
                            ins = e.matmul(po[j][:, 0:129], lhsT=PT[pi_][:, j * 128:(j + 1) * 128], rhs=VA[:, ki, g, :],
                                           start=first, stop=(idx == len(kl) - 1))
                        return ins
                    fw.op("pe", pv, reads=[b_PT[pi_], b_VA[ki], b_va1], writes=b_po[0:ns])
                oi = it["o"] % 2
                it["o"] += 1
                for j in range(ns):
                    if kind == "c":
                        fw.op("dve", lambda e: e.tensor_scalar(out=rden[:, j:j + 1], in0=po[j][:, 128:129],
                                                               scalar1=esink[:, h:h + 1], scalar2=None, op0=ALU.add),
                              reads=[b_po[j], b_es], writes=[b_rden])
                        fw.op("dve", lambda e: e.reciprocal(out=rden[:, j:j + 1], in_=rden[:, j:j + 1]),
                              reads=[b_rden], writes=[b_rden])
                    else:
                        fw.op("dve", lambda e: e.reciprocal(out=rden[:, j:j + 1], in_=po[j][:, 128:129]),
                              reads=[b_po[j]], writes=[b_rden])
                    fw.op("dve", lambda e: e.tensor_scalar(out=on[oi][:, j, :], in0=po[j][:, 0:128], scalar1=rden[:, j:j + 1],
                                                           scalar2=None, op0=ALU.mult),
                          reads=[b_po[j], b_rden], writes=[b_on[oi]])
                p = cnt["tr"] % 2
                cnt["tr"] += 1

                def tr2(e):
                    for j in range(ns):
                        ins = e.transpose(ptr[p][:, j * 128:(j + 1) * 128], on[oi][:, j, :], ident[:, :])
                    return ins
                fw.op("pe", tr2, reads=[b_on[oi], b_id], writes=[b_ptr[p]])
                fw.op("act", lambda e: e.copy(out=ost[oi][:, 0:qw], in_=ptr[p][:, 0:qw]), reads=[b_ptr[p]], writes=[b_ost[oi]])
                fw.dma("sp", S.OT[orow + h * 128:orow + (h + 1) * 128, oc0:oc0 + qw], ost[oi][:, 0:qw], reads=[b_ost[oi]])


def rope_table(cfg, half):
    n = 2 * cfg.T_OWN
    t = np.arange(n)
    if half == 1:
        t = n - 1 - t
    rows = (t // 64).astype(np.float32)
    cols = (t % 64).astype(np.float32)
    inv = (10000.0 ** (-np.arange(0, 64, 2, dtype=np.float32) / 64.0)).astype(np.float32)
    ang = np.concatenate([rows[:, None] * inv, cols[:, None] * inv], axis=-1).astype(np.float32)
    return np.ascontiguousarray(np.concatenate([np.cos(ang), np.sin(ang)], axis=1).astype(np.float32))


def phase_mlstm(fw, cfg, l, S, A, need_ctx):
    nc = fw.nc
    To, Tc, TA = cfg.T_OWN, cfg.T_CTX, cfg.T_ALL
    NT = TA // 128
    T_OC = To + Tc
    kscale = 128.0 ** -0.5
    with Phase(fw, "ml") as ph:
        identB = ph.sb([128, 128], BF16)
        b_idB = make_ident(fw, identB)
        identF = ph.sb([128, 128], F32)
        b_idF = make_ident(fw, identF)
        ones = ph.sb([128, 128], F32)
        U = [ph.sb([128, 128], F32) for _ in range(2)]
        NEGM = [ph.sb([128, 128], F32) for _ in range(2)]
        Sel = [ph.sb([128, 128], F32) for _ in range(2)]
        Bsel = ph.sb([64, 4], F32)
        b_c = Buf()
        fw.op("pool", lambda e: e.memset(ones[:, :], 1.0), writes=[b_c])
        for d in range(2):
            fw.op("pool", lambda e: e.memset(U[d][:, :], 1.0), writes=[b_c])
            fw.op("pool", lambda e: e.memset(NEGM[d][:, :], 0.0), writes=[b_c])
            fw.op("pool", lambda e: e.memset(Sel[d][:, :], 1.0), writes=[b_c])
        fw.op("pool", lambda e: e.memset(Bsel[:, :], 0.0), writes=[b_c])
        sg = [1, -1]
        for d in range(2):
            fw.op("pool", lambda e: e.affine_select(out=U[d][:, :], in_=U[d][:, :], pattern=[[sg[d], 128]], compare_op=ALU.is_ge,
                                                    fill=0.0, base=0, channel_multiplier=-sg[d]), reads=[b_c], writes=[b_c])
            fw.op("pool", lambda e: e.affine_select(out=NEGM[d][:, :], in_=NEGM[d][:, :], pattern=[[-sg[d], 128]],
                                                    compare_op=ALU.is_ge, fill=NEG, base=0, channel_multiplier=sg[d]),
                  reads=[b_c], writes=[b_c])
            last = 127 if d == 0 else 0
            fw.op("pool", lambda e: e.affine_select(out=Sel[d][:, :], in_=Sel[d][:, :], pattern=[[0, 128]], compare_op=ALU.is_equal,
                                                    fill=0.0, base=-last, channel_multiplier=1), reads=[b_c], writes=[b_c])
        for base in (0, -32):
            fw.op("pool", lambda e: e.affine_select(out=Bsel[:, :], in_=Bsel[:, :], pattern=[[-1, 4]], compare_op=ALU.not_equal,
                                                    fill=1.0, base=base, channel_multiplier=1), reads=[b_c], writes=[b_c])
        G = ph.sb([128, NT, 16], F32)
        IC = ph.sb([128, NT, 8], F32)
        LF = ph.sb([128, NT, 8], F32)
        gb = ph.sb([128, 16], F32)
        b_G, b_gate, b_gb, b_gb2 = bufs(4)
        fw.dma("sp", G[:, :, :], S.MG.rearrange("(t p) c -> p t c", p=128), writes=[b_G])
        load_bc(fw, "sp", gb[:, 0:8], A["m_ig_b"][l:l + 1, :], b_gb)
        load_bc(fw, "sp", gb[:, 8:16], A["m_fg_b"][l:l + 1, :], b_gb2)
        fw.op("dve", lambda e: e.tensor_tensor(out=IC[:, :, :], in0=G[:, :, 0:8], in1=gb[:, 0:8].unsqueeze(1).to_broadcast([128, NT, 8]),
                                               op=ALU.add), reads=[b_G, b_gb], writes=[b_gate])
        fw.op("dve", lambda e: e.tensor_tensor(out=LF[:, :, :], in0=G[:, :, 8:16], in1=gb[:, 8:16].unsqueeze(1).to_broadcast([128, NT, 8]),
                                               op=ALU.add), reads=[b_G, b_gb2, b_gate], writes=[b_gate])
        fw.op("act", lambda e: e.activation(out=LF[:, :, :], in_=LF[:, :, :], func=AF.Exp, scale=-1.0), reads=[b_gate], writes=[b_gate])
        fw.op("act", lambda e: e.activation(out=LF[:, :, :], in_=LF[:, :, :], func=AF.Ln, bias=1.0), reads=[b_gate], writes=[b_gate])
        fw.op("dve", lambda e: e.tensor_scalar(out=LF[:, :, :], in0=LF[:, :, :], scalar1=-1.0, scalar2=None, op0=ALU.mult),
              reads=[b_gate], writes=[b_gate])
        qT = ph.sb([128, 4, T_OC], BF16)
        kT = ph.sb([128, 4, T_OC], BF16)
        Ktm = ph.sb([128, NT, 4, 128], BF16)
        VA = ph.sb([128, NT, 4, 129], BF16)
        b_qT, b_kT, b_Ktm, b_VA = bufs(4)
        fw.op("pool", lambda e: e.memset(VA[:, :, :, 128:129], 1.0), writes=[b_VA])
        for h in range(4):
            fw.dma("sp", VA[:, :, h, 0:128], S.P[:, C_MV + h * 128:C_MV + (h + 1) * 128].rearrange("(t p) d -> p t d", p=128),
                   reads=[b_VA], writes=[b_VA])
        cw = ph.sb([128, 8, 3], F32)
        b_cw = Buf()
        fw.dma("sp", cw[:, :, :].rearrange("p c k -> p (c k)"), A["m_conv"][l], writes=[b_cw])
        with Phase(fw, "mlc") as pc:
            X = [pc.sb([128, TA], BF16) for _ in range(2)]
            acc = [pc.sb([128, TA], F32) for _ in range(2)]
            ktmp = pc.sb([128, TA], BF16)
            b_X, b_acc = bufs(2), bufs(2)
            b_ktmp = Buf()
            ptr = [pc.ps([128, 512], BF16) for _ in range(2)]
            b_ptr = bufs(2)
            n = 0
            tp = 0
            for qk in range(2):
                src = S.MQT if qk == 0 else S.MKT
                for hc in range(4):
                    s = n % 2
                    n += 1
                    fw.dma("sp", X[s][:, :], src[hc * 128:(hc + 1) * 128, :], writes=[b_X[s]])
                    w = cw[:, qk * 4 + hc, :]
                    for (a, b) in ((0, 2 * To), (2 * To, TA)):
                        fw.op("dve", lambda e: e.tensor_scalar(out=acc[s][:, a:b], in0=X[s][:, a:b], scalar1=w[:, 1:2], scalar2=None,
                                                               op0=ALU.mult), reads=[b_X[s], b_cw], writes=[b_acc[s]])
                        fw.op("dve", lambda e: e.scalar_tensor_tensor(out=acc[s][:, a + 1:b], in0=X[s][:, a:b - 1], scalar=w[:, 0:1],
                                                                      in1=acc[s][:, a + 1:b], op0=ALU.mult, op1=ALU.add),
                              reads=[b_X[s], b_cw, b_acc[s]], writes=[b_acc[s]])
                        fw.op("dve", lambda e: e.scalar_tensor_tensor(out=acc[s][:, a:b - 1], in0=X[s][:, a + 1:b], scalar=w[:, 2:3],
                                                                      in1=acc[s][:, a:b - 1], op0=ALU.mult, op1=ALU.add),
                              reads=[b_X[s], b_cw, b_acc[s]], writes=[b_acc[s]])
                    if qk == 0:
                        fw.op("act", lambda e: e.activation(out=qT[:, hc, 0:To], in_=acc[s][:, 0:To], func=AF.Silu),
                              reads=[b_acc[s]], writes=[b_qT])
                        fw.op("act", lambda e: e.activation(out=qT[:, hc, To:T_OC], in_=acc[s][:, 2 * To:TA], func=AF.Silu),
                              reads=[b_acc[s]], writes=[b_qT])
                    else:
                        fw.op("act", lambda e: e.activation(out=acc[s][:, :], in_=acc[s][:, :], func=AF.Silu),
                              reads=[b_acc[s]], writes=[b_acc[s]])
                        fw.op("dve", lambda e: e.tensor_scalar(out=ktmp[:, :], in0=acc[s][:, :], scalar1=kscale, scalar2=None,
                                                               op0=ALU.mult), reads=[b_acc[s]], writes=[b_ktmp])
                        fw.op("pool", lambda e: e.tensor_copy(out=kT[:, hc, 0:To], in_=ktmp[:, 0:To]), reads=[b_ktmp], writes=[b_kT])
                        fw.op("pool", lambda e: e.tensor_copy(out=kT[:, hc, To:T_OC], in_=ktmp[:, 2 * To:TA]), reads=[b_ktmp], writes=[b_kT])
                        for t0 in range(0, NT, 4):
                            tn = min(4, NT - t0)
                            p = tp % 2
                            tp += 1

                            def tr(e):
                                for tt in range(tn):
                                    ins = e.transpose(ptr[p][:, tt * 128:(tt + 1) * 128], ktmp[:, (t0 + tt) * 128:(t0 + tt + 1) * 128],
                                                      identB[:, :])
                                return ins
                            fw.op("pe", tr, reads=[b_ktmp, b_idB], writes=[b_ptr[p]])
                            fw.op("act", lambda e: e.copy(out=Ktm[:, t0:t0 + tn, hc, :],
                                                          in_=ptr[p][:, 0:tn * 128].rearrange("p (t d) -> p t d", t=tn)),
                                  reads=[b_ptr[p]], writes=[b_Ktm])
        pA = ph.ps([128, 512])
        pC = ph.ps([128, 512])
        pD = ph.ps([128, 512])
        pE = ph.ps([128, 512], BF16)
        pF = ph.ps([128, 512])
        pG = ph.ps([128, 512])
        b_pA, b_pC, b_pD, b_pE, b_pF, b_pG = bufs(6)
        Cst = [ph.sb([128, 4, 129], F32) for _ in range(2)]
        Cb = [ph.sb([128, 4, 129], BF16) for _ in range(2)]
        mst = [ph.sb([128, 4], F32) for _ in range(2)]
        b_C, b_Cb, b_m = bufs(2), bufs(2), bufs(2)
        for d in range(2):
            fw.op("pool", lambda e: e.memset(Cst[d][:, :, :], 0.0), writes=[b_C[d]])
            fw.op("pool", lambda e: e.memset(Cb[d][:, :, :], 0.0), writes=[b_Cb[d]])
            fw.op("pool", lambda e: e.memset(mst[d][:, :], NEG), writes=[b_m[d]])
        AB = ph.sb([128, 64], F32)
        R64 = ph.sb([64, 128], F32)
        X64 = ph.sb([64, 128], F32)
        Lall = ph.sb([64, 4, 128], F32)
        b_AB, b_R64, b_X64, b_Lall = bufs(4)
        fw.op("pool", lambda e: e.memset(AB[:, :], 0.0), writes=[b_AB])
        fw.op("pool", lambda e: e.memset(R64[:, :], 1.0), writes=[b_R64])
        fw.op("pool", lambda e: e.memset(X64[:, :], 1.0), writes=[b_X64])
        bsb = ph.sb([128, 4], F32)
        btot = ph.sb([128, 4], F32)
        bm = ph.sb([128, 4], F32)
        rowmax = ph.sb([128, 4], F32)
        mrow = ph.sb([128, 4], F32)
        E8 = ph.sb([128, 8], F32)
        small = ph.sb([128, 16], F32)
        mnew = ph.sb([128, 4], F32)
        ld = ph.sb([128, 4, 128], F32)
        Dm = ph.sb([128, 4, 128], F32)
        Wb = ph.sb([128, 4, 128], BF16)
        WT = ph.sb([128, 4, 128], BF16)
        kw = ph.sb([128, 4, 128], BF16)
        numt = ph.sb([128, 4, 129], F32)
        hout = [ph.sb([128, 4, 128], F32) for _ in range(2)]
        tmpC = ph.sb([128, 4, 129], F32)
        (b_bsb, b_btot, b_bm, b_rowmax, b_mrow, b_E8, b_small, b_mnew, b_ld, b_Dm, b_Wb, b_WT, b_kw, b_numt,
         b_tmpC) = bufs(15)
        b_hout = bufs(2)
        hcnt = [0]

        def v2(t):
            return t[:, 0:258].rearrange("p (h e) -> p h e", h=2)

        def step(d, ti, full, oc_tile):
            lf = LF[:, ti, d * 4:(d + 1) * 4]
            ic = IC[:, ti, d * 4:(d + 1) * 4]
            m = mst[d]

            def mm_b(e):
                e.matmul(pA[:, 0:4], lhsT=U[d][:, :], rhs=lf, start=True, stop=True)
                return e.matmul(pA[:, 4:8], lhsT=ones[:, :], rhs=lf, start=True, stop=True)
            fw.op("pe", mm_b, reads=[b_gate, b_c], writes=[b_pA])
            fw.op("dve", lambda e: e.tensor_copy(out=AB[:, 32:36], in_=pA[:, 0:4]), reads=[b_pA], writes=[b_AB, b_bsb])
            fw.op("dve", lambda e: e.tensor_tensor(out=AB[:, 0:4], in0=ic, in1=pA[:, 0:4], op=ALU.subtract),
                  reads=[b_pA, b_gate], writes=[b_AB])
            fw.op("act", lambda e: e.copy(out=btot[:, :], in_=pA[:, 4:8]), reads=[b_pA], writes=[b_btot])
            fw.op("dve", lambda e: e.tensor_tensor(out=bm[:, :], in0=AB[:, 32:36], in1=m[:, :], op=ALU.add),
                  reads=[b_AB, b_m[d]], writes=[b_bm])
            fw.op("pe", lambda e: e.transpose(pA[0:64, 16:144], AB[:, :], identF[:, :]), reads=[b_AB, b_idF], writes=[b_pA])
            fw.op("act", lambda e: e.copy(out=R64[0:32, :], in_=pA[0:32, 16:144]), reads=[b_pA], writes=[b_R64])
            fw.op("dve", lambda e: e.tensor_copy(out=X64[32:64, :], in_=pA[32:64, 16:144]), reads=[b_pA], writes=[b_X64])
            fw.op("dve", lambda e: e.tensor_tensor(out=Lall[:, :, :], in0=X64[:, :].unsqueeze(1).to_broadcast([64, 4, 128]),
                                                   in1=Bsel[:, :].unsqueeze(2).to_broadcast([64, 4, 128]), op=ALU.mult),
                  reads=[b_X64, b_c], writes=[b_Lall])

            def mm_ld(e):
                for h in range(4):
                    ins = e.matmul(pC[:, h * 128:(h + 1) * 128], lhsT=Lall[:, h, :], rhs=R64[:, :], start=True, stop=True)
                return ins
            fw.op("pe", mm_ld, reads=[b_Lall, b_R64], writes=[b_pC])
            fw.op("dve", lambda e: e.tensor_tensor(out=ld[:, :, :], in0=pC[:, :].rearrange("p (h s) -> p h s", h=4),
                                                   in1=NEGM[d][:, :].unsqueeze(1).to_broadcast([128, 4, 128]), op=ALU.add),
                  reads=[b_pC, b_c], writes=[b_ld])
            fw.op("dve", lambda e: e.tensor_reduce(out=rowmax[:, :], in_=ld[:, :, :], axis=AX.X, op=ALU.max),
                  reads=[b_ld], writes=[b_rowmax])
            if full:
                fw.op("dve", lambda e: e.tensor_tensor(out=mrow[:, :], in0=bm[:, :], in1=rowmax[:, :], op=ALU.max),
                      reads=[b_bm, b_rowmax], writes=[b_mrow])
                fw.op("dve", lambda e: e.tensor_tensor(out=ld[:, :, :], in0=ld[:, :, :],
                                                       in1=mrow[:, :].unsqueeze(2).to_broadcast([128, 4, 128]), op=ALU.subtract),
                      reads=[b_ld, b_mrow], writes=[b_ld])
                fw.op("dve", lambda e: e.tensor_scalar_max(out=ld[:, :, :], in0=ld[:, :, :], scalar1=-80.0), reads=[b_ld], writes=[b_ld])
                fw.op("act", lambda e: e.activation(out=Dm[:, :, :], in_=ld[:, :, :], func=AF.Exp), reads=[b_ld], writes=[b_Dm])
                oc0 = oc_tile * 128

                def mm_s(e):
                    for h in range(4):
                        ins = e.matmul(pD[:, h * 128:(h + 1) * 128], lhsT=qT[:, h, oc0:oc0 + 128], rhs=kT[:, h, oc0:oc0 + 128],
                                       start=True, stop=True)
                    return ins
                fw.op("pe", mm_s, reads=[b_qT, b_kT], writes=[b_pD])
                fw.op("dve", lambda e: e.tensor_tensor(out=Wb[:, :, :], in0=pD[:, :].rearrange("p (h s) -> p h s", h=4), in1=Dm[:, :, :],
                                                       op=ALU.mult), reads=[b_pD, b_Dm], writes=[b_Wb])

                def mm_t(e):
                    for h in range(4):
                        ins = e.transpose(pE[:, h * 128:(h + 1) * 128], Wb[:, h, :], identB[:, :])
                    return ins
                fw.op("pe", mm_t, reads=[b_Wb, b_idB], writes=[b_pE])
                fw.op("act", lambda e: e.copy(out=WT[:, :, :], in_=pE[:, :].rearrange("p (h j) -> p h j", h=4)), reads=[b_pE], writes=[b_WT])

                def mm_intra(e):
                    for h in range(4):
                        ins = e.matmul(v2(pF if h < 2 else pG)[:, h % 2, :], lhsT=WT[:, h, :], rhs=VA[:, ti, h, :], start=True, stop=True)
                    return ins
                fw.op("pe", mm_intra, reads=[b_WT, b_VA], writes=[b_pF, b_pG])

                def mm_inter(e):
                    for h in range(4):
                        ins = e.matmul(v2(pC if h < 2 else pD)[:, h % 2, :], lhsT=qT[:, h, oc0:oc0 + 128], rhs=Cb[d][:, h, :],
                                       start=True, stop=True)
                    return ins
                fw.op("pe", mm_inter, reads=[b_qT, b_Cb[d]], writes=[b_pC, b_pD])
                fw.op("dve", lambda e: e.tensor_tensor(out=E8[:, 0:4], in0=bm[:, :], in1=mrow[:, :], op=ALU.subtract),
                      reads=[b_bm, b_mrow], writes=[b_E8])
                fw.op("dve", lambda e: e.tensor_scalar(out=E8[:, 4:8], in0=mrow[:, :], scalar1=-1.0, scalar2=None, op0=ALU.mult),
                      reads=[b_mrow, b_E8], writes=[b_E8])
                fw.op("dve", lambda e: e.tensor_scalar_max(out=E8[:, :], in0=E8[:, :], scalar1=-80.0), reads=[b_E8], writes=[b_E8])
                fw.op("act", lambda e: e.activation(out=E8[:, :], in_=E8[:, :], func=AF.Exp), reads=[b_E8], writes=[b_E8])
                for hp, (pi_, pj_, bi_, bj_) in enumerate(((pC, pF, b_pC, b_pF), (pD, pG, b_pD, b_pG))):
                    hs = slice(hp * 2, hp * 2 + 2)
                    fw.op("dve", lambda e: e.tensor_tensor(out=numt[:, hs, :], in0=v2(pi_),
                                                           in1=E8[:, hs].unsqueeze(2).to_broadcast([128, 2, 129]), op=ALU.mult),
                          reads=[bi_, b_E8], writes=[b_numt])
                    fw.op("dve", lambda e: e.tensor_tensor(out=numt[:, hs, :], in0=numt[:, hs, :], in1=v2(pj_), op=ALU.add),
                          reads=[bj_, b_numt], writes=[b_numt])
                fw.op("dve", lambda e: e.tensor_scalar(out=small[:, 4:8], in0=numt[:, :, 128], scalar1=-1.0, scalar2=None, op0=ALU.mult),
                      reads=[b_numt], writes=[b_small])
                fw.op("dve", lambda e: e.tensor_tensor(out=small[:, 0:4], in0=numt[:, :, 128], in1=small[:, 4:8], op=ALU.max),
                      reads=[b_numt, b_small], writes=[b_small])
                fw.op("dve", lambda e: e.tensor_tensor(out=small[:, 0:4], in0=small[:, 0:4], in1=E8[:, 4:8], op=ALU.max),
                      reads=[b_small, b_E8], writes=[b_small])
                fw.op("dve", lambda e: e.reciprocal(out=small[:, 0:4], in_=small[:, 0:4]), reads=[b_small], writes=[b_small])
                hi = hcnt[0] % 2
                hcnt[0] += 1
                fw.op("dve", lambda e: e.tensor_tensor(out=hout[hi][:, :, :], in0=numt[:, :, 0:128],
                                                       in1=small[:, 0:4].unsqueeze(2).to_broadcast([128, 4, 128]), op=ALU.mult),
                      reads=[b_numt, b_small], writes=[b_hout[hi]])
                fw.dma("sp", S.HD[d, oc0:oc0 + 128, :], hout[hi][:, :, :].rearrange("p h e -> p (h e)"), reads=[b_hout[hi]])
            fw.op("pe", lambda e: e.matmul(pA[:, 8:12], lhsT=Sel[d][:, :], rhs=rowmax[:, :], start=True, stop=True),
                  reads=[b_rowmax, b_c], writes=[b_pA])
            fw.op("dve", lambda e: e.tensor_tensor(out=small[:, 8:12], in0=btot[:, :], in1=m[:, :], op=ALU.add),
                  reads=[b_btot, b_m[d]], writes=[b_small])
            fw.op("dve", lambda e: e.tensor_tensor(out=mnew[:, :], in0=small[:, 8:12], in1=pA[:, 8:12], op=ALU.max),
                  reads=[b_small, b_pA], writes=[b_mnew])
            fw.op("dve", lambda e: e.tensor_tensor(out=E8[:, 4:8], in0=small[:, 8:12], in1=mnew[:, :], op=ALU.subtract),
                  reads=[b_small, b_mnew, b_E8], writes=[b_E8])
            fw.op("dve", lambda e: e.tensor_tensor(out=E8[:, 0:4], in0=AB[:, 0:4], in1=btot[:, :], op=ALU.add),
                  reads=[b_AB, b_btot, b_E8], writes=[b_E8])
            fw.op("dve", lambda e: e.tensor_tensor(out=E8[:, 0:4], in0=E8[:, 0:4], in1=mnew[:, :], op=ALU.subtract),
                  reads=[b_E8, b_mnew], writes=[b_E8])
            fw.op("dve", lambda e: e.tensor_scalar_max(out=E8[:, :], in0=E8[:, :], scalar1=-80.0), reads=[b_E8], writes=[b_E8])
            fw.op("act", lambda e: e.activation(out=E8[:, :], in_=E8[:, :], func=AF.Exp), reads=[b_E8], writes=[b_E8])
            fw.op("dve", lambda e: e.tensor_tensor(out=kw[:, :, :], in0=Ktm[:, ti, :, :],
                                                   in1=E8[:, 0:4].unsqueeze(2).to_broadcast([128, 4, 128]), op=ALU.mult),
                  reads=[b_Ktm, b_E8], writes=[b_kw])

            def mm_dc(e):
                for h in range(4):
                    ins = e.matmul(v2(pF if h < 2 else pG)[:, h % 2, :], lhsT=kw[:, h, :], rhs=VA[:, ti, h, :], start=True, stop=True)
                return ins
            fw.op("pe", mm_dc, reads=[b_kw, b_VA], writes=[b_pF, b_pG])
            fw.op("dve", lambda e: e.tensor_tensor(out=tmpC[:, :, :], in0=Cst[d][:, :, :],
                                                   in1=E8[:, 4:8].unsqueeze(2).to_broadcast([128, 4, 129]), op=ALU.mult),
                  reads=[b_C[d], b_E8], writes=[b_tmpC])
            fw.op("dve", lambda e: e.tensor_tensor(out=Cst[d][:, 0:2, :], in0=tmpC[:, 0:2, :], in1=v2(pF), op=ALU.add),
                  reads=[b_tmpC, b_pF], writes=[b_C[d]])
            fw.op("dve", lambda e: e.tensor_tensor(out=Cst[d][:, 2:4, :], in0=tmpC[:, 2:4, :], in1=v2(pG), op=ALU.add),
                  reads=[b_tmpC, b_pG, b_C[d]], writes=[b_C[d]])
            fw.op("act", lambda e: e.copy(out=Cb[d][:, :, :], in_=Cst[d][:, :, :]), reads=[b_C[d]], writes=[b_Cb[d]])
            fw.op("dve", lambda e: e.tensor_copy(out=m[:, :], in_=mnew[:, :]), reads=[b_mnew], writes=[b_m[d]])

        n_own, n_ctx = cfg.nt_own, cfg.nt_ctx
        t_own0, t_oth0, t_ctx0 = 0, n_own, 2 * n_own
        near = [(t_ctx0 + i, need_ctx, n_own + i) for i in range(n_ctx)] + [(t_own0 + i, True, i) for i in range(n_own)]
        far = [(t_ctx0 + i, need_ctx, n_own + i) for i in reversed(range(n_ctx))]
        far += [(t_oth0 + i, False, None) for i in reversed(range(n_own))]
        far += [(t_own0 + i, True, i) for i in reversed(range(n_own))]
        for i in range(max(len(near), len(far))):
            if i < len(far):
                step(1, *far[i])
            if i < len(near):
                step(0, *near[i])


def small_inputs(inp, half):
    ig = np.asarray(inp["m_ig_b"]); fg = np.asarray(inp["m_fg_b"]); cv = np.asarray(inp["m_conv"])
    if half == 1:
        ig, fg, cv = ig[:, ::-1, :], fg[:, ::-1, :], cv[:, ::-1, :]
    out = {
        "m_ig_b": np.ascontiguousarray(ig.reshape(DEPTH, 8)), "m_fg_b": np.ascontiguousarray(fg.reshape(DEPTH, 8)),
        "m_conv": np.ascontiguousarray(cv.reshape(DEPTH, 3, 8, 128).transpose(0, 3, 2, 1).reshape(DEPTH, 128, 24)),
    }
    for k in ("a_qn_g", "a_kn_g", "c_sink", "m_norm_g", "norm1_g", "norm2_g", "b_rg", "b_re"):
        out[k] = np.ascontiguousarray(inp[k])
    out["final_g"] = np.ascontiguousarray(np.asarray(inp["final_g"])[None, :])
    return out


def _tiles_oc(cfg, x_own, xc, o_own, o_ctx, need_ctx):
    tl = [(t, t * 128, x_own[t * 128:(t + 1) * 128, :], 0, o_own[t * 128:(t + 1) * 128, :]) for t in range(cfg.nt_own)]
    if need_ctx:
        tl += [(cfg.nt_own + i, cfg.O_CTX + i * 128, xc[i * 128:(i + 1) * 128, :], 1, o_ctx[i * 128:(i + 1) * 128, :])
               for i in range(cfg.nt_ctx)]
    return tl


def phase_merge(fw, cfg, l, S, A, W, x_own, xc, o_own, o_ctx, need_ctx):
    nc = fw.nc
    To, Tc = cfg.T_OWN, cfg.T_CTX
    T_OC = To + Tc
    tiles = _tiles_oc(cfg, x_own, xc, o_own, o_ctx, need_ctx)
    with Phase(fw, "mg1") as ph:
        identB = ph.sb([128, 128], BF16)
        b_id = make_ident(fw, identB)
        gmn = ph.sb([128, 512], F32)
        b_gmn = Buf()
        load_bc(fw, "sp", gmn[:, :], A["m_norm_g"][l:l + 1, :], b_gmn)
        AT = ph.sb([128, 8, T_OC], BF16)
        CT = ph.sb([128, 4, T_OC], BF16)
        b_AT, b_CT = bufs(2)
        fw.dma("sp", AT[:, :, :], S.OT[0:1024, :].rearrange("(k p) t -> p k t", p=128), writes=[b_AT])
        fw.dma("sp", CT[:, :, :], S.OT[1536:2048, :].rearrange("(k p) t -> p k t", p=128), writes=[b_CT])
        Wa = ph.sb([128, 8, D], BF16)
        Wm = ph.sb([128, 4, D], BF16)
        Wc = ph.sb([128, 4, D], BF16)
        b_Wa, b_Wm, b_Wc = bufs(3)
        fw.dma("pool", Wa[:, :, :], W["w_br_a"].rearrange("(k p) c -> p k c", p=128), writes=[b_Wa])
        fw.dma("pool", Wm[:, :, :], W["w_br_m"].rearrange("(k p) c -> p k c", p=128), writes=[b_Wm])
        fw.dma("pool", Wc[:, :, :], W["w_br_c"].rearrange("(k p) c -> p k c", p=128), writes=[b_Wc])
        h0 = [ph.sb([128, 512], F32) for _ in range(2)]
        h1 = [ph.sb([128, 512], F32) for _ in range(2)]
        mo = [ph.sb([128, 512], BF16) for _ in range(2)]
        gg = [ph.sb([128, 3 * D], BF16) for _ in range(2)]
        b_h0, b_h1, b_mo, b_gg = bufs(2), bufs(2), bufs(2), bufs(2)
        sq = ph.sb([128, 512], F32)
        ss = ph.sb([128, 4], F32)
        sig = ph.sb([128, 512], F32)
        omb = ph.sb([128, 512], BF16)
        omT = [ph.sb([128, 4, 128], BF16) for _ in range(2)]
        z32 = ph.sb([128, 512], F32)
        t32 = ph.sb([128, 512], F32)
        zb = [ph.sb([128, D], BF16) for _ in range(2)]
        zTs = [ph.sb([128, NK, 128], BF16) for _ in range(2)]
        b_sq, b_ss, b_sig, b_omb, b_z32, b_t32 = bufs(6)
        b_omT, b_zb, b_zTs = bufs(2), bufs(2), bufs(2)
        ptr = [ph.ps([128, 512], BF16) for _ in range(2)]
        b_ptr = bufs(2)
        pa = [ph.ps([128, 512]) for _ in range(2)]
        pm = [ph.ps([128, 512]) for _ in range(2)]
        pc = [ph.ps([128, 512]) for _ in range(2)]
        b_pa, b_pm, b_pc = bufs(2), bufs(2), bufs(2)
        tp = 0
        pq = 0
        for n, (t, prow, xsrc, m, dst) in enumerate(tiles):
            s = n % 2
            tok = slice(t * 128, (t + 1) * 128)
            fw.dma("sp", h0[s][:, :], S.HD[0, t * 128:(t + 1) * 128, :], writes=[b_h0[s]])
            fw.dma("sp", h1[s][:, :], S.HD[1, t * 128:(t + 1) * 128, :], writes=[b_h1[s]])
            fw.dma("sp", mo[s][:, :], S.P[prow:prow + 128, C_MO:C_MO + 512], writes=[b_mo[s]])
            fw.dma("sp", gg[s][:, :], S.P[prow:prow + 128, C_G:C_G + 3 * D], writes=[b_gg[s]])
            fw.op("dve", lambda e: e.tensor_tensor(out=h0[s][:, :], in0=h0[s][:, :], in1=h1[s][:, :], op=ALU.add),
                  reads=[b_h0[s], b_h1[s]], writes=[b_h0[s]])
            fw.op("dve", lambda e: e.tensor_tensor(out=sq[:, :], in0=h0[s][:, :], in1=h0[s][:, :], op=ALU.mult),
                  reads=[b_h0[s]], writes=[b_sq])
            fw.op("dve", lambda e: e.tensor_reduce(out=ss[:, :], in_=sq[:, :].rearrange("p (h d) -> p h d", h=4), axis=AX.X, op=ALU.add),
                  reads=[b_sq], writes=[b_ss])
            _rstd(fw, ss[:, :], b_ss, 128)
            h3 = h0[s][:, :].rearrange("p (h d) -> p h d", h=4)
            fw.op("dve", lambda e: e.tensor_tensor(out=h3, in0=h3, in1=ss[:, :].unsqueeze(2).to_broadcast([128, 4, 128]), op=ALU.mult),
                  reads=[b_h0[s], b_ss], writes=[b_h0[s]])
            fw.op("dve", lambda e: e.tensor_tensor(out=h0[s][:, :], in0=h0[s][:, :], in1=gmn[:, :], op=ALU.mult),
                  reads=[b_h0[s], b_gmn], writes=[b_h0[s]])
            fw.op("act", lambda e: e.activation(out=sig[:, :], in_=mo[s][:, :], func=AF.Sigmoid), reads=[b_mo[s]], writes=[b_sig])
            fw.op("dve", lambda e: e.tensor_tensor(out=omb[:, :], in0=h0[s][:, :], in1=sig[:, :], op=ALU.mult),
                  reads=[b_h0[s], b_sig], writes=[b_omb])
            p = tp % 2
            tp += 1

            def tr(e):
                for k in range(4):
                    ins = e.transpose(ptr[p][:, k * 128:(k + 1) * 128], omb[:, k * 128:(k + 1) * 128], identB[:, :])
                return ins
            fw.op("pe", tr, reads=[b_omb, b_id], writes=[b_ptr[p]])
            fw.op("act", lambda e: e.copy(out=omT[s][:, :, :], in_=ptr[p][:, :].rearrange("p (k t) -> p k t", k=4)),
                  reads=[b_ptr[p]], writes=[b_omT[s]])
            fw.op("act", lambda e: e.activation(out=gg[s][:, :], in_=gg[s][:, :], func=AF.Sigmoid), reads=[b_gg[s]], writes=[b_gg[s]])
            for cb in range(4):
                q = pq % 2
                pq += 1
                cs = slice(cb * 512, (cb + 1) * 512)

                def mma(e):
                    for k in range(8):
                        ins = e.matmul(pa[q][:, :], lhsT=AT[:, k, tok], rhs=Wa[:, k, cs], start=(k == 0), stop=(k == 7))
                    return ins

                def mmm(e):
                    for k in range(4):
                        ins = e.matmul(pm[q][:, :], lhsT=omT[s][:, k, :], rhs=Wm[:, k, cs], start=(k == 0), stop=(k == 3))
                    return ins

                def mmc(e):
                    for k in range(4):
                        ins = e.matmul(pc[q][:, :], lhsT=CT[:, k, tok], rhs=Wc[:, k, cs], start=(k == 0), stop=(k == 3))
                    return ins
                fw.op("pe", mma, reads=[b_AT, b_Wa], writes=[b_pa[q]])
                fw.op("pe", mmm, reads=[b_omT[s], b_Wm], writes=[b_pm[q]])
                fw.op("pe", mmc, reads=[b_CT, b_Wc], writes=[b_pc[q]])
                fw.op("dve", lambda e: e.tensor_tensor(out=z32[:, :], in0=pa[q][:, :], in1=gg[s][:, cb * 512:(cb + 1) * 512], op=ALU.mult),
                      reads=[b_pa[q], b_gg[s]], writes=[b_z32])
                fw.op("dve", lambda e: e.tensor_tensor(out=t32[:, :], in0=pm[q][:, :], in1=gg[s][:, D + cb * 512:D + (cb + 1) * 512],
                                                       op=ALU.mult), reads=[b_pm[q], b_gg[s]], writes=[b_t32])
                fw.op("pool", lambda e: e.tensor_tensor(out=z32[:, :], in0=z32[:, :], in1=t32[:, :], op=ALU.add),
                      reads=[b_z32, b_t32], writes=[b_z32])
                fw.op("dve", lambda e: e.tensor_tensor(out=t32[:, :], in0=pc[q][:, :], in1=gg[s][:, 2 * D + cb * 512:2 * D + (cb + 1) * 512],
                                                       op=ALU.mult), reads=[b_pc[q], b_gg[s]], writes=[b_t32])
                fw.op("pool", lambda e: e.tensor_tensor(out=zb[s][:, cs], in0=z32[:, :], in1=t32[:, :], op=ALU.add),
                      reads=[b_z32, b_t32], writes=[b_zb[s]])
            for k4 in range(NK // 4):
                p = tp % 2
                tp += 1

                def tr2(e):
                    for kk in range(4):
                        k = k4 * 4 + kk
                        ins = e.transpose(ptr[p][:, kk * 128:(kk + 1) * 128], zb[s][:, k * 128:(k + 1) * 128], identB[:, :])
                    return ins
                fw.op("pe", tr2, reads=[b_zb[s], b_id], writes=[b_ptr[p]])
                fw.op("act", lambda e: e.copy(out=zTs[s][:, k4 * 4:(k4 + 1) * 4, :], in_=ptr[p][:, :].rearrange("p (k t) -> p k t", k=4)),
                      reads=[b_ptr[p]], writes=[b_zTs[s]])
            fw.dma("sp", S.ZT[:, tok].rearrange("(k p) t -> p k t", p=128), zTs[s][:, :, :], reads=[b_zTs[s]])
    with Phase(fw, "mg2") as ph:
        ZTs = ph.sb([128, NK, T_OC], BF16)
        Wo = ph.sb([128, NK, D], BF16)
        b_Z, b_Wo = bufs(2)
        fw.dma("sp", ZTs[:, :, :], S.ZT.rearrange("(k p) t -> p k t", p=128), writes=[b_Z])
        fw.dma("pool", Wo[:, :, :], W["w_out"].rearrange("(k p) c -> p k c", p=128), writes=[b_Wo])
        g1 = [ph.sb([128, D], F32) for _ in range(2)]
        b_g1 = bufs(2)
        for m in range(2):
            load_bc(fw, "sp", g1[m][:, :], S.mods[l][m:m + 1, 2 * D:3 * D], b_g1[m])
        xt = [ph.sb([128, D], F32) for _ in range(2)]
        xn = [ph.sb([128, D], F32) for _ in range(2)]
        b_xt, b_xn = bufs(2), bufs(2)
        py = [ph.ps([128, 512]) for _ in range(4)]
        b_py = bufs(4)
        pq = 0
        for n, (t, prow, xsrc, m, dst) in enumerate(tiles):
            s = n % 2
            tok = slice(t * 128, (t + 1) * 128)
            fw.dma("sp", xt[s][:, :], xsrc, writes=[b_xt[s]])
            for cb in range(4):
                q = pq % 4
                pq += 1
                cs = slice(cb * 512, (cb + 1) * 512)

                def mmo(e):
                    for k in range(NK):
                        ins = e.matmul(py[q][:, :], lhsT=ZTs[:, k, tok], rhs=Wo[:, k, cs], start=(k == 0), stop=(k == NK - 1))
                    return ins
                fw.op("pe", mmo, reads=[b_Z, b_Wo], writes=[b_py[q]])
                fw.op("dve", lambda e: e.tensor_tensor(out=xn[s][:, cs], in0=py[q][:, :], in1=g1[m][:, cs], op=ALU.mult),
                      reads=[b_py[q], b_g1[m]], writes=[b_xn[s]])
                fw.op("pool", lambda e: e.tensor_tensor(out=xn[s][:, cs], in0=xn[s][:, cs], in1=xt[s][:, cs], op=ALU.add),
                      reads=[b_xn[s], b_xt[s]], writes=[b_xn[s]])
            fw.dma("sp", dst, xn[s][:, :], reads=[b_xn[s]])


def phase_moe_router(fw, cfg, l, S, A, W, o_own, o_ctx, need_ctx):
    nc = fw.nc
    tiles = _tiles_oc(cfg, o_own, o_ctx, o_own, o_ctx, need_ctx)
    BIG = 1.0e4
    with Phase(fw, "mr") as ph:
        identB = ph.sb([128, 128], BF16)
        b_id = make_ident(fw, identB)
        gain = [ph.sb([128, D], F32) for _ in range(2)]
        shift = [ph.sb([128, D], F32) for _ in range(2)]
        tmpg = ph.sb([128, D], F32)
        b_gain, b_shift = bufs(2), bufs(2)
        b_tmp = Buf()
        load_bc(fw, "sp", tmpg[:, :], A["norm2_g"][l:l + 1, :], b_tmp)
        for m in range(2):
            load_bc(fw, "sp", shift[m][:, :], S.mods[l][m:m + 1, 3 * D:4 * D], b_shift[m])
            load_bc(fw, "sp", gain[m][:, :], S.mods[l][m:m + 1, 4 * D:5 * D], b_gain[m])
            fw.op("dve", lambda e: e.scalar_tensor_tensor(out=gain[m][:, :], in0=gain[m][:, :], scalar=1.0, in1=tmpg[:, :],
                                                          op0=ALU.add, op1=ALU.mult), reads=[b_gain[m], b_tmp], writes=[b_gain[m]])
        wr = ph.sb([128, NK, 20], F32)
        wrh = ph.sb([128, NK, 20], BF16)
        wrl = ph.sb([128, NK, 20], BF16)
        brb = ph.sb([128, 20], F32)
        b_wr, b_wrh, b_wrl, b_brb, b_brb2 = bufs(5)
        fw.dma("sp", wr[:, :, :], W["w_r"].rearrange("(k p) c -> p k c", p=128), writes=[b_wr])
        load_bc(fw, "sp", brb[:, 0:4], A["b_rg"][l:l + 1, :], b_brb)
        load_bc(fw, "sp", brb[:, 4:20], A["b_re"][l:l + 1, :], b_brb2)
        fw.op("act", lambda e: e.copy(out=wrh[:, :, :], in_=wr[:, :, :]), reads=[b_wr], writes=[b_wrh])
        fw.op("dve", lambda e: e.tensor_tensor(out=wrl[:, :, :], in0=wr[:, :, :], in1=wrh[:, :, :], op=ALU.subtract),
              reads=[b_wr, b_wrh], writes=[b_wrl])
        xt = [ph.sb([128, D], F32) for _ in range(2)]
        b_xt = bufs(2)
        junk = ph.sb([128, D], BF16)
        h32 = ph.sb([128, D], F32)
        hb = [ph.sb([128, D], BF16) for _ in range(2)]
        lb = [ph.sb([128, D], BF16) for _ in range(2)]
        hTt = [ph.sb([128, NK, 128], BF16) for _ in range(2)]
        lTt = [ph.sb([128, NK, 128], BF16) for _ in range(2)]
        ss = [ph.sb([128, 1], F32) for _ in range(2)]
        b_junk, b_h32 = bufs(2)
        b_hb, b_lb, b_hTt, b_lTt, b_ss = bufs(2), bufs(2), bufs(2), bufs(2), bufs(2)
        ptr = [ph.ps([128, 512], BF16) for _ in range(2)]
        b_ptr = bufs(2)
        plg = [ph.ps([128, 512]) for _ in range(2)]
        b_plg = bufs(2)
        lg = ph.sb([128, 20], F32)
        wk = ph.sb([128, 64], F32)
        cmb = [ph.sb([128, 16], F32) for _ in range(2)]
        b_lg, b_wk = bufs(2)
        b_cmb = bufs(2)
        tp = 0
        for n, (t, prow, xsrc, m, dst) in enumerate(tiles):
            s = n % 2
            tok = slice(t * 128, (t + 1) * 128)
            fw.dma("sp", xt[s][:, :], xsrc, writes=[b_xt[s]])
            fw.op("pool", lambda e: e.memset(ss[s][:, :], 0.0), writes=[b_ss[s]])
            fw.op("act", lambda e: e.activation(out=junk[:, :], in_=xt[s][:, :], func=AF.Square, accum_out=ss[s][:, 0:1]),
                  reads=[b_xt[s], b_ss[s]], writes=[b_junk, b_ss[s]])
            _rstd(fw, ss[s][:, :], b_ss[s], D)
            fw.op("dve", lambda e: e.scalar_tensor_tensor(out=h32[:, :], in0=xt[s][:, :], scalar=ss[s][:, 0:1], in1=gain[m][:, :],
                                                          op0=ALU.mult, op1=ALU.mult), reads=[b_xt[s], b_ss[s], b_gain[m]], writes=[b_h32])
            fw.op("pool", lambda e: e.tensor_tensor(out=h32[:, :], in0=h32[:, :], in1=shift[m][:, :], op=ALU.add),
                  reads=[b_h32, b_shift[m]], writes=[b_h32])
            fw.op("act", lambda e: e.copy(out=hb[s][:, :], in_=h32[:, :]), reads=[b_h32], writes=[b_hb[s]])
            fw.op("dve", lambda e: e.tensor_tensor(out=lb[s][:, :], in0=h32[:, :], in1=hb[s][:, :], op=ALU.subtract),
                  reads=[b_h32, b_hb[s]], writes=[b_lb[s]])
            for (srcb, b_src, dstT, b_dst) in ((hb[s], b_hb[s], hTt[s], b_hTt[s]), (lb[s], b_lb[s], lTt[s], b_lTt[s])):
                for k4 in range(NK // 4):
                    p = tp % 2
                    tp += 1

                    def tr(e):
                        for kk in range(4):
                            k = k4 * 4 + kk
                            ins = e.transpose(ptr[p][:, kk * 128:(kk + 1) * 128], srcb[:, k * 128:(k + 1) * 128], identB[:, :])
                        return ins
                    fw.op("pe", tr, reads=[b_src, b_id], writes=[b_ptr[p]])
                    fw.op("act", lambda e: e.copy(out=dstT[:, k4 * 4:(k4 + 1) * 4, :], in_=ptr[p][:, :].rearrange("p (k t) -> p k t", k=4)),
                          reads=[b_ptr[p]], writes=[b_dst])
            fw.dma("sp", S.HT2[:, tok].rearrange("(k p) t -> p k t", p=128), hTt[s][:, :, :], reads=[b_hTt[s]])

            def mml(e):
                i = 0
                for (L, R) in ((hTt[s], wrh), (lTt[s], wrh), (hTt[s], wrl)):
                    for k in range(NK):
                        ins = e.matmul(plg[s][:, 0:20], lhsT=L[:, k, :], rhs=R[:, k, :], start=(i == 0), stop=(i == 3 * NK - 1))
                        i += 1
                return ins
            fw.op("pe", mml, reads=[b_hTt[s], b_lTt[s], b_wrh, b_wrl], writes=[b_plg[s]])
            fw.op("dve", lambda e: e.tensor_tensor(out=lg[:, :], in0=plg[s][:, 0:20], in1=brb[:, :], op=ALU.add),
                  reads=[b_plg[s], b_brb, b_brb2], writes=[b_lg])
            GL, EL = lg[:, 0:4], lg[:, 4:20]
            gmax, gsum, oh, pg = wk[:, 0:1], wk[:, 1:2], wk[:, 4:8], wk[:, 2:3]
            esel, m1, m2 = wk[:, 8:24], wk[:, 3:4], wk[:, 24:25]
            mk1, mk2, e2 = wk[:, 32:48], wk[:, 48:64], wk[:, 8:24]
            w1, w2 = wk[:, 25:26], wk[:, 26:27]
            ops = [
                lambda e: e.tensor_reduce(out=gmax, in_=GL, axis=AX.X, op=ALU.max),
                lambda e: e.tensor_scalar(out=oh, in0=GL, scalar1=gmax, scalar2=None, op0=ALU.is_ge),
                lambda e: e.tensor_scalar(out=wk[:, 28:32], in0=GL, scalar1=gmax, scalar2=None, op0=ALU.subtract),
            ]
            for f in ops:
                fw.op("dve", f, reads=[b_lg, b_wk], writes=[b_wk])
            fw.op("act", lambda e: e.activation(out=wk[:, 28:32], in_=wk[:, 28:32], func=AF.Exp), reads=[b_wk], writes=[b_wk])
            ops = [
                lambda e: e.tensor_reduce(out=gsum, in_=wk[:, 28:32], axis=AX.X, op=ALU.add),
                lambda e: e.reciprocal(out=pg, in_=gsum),
                lambda e: e.tensor_scalar(out=wk[:, 28:32], in0=oh, scalar1=BIG, scalar2=-BIG, op0=ALU.mult, op1=ALU.add),
                lambda e: e.tensor_tensor(out=esel.rearrange("p (g x) -> p g x", g=4), in0=EL.rearrange("p (g x) -> p g x", g=4),
                                          in1=wk[:, 28:32].unsqueeze(2).to_broadcast([128, 4, 4]), op=ALU.add),
                lambda e: e.tensor_reduce(out=m1, in_=esel, axis=AX.X, op=ALU.max),
                lambda e: e.tensor_scalar(out=mk1, in0=esel, scalar1=m1, scalar2=None, op0=ALU.is_ge),
                lambda e: e.scalar_tensor_tensor(out=e2, in0=mk1, scalar=-BIG, in1=esel, op0=ALU.mult, op1=ALU.add),
                lambda e: e.tensor_reduce(out=m2, in_=e2, axis=AX.X, op=ALU.max),
                lambda e: e.tensor_scalar(out=mk2, in0=e2, scalar1=m2, scalar2=None, op0=ALU.is_ge),
                lambda e: e.tensor_tensor(out=w2, in0=m2, in1=m1, op=ALU.subtract),
            ]
            for f in ops:
                fw.op("dve", f, reads=[b_lg, b_wk], writes=[b_wk])
            fw.op("act", lambda e: e.activation(out=w2, in_=w2, func=AF.Exp), reads=[b_wk], writes=[b_wk])
            ops = [
                lambda e: e.tensor_scalar(out=w1, in0=w2, scalar1=1.0, scalar2=None, op0=ALU.add),
                lambda e: e.reciprocal(out=w1, in_=w1),
                lambda e: e.tensor_tensor(out=w2, in0=w2, in1=w1, op=ALU.mult),
                lambda e: e.tensor_tensor(out=w1, in0=w1, in1=pg, op=ALU.mult),
                lambda e: e.tensor_tensor(out=w2, in0=w2, in1=pg, op=ALU.mult),
                lambda e: e.tensor_scalar(out=mk1, in0=mk1, scalar1=w1, scalar2=None, op0=ALU.mult),
            ]
            for f in ops:
                fw.op("dve", f, reads=[b_wk], writes=[b_wk])
            fw.op("dve", lambda e: e.scalar_tensor_tensor(out=cmb[s][:, :], in0=mk2, scalar=w2, in1=mk1, op0=ALU.mult, op1=ALU.add),
                  reads=[b_wk], writes=[b_cmb[s]])
            fw.dma("sp", S.COMB[tok, :], cmb[s][:, :], reads=[b_cmb[s]])


def phase_moe_experts(fw, cfg, l, S, A, W, o_own, o_ctx, x2_own, x2_ctx, need_ctx, final_out=None):
    nc = fw.nc
    To, Tc = cfg.T_OWN, cfg.T_CTX
    tiles = _tiles_oc(cfg, o_own, o_ctx, x2_own, x2_ctx, need_ctx)
    SB = 4
    with Phase(fw, "mx") as ph:
        identF = ph.sb([128, 128], F32)
        b_idF = make_ident(fw, identF)
        sel = ph.sb([16, 16, 128], BF16)
        b_sel = Buf()
        fw.op("pool", lambda e: e.memset(sel[:, :, :], 0.0), writes=[b_sel])
        fw.op("pool", lambda e: e.affine_select(out=sel[:, :, :], in_=sel[:, :, :], pattern=[[-1, 16], [0, 128]], compare_op=ALU.not_equal,
                                                fill=1.0, base=0, channel_multiplier=1), reads=[b_sel], writes=[b_sel])
        g2 = [ph.sb([128, D], F32) for _ in range(2)]
        b_g2 = bufs(2)
        for m in range(2):
            load_bc(fw, "sp", g2[m][:, :], S.mods[l][m:m + 1, 5 * D:6 * D], b_g2[m])
        fgb = ph.sb([128, D], F32)
        b_fgb = Buf()
        if final_out is not None:
            load_bc(fw, "sp", fgb[:, :], A["final_g"][0:1, :], b_fgb)
        acc = ph.sb([128, SB, D], F32)
        hT = ph.sb([128, NK, SB * 128], BF16)
        cm = ph.sb([128, SB, 16], F32)
        cmT = ph.sb([16, SB * 128], BF16)
        cbc = [ph.sb([128, SB * 128], F32) for _ in range(2)]
        Wg = [ph.sb([128, NK, 512], BF16) for _ in range(2)]
        Wu = [ph.sb([128, NK, 512], BF16) for _ in range(2)]
        Wd = [ph.sb([128, 4, D], BF16) for _ in range(2)]
        midT = [ph.sb([128, 4, SB * 128], BF16) for _ in range(2)]
        sa = ph.sb([128, 512], F32)
        xt = [ph.sb([128, D], F32) for _ in range(2)]
        junk = ph.sb([128, D], BF16)
        ss = ph.sb([128, 1], F32)
        b_acc, b_hT, b_cm, b_cmT, b_sa, b_junk, b_ss = bufs(7)
        b_cbc, b_Wg, b_Wu, b_Wd, b_midT, b_xt = bufs(2), bufs(2), bufs(2), bufs(2), bufs(2), bufs(2)
        pa = [ph.ps([128, 512]) for _ in range(2)]
        pu = [ph.ps([128, 512]) for _ in range(2)]
        po = [ph.ps([128, 512]) for _ in range(3)]
        pcb = ph.ps([128, 512])
        b_pa, b_pu, b_po = bufs(2), bufs(2), bufs(3)
        b_pcb = Buf()
        wi = 0
        pi = 0
        oi = 0
        for sb0 in range(0, len(tiles), SB):
            tl = tiles[sb0:sb0 + SB]
            nt = len(tl)
            ntok = nt * 128
            tok0 = tl[0][0] * 128
            fw.dma("sp", hT[:, :, 0:ntok], S.HT2[:, tok0:tok0 + ntok].rearrange("(k p) t -> p k t", p=128), writes=[b_hT])
            fw.dma("sp", cm[:, 0:nt, :], S.COMB[tok0:tok0 + ntok, :].rearrange("(t p) c -> p t c", p=128), writes=[b_cm])
            for i in range(nt):
                fw.op("pe", lambda e: e.transpose(pcb[0:16, i * 128:(i + 1) * 128], cm[:, i, :], identF[:, :]),
                      reads=[b_cm, b_idF], writes=[b_pcb])
            fw.op("act", lambda e: e.copy(out=cmT[:, 0:ntok], in_=pcb[0:16, 0:ntok]), reads=[b_pcb], writes=[b_cmT])
            for ex in range(16):
                w = wi % 2
                wi += 1
                fw.dma("pool", Wg[w][:, :, :], W["w_gate"][ex].rearrange("(k p) f -> p k f", p=128), writes=[b_Wg[w]])
                fw.dma("pool", Wu[w][:, :, :], W["w_up"][ex].rearrange("(k p) f -> p k f", p=128), writes=[b_Wu[w]])
                fw.dma("pool", Wd[w][:, :, :], W["w_down"][ex].rearrange("(k p) c -> p k c", p=128), writes=[b_Wd[w]])
                for tb in range(0, ntok, 512):
                    tw = min(512, ntok - tb)
                    fw.op("pe", lambda e: e.matmul(pcb[:, 0:tw], lhsT=sel[:, ex, :], rhs=cmT[:, tb:tb + tw], start=True, stop=True),
                          reads=[b_sel, b_cmT], writes=[b_pcb])
                    fw.op("act", lambda e: e.copy(out=cbc[w][:, tb:tb + tw], in_=pcb[:, 0:tw]), reads=[b_pcb], writes=[b_cbc[w]])
                for fc in range(4):
                    for tb in range(0, ntok, 512):
                        tw = min(512, ntok - tb)
                        p = pi % 2
                        pi += 1

                        def mg(e):
                            for k in range(NK):
                                ins = e.matmul(pa[p][:, 0:tw], lhsT=Wg[w][:, k, fc * 128:(fc + 1) * 128], rhs=hT[:, k, tb:tb + tw],
                                               start=(k == 0), stop=(k == NK - 1))
                            return ins

                        def mu(e):
                            for k in range(NK):
                                ins = e.matmul(pu[p][:, 0:tw], lhsT=Wu[w][:, k, fc * 128:(fc + 1) * 128], rhs=hT[:, k, tb:tb + tw],
                                               start=(k == 0), stop=(k == NK - 1))
                            return ins
                        fw.op("pe", mg, reads=[b_Wg[w], b_hT], writes=[b_pa[p]])
                        fw.op("pe", mu, reads=[b_Wu[w], b_hT], writes=[b_pu[p]])
                        fw.op("act", lambda e: e.activation(out=sa[:, 0:tw], in_=pa[p][:, 0:tw], func=AF.Silu), reads=[b_pa[p]], writes=[b_sa])
                        fw.op("dve", lambda e: e.tensor_tensor(out=sa[:, 0:tw], in0=sa[:, 0:tw], in1=pu[p][:, 0:tw], op=ALU.mult),
                              reads=[b_sa, b_pu[p]], writes=[b_sa])
                        fw.op("pool", lambda e: e.tensor_tensor(out=midT[w][:, fc, tb:tb + tw], in0=sa[:, 0:tw], in1=cbc[w][:, tb:tb + tw],
                                                                op=ALU.mult), reads=[b_sa, b_cbc[w]], writes=[b_midT[w]])
                for i in range(nt):
                    for dc in range(4):
                        o = oi % 3
                        oi += 1

                        def md(e):
                            for fc in range(4):
                                ins = e.matmul(po[o][:, :], lhsT=midT[w][:, fc, i * 128:(i + 1) * 128], rhs=Wd[w][:, fc, dc * 512:(dc + 1) * 512],
                                               start=(fc == 0), stop=(fc == 3))
                            return ins
                        fw.op("pe", md, reads=[b_midT[w], b_Wd[w]], writes=[b_po[o]])
                        if ex == 0:
                            fw.op("act", lambda e: e.copy(out=acc[:, i, dc * 512:(dc + 1) * 512], in_=po[o][:, :]),
                                  reads=[b_po[o]], writes=[b_acc])
                        else:
                            fw.op("dve", lambda e: e.tensor_tensor(out=acc[:, i, dc * 512:(dc + 1) * 512], in0=acc[:, i, dc * 512:(dc + 1) * 512],
                                                                   in1=po[o][:, :], op=ALU.add), reads=[b_po[o], b_acc], writes=[b_acc])
            for i, (t, prow, xsrc, m, dst) in enumerate(tl):
                s = i % 2
                fw.dma("sp", xt[s][:, :], xsrc, writes=[b_xt[s]])
                fw.op("dve", lambda e: e.tensor_tensor(out=acc[:, i, :], in0=acc[:, i, :], in1=g2[m][:, :], op=ALU.mult),
                      reads=[b_acc, b_g2[m]], writes=[b_acc])
                fw.op("pool", lambda e: e.tensor_tensor(out=xt[s][:, :], in0=xt[s][:, :], in1=acc[:, i, :], op=ALU.add),
                      reads=[b_acc, b_xt[s]], writes=[b_xt[s]])
                if final_out is not None and m == 0:
                    fw.op("pool", lambda e: e.memset(ss[:, :], 0.0), writes=[b_ss])
                    fw.op("act", lambda e: e.activation(out=junk[:, :], in_=xt[s][:, :], func=AF.Square, accum_out=ss[:, 0:1]),
                          reads=[b_xt[s], b_ss], writes=[b_junk, b_ss])
                    _rstd(fw, ss[:, :], b_ss, D)
                    fw.op("dve", lambda e: e.scalar_tensor_tensor(out=xt[s][:, :], in0=xt[s][:, :], scalar=ss[:, 0:1], in1=fgb[:, :],
                                                                  op0=ALU.mult, op1=ALU.mult), reads=[b_xt[s], b_ss, b_fgb], writes=[b_xt[s]])
                    fw.dma("sp", final_out[t * 128:(t + 1) * 128, :], xt[s][:, :], reads=[b_xt[s]])
                else:
                    fw.dma("sp", dst, xt[s][:, :], reads=[b_xt[s]])


WEIGHT_KEYS = ("w_mod", "bmod2", "w_in", "g1row", "w_br_a", "w_br_m", "w_br_c", "w_out", "w_r", "w_gate", "w_up", "w_down")


def layer_weights(inp, l, half):
    w = layer_inputs(inp, l, half)
    for k in ("w_br_a", "w_br_m", "w_br_c", "w_out", "w_gate", "w_up", "w_down"):
        w[k] = np.ascontiguousarray(inp[k][l])
    w["w_r"] = np.ascontiguousarray(np.concatenate([np.asarray(inp["w_rg"][l]), np.asarray(inp["w_re"][l])], axis=1))
    return w


def emit_layer(fw, cfg, S, A, l, last, x_own, x_oth, xc, x2o, x2c, sfx=""):
    nc = fw.nc
    need_ctx = not last
    To, Tc = cfg.T_OWN, cfg.T_CTX
    x1o = nc.dram_tensor("x1o" + sfx, [To, D], F32, kind="Internal").ap()
    x1c = nc.dram_tensor("x1c" + sfx, [Tc, D], F32, kind="Internal").ap()
    W = {k: A[k + sfx] for k in WEIGHT_KEYS}
    phase_mods(fw, cfg, A["cvec"], W["w_mod"], W["bmod2"], S.mods[l])
    phase_inproj(fw, cfg, l, x_own, x_oth, xc, S.mods[l], W["g1row"], W["w_in"], S)
    phase_attn(fw, cfg, l, S, A, "a", need_ctx)
    phase_attn(fw, cfg, l, S, A, "c", need_ctx)
    phase_mlstm(fw, cfg, l, S, A, need_ctx)
    phase_merge(fw, cfg, l, S, A, W, x_own, xc, x1o, x1c, need_ctx)
    phase_moe_router(fw, cfg, l, S, A, W, x1o, x1c, need_ctx)
    phase_moe_experts(fw, cfg, l, S, A, W, x1o, x1c, x2o, x2c, need_ctx, final_out=(x2o if last else None))


def phase_exchange(fw, cfg, A, x2o, xoth):
    nc = fw.nc
    To = cfg.T_OWN
    nt = cfg.nt_own
    CH = 2
    nch = nt // CH
    Z = [nc.dram_tensor("xchgZ%d" % i, [2 * CH * 128, D], F32, kind="Internal").ap() for i in range(nch)]
    R = [nc.dram_tensor("xchgR%d" % i, [2 * CH * 128, D], F32, kind="Internal").ap() for i in range(nch)]
    b_Z, b_R = bufs(nch), bufs(nch)
    with Phase(fw, "xa") as ph:
        sv = ph.sb([128, 2], F32)
        b_sv = Buf()
        fw.dma("sp", sv[:, :], A["selv"], writes=[b_sv])
        xt = [ph.sb([128, D], F32) for _ in range(2)]
        z0 = [ph.sb([128, D], F32) for _ in range(2)]
        z1 = [ph.sb([128, D], F32) for _ in range(2)]
        b_xt, b_z0, b_z1 = bufs(2), bufs(2), bufs(2)
        for i in range(nt):
            s = i % 2
            c, t = i // CH, i % CH
            fw.dma("sp", xt[s][:, :], x2o[i * 128:(i + 1) * 128, :], writes=[b_xt[s]])
            fw.op("dve", lambda e: e.tensor_scalar(out=z0[s][:, :], in0=xt[s][:, :], scalar1=sv[:, 0:1], scalar2=None, op0=ALU.mult),
                  reads=[b_xt[s], b_sv], writes=[b_z0[s]])
            fw.op("pool", lambda e: e.tensor_scalar(out=z1[s][:, :], in0=xt[s][:, :], scalar1=sv[:, 1:2], scalar2=None, op0=ALU.mult),
                  reads=[b_xt[s], b_sv], writes=[b_z1[s]])
            fw.dma("sp", Z[c][t * 128:(t + 1) * 128, :], z0[s][:, :], reads=[b_z0[s]])
            fw.dma("sp", Z[c][CH * 128 + t * 128:CH * 128 + (t + 1) * 128, :], z1[s][:, :], reads=[b_z1[s]])
    for c in range(nch):
        fw.all_reduce(Z[c], R[c], [[0, 1], [2, 3], [4, 5], [6, 7]], writes=[b_R[c]])
    with Phase(fw, "xb") as ph:
        J = ph.sb([128, 2, 128], F32)
        b_J = Buf()
        fw.dma("sp", J[:, :, :], A["jsel"].rearrange("j r p -> r j p"), writes=[b_J])
        ra = [ph.sb([128, D], F32) for _ in range(2)]
        rb = [ph.sb([128, D], F32) for _ in range(2)]
        ot = [ph.sb([128, D], F32) for _ in range(2)]
        b_ra, b_rb, b_ot = bufs(2), bufs(2), bufs(2)
        pp = [ph.ps([128, 512]) for _ in range(4)]
        b_pp = bufs(4)
        pi = 0
        for i in range(nt):
            s = i % 2
            j = nt - 1 - i
            c, t = j // CH, j % CH
            fw.dma("sp", ra[s][:, :], R[c][t * 128:(t + 1) * 128, :], reads=[b_R[c]], writes=[b_ra[s]])
            fw.dma("sp", rb[s][:, :], R[c][CH * 128 + t * 128:CH * 128 + (t + 1) * 128, :], reads=[b_R[c]], writes=[b_rb[s]])
            for cb in range(4):
                p = pi % 4
                pi += 1
                cs = slice(cb * 512, (cb + 1) * 512)

                def mm(e):
                    e.matmul(pp[p][:, :], lhsT=J[:, 0, :], rhs=ra[s][:, cs], start=True, stop=False)
                    return e.matmul(pp[p][:, :], lhsT=J[:, 1, :], rhs=rb[s][:, cs], start=False, stop=True)
                fw.op("pe", mm, reads=[b_J, b_ra[s], b_rb[s]], writes=[b_pp[p]])
                if cb % 2 == 0:
                    fw.op("act", lambda e: e.copy(out=ot[s][:, cs], in_=pp[p][:, :]), reads=[b_pp[p]], writes=[b_ot[s]])
                else:
                    fw.op("dve", lambda e: e.tensor_copy(out=ot[s][:, cs], in_=pp[p][:, :]), reads=[b_pp[p]], writes=[b_ot[s]])
            fw.dma("sp", xoth[i * 128:(i + 1) * 128, :], ot[s][:, :], reads=[b_ot[s]])


def exchange_consts(half):
    selv = np.zeros((128, 2), np.float32)
    selv[:, half] = 1.0
    Jm = np.zeros((128, 128), np.float32)
    Jm[np.arange(128), 127 - np.arange(128)] = 1.0
    jsel = np.zeros((2, 128, 128), np.float32)
    jsel[1 - half] = Jm
    return {"selv": selv, "jsel": jsel}


def build_fused(cfg, shapes):
    nc = bass.Bass("TRN2", target_bir_lowering=False)
    fw = Fw(nc)
    A = {}
    for name, shp in shapes.items():
        A[name] = nc.dram_tensor(name, list(shp), F32, kind="ExternalInput").ap()
    S = Scratch(nc, cfg)
    To, Tc = cfg.T_OWN, cfg.T_CTX
    xm_o = nc.dram_tensor("xmid_o", [To, D], F32, kind="Internal").ap()
    xm_c = nc.dram_tensor("xmid_c", [Tc, D], F32, kind="Internal").ap()
    xm_oth = nc.dram_tensor("xmid_oth", [To, D], F32, kind="Internal").ap()
    out = nc.dram_tensor("out", [To, D], F32, kind="ExternalOutput").ap()
    dummy_c = nc.dram_tensor("xlast_c", [Tc, D], F32, kind="Internal").ap()
    emit_layer(fw, cfg, S, A, 0, False, A["x_own"], A["x_oth"], A["xc"], xm_o, xm_c, sfx="_0")
    phase_exchange(fw, cfg, A, xm_o, xm_oth)
    emit_layer(fw, cfg, S, A, 1, True, xm_o, xm_oth, xm_c, out, dummy_c, sfx="_1")
    fw.barrier()
    return nc


def build_layer(cfg, l, last, shapes):
    nc = bass.Bass("TRN2", target_bir_lowering=False)
    fw = Fw(nc)
    A = {}
    for name, shp in shapes.items():
        A[name] = nc.dram_tensor(name, list(shp), F32, kind="ExternalInput").ap()
    S = Scratch(nc, cfg)
    To, Tc = cfg.T_OWN, cfg.T_CTX
    x2o = nc.dram_tensor("x2o", [To, D], F32, kind="ExternalOutput").ap()
    x2c = nc.dram_tensor("x2c", [Tc, D], F32, kind="ExternalOutput").ap()
    emit_layer(fw, cfg, S, A, l, last, A["x_own"], A["x_oth"], A["xc"], x2o, x2c)
    fw.barrier()
    return nc


def run_fused(inp, cfg, cores):
    in_maps = []
    for (b, half) in cores:
        m = core_inputs(inp, cfg, b, half)
        m["rope"] = rope_table(cfg, half)
        m.update(small_inputs(inp, half))
        m.update(exchange_consts(half))
        for l in range(DEPTH):
            for k, v in layer_weights(inp, l, half).items():
                m[k + "_%d" % l] = v
        in_maps.append(m)
    shapes = {k: v.shape for k, v in in_maps[0].items()}
    nc = build_fused(cfg, shapes)
    res = run_bass_kernel_spmd(nc, in_maps, core_ids=list(range(len(cores))))
    To = cfg.T_OWN
    B = inp["x"].shape[0]
    x_new = np.zeros((B, 2 * To, D), np.float32)
    for (b, half), r in zip(cores, res.results):
        xo = np.asarray(r["out"], np.float32)
        if half == 0:
            x_new[b, :To] = xo
        else:
            x_new[b, To:] = xo[::-1]
    return x_new


def run_layer(inp, cfg, l, last, x_full, ctx_full, cores):
    in_maps = []
    cur = dict(inp)
    cur["x"] = x_full
    cur["ctx"] = ctx_full
    for (b, half) in cores:
        m = core_inputs(cur, cfg, b, half)
        m["rope"] = rope_table(cfg, half)
        m.update(small_inputs(inp, half))
        m.update(layer_weights(inp, l, half))
        in_maps.append(m)
    shapes = {k: v.shape for k, v in in_maps[0].items()}
    nc = build_layer(cfg, l, last, shapes)
    res = run_bass_kernel_spmd(nc, in_maps, core_ids=list(range(len(cores))))
    To = cfg.T_OWN
    B = x_full.shape[0]
    x_new = np.zeros((B, 2 * To, D), np.float32)
    c_new = np.zeros((B, cfg.T_CTX, D), np.float32)
    for (b, half), r in zip(cores, res.results):
        xo = np.asarray(r["x2o"], np.float32)
        if half == 0:
            x_new[b, :To] = xo
            c_new[b] = np.asarray(r["x2c"], np.float32)
        else:
            x_new[b, To:] = xo[::-1]
    return x_new, c_new


def kernel(**inputs):
    inp = {k: np.asarray(v) for k, v in inputs.items()}
    cfg = Cfg(nt_own=16, nt_ctx=2)
    cores = [(b, half) for b in range(4) for half in range(2)]
    return run_fused(inp, cfg, cores).astype(np.float32)
```

```python
from contextlib import ExitStack
import numpy as np
import concourse.bass as bass
import concourse.mybir as mybir
from concourse.bass_utils import run_bass_kernel_spmd

F32 = mybir.dt.float32
BF16 = mybir.dt.bfloat16
AF = mybir.ActivationFunctionType
ALU = mybir.AluOpType
AX = mybir.AxisListType

D = 2048
NK = D // 128
DEPTH = 2
EPS = 1e-6
NEG = -30000.0

ENGS = ("pe", "act", "dve", "pool", "sp")
RING = 12


class Buf:
    __slots__ = ("w", "r")

    def __init__(self):
        self.w = None
        self.r = {}


def bufs(n):
    return [Buf() for _ in range(n)]


class Fw:
    def __init__(self, nc):
        self.nc = nc
        self.e = dict(pe=nc.tensor, act=nc.scalar, dve=nc.vector, pool=nc.gpsimd, sp=nc.sync)
        self.semh = {}
        for k in ENGS:
            self.semh["e_" + k] = nc.alloc_semaphore("s_" + k)
        self.cnt = {k: 0 for k in ENGS}
        self.known = {k: {} for k in ENGS}
        self.rings = {}
        self.ring_n = {}
        self.ring_val = {}
        for q in ("sp", "pool", "act"):
            self.rings[q] = []
            self.ring_n[q] = 0
            for i in range(RING):
                key = "r_%s_%d" % (q, i)
                self.semh[key] = nc.alloc_semaphore(key)
                self.rings[q].append(key)
                self.ring_val[key] = 0

    def _wait(self, eng, dep):
        key, val, _ = dep
        if val <= 0 or self.known[eng].get(key, 0) >= val:
            return
        self.e[eng].wait_ge(self.semh[key], val)
        self.known[eng][key] = val

    def _sync(self, eng, issuer, reads, writes):
        for b in reads:
            if b.w is not None:
                self._wait(issuer, b.w)
        for b in writes:
            if b.w is not None and b.w[2] != eng:
                self._wait(issuer, b.w)
            for key, (val, pe) in b.r.items():
                if pe != eng:
                    self._wait(issuer, (key, val, pe))

    @staticmethod
    def _mark(dep, reads, writes):
        for b in reads:
            b.r[dep[0]] = (dep[1], dep[2])
        for b in writes:
            b.w = dep
            b.r = {}

    def op(self, eng, fn, reads=(), writes=()):
        self._sync(eng, eng, reads, writes)
        ins = fn(self.e[eng])
        self.cnt[eng] += 1
        ins.then_inc(self.semh["e_" + eng], 1)
        dep = ("e_" + eng, self.cnt[eng], eng)
        self._mark(dep, reads, writes)
        return dep

    def dma(self, q, out, in_, reads=(), writes=(), **kw):
        self._sync("dma", q, reads, writes)
        n = self.ring_n[q]
        self.ring_n[q] = n + 1
        key = self.rings[q][n % RING]
        prev = self.ring_val[key]
        self._wait(q, (key, prev, "dma"))
        self.e[q].dma_start(out=out, in_=in_, **kw).then_inc(self.semh[key], 16)
        self.ring_val[key] = prev + 16
        dep = (key, prev + 16, "dma")
        self._mark(dep, reads, writes)
        return dep

    def all_reduce(self, src, dst, groups, reads=(), writes=()):
        q = "pool"
        self._sync("dma", q, reads, writes)
        if "cc" not in self.semh:
            self.semh["cc"] = self.nc.alloc_semaphore("cc_sem")
            self.ring_val["cc"] = 0
        prev = self.ring_val["cc"]
        self._wait(q, ("cc", prev, "dma"))
        self.e[q].collective_compute("AllReduce", ALU.add, replica_groups=groups, ins=[src], outs=[dst]).then_inc(self.semh["cc"])
        self.ring_val["cc"] = prev + 1
        dep = ("cc", prev + 1, "dma")
        self._mark(dep, reads, writes)
        return dep

    def barrier(self):
        for k in ENGS:
            if k != "sp":
                self._wait("sp", ("e_" + k, self.cnt[k], k))
        for key, val in self.ring_val.items():
            self._wait("sp", (key, val, "dma"))
        self.e["sp"].sem_inc(self.semh["e_sp"], 1)
        self.cnt["sp"] += 1
        for k in ENGS:
            if k != "sp":
                self._wait(k, ("e_sp", self.cnt["sp"], "sp"))
        for k in ENGS:
            for kk in ENGS:
                self.known[k]["e_" + kk] = self.cnt[kk]
            for key, val in self.ring_val.items():
                self.known[k][key] = val


class Phase:
    def __init__(self, fw, name):
        self.fw = fw
        self.nc = fw.nc
        fw.nphase = getattr(fw, "nphase", 0) + 1
        self.name = "%s%d" % (name, fw.nphase)
        self.es = ExitStack()
        self.i = 0

    def __enter__(self):
        self.es.__enter__()
        return self

    def __exit__(self, *a):
        self.fw.barrier()
        return self.es.__exit__(*a)

    def sb(self, shape, dt):
        self.i += 1
        return self.es.enter_context(self.nc.sbuf_tensor("%s_s%d" % (self.name, self.i), list(shape), dt))

    def ps(self, shape, dt=F32):
        self.i += 1
        return self.es.enter_context(self.nc.psum_tensor("%s_p%d" % (self.name, self.i), list(shape), dt))


def make_ident(fw, t, n=128):
    b = Buf()
    fw.op("pool", lambda e: e.memset(t[:, :], 0.0), writes=[b])
    fw.op("pool", lambda e: e.affine_select(out=t[:, :], in_=t[:, :], pattern=[[-1, n]],
                                            compare_op=ALU.not_equal, fill=1.0, base=0,
                                            channel_multiplier=1), reads=[b], writes=[b])
    return b


C_AQ, C_AKV, C_MQ, C_MK, C_MV, C_MO, C_CQ, C_CKV, C_G, C_MG = 0, 1024, 1536, 2048, 2560, 3072, 3584, 4096, 4608, 10752
P_IN = 10768


class Cfg:
    def __init__(self, nt_own=16, nt_ctx=2):
        self.nt_own = nt_own
        self.nt_oth = nt_own
        self.nt_ctx = nt_ctx
        self.T_OWN = 128 * nt_own
        self.T_CTX = 128 * nt_ctx
        self.T_ALL = 2 * self.T_OWN + self.T_CTX
        self.O_OWN, self.O_OTH, self.O_CTX = 0, self.T_OWN, 2 * self.T_OWN


def phase_mods(fw, cfg, cvec, w_mod_l, bmod2_l, mods_l):
    nc = fw.nc
    with Phase(fw, "mod") as ph:
        cc = ph.sb([128, NK * 2], F32)
        S = ph.sb([128, NK * 2], BF16)
        bm = ph.sb([2, 6 * D], F32)
        out = ph.sb([2, 6 * D], F32)
        W = [ph.sb([128, NK, 512], BF16) for _ in range(2)]
        pp = [ph.ps([128, 512]) for _ in range(2)]
        b_cc, b_S, b_bm, b_out = bufs(4)
        b_W = bufs(2)
        b_pp = bufs(2)
        fw.dma("sp", cc[:, :], cvec, writes=[b_cc])
        fw.dma("sp", bm[:, :], bmod2_l, writes=[b_bm])
        fw.op("act", lambda e: e.activation(out=S[:, :], in_=cc[:, :], func=AF.Silu), reads=[b_cc], writes=[b_S])
        wv = w_mod_l.rearrange("(k p) c -> p k c", p=128)
        nblk = 6 * D // 512
        for j in range(nblk):
            s = j % 2
            fw.dma("pool", W[s][:, :, :], wv[:, :, j * 512:(j + 1) * 512], writes=[b_W[s]])

            def mm(e, s=s):
                for k in range(NK):
                    ins = e.matmul(pp[s][0:2, :], lhsT=S[:, 2 * k:2 * k + 2], rhs=W[s][:, k, :],
                                   start=(k == 0), stop=(k == NK - 1))
                return ins
            fw.op("pe", mm, reads=[b_S, b_W[s]], writes=[b_pp[s]])
            fw.op("dve", lambda e, s=s, j=j: e.tensor_tensor(out=out[:, j * 512:(j + 1) * 512], in0=pp[s][0:2, :],
                                                              in1=bm[:, j * 512:(j + 1) * 512], op=ALU.add),
                  reads=[b_pp[s], b_bm], writes=[b_out])
        fw.dma("sp", mods_l, out[:, :], reads=[b_out])


def load_bc(fw, q, dst, src_row, wb):
    return fw.dma(q, dst, src_row.partition_broadcast(128), writes=[wb])


def phase_inproj(fw, cfg, l, x_own, x_oth, xc, mods_l, g1row, w_in_l, S):
    nc = fw.nc
    T_OC = cfg.T_OWN + cfg.T_CTX
    passes = [
        ("oc", [(x_own, i, 0) for i in range(cfg.nt_own)] + [(xc, i, 1) for i in range(cfg.nt_ctx)]),
        ("oth", [(x_oth, i, 0) for i in range(cfg.nt_oth)]),
    ]
    with Phase(fw, "inp") as ph:
        ident = ph.sb([128, 128], BF16)
        b_id = make_ident(fw, ident)
        gain = [ph.sb([128, D], F32) for _ in range(2)]
        shift = [ph.sb([128, D], F32) for _ in range(2)]
        tmpg = ph.sb([128, D], F32)
        b_gain, b_shift = bufs(2), bufs(2)
        b_tmp = Buf()
        load_bc(fw, "sp", tmpg[:, :], g1row, b_tmp)
        for m in range(2):
            load_bc(fw, "sp", shift[m][:, :], mods_l[m:m + 1, 0:D], b_shift[m])
            load_bc(fw, "sp", gain[m][:, :], mods_l[m:m + 1, D:2 * D], b_gain[m])
            fw.op("dve", lambda e, m=m: e.scalar_tensor_tensor(out=gain[m][:, :], in0=gain[m][:, :], scalar=1.0,
                                                                in1=tmpg[:, :], op0=ALU.add, op1=ALU.mult),
                  reads=[b_gain[m], b_tmp], writes=[b_gain[m]])
        hT = ph.sb([128, NK, T_OC], BF16)
        xt = [ph.sb([128, D], F32)] * 2
        b_xt = [Buf()] * 2
        junk = ph.sb([128, D], BF16)
        b_junk = Buf()
        y32 = ph.sb([128, D], F32)
        b_y32 = Buf()
        hb = [ph.sb([128, D], BF16)] * 2
        b_hb = [Buf()] * 2
        ss = [ph.sb([128, 1], F32) for _ in range(2)]
        b_ss = bufs(2)
        ptr = [ph.ps([128, 512], BF16) for _ in range(2)]
        b_ptr = bufs(2)
        pmm = [ph.ps([128, 512]) for _ in range(4)]
        b_pmm = bufs(4)
        W = [ph.sb([128, NK, 512], BF16) for _ in range(2)]
        b_W = bufs(2)
        stage = [ph.sb([128, T_OC // 128, 512], BF16) for _ in range(2)]
        b_stage = bufs(2)
        stg32 = ph.sb([128, T_OC // 128, 16], F32)
        b_stg32 = Buf()
        wv = w_in_l.rearrange("(k p) c -> p k c", p=128)
        wi = 0
        ti = 0
        pi = 0
        for pname, tiles in passes:
            nt = len(tiles)
            b_hT = bufs(nt)
            for t, (src, i, m) in enumerate(tiles):
                s = ti % 2
                ti += 1
                fw.dma("sp", xt[s][:, :], src[i * 128:(i + 1) * 128, :], writes=[b_xt[s]])
                fw.op("pool", lambda e, s=s: e.memset(ss[s][:, :], 0.0), writes=[b_ss[s]])
                fw.op("act", lambda e, s=s: e.activation(out=junk[:, :], in_=xt[s][:, :], func=AF.Square,
                                                         accum_out=ss[s][:, 0:1]),
                      reads=[b_xt[s], b_ss[s]], writes=[b_junk, b_ss[s]])
                fw.op("dve", lambda e, s=s: e.tensor_scalar(out=ss[s][:, :], in0=ss[s][:, :], scalar1=1.0 / D,
                                                            scalar2=EPS, op0=ALU.mult, op1=ALU.add),
                      reads=[b_ss[s]], writes=[b_ss[s]])
                fw.op("act", lambda e, s=s: e.sqrt(out=ss[s][:, :], in_=ss[s][:, :]), reads=[b_ss[s]], writes=[b_ss[s]])
                fw.op("dve", lambda e, s=s: e.reciprocal(out=ss[s][:, :], in_=ss[s][:, :]),
                      reads=[b_ss[s]], writes=[b_ss[s]])
                fw.op("dve", lambda e, s=s, m=m: e.scalar_tensor_tensor(out=y32[:, :], in0=xt[s][:, :],
                                                                        scalar=ss[s][:, 0:1], in1=gain[m][:, :],
                                                                        op0=ALU.mult, op1=ALU.mult),
                      reads=[b_xt[s], b_ss[s], b_gain[m]], writes=[b_y32])
                fw.op("pool", lambda e, s=s, m=m: e.tensor_tensor(out=hb[s][:, :], in0=y32[:, :], in1=shift[m][:, :],
                                                                  op=ALU.add),
                      reads=[b_y32, b_shift[m]], writes=[b_hb[s]])
                for k4 in range(NK // 4):
                    p = pi % 2
                    pi += 1

                    def tr(e, s=s, k4=k4, p=p):
                        for kk in range(4):
                            k = k4 * 4 + kk
                            ins = e.transpose(ptr[p][:, kk * 128:(kk + 1) * 128], hb[s][:, k * 128:(k + 1) * 128],
                                              ident[:, :])
                        return ins
                    fw.op("pe", tr, reads=[b_hb[s], b_id], writes=[b_ptr[p]])
                    fw.op("act", lambda e, k4=k4, p=p, t=t: e.copy(
                        out=hT[:, k4 * 4:(k4 + 1) * 4, t * 128:(t + 1) * 128],
                        in_=ptr[p][:, :].rearrange("p (k t) -> p k t", k=4)),
                        reads=[b_ptr[p]], writes=[b_hT[t]])
            if pname == "oc":
                blocks = [("tm", c0, 512) for c0 in range(0, C_MQ, 512)]
                blocks += [("fm", C_MQ, 512), ("fm", C_MK, 512)]
                blocks += [("tm", c0, 512) for c0 in range(C_MV, C_MG, 512)]
                blocks += [("tm32", C_MG, 16)]
                segs = [(0, cfg.nt_own, cfg.O_OWN), (cfg.nt_own, cfg.nt_ctx, cfg.O_CTX)]
            else:
                blocks = [("tm", C_AKV, 512), ("fm", C_MQ, 512), ("fm", C_MK, 512), ("tm", C_MV, 512),
                          ("tm", C_CKV, 512), ("tm32", C_MG, 16)]
                segs = [(0, cfg.nt_oth, cfg.O_OTH)]
            for kind, c0, cw in blocks:
                s = wi % 2
                wi += 1
                fw.dma("pool", W[s][:, :, 0:cw], wv[:, :, c0:c0 + cw], writes=[b_W[s]])
                if kind in ("tm", "tm32"):
                    st = stage[s] if kind == "tm" else stg32
                    bst = b_stage[s] if kind == "tm" else b_stg32
                    for t in range(nt):
                        p = pi % 4
                        pi += 1

                        def mm(e, s=s, t=t, p=p, cw=cw):
                            for k in range(NK):
                                ins = e.matmul(pmm[p][:, 0:cw], lhsT=hT[:, k, t * 128:(t + 1) * 128],
                                               rhs=W[s][:, k, 0:cw], start=(k == 0), stop=(k == NK - 1))
                            return ins
                        fw.op("pe", mm, reads=[b_hT[t], b_W[s]], writes=[b_pmm[p]])
                        ev = "act" if t % 2 == 0 else "dve"
                        if ev == "act":
                            fw.op("act", lambda e, t=t, p=p, cw=cw, st=st: e.copy(out=st[:, t, 0:cw], in_=pmm[p][:, 0:cw]),
                                  reads=[b_pmm[p]], writes=[bst])
                        else:
                            fw.op("dve", lambda e, t=t, p=p, cw=cw, st=st: e.tensor_copy(out=st[:, t, 0:cw],
                                                                                         in_=pmm[p][:, 0:cw]),
                                  reads=[b_pmm[p]], writes=[bst])
                    dst = S.tm(c0, cw)
                    for (t0, ntl, o) in segs:
                        fw.dma("sp", dst[o:o + ntl * 128, :].rearrange("(t p) c -> p t c", p=128),
                               st[:, t0:t0 + ntl, 0:cw], reads=[bst])
                else:
                    ntok = nt * 128 if (pname == "oc" or c0 == C_MK) else min(512, nt * 128)
                    stT = stage[s][:, :, :].rearrange("p t c -> p (t c)")
                    for cc in range(4):
                        for tb in range(0, ntok, 512):
                            tw = min(512, ntok - tb)
                            p = pi % 4
                            pi += 1

                            def mm(e, s=s, cc=cc, tb=tb, tw=tw, p=p):
                                for k in range(NK):
                                    ins = e.matmul(pmm[p][:, 0:tw], lhsT=W[s][:, k, cc * 128:(cc + 1) * 128],
                                                   rhs=hT[:, k, tb:tb + tw], start=(k == 0), stop=(k == NK - 1))
                                return ins
                            fw.op("pe", mm, reads=[b_hT[tt] for tt in range(tb // 128, (tb + tw) // 128)] + [b_W[s]],
                                  writes=[b_pmm[p]])
                            if (tb // 512) % 2 == 0:
                                fw.op("act", lambda e, tb=tb, tw=tw, p=p, cc=cc: e.copy(
                                    out=stT[:, cc * ntok + tb:cc * ntok + tb + tw], in_=pmm[p][:, 0:tw]),
                                    reads=[b_pmm[p]], writes=[b_stage[s]])
                            else:
                                fw.op("dve", lambda e, tb=tb, tw=tw, p=p, cc=cc: e.tensor_copy(
                                    out=stT[:, cc * ntok + tb:cc * ntok + tb + tw], in_=pmm[p][:, 0:tw]),
                                    reads=[b_pmm[p]], writes=[b_stage[s]])
                    dstT = S.fm(c0)
                    for (t0, ntl, o) in segs:
                        a0, a1 = t0 * 128, min((t0 + ntl) * 128, ntok)
                        if a1 <= a0:
                            continue
                        fw.dma("sp", dstT[:, o:o + a1 - a0].rearrange("(cc p) t -> p cc t", p=128),
                               stT[:, 0:4 * ntok].rearrange("p (cc t) -> p cc t", cc=4)[:, :, a0:a1],
                               reads=[b_stage[s]])


class Scratch:
    def __init__(self, nc, cfg, taps=()):
        self.nc = nc
        self.cfg = cfg
        T = cfg.T_ALL

        def dt(name, shape, dtype):
            kind = "ExternalOutput" if name in taps else "Internal"
            return nc.dram_tensor(name, list(shape), dtype, kind=kind).ap()
        self.mods = [dt("mods%d" % l, [2, 6 * D], F32) for l in range(DEPTH)]
        self.P = dt("P_tm", [T, C_MG], BF16)
        self.MG = dt("MG", [T, 16], F32)
        self.MQT = dt("MQT", [512, T], BF16)
        self.MKT = dt("MKT", [512, T], BF16)
        self.HT2 = dt("HT2", [2048, cfg.T_OWN + cfg.T_CTX], BF16)
        self.COMB = dt("COMB", [cfg.T_OWN + cfg.T_CTX, 16], F32)
        self.ZT = dt("ZT", [2048, cfg.T_OWN + cfg.T_CTX], BF16)
        self.HD = dt("HD", [2, cfg.T_OWN + cfg.T_CTX, 512], F32)
        self.OT = dt("OT", [2048, cfg.T_OWN + cfg.T_CTX], BF16)

    def tm(self, c0, cw):
        if c0 == C_MG:
            return self.MG
        return self.P[:, c0:c0 + cw]

    def fm(self, c0):
        return self.MQT if c0 == C_MQ else self.MKT


def _win_perm(half):
    mg = np.arange(3584, 3600).reshape(2, 2, 4)
    if half == 1:
        mg = mg[:, ::-1, :]
    return np.concatenate([np.arange(0, 3584), np.arange(3600, 10768), mg.reshape(-1)])


def core_inputs(inp, cfg, b, half):
    To = cfg.T_OWN
    seq = np.asarray(inp["x"][b][:2 * To])
    ctx = np.asarray(inp["ctx"][b][:cfg.T_CTX])
    if half == 1:
        seq = seq[::-1]
        ctx = ctx[::-1]
    cv = np.stack([np.asarray(inp["c"][b]).reshape(NK, 128).T, np.asarray(inp["c_ctx"]).reshape(NK, 128).T], axis=2)
    return {
        "x_own": np.ascontiguousarray(seq[:To]),
        "x_oth": np.ascontiguousarray(seq[To:]),
        "xc": np.ascontiguousarray(ctx),
        "cvec": np.ascontiguousarray(cv.reshape(128, 2 * NK)),
    }


def layer_inputs(inp, l, half):
    return {
        "w_mod": np.ascontiguousarray(inp["w_mod"][l]),
        "bmod2": np.ascontiguousarray(np.stack([inp["b_mod"][l], inp["b_mod"][l]], 0)),
        "w_in": np.ascontiguousarray(np.asarray(inp["w_in"][l])[:, _win_perm(half)]),
        "g1row": np.ascontiguousarray(np.asarray(inp["norm1_g"][l])[None, :]),
    }


def _rstd(fw, ss, b_ss, n):
    fw.op("dve", lambda e: e.tensor_scalar(out=ss, in0=ss, scalar1=1.0 / n, scalar2=EPS, op0=ALU.mult, op1=ALU.add),
          reads=[b_ss], writes=[b_ss])
    fw.op("act", lambda e: e.sqrt(out=ss, in_=ss), reads=[b_ss], writes=[b_ss])
    fw.op("dve", lambda e: e.reciprocal(out=ss, in_=ss), reads=[b_ss], writes=[b_ss])


def phase_attn(fw, cfg, l, S, A, kind, need_ctx):
    nc = fw.nc
    To, Tc = cfg.T_OWN, cfg.T_CTX
    if kind == "a":
        Hq, Hkv, cq0, ckv0, orow = 8, 2, C_AQ, C_AKV, 0
    else:
        Hq, Hkv, cq0, ckv0, orow = 4, 2, C_CQ, C_CKV, 1536
    G = Hq // Hkv
    scale = 128.0 ** -0.5
    QB = min(512, To)
    nsub = QB // 128
    ktiles = [(cfg.O_OWN + i * 128, i * 128) for i in range(cfg.nt_own)]
    if kind == "a":
        ktiles += [(cfg.O_OTH + i * 128, To + i * 128) for i in range(cfg.nt_oth)]
    else:
        ktiles += [(cfg.O_OTH, To)]
    n_lat_k = len(ktiles)
    ktiles += [(cfg.O_CTX + i * 128, None) for i in range(cfg.nt_ctx)]
    nkt = len(ktiles)
    qtiles = [(cfg.O_OWN + i * 128, i * 128) for i in range(cfg.nt_own)]
    if need_ctx:
        qtiles += [(cfg.O_CTX + i * 128, None) for i in range(cfg.nt_ctx)]
    nqt = len(qtiles)
    with Phase(fw, "at" + kind) as ph:
        ident = ph.sb([128, 128], BF16)
        b_id = make_ident(fw, ident)
        KT = ph.sb([128, Hkv, nkt * 128], BF16)
        VA = ph.sb([128, nkt, Hkv, 129], BF16)
        QT = ph.sb([128, Hq, nqt * 128], BF16)
        b_KT, b_VA, b_QT = bufs(nkt), bufs(nkt), bufs(nqt)
        b_va1 = Buf()
        fw.op("pool", lambda e: e.memset(VA[:, :, :, 128:129], 1.0), writes=[b_va1])
        gq = ph.sb([128, 128], F32)
        gk = ph.sb([128, 128], F32)
        b_g = Buf()
        b_g2 = Buf()
        if kind == "a":
            load_bc(fw, "sp", gq[:, :], A["a_qn_g"][l:l + 1, :], b_g)
            load_bc(fw, "sp", gk[:, :], A["a_kn_g"][l:l + 1, :], b_g2)
            fw.op("dve", lambda e: e.tensor_scalar(out=gq[:, :], in0=gq[:, :], scalar1=scale, scalar2=None, op0=ALU.mult),
                  reads=[b_g, b_g2], writes=[b_g])
        esink = ph.sb([128, 4], F32)
        b_es = Buf()
        if kind == "c":
            load_bc(fw, "sp", esink[:, :], A["c_sink"][l:l + 1, :], b_es)
            fw.op("act", lambda e: e.activation(out=esink[:, :], in_=esink[:, :], func=AF.Exp), reads=[b_es], writes=[b_es])
        masks = {}
        b_mask = Buf()
        if kind == "c":
            mk_t = ph.sb([128, nsub + 2, QB], BF16)
            fw.op("pool", lambda e: e.memset(mk_t[:, :, :], 1.0), writes=[b_mask])
            for r in range(-1, nsub + 1):
                mv = mk_t[:, r + 1, :]
                fw.op("pool", lambda e, mv=mv, r=r: e.affine_select(out=mv, in_=mv, pattern=[[1, QB]], compare_op=ALU.is_ge,
                                                                    fill=0.0, base=128 - r * 128, channel_multiplier=-1),
                      reads=[b_mask], writes=[b_mask])
                fw.op("pool", lambda e, mv=mv, r=r: e.affine_select(out=mv, in_=mv, pattern=[[-1, QB]], compare_op=ALU.is_ge,
                                                                    fill=0.0, base=128 + r * 128, channel_multiplier=1),
                      reads=[b_mask], writes=[b_mask])
                masks[r] = mv
        HM = max(Hq, 2 * Hkv)
        ld = [ph.sb([128, HM * 128], BF16) for _ in range(2)]
        b_ld = bufs(2)
        rp = [ph.sb([128, 128], F32) for _ in range(2)]
        b_rp = bufs(2)
        x32 = ph.sb([128, Hq * 128], F32)
        sq = ph.sb([128, Hq * 128], F32)
        t1 = ph.sb([128, Hq * 64], F32)
        t2 = ph.sb([128, Hq * 64], F32)
        xr = [ph.sb([128, Hq * 128], BF16) for _ in range(2)]
        b_x32, b_sq, b_t1, b_t2 = bufs(4)
        b_xr = bufs(2)
        ssn = ph.sb([128, Hq], F32)
        b_ssn = Buf()
        ptr = [ph.ps([128, 512], BF16) for _ in range(2)]
        b_ptr = bufs(2)
        cnt = {"ld": 0, "tr": 0, "xr": 0}

        def prep(row, rrow, c0, H, gt, dests, vdest=None):
            s = cnt["ld"] % 2
            cnt["ld"] += 1
            w = H * 128 + (Hkv * 128 if vdest is not None else 0)
            fw.dma("sp", ld[s][:, 0:w], S.P[row:row + 128, c0:c0 + w], writes=[b_ld[s]])
            if rrow is not None:
                fw.dma("sp", rp[s][:, :], A["rope"][rrow:rrow + 128, :], writes=[b_rp[s]])
            if vdest is not None:
                fw.op("pool", lambda e: e.tensor_copy(out=vdest[0], in_=ld[s][:, H * 128:w].rearrange("p (h d) -> p h d", h=Hkv)),
                      reads=[b_ld[s], b_va1], writes=[vdest[1]])
            xs = x32[:, 0:H * 128]
            if gt is not None:
                fw.op("act", lambda e: e.copy(out=xs, in_=ld[s][:, 0:H * 128]), reads=[b_ld[s]], writes=[b_x32])
                fw.op("dve", lambda e: e.tensor_tensor(out=sq[:, 0:H * 128], in0=xs, in1=xs, op=ALU.mult),
                      reads=[b_x32], writes=[b_sq])
                fw.op("dve", lambda e: e.tensor_reduce(out=ssn[:, 0:H], in_=sq[:, 0:H * 128].rearrange("p (h d) -> p h d", h=H),
                                                       axis=AX.X, op=ALU.add), reads=[b_sq], writes=[b_ssn])
                _rstd(fw, ssn[:, 0:H], b_ssn, 128)
                x3 = xs.rearrange("p (h d) -> p h d", h=H)
                fw.op("dve", lambda e: e.tensor_tensor(out=x3, in0=x3, in1=ssn[:, 0:H].unsqueeze(2).to_broadcast([128, H, 128]),
                                                       op=ALU.mult), reads=[b_x32, b_ssn], writes=[b_x32])
                fw.op("dve", lambda e: e.tensor_tensor(out=x3, in0=x3, in1=gt[:, :].unsqueeze(1).to_broadcast([128, H, 128]),
                                                       op=ALU.mult), reads=[b_x32, b_g], writes=[b_x32])
            else:
                sc = scale if dests[0][2] == "q" else 1.0
                fw.op("act", lambda e: e.activation(out=xs, in_=ld[s][:, 0:H * 128], func=AF.Copy, scale=sc),
                      reads=[b_ld[s]], writes=[b_x32])
            xi = cnt["xr"] % 2
            cnt["xr"] += 1
            xo = xr[xi][:, 0:H * 128]
            if rrow is not None:
                x5 = xs.rearrange("p (h a b f) -> p h a b f", h=H, a=2, b=2)
                o5 = xo.rearrange("p (h a b f) -> p h a b f", h=H, a=2, b=2)
                cs = rp[s][:, 0:64].rearrange("p (a f) -> p a f", a=2).unsqueeze(1).to_broadcast([128, H, 2, 32])
                sn = rp[s][:, 64:128].rearrange("p (a f) -> p a f", a=2).unsqueeze(1).to_broadcast([128, H, 2, 32])
                x1, x2 = x5[:, :, :, 0, :], x5[:, :, :, 1, :]
                u1 = t1[:, 0:H * 64].rearrange("p (h a f) -> p h a f", h=H, a=2)
                u2 = t2[:, 0:H * 64].rearrange("p (h a f) -> p h a f", h=H, a=2)
                fw.op("dve", lambda e: e.tensor_tensor(out=u1, in0=x1, in1=cs, op=ALU.mult), reads=[b_x32, b_rp[s]], writes=[b_t1])
                fw.op("dve", lambda e: e.tensor_tensor(out=u2, in0=x2, in1=sn, op=ALU.mult), reads=[b_x32, b_rp[s]], writes=[b_t2])
                fw.op("dve", lambda e: e.tensor_tensor(out=o5[:, :, :, 0, :], in0=u1, in1=u2, op=ALU.subtract),
                      reads=[b_t1, b_t2], writes=[b_xr[xi]])
                fw.op("dve", lambda e: e.tensor_tensor(out=u1, in0=x2, in1=cs, op=ALU.mult), reads=[b_x32, b_rp[s]], writes=[b_t1])
                fw.op("dve", lambda e: e.tensor_tensor(out=u2, in0=x1, in1=sn, op=ALU.mult), reads=[b_x32, b_rp[s]], writes=[b_t2])
                fw.op("dve", lambda e: e.tensor_tensor(out=o5[:, :, :, 1, :], in0=u1, in1=u2, op=ALU.add),
                      reads=[b_t1, b_t2], writes=[b_xr[xi]])
            else:
                fw.op("dve", lambda e: e.tensor_copy(out=xo, in_=xs), reads=[b_x32], writes=[b_xr[xi]])
            for h0 in range(0, H, 4):
                hn = min(4, H - h0)
                p = cnt["tr"] % 2
                cnt["tr"] += 1

                def tr(e):
                    for hh in range(hn):
                        ins = e.transpose(ptr[p][:, hh * 128:(hh + 1) * 128], xo[:, (h0 + hh) * 128:(h0 + hh + 1) * 128], ident[:, :])
                    return ins
                fw.op("pe", tr, reads=[b_xr[xi], b_id], writes=[b_ptr[p]])
                fw.op("act", lambda e: e.copy(out=dests[0][0][:, h0:h0 + hn, dests[0][3]:dests[0][3] + 128],
                                              in_=ptr[p][:, 0:hn * 128].rearrange("p (h t) -> p h t", h=hn)),
                      reads=[b_ptr[p]], writes=[dests[0][1]])

        for ki, (row, rrow) in enumerate(ktiles):
            prep(row, rrow, ckv0, Hkv, gk if kind == "a" else None, [(KT, b_KT[ki], "k", ki * 128)],
                 vdest=(VA[:, ki, :, 0:128], b_VA[ki]))
        for qi, (row, rrow) in enumerate(qtiles):
            prep(row, rrow, cq0, Hq, gq if kind == "a" else None, [(QT, b_QT[qi], "q", qi * 128)])

        if getattr(S, "dbg", None) and kind in S.dbg:
            dq = nc.dram_tensor("dbgQT" + kind, [128, Hq, nqt * 128], BF16, kind="ExternalOutput").ap()
            dk = nc.dram_tensor("dbgKT" + kind, [128, Hkv, nkt * 128], BF16, kind="ExternalOutput").ap()
            dv = nc.dram_tensor("dbgVA" + kind, [128, nkt, Hkv, 129], BF16, kind="ExternalOutput").ap()
            fw.dma("sp", dq, QT[:, :, :], reads=b_QT)
            fw.dma("sp", dk, KT[:, :, :], reads=b_KT)
            fw.dma("sp", dv, VA[:, :, :, :], reads=b_VA + [b_va1])
        pst = [ph.ps([128, 512]) for _ in range(2)]
        b_pst = bufs(2)
        po = [ph.ps([128, 512]) for _ in range(4)]
        b_po = bufs(4)
        PT = [ph.sb([128, 512], BF16) for _ in range(3)]
        b_PT = bufs(3)
        rden = ph.sb([128, 4], F32)
        b_rden = Buf()
        on = [ph.sb([128, 4, 128], BF16) for _ in range(2)]
        b_on = bufs(2)
        ost = [ph.sb([128, 512], BF16) for _ in range(2)]
        b_ost = bufs(2)
        it = {"s": 0, "p": 0, "o": 0}
        qblocks = []
        for q0 in range(0, To, QB):
            i0 = q0 // 128
            if kind == "a":
                kl = [(ki, None) for ki in range(nkt)]
            else:
                kl = []
                for r in range(-1, nsub + 1):
                    kt = i0 + r
                    if 0 <= kt <= cfg.nt_own:
                        kl.append((kt, r))
                kl += [(n_lat_k + i, None) for i in range(cfg.nt_ctx)]
            qblocks.append((q0, nsub, kl, q0))
        if need_ctx:
            qblocks.append((To, cfg.nt_ctx, [(n_lat_k + i, None) for i in range(cfg.nt_ctx)], To))
        for h in range(Hq):
            g = h // G
            for (q0, ns, kl, oc0) in qblocks:
                qw = ns * 128
                def emit_s(ii):
                    ki_, r_ = kl[ii]
                    sp__ = it["s"] % 2
                    it["s"] += 1
                    fw.op("pe", lambda e: e.matmul(pst[sp__][:, 0:qw], lhsT=KT[:, g, ki_ * 128:(ki_ + 1) * 128],
                                                   rhs=QT[:, h, q0:q0 + qw], start=True, stop=True),
                          reads=[b_KT[ki_]] + [b_QT[q0 // 128 + j] for j in range(ns)], writes=[b_pst[sp__]])
                    return sp__
                sq_ = [emit_s(0)]
                for idx, (ki, r) in enumerate(kl):
                    if idx + 1 < len(kl):
                        sq_.append(emit_s(idx + 1))
                    sp_ = sq_[idx]
                    pi_ = it["p"] % 3
                    it["p"] += 1
                    fw.op("act", lambda e: e.activation(out=PT[pi_][:, 0:qw], in_=pst[sp_][:, 0:qw], func=AF.Exp),
                          reads=[b_pst[sp_]], writes=[b_PT[pi_]])
                    if r is not None:
                        fw.op("pool", lambda e: e.tensor_tensor(out=PT[pi_][:, 0:qw], in0=PT[pi_][:, 0:qw], in1=masks[r][:, 0:qw],
                                                                op=ALU.mult), reads=[b_PT[pi_], b_mask], writes=[b_PT[pi_]])

                    def pv(e):
                        ins = None
                        for j in range(ns):
                            if r is not None and abs(r - j) > 1:
                                continue
                            first = (idx == 0) if r is None else (ki == max(0, q0 // 128 + j - 1))
                            ins = e.matmul(po[j][:, 0:129], lhsT=PT[pi_][:, j * 128:(j + 1) * 128], rhs=VA[:, ki, g, :],
                                           start=first, stop=(idx == len(kl) - 1))
                        return ins
                    fw.op("pe", pv, reads=[b_PT[pi_], b_VA[ki], b_va1], writes=b_po[0:ns])
                oi = it["o"] % 2
                it["o"] += 1
                for j in range(ns):
                    if kind == "c":
                        fw.op("dve", lambda e: e.tensor_scalar(out=rden[:, j:j + 1], in0=po[j][:, 128:129],
                                                               scalar1=esink[:, h:h + 1], scalar2=None, op0=ALU.add),
                              reads=[b_po[j], b_es], writes=[b_rden])
                        fw.op("dve", lambda e: e.reciprocal(out=rden[:, j:j + 1], in_=rden[:, j:j + 1]),
                              reads=[b_rden], writes=[b_rden])
                    else:
                        fw.op("dve", lambda e: e.reciprocal(out=rden[:, j:j + 1], in_=po[j][:, 128:129]),
                              reads=[b_po[j]], writes=[b_rden])
                    fw.op("dve", lambda e: e.tensor_scalar(out=on[oi][:, j, :], in0=po[j][:, 0:128], scalar1=rden[:, j:j + 1],
                                                           scalar2=None, op0=ALU.mult),
                          reads=[b_po[j], b_rden], writes=[b_on[oi]])
                p = cnt["tr"] % 2
                cnt["tr"] += 1

                def tr2(e):
                    for j in range(ns):
                        ins = e.transpose(ptr[p][:, j * 128:(j + 1) * 128], on[oi][:, j, :], ident[:, :])
                    return ins
                fw.op("pe", tr2, reads=[b_on[oi], b_id], writes=[b_ptr[p]])
                fw.op("act", lambda e: e.copy(out=ost[oi][:, 0:qw], in_=ptr[p][:, 0:qw]), reads=[b_ptr[p]], writes=[b_ost[oi]])
                fw.dma("sp", S.OT[orow + h * 128:orow + (h + 1) * 128, oc0:oc0 + qw], ost[oi][:, 0:qw], reads=[b_ost[oi]])


def rope_table(cfg, half):
    n = 2 * cfg.T_OWN
    t = np.arange(n)
    if half == 1:
        t = n - 1 - t
    rows = (t // 64).astype(np.float32)
    cols = (t % 64).astype(np.float32)
    inv = (10000.0 ** (-np.arange(0, 64, 2, dtype=np.float32) / 64.0)).astype(np.float32)
    ang = np.concatenate([rows[:, None] * inv, cols[:, None] * inv], axis=-1).astype(np.float32)
    return np.ascontiguousarray(np.concatenate([np.cos(ang), np.sin(ang)], axis=1).astype(np.float32))


def phase_mlstm(fw, cfg, l, S, A, need_ctx):
    nc = fw.nc
    To, Tc, TA = cfg.T_OWN, cfg.T_CTX, cfg.T_ALL
    NT = TA // 128
    T_OC = To + Tc
    kscale = 128.0 ** -0.5
    with Phase(fw, "ml") as ph:
        identB = ph.sb([128, 128], BF16)
        b_idB = make_ident(fw, identB)
        identF = ph.sb([128, 128], F32)
        b_idF = make_ident(fw, identF)
        ones = ph.sb([128, 128], F32)
        U = [ph.sb([128, 128], F32) for _ in range(2)]
        NEGM = [ph.sb([128, 128], F32) for _ in range(2)]
        Sel = [ph.sb([128, 128], F32) for _ in range(2)]
        Bsel = ph.sb([64, 4], F32)
        b_c = Buf()
        fw.op("pool", lambda e: e.memset(ones[:, :], 1.0), writes=[b_c])
        for d in range(2):
            fw.op("pool", lambda e: e.memset(U[d][:, :], 1.0), writes=[b_c])
            fw.op("pool", lambda e: e.memset(NEGM[d][:, :], 0.0), writes=[b_c])
            fw.op("pool", lambda e: e.memset(Sel[d][:, :], 1.0), writes=[b_c])
        fw.op("pool", lambda e: e.memset(Bsel[:, :], 0.0), writes=[b_c])
        sg = [1, -1]
        for d in range(2):
            fw.op("pool", lambda e: e.affine_select(out=U[d][:, :], in_=U[d][:, :], pattern=[[sg[d], 128]], compare_op=ALU.is_ge,
                                                    fill=0.0, base=0, channel_multiplier=-sg[d]), reads=[b_c], writes=[b_c])
            fw.op("pool", lambda e: e.affine_select(out=NEGM[d][:, :], in_=NEGM[d][:, :], pattern=[[-sg[d], 128]],
                                                    compare_op=ALU.is_ge, fill=NEG, base=0, channel_multiplier=sg[d]),
                  reads=[b_c], writes=[b_c])
            last = 127 if d == 0 else 0
            fw.op("pool", lambda e: e.affine_select(out=Sel[d][:, :], in_=Sel[d][:, :], pattern=[[0, 128]], compare_op=ALU.is_equal,
                                                    fill=0.0, base=-last, channel_multiplier=1), reads=[b_c], writes=[b_c])
        for base in (0, -32):
            fw.op("pool", lambda e: e.affine_select(out=Bsel[:, :], in_=Bsel[:, :], pattern=[[-1, 4]], compare_op=ALU.not_equal,
                                                    fill=1.0, base=base, channel_multiplier=1), reads=[b_c], writes=[b_c])
        G = ph.sb([128, NT, 16], F32)
        IC = ph.sb([128, NT, 8], F32)
        LF = ph.sb([128, NT, 8], F32)
        gb = ph.sb([128, 16], F32)
        b_G, b_gate, b_gb, b_gb2 = bufs(4)
        fw.dma("sp", G[:, :, :], S.MG.rearrange("(t p) c -> p t c", p=128), writes=[b_G])
        load_bc(fw, "sp", gb[:, 0:8], A["m_ig_b"][l:l + 1, :], b_gb)
        load_bc(fw, "sp", gb[:, 8:16], A["m_fg_b"][l:l + 1, :], b_gb2)
        fw.op("dve", lambda e: e.tensor_tensor(out=IC[:, :, :], in0=G[:, :, 0:8], in1=gb[:, 0:8].unsqueeze(1).to_broadcast([128, NT, 8]),
                                               op=ALU.add), reads=[b_G, b_gb], writes=[b_gate])
        fw.op("dve", lambda e: e.tensor_tensor(out=LF[:, :, :], in0=G[:, :, 8:16], in1=gb[:, 8:16].unsqueeze(1).to_broadcast([128, NT, 8]),
                                               op=ALU.add), reads=[b_G, b_gb2, b_gate], writes=[b_gate])
        fw.op("act", lambda e: e.activation(out=LF[:, :, :], in_=LF[:, :, :], func=AF.Exp, scale=-1.0), reads=[b_gate], writes=[b_gate])
        fw.op("act", lambda e: e.activation(out=LF[:, :, :], in_=LF[:, :, :], func=AF.Ln, bias=1.0), reads=[b_gate], writes=[b_gate])
        fw.op("dve", lambda e: e.tensor_scalar(out=LF[:, :, :], in0=LF[:, :, :], scalar1=-1.0, scalar2=None, op0=ALU.mult),
              reads=[b_gate], writes=[b_gate])
        qT = ph.sb([128, 4, T_OC], BF16)
        kT = ph.sb([128, 4, T_OC], BF16)
        Ktm = ph.sb([128, NT, 4, 128], BF16)
        VA = ph.sb([128, NT, 4, 129], BF16)
        b_qT, b_kT, b_Ktm, b_VA = bufs(4)
        fw.op("pool", lambda e: e.memset(VA[:, :, :, 128:129], 1.0), writes=[b_VA])
        for h in range(4):
            fw.dma("sp", VA[:, :, h, 0:128], S.P[:, C_MV + h * 128:C_MV + (h + 1) * 128].rearrange("(t p) d -> p t d", p=128),
                   reads=[b_VA], writes=[b_VA])
        cw = ph.sb([128, 8, 3], F32)
        b_cw = Buf()
        fw.dma("sp", cw[:, :, :].rearrange("p c k -> p (c k)"), A["m_conv"][l], writes=[b_cw])
        with Phase(fw, "mlc") as pc:
            X = [pc.sb([128, TA], BF16) for _ in range(2)]
            acc = [pc.sb([128, TA], F32) for _ in range(2)]
            ktmp = pc.sb([128, TA], BF16)
            b_X, b_acc = bufs(2), bufs(2)
            b_ktmp = Buf()
            ptr = [pc.ps([128, 512], BF16) for _ in range(2)]
            b_ptr = bufs(2)
            n = 0
            tp = 0
            for qk in range(2):
                src = S.MQT if qk == 0 else S.MKT
                for hc in range(4):
                    s = n % 2
                    n += 1
                    fw.dma("sp", X[s][:, :], src[hc * 128:(hc + 1) * 128, :], writes=[b_X[s]])
                    w = cw[:, qk * 4 + hc, :]
                    for (a, b) in ((0, 2 * To), (2 * To, TA)):
                        fw.op("dve", lambda e: e.tensor_scalar(out=acc[s][:, a:b], in0=X[s][:, a:b], scalar1=w[:, 1:2], scalar2=None,
                                                               op0=ALU.mult), reads=[b_X[s], b_cw], writes=[b_acc[s]])
                        fw.op("dve", lambda e: e.scalar_tensor_tensor(out=acc[s][:, a + 1:b], in0=X[s][:, a:b - 1], scalar=w[:, 0:1],
                                                                      in1=acc[s][:, a + 1:b], op0=ALU.mult, op1=ALU.add),
                              reads=[b_X[s], b_cw, b_acc[s]], writes=[b_acc[s]])
                        fw.op("dve", lambda e: e.scalar_tensor_tensor(out=acc[s][:, a:b - 1], in0=X[s][:, a + 1:b], scalar=w[:, 2:3],
                                                                      in1=acc[s][:, a:b - 1], op0=ALU.mult, op1=ALU.add),
                              reads=[b_X[s], b_cw, b_acc[s]], writes=[b_acc[s]])
                    if qk == 0:
                        fw.op("act", lambda e: e.activation(out=qT[:, hc, 0:To], in_=acc[s][:, 0:To], func=AF.Silu),
                              reads=[b_acc[s]], writes=[b_qT])
                        fw.op("act", lambda e: e.activation(out=qT[:, hc, To:T_OC], in_=acc[s][:, 2 * To:TA], func=AF.Silu),
                              reads=[b_acc[s]], writes=[b_qT])
                    else:
                        fw.op("act", lambda e: e.activation(out=acc[s][:, :], in_=acc[s][:, :], func=AF.Silu),
                              reads=[b_acc[s]], writes=[b_acc[s]])
                        fw.op("dve", lambda e: e.tensor_scalar(out=ktmp[:, :], in0=acc[s][:, :], scalar1=kscale, scalar2=None,
                                                               op0=ALU.mult), reads=[b_acc[s]], writes=[b_ktmp])
                        fw.op("pool", lambda e: e.tensor_copy(out=kT[:, hc, 0:To], in_=ktmp[:, 0:To]), reads=[b_ktmp], writes=[b_kT])
                        fw.op("pool", lambda e: e.tensor_copy(out=kT[:, hc, To:T_OC], in_=ktmp[:, 2 * To:TA]), reads=[b_ktmp], writes=[b_kT])
                        for t0 in range(0, NT, 4):
                            tn = min(4, NT - t0)
                            p = tp % 2
                            tp += 1

                            def tr(e):
                                for tt in range(tn):
                                    ins = e.transpose(ptr[p][:, tt * 128:(tt + 1) * 128], ktmp[:, (t0 + tt) * 128:(t0 + tt + 1) * 128],
                                                      identB[:, :])
                                return ins
                            fw.op("pe", tr, reads=[b_ktmp, b_idB], writes=[b_ptr[p]])
                            fw.op("act", lambda e: e.copy(out=Ktm[:, t0:t0 + tn, hc, :],
                                                          in_=ptr[p][:, 0:tn * 128].rearrange("p (t d) -> p t d", t=tn)),
                                  reads=[b_ptr[p]], writes=[b_Ktm])
        pA = ph.ps([128, 512])
        pC = ph.ps([128, 512])
        pD = ph.ps([128, 512])
        pE = ph.ps([128, 512], BF16)
        pF = ph.ps([128, 512])
        pG = ph.ps([128, 512])
        b_pA, b_pC, b_pD, b_pE, b_pF, b_pG = bufs(6)
        Cst = [ph.sb([128, 4, 129], F32) for _ in range(2)]
        Cb = [ph.sb([128, 4, 129], BF16) for _ in range(2)]
        mst = [ph.sb([128, 4], F32) for _ in range(2)]
        b_C, b_Cb, b_m = bufs(2), bufs(2), bufs(2)
        for d in range(2):
            fw.op("pool", lambda e: e.memset(Cst[d][:, :, :], 0.0), writes=[b_C[d]])
            fw.op("pool", lambda e: e.memset(Cb[d][:, :, :], 0.0), writes=[b_Cb[d]])
            fw.op("pool", lambda e: e.memset(mst[d][:, :], NEG), writes=[b_m[d]])
        AB = ph.sb([128, 64], F32)
        R64 = ph.sb([64, 128], F32)
        X64 = ph.sb([64, 128], F32)
        Lall = ph.sb([64, 4, 128], F32)
        b_AB, b_R64, b_X64, b_Lall = bufs(4)
        fw.op("pool", lambda e: e.memset(AB[:, :], 0.0), writes=[b_AB])
        fw.op("pool", lambda e: e.memset(R64[:, :], 1.0), writes=[b_R64])
        fw.op("pool", lambda e: e.memset(X64[:, :], 1.0), writes=[b_X64])
        bsb = ph.sb([128, 4], F32)
        btot = ph.sb([128, 4], F32)
        bm = ph.sb([128, 4], F32)
        rowmax = ph.sb([128, 4], F32)
        mrow = ph.sb([128, 4], F32)
        E8 = ph.sb([128, 8], F32)
        small = ph.sb([128, 16], F32)
        mnew = ph.sb([128, 4], F32)
        ld = ph.sb([128, 4, 128], F32)
        Dm = ph.sb([128, 4, 128], F32)
        Wb = ph.sb([128, 4, 128], BF16)
        WT = ph.sb([128, 4, 128], BF16)
        kw = ph.sb([128, 4, 128], BF16)
        numt = ph.sb([128, 4, 129], F32)
        hout = [ph.sb([128, 4, 128], F32) for _ in range(2)]
        tmpC = ph.sb([128, 4, 129], F32)
        (b_bsb, b_btot, b_bm, b_rowmax, b_mrow, b_E8, b_small, b_mnew, b_ld, b_Dm, b_Wb, b_WT, b_kw, b_numt,
         b_tmpC) = bufs(15)
        b_hout = bufs(2)
        hcnt = [0]

        def v2(t):
            return t[:, 0:258].rearrange("p (h e) -> p h e", h=2)

        def step(d, ti, full, oc_tile):
            lf = LF[:, ti, d * 4:(d + 1) * 4]
            ic = IC[:, ti, d * 4:(d + 1) * 4]
            m = mst[d]

            def mm_b(e):
                e.matmul(pA[:, 0:4], lhsT=U[d][:, :], rhs=lf, start=True, stop=True)
                return e.matmul(pA[:, 4:8], lhsT=ones[:, :], rhs=lf, start=True, stop=True)
            fw.op("pe", mm_b, reads=[b_gate, b_c], writes=[b_pA])
            fw.op("dve", lambda e: e.tensor_copy(out=AB[:, 32:36], in_=pA[:, 0:4]), reads=[b_pA], writes=[b_AB, b_bsb])
            fw.op("dve", lambda e: e.tensor_tensor(out=AB[:, 0:4], in0=ic, in1=pA[:, 0:4], op=ALU.subtract),
                  reads=[b_pA, b_gate], writes=[b_AB])
            fw.op("act", lambda e: e.copy(out=btot[:, :], in_=pA[:, 4:8]), reads=[b_pA], writes=[b_btot])
            fw.op("dve", lambda e: e.tensor_tensor(out=bm[:, :], in0=AB[:, 32:36], in1=m[:, :], op=ALU.add),
                  reads=[b_AB, b_m[d]], writes=[b_bm])
            fw.op("pe", lambda e: e.transpose(pA[0:64, 16:144], AB[:, :], identF[:, :]), reads=[b_AB, b_idF], writes=[b_pA])
            fw.op("act", lambda e: e.copy(out=R64[0:32, :], in_=pA[0:32, 16:144]), reads=[b_pA], writes=[b_R64])
            fw.op("dve", lambda e: e.tensor_copy(out=X64[32:64, :], in_=pA[32:64, 16:144]), reads=[b_pA], writes=[b_X64])
            fw.op("dve", lambda e: e.tensor_tensor(out=Lall[:, :, :], in0=X64[:, :].unsqueeze(1).to_broadcast([64, 4, 128]),
                                                   in1=Bsel[:, :].unsqueeze(2).to_broadcast([64, 4, 128]), op=ALU.mult),
                  reads=[b_X64, b_c], writes=[b_Lall])

            def mm_ld(e):
                for h in range(4):
                    ins = e.matmul(pC[:, h * 128:(h + 1) * 128], lhsT=Lall[:, h, :], rhs=R64[:, :], start=True, stop=True)
                return ins
            fw.op("pe", mm_ld, reads=[b_Lall, b_R64], writes=[b_pC])
            fw.op("dve", lambda e: e.tensor_tensor(out=ld[:, :, :], in0=pC[:, :].rearrange("p (h s) -> p h s", h=4),
                                                   in1=NEGM[d][:, :].unsqueeze(1).to_broadcast([128, 4, 128]), op=ALU.add),
                  reads=[b_pC, b_c], writes=[b_ld])
            fw.op("dve", lambda e: e.tensor_reduce(out=rowmax[:, :], in_=ld[:, :, :], axis=AX.X, op=ALU.max),
                  reads=[b_ld], writes=[b_rowmax])
            if full:
                fw.op("dve", lambda e: e.tensor_tensor(out=mrow[:, :], in0=bm[:, :], in1=rowmax[:, :], op=ALU.max),
                      reads=[b_bm, b_rowmax], writes=[b_mrow])
                fw.op("dve", lambda e: e.tensor_tensor(out=ld[:, :, :], in0=ld[:, :, :],
                                                       in1=mrow[:, :].unsqueeze(2).to_broadcast([128, 4, 128]), op=ALU.subtract),
                      reads=[b_ld, b_mrow], writes=[b_ld])
                fw.op("dve", lambda e: e.tensor_scalar_max(out=ld[:, :, :], in0=ld[:, :, :], scalar1=-80.0), reads=[b_ld], writes=[b_ld])
                fw.op("act", lambda e: e.activation(out=Dm[:, :, :], in_=ld[:, :, :], func=AF.Exp), reads=[b_ld], writes=[b_Dm])
                oc0 = oc_tile * 128

                def mm_s(e):
                    for h in range(4):
                        ins = e.matmul(pD[:, h * 128:(h + 1) * 128], lhsT=qT[:, h, oc0:oc0 + 128], rhs=kT[:, h, oc0:oc0 + 128],
                                       start=True, stop=True)
                    return ins
                fw.op("pe", mm_s, reads=[b_qT, b_kT], writes=[b_pD])
                fw.op("dve", lambda e: e.tensor_tensor(out=Wb[:, :, :], in0=pD[:, :].rearrange("p (h s) -> p h s", h=4), in1=Dm[:, :, :],
                                                       op=ALU.mult), reads=[b_pD, b_Dm], writes=[b_Wb])

                def mm_t(e):
                    for h in range(4):
                        ins = e.transpose(pE[:, h * 128:(h + 1) * 128], Wb[:, h, :], identB[:, :])
                    return ins
                fw.op("pe", mm_t, reads=[b_Wb, b_idB], writes=[b_pE])
                fw.op("act", lambda e: e.copy(out=WT[:, :, :], in_=pE[:, :].rearrange("p (h j) -> p h j", h=4)), reads=[b_pE], writes=[b_WT])

                def mm_intra(e):
                    for h in range(4):
                        ins = e.matmul(v2(pF if h < 2 else pG)[:, h % 2, :], lhsT=WT[:, h, :], rhs=VA[:, ti, h, :], start=True, stop=True)
                    return ins
                fw.op("pe", mm_intra, reads=[b_WT, b_VA], writes=[b_pF, b_pG])

                def mm_inter(e):
                    for h in range(4):
                        ins = e.matmul(v2(pC if h < 2 else pD)[:, h % 2, :], lhsT=qT[:, h, oc0:oc0 + 128], rhs=Cb[d][:, h, :],
                                       start=True, stop=True)
                    return ins
                fw.op("pe", mm_inter, reads=[b_qT, b_Cb[d]], writes=[b_pC, b_pD])
                fw.op("dve", lambda e: e.tensor_tensor(out=E8[:, 0:4], in0=bm[:, :], in1=mrow[:, :], op=ALU.subtract),
                      reads=[b_bm, b_mrow], writes=[b_E8])
                fw.op("dve", lambda e: e.tensor_scalar(out=E8[:, 4:8], in0=mrow[:, :], scalar1=-1.0, scalar2=None, op0=ALU.mult),
                      reads=[b_mrow, b_E8], writes=[b_E8])
                fw.op("dve", lambda e: e.tensor_scalar_max(out=E8[:, :], in0=E8[:, :], scalar1=-80.0), reads=[b_E8], writes=[b_E8])
                fw.op("act", lambda e: e.activation(out=E8[:, :], in_=E8[:, :], func=AF.Exp), reads=[b_E8], writes=[b_E8])
                for hp, (pi_, pj_, bi_, bj_) in enumerate(((pC, pF, b_pC, b_pF), (pD, pG, b_pD, b_pG))):
                    hs = slice(hp * 2, hp * 2 + 2)
                    fw.op("dve", lambda e: e.tensor_tensor(out=numt[:, hs, :], in0=v2(pi_),
                                                           in1=E8[:, hs].unsqueeze(2).to_broadcast([128, 2, 129]), op=ALU.mult),
                          reads=[bi_, b_E8], writes=[b_numt])
                    fw.op("dve", lambda e: e.tensor_tensor(out=numt[:, hs, :], in0=numt[:, hs, :], in1=v2(pj_), op=ALU.add),
                          reads=[bj_, b_numt], writes=[b_numt])
                fw.op("dve", lambda e: e.tensor_scalar(out=small[:, 4:8], in0=numt[:, :, 128], scalar1=-1.0, scalar2=None, op0=ALU.mult),
                      reads=[b_numt], writes=[b_small])
                fw.op("dve", lambda e: e.tensor_tensor(out=small[:, 0:4], in0=numt[:, :, 128], in1=small[:, 4:8], op=ALU.max),
                      reads=[b_numt, b_small], writes=[b_small])
                fw.op("dve", lambda e: e.tensor_tensor(out=small[:, 0:4], in0=small[:, 0:4], in1=E8[:, 4:8], op=ALU.max),
                      reads=[b_small, b_E8], writes=[b_small])
                fw.op("dve", lambda e: e.reciprocal(out=small[:, 0:4], in_=small[:, 0:4]), reads=[b_small], writes=[b_small])
                hi = hcnt[0] % 2
                hcnt[0] += 1
                fw.op("dve", lambda e: e.tensor_tensor(out=hout[hi][:, :, :], in0=numt[:, :, 0:128],
                                                       in1=small[:, 0:4].unsqueeze(2).to_broadcast([128, 4, 128]), op=ALU.mult),
                      reads=[b_numt, b_small], writes=[b_hout[hi]])
                fw.dma("sp", S.HD[d, oc0:oc0 + 128, :], hout[hi][:, :, :].rearrange("p h e -> p (h e)"), reads=[b_hout[hi]])
            fw.op("pe", lambda e: e.matmul(pA[:, 8:12], lhsT=Sel[d][:, :], rhs=rowmax[:, :], start=True, stop=True),
                  reads=[b_rowmax, b_c], writes=[b_pA])
            fw.op("dve", lambda e: e.tensor_tensor(out=small[:, 8:12], in0=btot[:, :], in1=m[:, :], op=ALU.add),
                  reads=[b_btot, b_m[d]], writes=[b_small])
            fw.op("dve", lambda e: e.tensor_tensor(out=mnew[:, :], in0=small[:, 8:12], in1=pA[:, 8:12], op=ALU.max),
                  reads=[b_small, b_pA], writes=[b_mnew])
            fw.op("dve", lambda e: e.tensor_tensor(out=E8[:, 4:8], in0=small[:, 8:12], in1=mnew[:, :], op=ALU.subtract),
                  reads=[b_small, b_mnew, b_E8], writes=[b_E8])
            fw.op("dve", lambda e: e.tensor_tensor(out=E8[:, 0:4], in0=AB[:, 0:4], in1=btot[:, :], op=ALU.add),
                  reads=[b_AB, b_btot, b_E8], writes=[b_E8])
            fw.op("dve", lambda e: e.tensor_tensor(out=E8[:, 0:4], in0=E8[:, 0:4], in1=mnew[:, :], op=ALU.subtract),
                  reads=[b_E8, b_mnew], writes=[b_E8])
            fw.op("dve", lambda e: e.tensor_scalar_max(out=E8[:, :], in0=E8[:, :], scalar1=-80.0), reads=[b_E8], writes=[b_E8])
            fw.op("act", lambda e: e.activation(out=E8[:, :], in_=E8[:, :], func=AF.Exp), reads=[b_E8], writes=[b_E8])
            fw.op("dve", lambda e: e.tensor_tensor(out=kw[:, :, :], in0=Ktm[:, ti, :, :],
                                                   in1=E8[:, 0:4].unsqueeze(2).to_broadcast([128, 4, 128]), op=ALU.mult),
                  reads=[b_Ktm, b_E8], writes=[b_kw])

            def mm_dc(e):
                for h in range(4):
                    ins = e.matmul(v2(pF if h < 2 else pG)[:, h % 2, :], lhsT=kw[:, h, :], rhs=VA[:, ti, h, :], start=True, stop=True)
                return ins
            fw.op("pe", mm_dc, reads=[b_kw, b_VA], writes=[b_pF, b_pG])
            fw.op("dve", lambda e: e.tensor_tensor(out=tmpC[:, :, :], in0=Cst[d][:, :, :],
                                                   in1=E8[:, 4:8].unsqueeze(2).to_broadcast([128, 4, 129]), op=ALU.mult),
                  reads=[b_C[d], b_E8], writes=[b_tmpC])
            fw.op("dve", lambda e: e.tensor_tensor(out=Cst[d][:, 0:2, :], in0=tmpC[:, 0:2, :], in1=v2(pF), op=ALU.add),
                  reads=[b_tmpC, b_pF], writes=[b_C[d]])
            fw.op("dve", lambda e: e.tensor_tensor(out=Cst[d][:, 2:4, :], in0=tmpC[:, 2:4, :], in1=v2(pG), op=ALU.add),
                  reads=[b_tmpC, b_pG, b_C[d]], writes=[b_C[d]])
            fw.op("act", lambda e: e.copy(out=Cb[d][:, :, :], in_=Cst[d][:, :, :]), reads=[b_C[d]], writes=[b_Cb[d]])
            fw.op("dve", lambda e: e.tensor_copy(out=m[:, :], in_=mnew[:, :]), reads=[b_mnew], writes=[b_m[d]])

        n_own, n_ctx = cfg.nt_own, cfg.nt_ctx
        t_own0, t_oth0, t_ctx0 = 0, n_own, 2 * n_own
        near = [(t_ctx0 + i, need_ctx, n_own + i) for i in range(n_ctx)] + [(t_own0 + i, True, i) for i in range(n_own)]
        far = [(t_ctx0 + i, need_ctx, n_own + i) for i in reversed(range(n_ctx))]
        far += [(t_oth0 + i, False, None) for i in reversed(range(n_own))]
        far += [(t_own0 + i, True, i) for i in reversed(range(n_own))]
        for i in range(max(len(near), len(far))):
            if i < len(far):
                step(1, *far[i])
            if i < len(near):
                step(0, *near[i])


def small_inputs(inp, half):
    ig = np.asarray(inp["m_ig_b"]); fg = np.asarray(inp["m_fg_b"]); cv = np.asarray(inp["m_conv"])
    if half == 1:
        ig, fg, cv = ig[:, ::-1, :], fg[:, ::-1, :], cv[:, ::-1, :]
    out = {
        "m_ig_b": np.ascontiguousarray(ig.reshape(DEPTH, 8)), "m_fg_b": np.ascontiguousarray(fg.reshape(DEPTH, 8)),
        "m_conv": np.ascontiguousarray(cv.reshape(DEPTH, 3, 8, 128).transpose(0, 3, 2, 1).reshape(DEPTH, 128, 24)),
    }
    for k in ("a_qn_g", "a_kn_g", "c_sink", "m_norm_g", "norm1_g", "norm2_g", "b_rg", "b_re"):
        out[k] = np.ascontiguousarray(inp[k])
    out["final_g"] = np.ascontiguousarray(np.asarray(inp["final_g"])[None, :])
    return out


def _tiles_oc(cfg, x_own, xc, o_own, o_ctx, need_ctx):
    tl = [(t, t * 128, x_own[t * 128:(t + 1) * 128, :], 0, o_own[t * 128:(t + 1) * 128, :]) for t in range(cfg.nt_own)]
    if need_ctx:
        tl += [(cfg.nt_own + i, cfg.O_CTX + i * 128, xc[i * 128:(i + 1) * 128, :], 1, o_ctx[i * 128:(i + 1) * 128, :])
               for i in range(cfg.nt_ctx)]
    return tl


def phase_merge(fw, cfg, l, S, A, W, x_own, xc, o_own, o_ctx, need_ctx):
    nc = fw.nc
    To, Tc = cfg.T_OWN, cfg.T_CTX
    T_OC = To + Tc
    tiles = _tiles_oc(cfg, x_own, xc, o_own, o_ctx, need_ctx)
    with Phase(fw, "mg1") as ph:
        identB = ph.sb([128, 128], BF16)
        b_id = make_ident(fw, identB)
        gmn = ph.sb([128, 512], F32)
        b_gmn = Buf()
        load_bc(fw, "sp", gmn[:, :], A["m_norm_g"][l:l + 1, :], b_gmn)
        AT = ph.sb([128, 8, T_OC], BF16)
        CT = ph.sb([128, 4, T_OC], BF16)
        b_AT, b_CT = bufs(2)
        fw.dma("sp", AT[:, :, :], S.OT[0:1024, :].rearrange("(k p) t -> p k t", p=128), writes=[b_AT])
        fw.dma("sp", CT[:, :, :], S.OT[1536:2048, :].rearrange("(k p) t -> p k t", p=128), writes=[b_CT])
        Wa = ph.sb([128, 8, D], BF16)
        Wm = ph.sb([128, 4, D], BF16)
        Wc = ph.sb([128, 4, D], BF16)
        b_Wa, b_Wm, b_Wc = bufs(3)
        fw.dma("pool", Wa[:, :, :], W["w_br_a"].rearrange("(k p) c -> p k c", p=128), writes=[b_Wa])
        fw.dma("pool", Wm[:, :, :], W["w_br_m"].rearrange("(k p) c -> p k c", p=128), writes=[b_Wm])
        fw.dma("pool", Wc[:, :, :], W["w_br_c"].rearrange("(k p) c -> p k c", p=128), writes=[b_Wc])
        h0 = [ph.sb([128, 512], F32) for _ in range(2)]
        h1 = [ph.sb([128, 512], F32) for _ in range(2)]
        mo = [ph.sb([128, 512], BF16) for _ in range(2)]
        gg = [ph.sb([128, 3 * D], BF16) for _ in range(2)]
        b_h0, b_h1, b_mo, b_gg = bufs(2), bufs(2), bufs(2), bufs(2)
        sq = ph.sb([128, 512], F32)
        ss = ph.sb([128, 4], F32)
        sig = ph.sb([128, 512], F32)
        omb = ph.sb([128, 512], BF16)
        omT = [ph.sb([128, 4, 128], BF16) for _ in range(2)]
        z32 = ph.sb([128, 512], F32)
        t32 = ph.sb([128, 512], F32)
        zb = [ph.sb([128, D], BF16) for _ in range(2)]
        zTs = [ph.sb([128, NK, 128], BF16) for _ in range(2)]
        b_sq, b_ss, b_sig, b_omb, b_z32, b_t32 = bufs(6)
        b_omT, b_zb, b_zTs = bufs(2), bufs(2), bufs(2)
        ptr = [ph.ps([128, 512], BF16) for _ in range(2)]
        b_ptr = bufs(2)
        pa = [ph.ps([128, 512]) for _ in range(2)]
        pm = [ph.ps([128, 512]) for _ in range(2)]
        pc = [ph.ps([128, 512]) for _ in range(2)]
        b_pa, b_pm, b_pc = bufs(2), bufs(2), bufs(2)
        tp = 0
        pq = 0
        for n, (t, prow, xsrc, m, dst) in enumerate(tiles):
            s = n % 2
            tok = slice(t * 128, (t + 1) * 128)
            fw.dma("sp", h0[s][:, :], S.HD[0, t * 128:(t + 1) * 128, :], writes=[b_h0[s]])
            fw.dma("sp", h1[s][:, :], S.HD[1, t * 128:(t + 1) * 128, :], writes=[b_h1[s]])
            fw.dma("sp", mo[s][:, :], S.P[prow:prow + 128, C_MO:C_MO + 512], writes=[b_mo[s]])
            fw.dma("sp", gg[s][:, :], S.P[prow:prow + 128, C_G:C_G + 3 * D], writes=[b_gg[s]])
            fw.op("dve", lambda e: e.tensor_tensor(out=h0[s][:, :], in0=h0[s][:, :], in1=h1[s][:, :], op=ALU.add),
                  reads=[b_h0[s], b_h1[s]], writes=[b_h0[s]])
            fw.op("dve", lambda e: e.tensor_tensor(out=sq[:, :], in0=h0[s][:, :], in1=h0[s][:, :], op=ALU.mult),
                  reads=[b_h0[s]], writes=[b_sq])
            fw.op("dve", lambda e: e.tensor_reduce(out=ss[:, :], in_=sq[:, :].rearrange("p (h d) -> p h d", h=4), axis=AX.X, op=ALU.add),
                  reads=[b_sq], writes=[b_ss])
            _rstd(fw, ss[:, :], b_ss, 128)
            h3 = h0[s][:, :].rearrange("p (h d) -> p h d", h=4)
            fw.op("dve", lambda e: e.tensor_tensor(out=h3, in0=h3, in1=ss[:, :].unsqueeze(2).to_broadcast([128, 4, 128]), op=ALU.mult),
                  reads=[b_h0[s], b_ss], writes=[b_h0[s]])
            fw.op("dve", lambda e: e.tensor_tensor(out=h0[s][:, :], in0=h0[s][:, :], in1=gmn[:, :], op=ALU.mult),
                  reads=[b_h0[s], b_gmn], writes=[b_h0[s]])
            fw.op("act", lambda e: e.activation(out=sig[:, :], in_=mo[s][:, :], func=AF.Sigmoid), reads=[b_mo[s]], writes=[b_sig])
            fw.op("dve", lambda e: e.tensor_tensor(out=omb[:, :], in0=h0[s][:, :], in1=sig[:, :], op=ALU.mult),
                  reads=[b_h0[s], b_sig], writes=[b_omb])
            p = tp % 2
            tp += 1

            def tr(e):
                for k in range(4):
                    ins = e.transpose(ptr[p][:, k * 128:(k + 1) * 128], omb[:, k * 128:(k + 1) * 128], identB[:, :])
                return ins
            fw.op("pe", tr, reads=[b_omb, b_id], writes=[b_ptr[p]])
            fw.op("act", lambda e: e.copy(out=omT[s][:, :, :], in_=ptr[p][:, :].rearrange("p (k t) -> p k t", k=4)),
                  reads=[b_ptr[p]], writes=[b_omT[s]])
            fw.op("act", lambda e: e.activation(out=gg[s][:, :], in_=gg[s][:, :], func=AF.Sigmoid), reads=[b_gg[s]], writes=[b_gg[s]])
            for cb in range(4):
                q = pq % 2
                pq += 1
                cs = slice(cb * 512, (cb + 1) * 512)

                def mma(e):
                    for k in range(8):
                        ins = e.matmul(pa[q][:, :], lhsT=AT[:, k, tok], rhs=Wa[:, k, cs], start=(k == 0), stop=(k == 7))
                    return ins

                def mmm(e):
                    for k in range(4):
                        ins = e.matmul(pm[q][:, :], lhsT=omT[s][:, k, :], rhs=Wm[:, k, cs], start=(k == 0), stop=(k == 3))
                    return ins

                def mmc(e):
                    for k in range(4):
                        ins = e.matmul(pc[q][:, :], lhsT=CT[:, k, tok], rhs=Wc[:, k, cs], start=(k == 0), stop=(k == 3))
                    return ins
                fw.op("pe", mma, reads=[b_AT, b_Wa], writes=[b_pa[q]])
                fw.op("pe", mmm, reads=[b_omT[s], b_Wm], writes=[b_pm[q]])
                fw.op("pe", mmc, reads=[b_CT, b_Wc], writes=[b_pc[q]])
                fw.op("dve", lambda e: e.tensor_tensor(out=z32[:, :], in0=pa[q][:, :], in1=gg[s][:, cb * 512:(cb + 1) * 512], op=ALU.mult),
                      reads=[b_pa[q], b_gg[s]], writes=[b_z32])
                fw.op("dve", lambda e: e.tensor_tensor(out=t32[:, :], in0=pm[q][:, :], in1=gg[s][:, D + cb * 512:D + (cb + 1) * 512],
                                                       op=ALU.mult), reads=[b_pm[q], b_gg[s]], writes=[b_t32])
                fw.op("pool", lambda e: e.tensor_tensor(out=z32[:, :], in0=z32[:, :], in1=t32[:, :], op=ALU.add),
                      reads=[b_z32, b_t32], writes=[b_z32])
                fw.op("dve", lambda e: e.tensor_tensor(out=t32[:, :], in0=pc[q][:, :], in1=gg[s][:, 2 * D + cb * 512:2 * D + (cb + 1) * 512],
                                                       op=ALU.mult), reads=[b_pc[q], b_gg[s]], writes=[b_t32])
                fw.op("pool", lambda e: e.tensor_tensor(out=zb[s][:, cs], in0=z32[:, :], in1=t32[:, :], op=ALU.add),
                      reads=[b_z32, b_t32], writes=[b_zb[s]])
            for k4 in range(NK // 4):
                p = tp % 2
                tp += 1

                def tr2(e):
                    for kk in range(4):
                        k = k4 * 4 + kk
                        ins = e.transpose(ptr[p][:, kk * 128:(kk + 1) * 128], zb[s][:, k * 128:(k + 1) * 128], identB[:, :])
                    return ins
                fw.op("pe", tr2, reads=[b_zb[s], b_id], writes=[b_ptr[p]])
                fw.op("act", lambda e: e.copy(out=zTs[s][:, k4 * 4:(k4 + 1) * 4, :], in_=ptr[p][:, :].rearrange("p (k t) -> p k t", k=4)),
                      reads=[b_ptr[p]], writes=[b_zTs[s]])
            fw.dma("sp", S.ZT[:, tok].rearrange("(k p) t -> p k t", p=128), zTs[s][:, :, :], reads=[b_zTs[s]])
    with Phase(fw, "mg2") as ph:
        ZTs = ph.sb([128, NK, T_OC], BF16)
        Wo = ph.sb([128, NK, D], BF16)
        b_Z, b_Wo = bufs(2)
        fw.dma("sp", ZTs[:, :, :], S.ZT.rearrange("(k p) t -> p k t", p=128), writes=[b_Z])
        fw.dma("pool", Wo[:, :, :], W["w_out"].rearrange("(k p) c -> p k c", p=128), writes=[b_Wo])
        g1 = [ph.sb([128, D], F32) for _ in range(2)]
        b_g1 = bufs(2)
        for m in range(2):
            load_bc(fw, "sp", g1[m][:, :], S.mods[l][m:m + 1, 2 * D:3 * D], b_g1[m])
        xt = [ph.sb([128, D], F32) for _ in range(2)]
        xn = [ph.sb([128, D], F32) for _ in range(2)]
        b_xt, b_xn = bufs(2), bufs(2)
        py = [ph.ps([128, 512]) for _ in range(4)]
        b_py = bufs(4)
        pq = 0
        for n, (t, prow, xsrc, m, dst) in enumerate(tiles):
            s = n % 2
            tok = slice(t * 128, (t + 1) * 128)
            fw.dma("sp", xt[s][:, :], xsrc, writes=[b_xt[s]])
            for cb in range(4):
                q = pq % 4
                pq += 1
                cs = slice(cb * 512, (cb + 1) * 512)

                def mmo(e):
                    for k in range(NK):
                        ins = e.matmul(py[q][:, :], lhsT=ZTs[:, k, tok], rhs=Wo[:, k, cs], start=(k == 0), stop=(k == NK - 1))
                    return ins
                fw.op("pe", mmo, reads=[b_Z, b_Wo], writes=[b_py[q]])
                fw.op("dve", lambda e: e.tensor_tensor(out=xn[s][:, cs], in0=py[q][:, :], in1=g1[m][:, cs], op=ALU.mult),
                      reads=[b_py[q], b_g1[m]], writes=[b_xn[s]])
                fw.op("pool", lambda e: e.tensor_tensor(out=xn[s][:, cs], in0=xn[s][:, cs], in1=xt[s][:, cs], op=ALU.add),
                      reads=[b_xn[s], b_xt[s]], writes=[b_xn[s]])
            fw.dma("sp", dst, xn[s][:, :], reads=[b_xn[s]])


def phase_moe_router(fw, cfg, l, S, A, W, o_own, o_ctx, need_ctx):
    nc = fw.nc
    tiles = _tiles_oc(cfg, o_own, o_ctx, o_own, o_ctx, need_ctx)
    BIG = 1.0e4
    with Phase(fw, "mr") as ph:
        identB = ph.sb([128, 128], BF16)
        b_id = make_ident(fw, identB)
        gain = [ph.sb([128, D], F32) for _ in range(2)]
        shift = [ph.sb([128, D], F32) for _ in range(2)]
        tmpg = ph.sb([128, D], F32)
        b_gain, b_shift = bufs(2), bufs(2)
        b_tmp = Buf()
        load_bc(fw, "sp", tmpg[:, :], A["norm2_g"][l:l + 1, :], b_tmp)
        for m in range(2):
            load_bc(fw, "sp", shift[m][:, :], S.mods[l][m:m + 1, 3 * D:4 * D], b_shift[m])
            load_bc(fw, "sp", gain[m][:, :], S.mods[l][m:m + 1, 4 * D:5 * D], b_gain[m])
            fw.op("dve", lambda e: e.scalar_tensor_tensor(out=gain[m][:, :], in0=gain[m][:, :], scalar=1.0, in1=tmpg[:, :],
                                                          op0=ALU.add, op1=ALU.mult), reads=[b_gain[m], b_tmp], writes=[b_gain[m]])
        wr = ph.sb([128, NK, 20], F32)
        wrh = ph.sb([128, NK, 20], BF16)
        wrl = ph.sb([128, NK, 20], BF16)
        brb = ph.sb([128, 20], F32)
        b_wr, b_wrh, b_wrl, b_brb, b_brb2 = bufs(5)
        fw.dma("sp", wr[:, :, :], W["w_r"].rearrange("(k p) c -> p k c", p=128), writes=[b_wr])
        load_bc(fw, "sp", brb[:, 0:4], A["b_rg"][l:l + 1, :], b_brb)
        load_bc(fw, "sp", brb[:, 4:20], A["b_re"][l:l + 1, :], b_brb2)
        fw.op("act", lambda e: e.copy(out=wrh[:, :, :], in_=wr[:, :, :]), reads=[b_wr], writes=[b_wrh])
        fw.op("dve", lambda e: e.tensor_tensor(out=wrl[:, :, :], in0=wr[:, :, :], in1=wrh[:, :, :], op=ALU.subtract),
              reads=[b_wr, b_wrh], writes=[b_wrl])
        xt = [ph.sb([128, D], F32) for _ in range(2)]
        b_xt = bufs(2)
        junk = ph.sb([128, D], BF16)
        h32 = ph.sb([128, D], F32)
        hb = [ph.sb([128, D], BF16) for _ in range(2)]
        lb = [ph.sb([128, D], BF16) for _ in range(2)]
        hTt = [ph.sb([128, NK, 128], BF16) for _ in range(2)]
        lTt = [ph.sb([128, NK, 128], BF16) for _ in range(2)]
        ss = [ph.sb([128, 1], F32) for _ in range(2)]
        b_junk, b_h32 = bufs(2)
        b_hb, b_lb, b_hTt, b_lTt, b_ss = bufs(2), bufs(2), bufs(2), bufs(2), bufs(2)
        ptr = [ph.ps([128, 512], BF16) for _ in range(2)]
        b_ptr = bufs(2)
        plg = [ph.ps([128, 512]) for _ in range(2)]
        b_plg = bufs(2)
        NTM = cfg.nt_own + cfg.nt_ctx
        lgall = ph.sb([128, NTM, 20], F32)
        rs = ph.sb([128, 7, NTM], F32)
        r4 = ph.sb([128, 3, NTM, 4], F32)
        r16 = ph.sb([128, 4, NTM, 16], F32)
        b_lg, b_wk = bufs(2)
        tp = 0
        for n, (t, prow, xsrc, m, dst) in enumerate(tiles):
            s = n % 2
            tok = slice(t * 128, (t + 1) * 128)
            fw.dma("sp", xt[s][:, :], xsrc, writes=[b_xt[s]])
            fw.op("pool", lambda e: e.memset(ss[s][:, :], 0.0), writes=[b_ss[s]])
            fw.op("act", lambda e: e.activation(out=junk[:, :], in_=xt[s][:, :], func=AF.Square, accum_out=ss[s][:, 0:1]),
                  reads=[b_xt[s], b_ss[s]], writes=[b_junk, b_ss[s]])
            _rstd(fw, ss[s][:, :], b_ss[s], D)
            fw.op("dve", lambda e: e.scalar_tensor_tensor(out=h32[:, :], in0=xt[s][:, :], scalar=ss[s][:, 0:1], in1=gain[m][:, :],
                                                          op0=ALU.mult, op1=ALU.mult), reads=[b_xt[s], b_ss[s], b_gain[m]], writes=[b_h32])
            fw.op("pool", lambda e: e.tensor_tensor(out=h32[:, :], in0=h32[:, :], in1=shift[m][:, :], op=ALU.add),
                  reads=[b_h32, b_shift[m]], writes=[b_h32])
            fw.op("act", lambda e: e.copy(out=hb[s][:, :], in_=h32[:, :]), reads=[b_h32], writes=[b_hb[s]])
            fw.op("dve", lambda e: e.tensor_tensor(out=lb[s][:, :], in0=h32[:, :], in1=hb[s][:, :], op=ALU.subtract),
                  reads=[b_h32, b_hb[s]], writes=[b_lb[s]])
            for (srcb, b_src, dstT, b_dst) in ((hb[s], b_hb[s], hTt[s], b_hTt[s]), (lb[s], b_lb[s], lTt[s], b_lTt[s])):
                for k4 in range(NK // 4):
                    p = tp % 2
                    tp += 1

                    def tr(e):
                        for kk in range(4):
                            k = k4 * 4 + kk
                            ins = e.transpose(ptr[p][:, kk * 128:(kk + 1) * 128], srcb[:, k * 128:(k + 1) * 128], identB[:, :])
                        return ins
                    fw.op("pe", tr, reads=[b_src, b_id], writes=[b_ptr[p]])
                    fw.op("act", lambda e: e.copy(out=dstT[:, k4 * 4:(k4 + 1) * 4, :], in_=ptr[p][:, :].rearrange("p (k t) -> p k t", k=4)),
                          reads=[b_ptr[p]], writes=[b_dst])
            fw.dma("sp", S.HT2[:, tok].rearrange("(k p) t -> p k t", p=128), hTt[s][:, :, :], reads=[b_hTt[s]])

            def mml(e):
                i = 0
                for (L, R) in ((hTt[s], wrh), (lTt[s], wrh), (hTt[s], wrl)):
                    for k in range(NK):
                        ins = e.matmul(plg[s][:, 0:20], lhsT=L[:, k, :], rhs=R[:, k, :], start=(i == 0), stop=(i == 3 * NK - 1))
                        i += 1
                return ins
            fw.op("pe", mml, reads=[b_hTt[s], b_lTt[s], b_wrh, b_wrl], writes=[b_plg[s]])
            fw.op("dve", lambda e: e.tensor_tensor(out=lgall[:, n, :], in0=plg[s][:, 0:20], in1=brb[:, :], op=ALU.add),
                  reads=[b_plg[s], b_brb, b_brb2], writes=[b_lg])
        T = len(tiles)
        GL, EL = lgall[:, 0:T, 0:4], lgall[:, 0:T, 4:20]
        gmax, gsum, pg, m1, m2, w1, w2 = (rs[:, i, 0:T] for i in range(7))
        oh, ex, pen = r4[:, 0, 0:T, :], r4[:, 1, 0:T, :], r4[:, 2, 0:T, :]
        esel, mk1, mk2, e2 = r16[:, 0, 0:T, :], r16[:, 1, 0:T, :], r16[:, 2, 0:T, :], r16[:, 3, 0:T, :]

        def bc4(v):
            return v.unsqueeze(2).to_broadcast([128, T, 4])

        def bc16(v):
            return v.unsqueeze(2).to_broadcast([128, T, 16])
        seq = [
            ("dve", lambda e: e.tensor_reduce(out=gmax, in_=GL, axis=AX.X, op=ALU.max)),
            ("dve", lambda e: e.tensor_tensor(out=oh, in0=GL, in1=bc4(gmax), op=ALU.is_ge)),
            ("dve", lambda e: e.tensor_tensor(out=ex, in0=GL, in1=bc4(gmax), op=ALU.subtract)),
            ("act", lambda e: e.activation(out=ex, in_=ex, func=AF.Exp)),
            ("dve", lambda e: e.tensor_reduce(out=gsum, in_=ex, axis=AX.X, op=ALU.add)),
            ("dve", lambda e: e.reciprocal(out=pg, in_=gsum)),
            ("dve", lambda e: e.tensor_scalar(out=pen, in0=oh, scalar1=BIG, scalar2=-BIG, op0=ALU.mult, op1=ALU.add)),
            ("dve", lambda e: e.tensor_tensor(out=esel.rearrange("p t (g x) -> p t g x", g=4), in0=EL.rearrange("p t (g x) -> p t g x", g=4),
                                              in1=pen.unsqueeze(3).to_broadcast([128, T, 4, 4]), op=ALU.add)),
            ("dve", lambda e: e.tensor_reduce(out=m1, in_=esel, axis=AX.X, op=ALU.max)),
            ("dve", lambda e: e.tensor_tensor(out=mk1, in0=esel, in1=bc16(m1), op=ALU.is_ge)),
            ("dve", lambda e: e.scalar_tensor_tensor(out=e2, in0=mk1, scalar=-BIG, in1=esel, op0=ALU.mult, op1=ALU.add)),
            ("dve", lambda e: e.tensor_reduce(out=m2, in_=e2, axis=AX.X, op=ALU.max)),
            ("dve", lambda e: e.tensor_tensor(out=mk2, in0=e2, in1=bc16(m2), op=ALU.is_ge)),
            ("dve", lambda e: e.tensor_tensor(out=w2, in0=m2, in1=m1, op=ALU.subtract)),
            ("act", lambda e: e.activation(out=w2, in_=w2, func=AF.Exp)),
            ("dve", lambda e: e.tensor_scalar(out=w1, in0=w2, scalar1=1.0, scalar2=None, op0=ALU.add)),
            ("dve", lambda e: e.reciprocal(out=w1, in_=w1)),
            ("dve", lambda e: e.tensor_tensor(out=w2, in0=w2, in1=w1, op=ALU.mult)),
            ("dve", lambda e: e.tensor_tensor(out=w1, in0=w1, in1=pg, op=ALU.mult)),
            ("dve", lambda e: e.tensor_tensor(out=w2, in0=w2, in1=pg, op=ALU.mult)),
            ("dve", lambda e: e.tensor_tensor(out=mk1, in0=mk1, in1=bc16(w1), op=ALU.mult)),
            ("dve", lambda e: e.tensor_tensor(out=mk2, in0=mk2, in1=bc16(w2), op=ALU.mult)),
            ("dve", lambda e: e.tensor_tensor(out=esel, in0=mk1, in1=mk2, op=ALU.add)),
        ]
        for eng, f in seq:
            fw.op(eng, f, reads=[b_lg, b_wk], writes=[b_wk])
        fw.dma("sp", S.COMB[0:T * 128, :].rearrange("(t p) c -> p t c", p=128), esel, reads=[b_wk])


def phase_moe_experts(fw, cfg, l, S, A, W, o_own, o_ctx, x2_own, x2_ctx, need_ctx, final_out=None):
    nc = fw.nc
    To, Tc = cfg.T_OWN, cfg.T_CTX
    tiles = _tiles_oc(cfg, o_own, o_ctx, x2_own, x2_ctx, need_ctx)
    SB = 4
    with Phase(fw, "mx") as ph:
        identF = ph.sb([128, 128], F32)
        b_idF = make_ident(fw, identF)
        sel = ph.sb([16, 16, 128], BF16)
        b_sel = Buf()
        fw.op("pool", lambda e: e.memset(sel[:, :, :], 0.0), writes=[b_sel])
        fw.op("pool", lambda e: e.affine_select(out=sel[:, :, :], in_=sel[:, :, :], pattern=[[-1, 16], [0, 128]], compare_op=ALU.not_equal,
                                                fill=1.0, base=0, channel_multiplier=1), reads=[b_sel], writes=[b_sel])
        g2 = [ph.sb([128, D], F32) for _ in range(2)]
        b_g2 = bufs(2)
        for m in range(2):
            load_bc(fw, "sp", g2[m][:, :], S.mods[l][m:m + 1, 5 * D:6 * D], b_g2[m])
        fgb = ph.sb([128, D], F32)
        b_fgb = Buf()
        if final_out is not None:
            load_bc(fw, "sp", fgb[:, :], A["final_g"][0:1, :], b_fgb)
        acc = ph.sb([128, SB, D], F32)
        hT = ph.sb([128, NK, SB * 128], BF16)
        cm = ph.sb([128, SB, 16], F32)
        cmT = ph.sb([16, SB * 128], BF16)
        cbc = [ph.sb([128, SB * 128], F32) for _ in range(2)]
        Wg = [ph.sb([128, NK, 512], BF16) for _ in range(2)]
        Wu = [ph.sb([128, NK, 512], BF16) for _ in range(2)]
        Wd = [ph.sb([128, 4, D], BF16) for _ in range(2)]
        midT = [ph.sb([128, 4, SB * 128], BF16) for _ in range(2)]
        sa = ph.sb([128, 512], F32)
        xt = [ph.sb([128, D], F32) for _ in range(2)]
        junk = ph.sb([128, D], BF16)
        ss = ph.sb([128, 1], F32)
        b_acc, b_hT, b_cm, b_cmT, b_sa, b_junk, b_ss = bufs(7)
        b_cbc, b_Wg, b_Wu, b_Wd, b_midT, b_xt = bufs(2), bufs(2), bufs(2), bufs(2), bufs(2), bufs(2)
        pa = [ph.ps([128, 512]) for _ in range(2)]
        pu = [ph.ps([128, 512]) for _ in range(2)]
        po = [ph.ps([128, 512]) for _ in range(3)]
        pcb = ph.ps([128, 512])
        b_pa, b_pu, b_po = bufs(2), bufs(2), bufs(3)
        b_pcb = Buf()
        wi = 0
        pi = 0
        oi = 0
        for sb0 in range(0, len(tiles), SB):
            tl = tiles[sb0:sb0 + SB]
            nt = len(tl)
            ntok = nt * 128
            tok0 = tl[0][0] * 128
            fw.dma("sp", hT[:, :, 0:ntok], S.HT2[:, tok0:tok0 + ntok].rearrange("(k p) t -> p k t", p=128), writes=[b_hT])
            fw.dma("sp", cm[:, 0:nt, :], S.COMB[tok0:tok0 + ntok, :].rearrange("(t p) c -> p t c", p=128), writes=[b_cm])
            for i in range(nt):
                fw.op("pe", lambda e: e.transpose(pcb[0:16, i * 128:(i + 1) * 128], cm[:, i, :], identF[:, :]),
                      reads=[b_cm, b_idF], writes=[b_pcb])
            fw.op("act", lambda e: e.copy(out=cmT[:, 0:ntok], in_=pcb[0:16, 0:ntok]), reads=[b_pcb], writes=[b_cmT])
            for ex in range(16):
                w = wi % 2
                wi += 1
                fw.dma("pool", Wg[w][:, :, :], W["w_gate"][ex].rearrange("(k p) f -> p k f", p=128), writes=[b_Wg[w]])
                fw.dma("pool", Wu[w][:, :, :], W["w_up"][ex].rearrange("(k p) f -> p k f", p=128), writes=[b_Wu[w]])
                fw.dma("pool", Wd[w][:, :, :], W["w_down"][ex].rearrange("(k p) c -> p k c", p=128), writes=[b_Wd[w]])
                for tb in range(0, ntok, 512):
                    tw = min(512, ntok - tb)
                    fw.op("pe", lambda e: e.matmul(pcb[:, 0:tw], lhsT=sel[:, ex, :], rhs=cmT[:, tb:tb + tw], start=True, stop=True),
                          reads=[b_sel, b_cmT], writes=[b_pcb])
                    fw.op("act", lambda e: e.copy(out=cbc[w][:, tb:tb + tw], in_=pcb[:, 0:tw]), reads=[b_pcb], writes=[b_cbc[w]])
                for fc in range(4):
                    for tb in range(0, ntok, 512):
                        tw = min(512, ntok - tb)
                        p = pi % 2
                        pi += 1

                        def mg(e):
                            for k in range(NK):
                                ins = e.matmul(pa[p][:, 0:tw], lhsT=Wg[w][:, k, fc * 128:(fc + 1) * 128], rhs=hT[:, k, tb:tb + tw],
                                               start=(k == 0), stop=(k == NK - 1))
                            return ins

                        def mu(e):
                            for k in range(NK):
                                ins = e.matmul(pu[p][:, 0:tw], lhsT=Wu[w][:, k, fc * 128:(fc + 1) * 128], rhs=hT[:, k, tb:tb + tw],
                                               start=(k == 0), stop=(k == NK - 1))
                            return ins
                        fw.op("pe", mg, reads=[b_Wg[w], b_hT], writes=[b_pa[p]])
                        fw.op("pe", mu, reads=[b_Wu[w], b_hT], writes=[b_pu[p]])
                        fw.op("act", lambda e: e.activation(out=sa[:, 0:tw], in_=pa[p][:, 0:tw], func=AF.Silu), reads=[b_pa[p]], writes=[b_sa])
                        fw.op("dve", lambda e: e.tensor_tensor(out=sa[:, 0:tw], in0=sa[:, 0:tw], in1=pu[p][:, 0:tw], op=ALU.mult),
                              reads=[b_sa, b_pu[p]], writes=[b_sa])
                        fw.op("pool", lambda e: e.tensor_tensor(out=midT[w][:, fc, tb:tb + tw], in0=sa[:, 0:tw], in1=cbc[w][:, tb:tb + tw],
                                                                op=ALU.mult), reads=[b_sa, b_cbc[w]], writes=[b_midT[w]])
                for i in range(nt):
                    for dc in range(4):
                        o = oi % 3
                        oi += 1

                        def md(e):
                            for fc in range(4):
                                ins = e.matmul(po[o][:, :], lhsT=midT[w][:, fc, i * 128:(i + 1) * 128], rhs=Wd[w][:, fc, dc * 512:(dc + 1) * 512],
                                               start=(fc == 0), stop=(fc == 3))
                            return ins
                        fw.op("pe", md, reads=[b_midT[w], b_Wd[w]], writes=[b_po[o]])
                        if ex == 0:
                            fw.op("act", lambda e: e.copy(out=acc[:, i, dc * 512:(dc + 1) * 512], in_=po[o][:, :]),
                                  reads=[b_po[o]], writes=[b_acc])
                        else:
                            fw.op("dve", lambda e: e.tensor_tensor(out=acc[:, i, dc * 512:(dc + 1) * 512], in0=acc[:, i, dc * 512:(dc + 1) * 512],
                                                                   in1=po[o][:, :], op=ALU.add), reads=[b_po[o], b_acc], writes=[b_acc])
            for i, (t, prow, xsrc, m, dst) in enumerate(tl):
                s = i % 2
                fw.dma("sp", xt[s][:, :], xsrc, writes=[b_xt[s]])
                fw.op("dve", lambda e: e.tensor_tensor(out=acc[:, i, :], in0=acc[:, i, :], in1=g2[m][:, :], op=ALU.mult),
                      reads=[b_acc, b_g2[m]], writes=[b_acc])
                fw.op("pool", lambda e: e.tensor_tensor(out=xt[s][:, :], in0=xt[s][:, :], in1=acc[:, i, :], op=ALU.add),
                      reads=[b_acc, b_xt[s]], writes=[b_xt[s]])
                if final_out is not None and m == 0:
                    fw.op("pool", lambda e: e.memset(ss[:, :], 0.0), writes=[b_ss])
                    fw.op("act", lambda e: e.activation(out=junk[:, :], in_=xt[s][:, :], func=AF.Square, accum_out=ss[:, 0:1]),
                          reads=[b_xt[s], b_ss], writes=[b_junk, b_ss])
                    _rstd(fw, ss[:, :], b_ss, D)
                    fw.op("dve", lambda e: e.scalar_tensor_tensor(out=xt[s][:, :], in0=xt[s][:, :], scalar=ss[:, 0:1], in1=fgb[:, :],
                                                                  op0=ALU.mult, op1=ALU.mult), reads=[b_xt[s], b_ss, b_fgb], writes=[b_xt[s]])
                    fw.dma("sp", final_out[t * 128:(t + 1) * 128, :], xt[s][:, :], reads=[b_xt[s]])
                else:
                    fw.dma("sp", dst, xt[s][:, :], reads=[b_xt[s]])


WEIGHT_KEYS = ("w_mod", "bmod2", "w_in", "g1row", "w_br_a", "w_br_m", "w_br_c", "w_out", "w_r", "w_gate", "w_up", "w_down")


def layer_weights(inp, l, half):
    w = layer_inputs(inp, l, half)
    for k in ("w_br_a", "w_br_m", "w_br_c", "w_out", "w_gate", "w_up", "w_down"):
        w[k] = np.ascontiguousarray(inp[k][l])
    w["w_r"] = np.ascontiguousarray(np.concatenate([np.asarray(inp["w_rg"][l]), np.asarray(inp["w_re"][l])], axis=1))
    return w


def emit_layer(fw, cfg, S, A, l, last, x_own, x_oth, xc, x2o, x2c, sfx=""):
    nc = fw.nc
    need_ctx = not last
    To, Tc = cfg.T_OWN, cfg.T_CTX
    x1o = nc.dram_tensor("x1o" + sfx, [To, D], F32, kind="Internal").ap()
    x1c = nc.dram_tensor("x1c" + sfx, [Tc, D], F32, kind="Internal").ap()
    W = {k: A[k + sfx] for k in WEIGHT_KEYS}
    phase_mods(fw, cfg, A["cvec"], W["w_mod"], W["bmod2"], S.mods[l])
    phase_inproj(fw, cfg, l, x_own, x_oth, xc, S.mods[l], W["g1row"], W["w_in"], S)
    phase_attn(fw, cfg, l, S, A, "a", need_ctx)
    phase_attn(fw, cfg, l, S, A, "c", need_ctx)
    phase_mlstm(fw, cfg, l, S, A, need_ctx)
    phase_merge(fw, cfg, l, S, A, W, x_own, xc, x1o, x1c, need_ctx)
    phase_moe_router(fw, cfg, l, S, A, W, x1o, x1c, need_ctx)
    phase_moe_experts(fw, cfg, l, S, A, W, x1o, x1c, x2o, x2c, need_ctx, final_out=(x2o if last else None))


def phase_exchange(fw, cfg, A, x2o, xoth):
    nc = fw.nc
    To = cfg.T_OWN
    nt = cfg.nt_own
    CH = 2
    nch = nt // CH
    Z = [nc.dram_tensor("xchgZ%d" % i, [2 * CH * 128, D], F32, kind="Internal").ap() for i in range(nch)]
    R = [nc.dram_tensor("xchgR%d" % i, [2 * CH * 128, D], F32, kind="Internal").ap() for i in range(nch)]
    b_Z, b_R = bufs(nch), bufs(nch)
    with Phase(fw, "xa") as ph:
        sv = ph.sb([128, 2], F32)
        b_sv = Buf()
        fw.dma("sp", sv[:, :], A["selv"], writes=[b_sv])
        xt = [ph.sb([128, D], F32) for _ in range(2)]
        z0 = [ph.sb([128, D], F32) for _ in range(2)]
        z1 = [ph.sb([128, D], F32) for _ in range(2)]
        b_xt, b_z0, b_z1 = bufs(2), bufs(2), bufs(2)
        for i in range(nt):
            s = i % 2
            c, t = i // CH, i % CH
            fw.dma("sp", xt[s][:, :], x2o[i * 128:(i + 1) * 128, :], writes=[b_xt[s]])
            fw.op("dve", lambda e: e.tensor_scalar(out=z0[s][:, :], in0=xt[s][:, :], scalar1=sv[:, 0:1], scalar2=None, op0=ALU.mult),
                  reads=[b_xt[s], b_sv], writes=[b_z0[s]])
            fw.op("act", lambda e: e.activation(out=z1[s][:, :], in_=xt[s][:, :], func=AF.Copy, scale=sv[:, 1:2]),
                  reads=[b_xt[s], b_sv], writes=[b_z1[s]])
            fw.dma("sp", Z[c][t * 128:(t + 1) * 128, :], z0[s][:, :], reads=[b_z0[s]])
            fw.dma("sp", Z[c][CH * 128 + t * 128:CH * 128 + (t + 1) * 128, :], z1[s][:, :], reads=[b_z1[s]])
    for c in range(nch):
        fw.all_reduce(Z[c], R[c], [[0, 1], [2, 3], [4, 5], [6, 7]], writes=[b_R[c]])
    with Phase(fw, "xb") as ph:
        J = ph.sb([128, 2, 128], F32)
        b_J = Buf()
        fw.dma("sp", J[:, :, :], A["jsel"].rearrange("j r p -> r j p"), writes=[b_J])
        ra = [ph.sb([128, D], F32) for _ in range(2)]
        rb = [ph.sb([128, D], F32) for _ in range(2)]
        ot = [ph.sb([128, D], F32) for _ in range(2)]
        b_ra, b_rb, b_ot = bufs(2), bufs(2), bufs(2)
        pp = [ph.ps([128, 512]) for _ in range(4)]
        b_pp = bufs(4)
        pi = 0
        for i in range(nt):
            s = i % 2
            j = nt - 1 - i
            c, t = j // CH, j % CH
            fw.dma("sp", ra[s][:, :], R[c][t * 128:(t + 1) * 128, :], reads=[b_R[c]], writes=[b_ra[s]])
            fw.dma("sp", rb[s][:, :], R[c][CH * 128 + t * 128:CH * 128 + (t + 1) * 128, :], reads=[b_R[c]], writes=[b_rb[s]])
            for cb in range(4):
                p = pi % 4
                pi += 1
                cs = slice(cb * 512, (cb + 1) * 512)

                def mm(e):
                    e.matmul(pp[p][:, :], lhsT=J[:, 0, :], rhs=ra[s][:, cs], start=True, stop=False)
                    return e.matmul(pp[p][:, :], lhsT=J[:, 1, :], rhs=rb[s][:, cs], start=False, stop=True)
                fw.op("pe", mm, reads=[b_J, b_ra[s], b_rb[s]], writes=[b_pp[p]])
                if cb % 2 == 0:
                    fw.op("act", lambda e: e.copy(out=ot[s][:, cs], in_=pp[p][:, :]), reads=[b_pp[p]], writes=[b_ot[s]])
                else:
                    fw.op("dve", lambda e: e.tensor_copy(out=ot[s][:, cs], in_=pp[p][:, :]), reads=[b_pp[p]], writes=[b_ot[s]])
            fw.dma("sp", xoth[i * 128:(i + 1) * 128, :], ot[s][:, :], reads=[b_ot[s]])


def exchange_consts(half):
    selv = np.zeros((128, 2), np.float32)
    selv[:, half] = 1.0
    Jm = np.zeros((128, 128), np.float32)
    Jm[np.arange(128), 127 - np.arange(128)] = 1.0
    jsel = np.zeros((2, 128, 128), np.float32)
    jsel[1 - half] = Jm
    return {"selv": selv, "jsel": jsel}


def build_fused(cfg, shapes):
    nc = bass.Bass("TRN2", target_bir_lowering=False)
    fw = Fw(nc)
    A = {}
    for name, shp in shapes.items():
        A[name] = nc.dram_tensor(name, list(shp), F32, kind="ExternalInput").ap()
    S = Scratch(nc, cfg)
    To, Tc = cfg.T_OWN, cfg.T_CTX
    xm_o = nc.dram_tensor("xmid_o", [To, D], F32, kind="Internal").ap()
    xm_c = nc.dram_tensor("xmid_c", [Tc, D], F32, kind="Internal").ap()
    xm_oth = nc.dram_tensor("xmid_oth", [To, D], F32, kind="Internal").ap()
    out = nc.dram_tensor("out", [To, D], F32, kind="ExternalOutput").ap()
    dummy_c = nc.dram_tensor("xlast_c", [Tc, D], F32, kind="Internal").ap()
    emit_layer(fw, cfg, S, A, 0, False, A["x_own"], A["x_oth"], A["xc"], xm_o, xm_c, sfx="_0")
    phase_exchange(fw, cfg, A, xm_o, xm_oth)
    emit_layer(fw, cfg, S, A, 1, True, xm_o, xm_oth, xm_c, out, dummy_c, sfx="_1")
    fw.barrier()
    return nc


def build_layer(cfg, l, last, shapes):
    nc = bass.Bass("TRN2", target_bir_lowering=False)
    fw = Fw(nc)
    A = {}
    for name, shp in shapes.items():
        A[name] = nc.dram_tensor(name, list(shp), F32, kind="ExternalInput").ap()
    S = Scratch(nc, cfg)
    To, Tc = cfg.T_OWN, cfg.T_CTX
    x2o = nc.dram_tensor("x2o", [To, D], F32, kind="ExternalOutput").ap()
    x2c = nc.dram_tensor("x2c", [Tc, D], F32, kind="ExternalOutput").ap()
    emit_layer(fw, cfg, S, A, l, last, A["x_own"], A["x_oth"], A["xc"], x2o, x2c)
    fw.barrier()
    return nc


def run_fused(inp, cfg, cores):
    in_maps = []
    for (b, half) in cores:
        m = core_inputs(inp, cfg, b, half)
        m["rope"] = rope_table(cfg, half)
        m.update(small_inputs(inp, half))
        m.update(exchange_consts(half))
        for l in range(DEPTH):
            for k, v in layer_weights(inp, l, half).items():
                m[k + "_%d" % l] = v
        in_maps.append(m)
    shapes = {k: v.shape for k, v in in_maps[0].items()}
    nc = build_fused(cfg, shapes)
    res = run_bass_kernel_spmd(nc, in_maps, core_ids=list(range(len(cores))))
    To = cfg.T_OWN
    B = inp["x"].shape[0]
    x_new = np.zeros((B, 2 * To, D), np.float32)
    for (b, half), r in zip(cores, res.results):
        xo = np.asarray(r["out"], np.float32)
        if half == 0:
            x_new[b, :To] = xo
        else:
            x_new[b, To:] = xo[::-1]
    return x_new


def run_layer(inp, cfg, l, last, x_full, ctx_full, cores):
    in_maps = []
    cur = dict(inp)
    cur["x"] = x_full
    cur["ctx"] = ctx_full
    for (b, half) in cores:
        m = core_inputs(cur, cfg, b, half)
        m["rope"] = rope_table(cfg, half)
        m.update(small_inputs(inp, half))
        m.update(layer_weights(inp, l, half))
        in_maps.append(m)
    shapes = {k: v.shape for k, v in in_maps[0].items()}
    nc = build_layer(cfg, l, last, shapes)
    res = run_bass_kernel_spmd(nc, in_maps, core_ids=list(range(len(cores))))
    To = cfg.T_OWN
    B = x_full.shape[0]
    x_new = np.zeros((B, 2 * To, D), np.float32)
    c_new = np.zeros((B, cfg.T_CTX, D), np.float32)
    for (b, half), r in zip(cores, res.results):
        xo = np.asarray(r["x2o"], np.float32)
        if half == 0:
            x_new[b, :To] = xo
            c_new[b] = np.asarray(r["x2c"], np.float32)
        else:
            x_new[b, To:] = xo[::-1]
    return x_new, c_new


def kernel(**inputs):
    inp = {k: np.asarray(v) for k, v in inputs.items()}
    cfg = Cfg(nt_own=16, nt_ctx=2)
    cores = [(b, half) for b in range(4) for half in range(2)]
    return run_fused(inp, cfg, cores).astype(np.float32)
```

```python
from contextlib import ExitStack
import numpy as np
import concourse.bass as bass
import concourse.mybir as mybir
from concourse.bass_utils import run_bass_kernel_spmd

F32 = mybir.dt.float32
BF16 = mybir.dt.bfloat16
AF = mybir.ActivationFunctionType
ALU = mybir.AluOpType
AX = mybir.AxisListType

D = 2048
NK = D // 128
DEPTH = 2
EPS = 1e-6
NEG = -30000.0

ENGS = ("pe", "act", "dve", "pool", "sp")
RING = 12


class Buf:
    __slots__ = ("w", "r")

    def __init__(self):
        self.w = None
        self.r = {}


def bufs(n):
    return [Buf() for _ in range(n)]


class Fw:
    def __init__(self, nc):
        self.nc = nc
        self.e = dict(pe=nc.tensor, act=nc.scalar, dve=nc.vector, pool=nc.gpsimd, sp=nc.sync)
        self.semh = {}
        for k in ENGS:
            self.semh["e_" + k] = nc.alloc_semaphore("s_" + k)
        self.cnt = {k: 0 for k in ENGS}
        self.known = {k: {} for k in ENGS}
        self.rings = {}
        self.ring_n = {}
        self.ring_val = {}
        for q in ("sp", "pool", "act"):
            self.rings[q] = []
            self.ring_n[q] = 0
            for i in range(RING):
                key = "r_%s_%d" % (q, i)
                self.semh[key] = nc.alloc_semaphore(key)
                self.rings[q].append(key)
                self.ring_val[key] = 0

    def _wait(self, eng, dep):
        key, val, _ = dep
        if val <= 0 or self.known[eng].get(key, 0) >= val:
            return
        self.e[eng].wait_ge(self.semh[key], val)
        self.known[eng][key] = val

    def _sync(self, eng, issuer, reads, writes):
        for b in reads:
            if b.w is not None:
                self._wait(issuer, b.w)
        for b in writes:
            if b.w is not None and b.w[2] != eng:
                self._wait(issuer, b.w)
            for key, (val, pe) in b.r.items():
                if pe != eng:
                    self._wait(issuer, (key, val, pe))

    @staticmethod
    def _mark(dep, reads, writes):
        for b in reads:
            b.r[dep[0]] = (dep[1], dep[2])
        for b in writes:
            b.w = dep
            b.r = {}

    def op(self, eng, fn, reads=(), writes=()):
        self._sync(eng, eng, reads, writes)
        ins = fn(self.e[eng])
        self.cnt[eng] += 1
        ins.then_inc(self.semh["e_" + eng], 1)
        dep = ("e_" + eng, self.cnt[eng], eng)
        self._mark(dep, reads, writes)
        return dep

    def dma(self, q, out, in_, reads=(), writes=(), **kw):
        self._sync("dma", q, reads, writes)
        n = self.ring_n[q]
        self.ring_n[q] = n + 1
        key = self.rings[q][n % RING]
        prev = self.ring_val[key]
        self._wait(q, (key, prev, "dma"))
        self.e[q].dma_start(out=out, in_=in_, **kw).then_inc(self.semh[key], 16)
        self.ring_val[key] = prev + 16
        dep = (key, prev + 16, "dma")
        self._mark(dep, reads, writes)
        return dep

    def all_reduce(self, src, dst, groups, reads=(), writes=()):
        q = "pool"
        self._sync("dma", q, reads, writes)
        if "cc" not in self.semh:
            self.semh["cc"] = self.nc.alloc_semaphore("cc_sem")
            self.ring_val["cc"] = 0
        prev = self.ring_val["cc"]
        self._wait(q, ("cc", prev, "dma"))
        self.e[q].collective_compute("AllReduce", ALU.add, replica_groups=groups, ins=[src], outs=[dst]).then_inc(self.semh["cc"])
        self.ring_val["cc"] = prev + 1
        dep = ("cc", prev + 1, "dma")
        self._mark(dep, reads, writes)
        return dep

    def barrier(self):
        for k in ENGS:
            if k != "sp":
                self._wait("sp", ("e_" + k, self.cnt[k], k))
        for key, val in self.ring_val.items():
            self._wait("sp", (key, val, "dma"))
        self.e["sp"].sem_inc(self.semh["e_sp"], 1)
        self.cnt["sp"] += 1
        for k in ENGS:
            if k != "sp":
                self._wait(k, ("e_sp", self.cnt["sp"], "sp"))
        for k in ENGS:
            for kk in ENGS:
                self.known[k]["e_" + kk] = self.cnt[kk]
            for key, val in self.ring_val.items():
                self.known[k][key] = val


class Phase:
    def __init__(self, fw, name):
        self.fw = fw
        self.nc = fw.nc
        fw.nphase = getattr(fw, "nphase", 0) + 1
        self.name = "%s%d" % (name, fw.nphase)
        self.es = ExitStack()
        self.i = 0

    def __enter__(self):
        self.es.__enter__()
        return self

    def __exit__(self, *a):
        self.fw.barrier()
        return self.es.__exit__(*a)

    def sb(self, shape, dt):
        self.i += 1
        return self.es.enter_context(self.nc.sbuf_tensor("%s_s%d" % (self.name, self.i), list(shape), dt))

    def ps(self, shape, dt=F32):
        self.i += 1
        return self.es.enter_context(self.nc.psum_tensor("%s_p%d" % (self.name, self.i), list(shape), dt))


def make_ident(fw, t, n=128):
    b = Buf()
    fw.op("pool", lambda e: e.memset(t[:, :], 0.0), writes=[b])
    fw.op("pool", lambda e: e.affine_select(out=t[:, :], in_=t[:, :], pattern=[[-1, n]],
                                            compare_op=ALU.not_equal, fill=1.0, base=0,
                                            channel_multiplier=1), reads=[b], writes=[b])
    return b


C_AQ, C_AKV, C_MQ, C_MK, C_MV, C_MO, C_CQ, C_CKV, C_G, C_MG = 0, 1024, 1536, 2048, 2560, 3072, 3584, 4096, 4608, 10752
P_IN = 10768


class Cfg:
    def __init__(self, nt_own=16, nt_ctx=2):
        self.nt_own = nt_own
        self.nt_oth = nt_own
        self.nt_ctx = nt_ctx
        self.T_OWN = 128 * nt_own
        self.T_CTX = 128 * nt_ctx
        self.T_ALL = 2 * self.T_OWN + self.T_CTX
        self.O_OWN, self.O_OTH, self.O_CTX = 0, self.T_OWN, 2 * self.T_OWN


def phase_mods(fw, cfg, cvec, w_mod_l, bmod2_l, mods_l):
    nc = fw.nc
    with Phase(fw, "mod") as ph:
        cc = ph.sb([128, NK * 2], F32)
        S = ph.sb([128, NK * 2], BF16)
        bm = ph.sb([2, 6 * D], F32)
        out = ph.sb([2, 6 * D], F32)
        W = [ph.sb([128, NK, 512], BF16) for _ in range(2)]
        pp = [ph.ps([128, 512]) for _ in range(2)]
        b_cc, b_S, b_bm, b_out = bufs(4)
        b_W = bufs(2)
        b_pp = bufs(2)
        fw.dma("sp", cc[:, :], cvec, writes=[b_cc])
        fw.dma("sp", bm[:, :], bmod2_l, writes=[b_bm])
        fw.op("act", lambda e: e.activation(out=S[:, :], in_=cc[:, :], func=AF.Silu), reads=[b_cc], writes=[b_S])
        wv = w_mod_l.rearrange("(k p) c -> p k c", p=128)
        nblk = 6 * D // 512
        for j in range(nblk):
            s = j % 2
            fw.dma("pool", W[s][:, :, :], wv[:, :, j * 512:(j + 1) * 512], writes=[b_W[s]])

            def mm(e, s=s):
                for k in range(NK):
                    ins = e.matmul(pp[s][0:2, :], lhsT=S[:, 2 * k:2 * k + 2], rhs=W[s][:, k, :],
                                   start=(k == 0), stop=(k == NK - 1))
                return ins
            fw.op("pe", mm, reads=[b_S, b_W[s]], writes=[b_pp[s]])
            fw.op("dve", lambda e, s=s, j=j: e.tensor_tensor(out=out[:, j * 512:(j + 1) * 512], in0=pp[s][0:2, :],
                                                              in1=bm[:, j * 512:(j + 1) * 512], op=ALU.add),
                  reads=[b_pp[s], b_bm], writes=[b_out])
        fw.dma("sp", mods_l, out[:, :], reads=[b_out])


def load_bc(fw, q, dst, src_row, wb):
    return fw.dma(q, dst, src_row.partition_broadcast(128), writes=[wb])


def phase_inproj(fw, cfg, l, x_own, x_oth, xc, mods_l, g1row, w_in_l, S, which=("oc", "oth")):
    nc = fw.nc
    T_OC = cfg.T_OWN + cfg.T_CTX
    passes = [
        ("oc", [(x_own, i, 0) for i in range(cfg.nt_own)] + [(xc, i, 1) for i in range(cfg.nt_ctx)]),
        ("oth", [(x_oth, i, 0) for i in range(cfg.nt_oth)]),
    ]
    with Phase(fw, "inp") as ph:
        ident = ph.sb([128, 128], BF16)
        b_id = make_ident(fw, ident)
        gain = [ph.sb([128, D], F32) for _ in range(2)]
        shift = [ph.sb([128, D], F32) for _ in range(2)]
        tmpg = ph.sb([128, D], F32)
        b_gain, b_shift = bufs(2), bufs(2)
        b_tmp = Buf()
        load_bc(fw, "sp", tmpg[:, :], g1row, b_tmp)
        for m in range(2):
            load_bc(fw, "sp", shift[m][:, :], mods_l[m:m + 1, 0:D], b_shift[m])
            load_bc(fw, "sp", gain[m][:, :], mods_l[m:m + 1, D:2 * D], b_gain[m])
            fw.op("dve", lambda e, m=m: e.scalar_tensor_tensor(out=gain[m][:, :], in0=gain[m][:, :], scalar=1.0,
                                                                in1=tmpg[:, :], op0=ALU.add, op1=ALU.mult),
                  reads=[b_gain[m], b_tmp], writes=[b_gain[m]])
        hT = ph.sb([128, NK, T_OC], BF16)
        xt = [ph.sb([128, D], F32)] * 2
        b_xt = [Buf()] * 2
        junk = ph.sb([128, D], BF16)
        b_junk = Buf()
        y32 = ph.sb([128, D], F32)
        b_y32 = Buf()
        hb = [ph.sb([128, D], BF16)] * 2
        b_hb = [Buf()] * 2
        ss = [ph.sb([128, 1], F32) for _ in range(2)]
        b_ss = bufs(2)
        ptr = [ph.ps([128, 512], BF16) for _ in range(2)]
        b_ptr = bufs(2)
        pmm = [ph.ps([128, 512]) for _ in range(4)]
        b_pmm = bufs(4)
        W = [ph.sb([128, NK, 512], BF16) for _ in range(2)]
        b_W = bufs(2)
        stage = [ph.sb([128, T_OC // 128, 512], BF16) for _ in range(2)]
        b_stage = bufs(2)
        stg32 = ph.sb([128, T_OC // 128, 16], F32)
        b_stg32 = Buf()
        wv = w_in_l.rearrange("(k p) c -> p k c", p=128)
        wi = 0
        ti = 0
        pi = 0
        for pname, tiles in passes:
            if pname not in which:
                continue
            nt = len(tiles)
            b_hT = bufs(nt)
            for t, (src, i, m) in enumerate(tiles):
                s = ti % 2
                ti += 1
                fw.dma("sp", xt[s][:, :], src[i * 128:(i + 1) * 128, :], writes=[b_xt[s]])
                fw.op("pool", lambda e, s=s: e.memset(ss[s][:, :], 0.0), writes=[b_ss[s]])
                fw.op("act", lambda e, s=s: e.activation(out=junk[:, :], in_=xt[s][:, :], func=AF.Square,
                                                         accum_out=ss[s][:, 0:1]),
                      reads=[b_xt[s], b_ss[s]], writes=[b_junk, b_ss[s]])
                fw.op("dve", lambda e, s=s: e.tensor_scalar(out=ss[s][:, :], in0=ss[s][:, :], scalar1=1.0 / D,
                                                            scalar2=EPS, op0=ALU.mult, op1=ALU.add),
                      reads=[b_ss[s]], writes=[b_ss[s]])
                fw.op("act", lambda e, s=s: e.sqrt(out=ss[s][:, :], in_=ss[s][:, :]), reads=[b_ss[s]], writes=[b_ss[s]])
                fw.op("dve", lambda e, s=s: e.reciprocal(out=ss[s][:, :], in_=ss[s][:, :]),
                      reads=[b_ss[s]], writes=[b_ss[s]])
                fw.op("dve", lambda e, s=s, m=m: e.scalar_tensor_tensor(out=y32[:, :], in0=xt[s][:, :],
                                                                        scalar=ss[s][:, 0:1], in1=gain[m][:, :],
                                                                        op0=ALU.mult, op1=ALU.mult),
                      reads=[b_xt[s], b_ss[s], b_gain[m]], writes=[b_y32])
                fw.op("pool", lambda e, s=s, m=m: e.tensor_tensor(out=hb[s][:, :], in0=y32[:, :], in1=shift[m][:, :],
                                                                  op=ALU.add),
                      reads=[b_y32, b_shift[m]], writes=[b_hb[s]])
                for k4 in range(NK // 4):
                    p = pi % 2
                    pi += 1

                    def tr(e, s=s, k4=k4, p=p):
                        for kk in range(4):
                            k = k4 * 4 + kk
                            ins = e.transpose(ptr[p][:, kk * 128:(kk + 1) * 128], hb[s][:, k * 128:(k + 1) * 128],
                                              ident[:, :])
                        return ins
                    fw.op("pe", tr, reads=[b_hb[s], b_id], writes=[b_ptr[p]])
                    fw.op("act", lambda e, k4=k4, p=p, t=t: e.copy(
                        out=hT[:, k4 * 4:(k4 + 1) * 4, t * 128:(t + 1) * 128],
                        in_=ptr[p][:, :].rearrange("p (k t) -> p k t", k=4)),
                        reads=[b_ptr[p]], writes=[b_hT[t]])
            if pname == "oc":
                blocks = [("tm", c0, 512) for c0 in range(0, C_MQ, 512)]
                blocks += [("fm", C_MQ, 512), ("fm", C_MK, 512)]
                blocks += [("tm", c0, 512) for c0 in range(C_MV, C_MG, 512)]
                blocks += [("tm32", C_MG, 16)]
                segs = [(0, cfg.nt_own, cfg.O_OWN), (cfg.nt_own, cfg.nt_ctx, cfg.O_CTX)]
            else:
                blocks = [("tm", C_AKV, 512), ("fm", C_MQ, 512), ("fm", C_MK, 512), ("tm", C_MV, 512),
                          ("tm", C_CKV, 512), ("tm32", C_MG, 16)]
                segs = [(0, cfg.nt_oth, cfg.O_OTH)]
            for kind, c0, cw in blocks:
                s = wi % 2
                wi += 1
                fw.dma("pool", W[s][:, :, 0:cw], wv[:, :, c0:c0 + cw], writes=[b_W[s]])
                if kind in ("tm", "tm32"):
                    st = stage[s] if kind == "tm" else stg32
                    bst = b_stage[s] if kind == "tm" else b_stg32
                    ntl_ = 1 if (pname == "oth" and c0 == C_CKV) else nt
                    for t in range(ntl_):
                        p = pi % 4
                        pi += 1

                        def mm(e, s=s, t=t, p=p, cw=cw):
                            for k in range(NK):
                                ins = e.matmul(pmm[p][:, 0:cw], lhsT=hT[:, k, t * 128:(t + 1) * 128],
                                               rhs=W[s][:, k, 0:cw], start=(k == 0), stop=(k == NK - 1))
                            return ins
                        fw.op("pe", mm, reads=[b_hT[t], b_W[s]], writes=[b_pmm[p]])
                        ev = "act" if t % 2 == 0 else "dve"
                        if ev == "act":
                            fw.op("act", lambda e, t=t, p=p, cw=cw, st=st: e.copy(out=st[:, t, 0:cw], in_=pmm[p][:, 0:cw]),
                                  reads=[b_pmm[p]], writes=[bst])
                        else:
                            fw.op("dve", lambda e, t=t, p=p, cw=cw, st=st: e.tensor_copy(out=st[:, t, 0:cw],
                                                                                         in_=pmm[p][:, 0:cw]),
                                  reads=[b_pmm[p]], writes=[bst])
                    dst = S.tm(c0, cw)
                    for (t0, ntl, o) in segs:
                        ntl = min(ntl, ntl_ - t0)
                        if ntl <= 0:
                            continue
                        fw.dma("sp", dst[o:o + ntl * 128, :].rearrange("(t p) c -> p t c", p=128),
                               st[:, t0:t0 + ntl, 0:cw], reads=[bst])
                else:
                    ntok = nt * 128 if (pname == "oc" or c0 == C_MK) else 128
                    stT = stage[s][:, :, :].rearrange("p t c -> p (t c)")
                    for cc in range(4):
                        for tb in range(0, ntok, 512):
                            tw = min(512, ntok - tb)
                            p = pi % 4
                            pi += 1

                            def mm(e, s=s, cc=cc, tb=tb, tw=tw, p=p):
                                for k in range(NK):
                                    ins = e.matmul(pmm[p][:, 0:tw], lhsT=W[s][:, k, cc * 128:(cc + 1) * 128],
                                                   rhs=hT[:, k, tb:tb + tw], start=(k == 0), stop=(k == NK - 1))
                                return ins
                            fw.op("pe", mm, reads=[b_hT[tt] for tt in range(tb // 128, (tb + tw) // 128)] + [b_W[s]],
                                  writes=[b_pmm[p]])
                            if (tb // 512) % 2 == 0:
                                fw.op("act", lambda e, tb=tb, tw=tw, p=p, cc=cc: e.copy(
                                    out=stT[:, cc * ntok + tb:cc * ntok + tb + tw], in_=pmm[p][:, 0:tw]),
                                    reads=[b_pmm[p]], writes=[b_stage[s]])
                            else:
                                fw.op("dve", lambda e, tb=tb, tw=tw, p=p, cc=cc: e.tensor_copy(
                                    out=stT[:, cc * ntok + tb:cc * ntok + tb + tw], in_=pmm[p][:, 0:tw]),
                                    reads=[b_pmm[p]], writes=[b_stage[s]])
                    dstT = S.fm(c0)
                    for (t0, ntl, o) in segs:
                        a0, a1 = t0 * 128, min((t0 + ntl) * 128, ntok)
                        if a1 <= a0:
                            continue
                        fw.dma("sp", dstT[:, o:o + a1 - a0].rearrange("(cc p) t -> p cc t", p=128),
                               stT[:, 0:4 * ntok].rearrange("p (cc t) -> p cc t", cc=4)[:, :, a0:a1],
                               reads=[b_stage[s]])


class Scratch:
    def __init__(self, nc, cfg, taps=()):
        self.nc = nc
        self.cfg = cfg
        T = cfg.T_ALL

        def dt(name, shape, dtype):
            kind = "ExternalOutput" if name in taps else "Internal"
            return nc.dram_tensor(name, list(shape), dtype, kind=kind).ap()
        self.mods = [dt("mods%d" % l, [2, 6 * D], F32) for l in range(DEPTH)]
        self.P = dt("P_tm", [T, C_MG], BF16)
        self.MG = dt("MG", [T, 16], F32)
        self.MQT = dt("MQT", [512, T], BF16)
        self.MKT = dt("MKT", [512, T], BF16)
        self.HT2 = dt("HT2", [2048, cfg.T_OWN + cfg.T_CTX], BF16)
        self.COMB = dt("COMB", [cfg.T_OWN + cfg.T_CTX, 16], F32)
        self.ZT = dt("ZT", [2048, cfg.T_OWN + cfg.T_CTX], BF16)
        self.HD = dt("HD", [2, cfg.T_OWN + cfg.T_CTX, 512], F32)
        self.OT = dt("OT", [2048, cfg.T_OWN + cfg.T_CTX], BF16)

    def tm(self, c0, cw):
        if c0 == C_MG:
            return self.MG
        return self.P[:, c0:c0 + cw]

    def fm(self, c0):
        return self.MQT if c0 == C_MQ else self.MKT


def _win_perm(half):
    mg = np.arange(3584, 3600).reshape(2, 2, 4)
    if half == 1:
        mg = mg[:, ::-1, :]
    return np.concatenate([np.arange(0, 3584), np.arange(3600, 10768), mg.reshape(-1)])


def core_inputs(inp, cfg, b, half):
    To = cfg.T_OWN
    seq = np.asarray(inp["x"][b][:2 * To])
    ctx = np.asarray(inp["ctx"][b][:cfg.T_CTX])
    if half == 1:
        seq = seq[::-1]
        ctx = ctx[::-1]
    cv = np.stack([np.asarray(inp["c"][b]).reshape(NK, 128).T, np.asarray(inp["c_ctx"]).reshape(NK, 128).T], axis=2)
    return {
        "x_own": np.ascontiguousarray(seq[:To]),
        "x_oth": np.ascontiguousarray(seq[To:]),
        "xc": np.ascontiguousarray(ctx),
        "cvec": np.ascontiguousarray(cv.reshape(128, 2 * NK)),
    }


def layer_inputs(inp, l, half):
    return {
        "w_mod": np.ascontiguousarray(inp["w_mod"][l]),
        "bmod2": np.ascontiguousarray(np.stack([inp["b_mod"][l], inp["b_mod"][l]], 0)),
        "w_in": np.ascontiguousarray(np.asarray(inp["w_in"][l])[:, _win_perm(half)]),
        "g1row": np.ascontiguousarray(np.asarray(inp["norm1_g"][l])[None, :]),
    }


def _rstd(fw, ss, b_ss, n):
    fw.op("dve", lambda e: e.tensor_scalar(out=ss, in0=ss, scalar1=1.0 / n, scalar2=EPS, op0=ALU.mult, op1=ALU.add),
          reads=[b_ss], writes=[b_ss])
    fw.op("act", lambda e: e.sqrt(out=ss, in_=ss), reads=[b_ss], writes=[b_ss])
    fw.op("dve", lambda e: e.reciprocal(out=ss, in_=ss), reads=[b_ss], writes=[b_ss])


def phase_attn(fw, cfg, l, S, A, kind, need_ctx):
    nc = fw.nc
    To, Tc = cfg.T_OWN, cfg.T_CTX
    if kind == "a":
        Hq, Hkv, cq0, ckv0, orow = 8, 2, C_AQ, C_AKV, 0
    else:
        Hq, Hkv, cq0, ckv0, orow = 4, 2, C_CQ, C_CKV, 1536
    G = Hq // Hkv
    scale = 128.0 ** -0.5
    QB = min(512, To)
    nsub = QB // 128
    ktiles = [(cfg.O_OWN + i * 128, i * 128) for i in range(cfg.nt_own)]
    if kind == "a":
        ktiles += [(cfg.O_OTH + i * 128, To + i * 128) for i in range(cfg.nt_oth)]
    else:
        ktiles += [(cfg.O_OTH, To)]
    n_lat_k = len(ktiles)
    ktiles += [(cfg.O_CTX + i * 128, None) for i in range(cfg.nt_ctx)]
    nkt = len(ktiles)
    qtiles = [(cfg.O_OWN + i * 128, i * 128) for i in range(cfg.nt_own)]
    if need_ctx:
        qtiles += [(cfg.O_CTX + i * 128, None) for i in range(cfg.nt_ctx)]
    nqt = len(qtiles)
    with Phase(fw, "at" + kind) as ph:
        ident = ph.sb([128, 128], BF16)
        b_id = make_ident(fw, ident)
        KT = ph.sb([128, Hkv, nkt * 128], BF16)
        VA = ph.sb([128, nkt, Hkv, 129], BF16)
        QT = ph.sb([128, Hq, nqt * 128], BF16)
        b_KT, b_VA, b_QT = bufs(nkt), bufs(nkt), bufs(nqt)
        b_va1 = Buf()
        fw.op("pool", lambda e: e.memset(VA[:, :, :, 128:129], 1.0), writes=[b_va1])
        gq = ph.sb([128, 128], F32)
        gk = ph.sb([128, 128], F32)
        b_g = Buf()
        b_g2 = Buf()
        if kind == "a":
            load_bc(fw, "sp", gq[:, :], A["a_qn_g"][l:l + 1, :], b_g)
            load_bc(fw, "sp", gk[:, :], A["a_kn_g"][l:l + 1, :], b_g2)
            fw.op("dve", lambda e: e.tensor_scalar(out=gq[:, :], in0=gq[:, :], scalar1=scale, scalar2=None, op0=ALU.mult),
                  reads=[b_g, b_g2], writes=[b_g])
        esink = ph.sb([128, 4], F32)
        b_es = Buf()
        if kind == "c":
            load_bc(fw, "sp", esink[:, :], A["c_sink"][l:l + 1, :], b_es)
            fw.op("act", lambda e: e.activation(out=esink[:, :], in_=esink[:, :], func=AF.Exp), reads=[b_es], writes=[b_es])
        masks = {}
        b_mask = Buf()
        if kind == "c":
            mk_t = ph.sb([128, nsub + 2, QB], BF16)
            fw.op("pool", lambda e: e.memset(mk_t[:, :, :], 1.0), writes=[b_mask])
            for r in range(-1, nsub + 1):
                mv = mk_t[:, r + 1, :]
                fw.op("pool", lambda e, mv=mv, r=r: e.affine_select(out=mv, in_=mv, pattern=[[1, QB]], compare_op=ALU.is_ge,
                                                                    fill=0.0, base=128 - r * 128, channel_multiplier=-1),
                      reads=[b_mask], writes=[b_mask])
                fw.op("pool", lambda e, mv=mv, r=r: e.affine_select(out=mv, in_=mv, pattern=[[-1, QB]], compare_op=ALU.is_ge,
                                                                    fill=0.0, base=128 + r * 128, channel_multiplier=1),
                      reads=[b_mask], writes=[b_mask])
                masks[r] = mv
        HM = max(Hq, 2 * Hkv)
        ld = [ph.sb([128, HM * 128], BF16) for _ in range(2)]
        b_ld = bufs(2)
        rp = [ph.sb([128, 128], F32) for _ in range(2)]
        b_rp = bufs(2)
        x32 = ph.sb([128, Hq * 128], F32)
        sq = ph.sb([128, Hq * 128], F32)
        t1 = ph.sb([128, Hq * 64], F32)
        t2 = ph.sb([128, Hq * 64], F32)
        xr = [ph.sb([128, Hq * 128], BF16) for _ in range(2)]
        b_x32, b_sq, b_t1, b_t2 = bufs(4)
        b_xr = bufs(2)
        ssn = ph.sb([128, Hq], F32)
        b_ssn = Buf()
        ptr = [ph.ps([128, 512], BF16) for _ in range(2)]
        b_ptr = bufs(2)
        cnt = {"ld": 0, "tr": 0, "xr": 0}

        def prep(row, rrow, c0, H, gt, dests, vdest=None):
            s = cnt["ld"] % 2
            cnt["ld"] += 1
            w = H * 128 + (Hkv * 128 if vdest is not None else 0)
            fw.dma("sp", ld[s][:, 0:w], S.P[row:row + 128, c0:c0 + w], writes=[b_ld[s]])
            if rrow is not None:
                fw.dma("sp", rp[s][:, :], A["rope"][rrow:rrow + 128, :], writes=[b_rp[s]])
            if vdest is not None:
                fw.op("pool", lambda e: e.tensor_copy(out=vdest[0], in_=ld[s][:, H * 128:w].rearrange("p (h d) -> p h d", h=Hkv)),
                      reads=[b_ld[s], b_va1], writes=[vdest[1]])
            xs = x32[:, 0:H * 128]
            if gt is not None:
                fw.op("act", lambda e: e.copy(out=xs, in_=ld[s][:, 0:H * 128]), reads=[b_ld[s]], writes=[b_x32])
                fw.op("dve", lambda e: e.tensor_tensor(out=sq[:, 0:H * 128], in0=xs, in1=xs, op=ALU.mult),
                      reads=[b_x32], writes=[b_sq])
                fw.op("dve", lambda e: e.tensor_reduce(out=ssn[:, 0:H], in_=sq[:, 0:H * 128].rearrange("p (h d) -> p h d", h=H),
                                                       axis=AX.X, op=ALU.add), reads=[b_sq], writes=[b_ssn])
                _rstd(fw, ssn[:, 0:H], b_ssn, 128)
                x3 = xs.rearrange("p (h d) -> p h d", h=H)
                fw.op("dve", lambda e: e.tensor_tensor(out=x3, in0=x3, in1=ssn[:, 0:H].unsqueeze(2).to_broadcast([128, H, 128]),
                                                       op=ALU.mult), reads=[b_x32, b_ssn], writes=[b_x32])
                fw.op("dve", lambda e: e.tensor_tensor(out=x3, in0=x3, in1=gt[:, :].unsqueeze(1).to_broadcast([128, H, 128]),
                                                       op=ALU.mult), reads=[b_x32, b_g], writes=[b_x32])
            else:
                sc = scale if dests[0][2] == "q" else 1.0
                fw.op("act", lambda e: e.activation(out=xs, in_=ld[s][:, 0:H * 128], func=AF.Copy, scale=sc),
                      reads=[b_ld[s]], writes=[b_x32])
            xi = cnt["xr"] % 2
            cnt["xr"] += 1
            xo = xr[xi][:, 0:H * 128]
            if rrow is not None:
                x5 = xs.rearrange("p (h a b f) -> p h a b f", h=H, a=2, b=2)
                o5 = xo.rearrange("p (h a b f) -> p h a b f", h=H, a=2, b=2)
                cs = rp[s][:, 0:64].rearrange("p (a f) -> p a f", a=2).unsqueeze(1).to_broadcast([128, H, 2, 32])
                sn = rp[s][:, 64:128].rearrange("p (a f) -> p a f", a=2).unsqueeze(1).to_broadcast([128, H, 2, 32])
                x1, x2 = x5[:, :, :, 0, :], x5[:, :, :, 1, :]
                u1 = t1[:, 0:H * 64].rearrange("p (h a f) -> p h a f", h=H, a=2)
                u2 = t2[:, 0:H * 64].rearrange("p (h a f) -> p h a f", h=H, a=2)
                fw.op("dve", lambda e: e.tensor_tensor(out=u1, in0=x1, in1=cs, op=ALU.mult), reads=[b_x32, b_rp[s]], writes=[b_t1])
                fw.op("dve", lambda e: e.tensor_tensor(out=u2, in0=x2, in1=sn, op=ALU.mult), reads=[b_x32, b_rp[s]], writes=[b_t2])
                fw.op("dve", lambda e: e.tensor_tensor(out=o5[:, :, :, 0, :], in0=u1, in1=u2, op=ALU.subtract),
                      reads=[b_t1, b_t2], writes=[b_xr[xi]])
                fw.op("dve", lambda e: e.tensor_tensor(out=u1, in0=x2, in1=cs, op=ALU.mult), reads=[b_x32, b_rp[s]], writes=[b_t1])
                fw.op("dve", lambda e: e.tensor_tensor(out=u2, in0=x1, in1=sn, op=ALU.mult), reads=[b_x32, b_rp[s]], writes=[b_t2])
                fw.op("dve", lambda e: e.tensor_tensor(out=o5[:, :, :, 1, :], in0=u1, in1=u2, op=ALU.add),
                      reads=[b_t1, b_t2], writes=[b_xr[xi]])
            else:
                fw.op("dve", lambda e: e.tensor_copy(out=xo, in_=xs), reads=[b_x32], writes=[b_xr[xi]])
            for h0 in range(0, H, 4):
                hn = min(4, H - h0)
                p = cnt["tr"] % 2
                cnt["tr"] += 1

                def tr(e):
                    for hh in range(hn):
                        ins = e.transpose(ptr[p][:, hh * 128:(hh + 1) * 128], xo[:, (h0 + hh) * 128:(h0 + hh + 1) * 128], ident[:, :])
                    return ins
                fw.op("pe", tr, reads=[b_xr[xi], b_id], writes=[b_ptr[p]])
                fw.op("act", lambda e: e.copy(out=dests[0][0][:, h0:h0 + hn, dests[0][3]:dests[0][3] + 128],
                                              in_=ptr[p][:, 0:hn * 128].rearrange("p (h t) -> p h t", h=hn)),
                      reads=[b_ptr[p]], writes=[dests[0][1]])

        for ki, (row, rrow) in enumerate(ktiles):
            prep(row, rrow, ckv0, Hkv, gk if kind == "a" else None, [(KT, b_KT[ki], "k", ki * 128)],
                 vdest=(VA[:, ki, :, 0:128], b_VA[ki]))
        for qi, (row, rrow) in enumerate(qtiles):
            prep(row, rrow, cq0, Hq, gq if kind == "a" else None, [(QT, b_QT[qi], "q", qi * 128)])

        if getattr(S, "dbg", None) and kind in S.dbg:
            dq = nc.dram_tensor("dbgQT" + kind, [128, Hq, nqt * 128], BF16, kind="ExternalOutput").ap()
            dk = nc.dram_tensor("dbgKT" + kind, [128, Hkv, nkt * 128], BF16, kind="ExternalOutput").ap()
            dv = nc.dram_tensor("dbgVA" + kind, [128, nkt, Hkv, 129], BF16, kind="ExternalOutput").ap()
            fw.dma("sp", dq, QT[:, :, :], reads=b_QT)
            fw.dma("sp", dk, KT[:, :, :], reads=b_KT)
            fw.dma("sp", dv, VA[:, :, :, :], reads=b_VA + [b_va1])
        pst = [ph.ps([128, 512]) for _ in range(2)]
        b_pst = bufs(2)
        po = [ph.ps([128, 512]) for _ in range(4)]
        b_po = bufs(4)
        PT = [ph.sb([128, 512], BF16) for _ in range(3)]
        b_PT = bufs(3)
        rden = ph.sb([128, 4], F32)
        b_rden = Buf()
        on = [ph.sb([128, 4, 128], BF16) for _ in range(2)]
        b_on = bufs(2)
        ost = [ph.sb([128, 512], BF16) for _ in range(2)]
        b_ost = bufs(2)
        it = {"s": 0, "p": 0, "o": 0}
        qblocks = []
        for q0 in range(0, To, QB):
            i0 = q0 // 128
            if kind == "a":
                kl = [(ki, None) for ki in range(nkt)]
            else:
                kl = []
                for r in range(-1, nsub + 1):
                    kt = i0 + r
                    if 0 <= kt <= cfg.nt_own:
                        kl.append((kt, r))
                kl += [(n_lat_k + i, None) for i in range(cfg.nt_ctx)]
            qblocks.append((q0, nsub, kl, q0))
        if need_ctx:
            qblocks.append((To, cfg.nt_ctx, [(n_lat_k + i, None) for i in range(cfg.nt_ctx)], To))
        for h in range(Hq):
            g = h // G
            for (q0, ns, kl, oc0) in qblocks:
                qw = ns * 128
                def emit_s(ii):
                    ki_, r_ = kl[ii]
                    sp__ = it["s"] % 2
                    it["s"] += 1
                    fw.op("pe", lambda e: e.matmul(pst[sp__][:, 0:qw], lhsT=KT[:, g, ki_ * 128:(ki_ + 1) * 128],
                                                   rhs=QT[:, h, q0:q0 + qw], start=True, stop=True),
                          reads=[b_KT[ki_]] + [b_QT[q0 // 128 + j] for j in range(ns)], writes=[b_pst[sp__]])
                    return sp__
                sq_ = [emit_s(0)]
                for idx, (ki, r) in enumerate(kl):
                    if idx + 1 < len(kl):
                        sq_.append(emit_s(idx + 1))
                    sp_ = sq_[idx]
                    pi_ = it["p"] % 3
                    it["p"] += 1
                    fw.op("act", lambda e: e.activation(out=PT[pi_][:, 0:qw], in_=pst[sp_][:, 0:qw], func=AF.Exp),
                          reads=[b_pst[sp_]], writes=[b_PT[pi_]])
                    if r is not None:
                        fw.op("pool", lambda e: e.tensor_tensor(out=PT[pi_][:, 0:qw], in0=PT[pi_][:, 0:qw], in1=masks[r][:, 0:qw],
                                                                op=ALU.mult), reads=[b_PT[pi_], b_mask], writes=[b_PT[pi_]])

                    def pv(e):
                        ins = None
                        for j in range(ns):
                            if r is not None and abs(r - j) > 1:
                                continue
                            first = (idx == 0) if r is None else (ki == max(0, q0 // 128 + j - 1))
                            ins = e.matmul(po[j][:, 0:129], lhsT=PT[pi_][:, j * 128:(j + 1) * 128], rhs=VA[:, ki, g, :],
                                           start=first, stop=(idx == len(kl) - 1))
                        return ins
                    fw.op("pe", pv, reads=[b_PT[pi_], b_VA[ki], b_va1], writes=b_po[0:ns])
                oi = it["o"] % 2
                it["o"] += 1
                for j in range(ns):
                    if kind == "c":
                        fw.op("dve", lambda e: e.tensor_scalar(out=rden[:, j:j + 1], in0=po[j][:, 128:129],
                                                               scalar1=esink[:, h:h + 1], scalar2=None, op0=ALU.add),
                              reads=[b_po[j], b_es], writes=[b_rden])
                        fw.op("dve", lambda e: e.reciprocal(out=rden[:, j:j + 1], in_=rden[:, j:j + 1]),
                              reads=[b_rden], writes=[b_rden])
                    else:
                        fw.op("dve", lambda e: e.reciprocal(out=rden[:, j:j + 1], in_=po[j][:, 128:129]),
                              reads=[b_po[j]], writes=[b_rden])
                    fw.op("dve", lambda e: e.tensor_scalar(out=on[oi][:, j, :], in0=po[j][:, 0:128], scalar1=rden[:, j:j + 1],
                                                           scalar2=None, op0=ALU.mult),
                          reads=[b_po[j], b_rden], writes=[b_on[oi]])
                p = cnt["tr"] % 2
                cnt["tr"] += 1

                def tr2(e):
                    for j in range(ns):
                        ins = e.transpose(ptr[p][:, j * 128:(j + 1) * 128], on[oi][:, j, :], ident[:, :])
                    return ins
                fw.op("pe", tr2, reads=[b_on[oi], b_id], writes=[b_ptr[p]])
                fw.op("act", lambda e: e.copy(out=ost[oi][:, 0:qw], in_=ptr[p][:, 0:qw]), reads=[b_ptr[p]], writes=[b_ost[oi]])
                fw.dma("sp", S.OT[orow + h * 128:orow + (h + 1) * 128, oc0:oc0 + qw], ost[oi][:, 0:qw], reads=[b_ost[oi]])


def rope_table(cfg, half):
    n = 2 * cfg.T_OWN
    t = np.arange(n)
    if half == 1:
        t = n - 1 - t
    rows = (t // 64).astype(np.float32)
    cols = (t % 64).astype(np.float32)
    inv = (10000.0 ** (-np.arange(0, 64, 2, dtype=np.float32) / 64.0)).astype(np.float32)
    ang = np.concatenate([rows[:, None] * inv, cols[:, None] * inv], axis=-1).astype(np.float32)
    return np.ascontiguousarray(np.concatenate([np.cos(ang), np.sin(ang)], axis=1).astype(np.float32))


def phase_mlstm(fw, cfg, l, S, A, need_ctx):
    nc = fw.nc
    To, Tc, TA = cfg.T_OWN, cfg.T_CTX, cfg.T_ALL
    NT = TA // 128
    T_OC = To + Tc
    kscale = 128.0 ** -0.5
    with Phase(fw, "ml") as ph:
        identB = ph.sb([128, 128], BF16)
        b_idB = make_ident(fw, identB)
        identF = ph.sb([128, 128], F32)
        b_idF = make_ident(fw, identF)
        ones = ph.sb([128, 128], F32)
        U = [ph.sb([128, 128], F32) for _ in range(2)]
        NEGM = [ph.sb([128, 128], F32) for _ in range(2)]
        Sel = [ph.sb([128, 128], F32) for _ in range(2)]
        Bsel = ph.sb([64, 4], F32)
        b_c = Buf()
        fw.op("pool", lambda e: e.memset(ones[:, :], 1.0), writes=[b_c])
        for d in range(2):
            fw.op("pool", lambda e: e.memset(U[d][:, :], 1.0), writes=[b_c])
            fw.op("pool", lambda e: e.memset(NEGM[d][:, :], 0.0), writes=[b_c])
            fw.op("pool", lambda e: e.memset(Sel[d][:, :], 1.0), writes=[b_c])
        fw.op("pool", lambda e: e.memset(Bsel[:, :], 0.0), writes=[b_c])
        sg = [1, -1]
        for d in range(2):
            fw.op("pool", lambda e: e.affine_select(out=U[d][:, :], in_=U[d][:, :], pattern=[[sg[d], 128]], compare_op=ALU.is_ge,
                                                    fill=0.0, base=0, channel_multiplier=-sg[d]), reads=[b_c], writes=[b_c])
            fw.op("pool", lambda e: e.affine_select(out=NEGM[d][:, :], in_=NEGM[d][:, :], pattern=[[-sg[d], 128]],
                                                    compare_op=ALU.is_ge, fill=NEG, base=0, channel_multiplier=sg[d]),
                  reads=[b_c], writes=[b_c])
            last = 127 if d == 0 else 0
            fw.op("pool", lambda e: e.affine_select(out=Sel[d][:, :], in_=Sel[d][:, :], pattern=[[0, 128]], compare_op=ALU.is_equal,
                                                    fill=0.0, base=-last, channel_multiplier=1), reads=[b_c], writes=[b_c])
        for base in (0, -32):
            fw.op("pool", lambda e: e.affine_select(out=Bsel[:, :], in_=Bsel[:, :], pattern=[[-1, 4]], compare_op=ALU.not_equal,
                                                    fill=1.0, base=base, channel_multiplier=1), reads=[b_c], writes=[b_c])
        G = ph.sb([128, NT, 16], F32)
        IC = ph.sb([128, NT, 8], F32)
        LF = ph.sb([128, NT, 8], F32)
        gb = ph.sb([128, 16], F32)
        b_G, b_gate, b_gb, b_gb2 = bufs(4)
        fw.dma("sp", G[:, :, :], S.MG.rearrange("(t p) c -> p t c", p=128), writes=[b_G])
        load_bc(fw, "sp", gb[:, 0:8], A["m_ig_b"][l:l + 1, :], b_gb)
        load_bc(fw, "sp", gb[:, 8:16], A["m_fg_b"][l:l + 1, :], b_gb2)
        fw.op("dve", lambda e: e.tensor_tensor(out=IC[:, :, :], in0=G[:, :, 0:8], in1=gb[:, 0:8].unsqueeze(1).to_broadcast([128, NT, 8]),
                                               op=ALU.add), reads=[b_G, b_gb], writes=[b_gate])
        fw.op("dve", lambda e: e.tensor_tensor(out=LF[:, :, :], in0=G[:, :, 8:16], in1=gb[:, 8:16].unsqueeze(1).to_broadcast([128, NT, 8]),
                                               op=ALU.add), reads=[b_G, b_gb2, b_gate], writes=[b_gate])
        fw.op("act", lambda e: e.activation(out=LF[:, :, :], in_=LF[:, :, :], func=AF.Exp, scale=-1.0), reads=[b_gate], writes=[b_gate])
        fw.op("act", lambda e: e.activation(out=LF[:, :, :], in_=LF[:, :, :], func=AF.Ln, bias=1.0), reads=[b_gate], writes=[b_gate])
        fw.op("dve", lambda e: e.tensor_scalar(out=LF[:, :, :], in0=LF[:, :, :], scalar1=-1.0, scalar2=None, op0=ALU.mult),
              reads=[b_gate], writes=[b_gate])
        qT = ph.sb([128, 4, T_OC], BF16)
        kT = ph.sb([128, 4, T_OC], BF16)
        Ktm = ph.sb([128, NT, 4, 128], BF16)
        VA = ph.sb([128, NT, 4, 129], BF16)
        b_qT, b_kT, b_Ktm, b_VA = bufs(4)
        fw.op("pool", lambda e: e.memset(VA[:, :, :, 128:129], 1.0), writes=[b_VA])
        for h in range(4):
            fw.dma("sp", VA[:, :, h, 0:128], S.P[:, C_MV + h * 128:C_MV + (h + 1) * 128].rearrange("(t p) d -> p t d", p=128),
                   reads=[b_VA], writes=[b_VA])
        cw = ph.sb([128, 8, 3], F32)
        b_cw = Buf()
        fw.dma("sp", cw[:, :, :].rearrange("p c k -> p (c k)"), A["m_conv"][l], writes=[b_cw])
        with Phase(fw, "mlc") as pc:
            X = [pc.sb([128, TA], BF16) for _ in range(2)]
            acc = [pc.sb([128, TA], F32) for _ in range(2)]
            ktmp = pc.sb([128, TA], BF16)
            b_X, b_acc = bufs(2), bufs(2)
            b_ktmp = Buf()
            ptr = [pc.ps([128, 512], BF16) for _ in range(2)]
            b_ptr = bufs(2)
            n = 0
            tp = 0
            for qk in range(2):
                src = S.MQT if qk == 0 else S.MKT
                for hc in range(4):
                    s = n % 2
                    n += 1
                    if qk == 0:
                        fw.dma("sp", X[s][:, 0:To + 128], src[hc * 128:(hc + 1) * 128, 0:To + 128], writes=[b_X[s]])
                        fw.dma("sp", X[s][:, 2 * To:TA], src[hc * 128:(hc + 1) * 128, 2 * To:TA], reads=[b_X[s]], writes=[b_X[s]])
                    else:
                        fw.dma("sp", X[s][:, :], src[hc * 128:(hc + 1) * 128, :], writes=[b_X[s]])
                    w = cw[:, qk * 4 + hc, :]
                    for (a, b) in (((0, To + 128) if qk == 0 else (0, 2 * To)), (2 * To, TA)):
                        fw.op("dve", lambda e: e.tensor_scalar(out=acc[s][:, a:b], in0=X[s][:, a:b], scalar1=w[:, 1:2], scalar2=None,
                                                               op0=ALU.mult), reads=[b_X[s], b_cw], writes=[b_acc[s]])
                        fw.op("dve", lambda e: e.scalar_tensor_tensor(out=acc[s][:, a + 1:b], in0=X[s][:, a:b - 1], scalar=w[:, 0:1],
                                                                      in1=acc[s][:, a + 1:b], op0=ALU.mult, op1=ALU.add),
                              reads=[b_X[s], b_cw, b_acc[s]], writes=[b_acc[s]])
                        fw.op("dve", lambda e: e.scalar_tensor_tensor(out=acc[s][:, a:b - 1], in0=X[s][:, a + 1:b], scalar=w[:, 2:3],
                                                                      in1=acc[s][:, a:b - 1], op0=ALU.mult, op1=ALU.add),
                              reads=[b_X[s], b_cw, b_acc[s]], writes=[b_acc[s]])
                    if qk == 0:
                        fw.op("act", lambda e: e.activation(out=qT[:, hc, 0:To], in_=acc[s][:, 0:To], func=AF.Silu),
                              reads=[b_acc[s]], writes=[b_qT])
                        fw.op("act", lambda e: e.activation(out=qT[:, hc, To:T_OC], in_=acc[s][:, 2 * To:TA], func=AF.Silu),
                              reads=[b_acc[s]], writes=[b_qT])
                    else:
                        fw.op("act", lambda e: e.activation(out=acc[s][:, :], in_=acc[s][:, :], func=AF.Silu),
                              reads=[b_acc[s]], writes=[b_acc[s]])
                        fw.op("dve", lambda e: e.tensor_scalar(out=ktmp[:, :], in0=acc[s][:, :], scalar1=kscale, scalar2=None,
                                                               op0=ALU.mult), reads=[b_acc[s]], writes=[b_ktmp])
                        fw.op("pool", lambda e: e.tensor_copy(out=kT[:, hc, 0:To], in_=ktmp[:, 0:To]), reads=[b_ktmp], writes=[b_kT])
                        fw.op("pool", lambda e: e.tensor_copy(out=kT[:, hc, To:T_OC], in_=ktmp[:, 2 * To:TA]), reads=[b_ktmp], writes=[b_kT])
                        for t0 in range(0, NT, 4):
                            tn = min(4, NT - t0)
                            p = tp % 2
                            tp += 1

                            def tr(e):
                                for tt in range(tn):
                                    ins = e.transpose(ptr[p][:, tt * 128:(tt + 1) * 128], ktmp[:, (t0 + tt) * 128:(t0 + tt + 1) * 128],
                                                      identB[:, :])
                                return ins
                            fw.op("pe", tr, reads=[b_ktmp, b_idB], writes=[b_ptr[p]])
                            fw.op("act", lambda e: e.copy(out=Ktm[:, t0:t0 + tn, hc, :],
                                                          in_=ptr[p][:, 0:tn * 128].rearrange("p (t d) -> p t d", t=tn)),
                                  reads=[b_ptr[p]], writes=[b_Ktm])
        pA = ph.ps([128, 512])
        pC = ph.ps([128, 512])
        pD = ph.ps([128, 512])
        pE = ph.ps([128, 512], BF16)
        pF = ph.ps([128, 512])
        pG = ph.ps([128, 512])
        b_pA, b_pC, b_pD, b_pE, b_pF, b_pG = bufs(6)
        Cst = [ph.sb([128, 4, 129], F32) for _ in range(2)]
        Cb = [ph.sb([128, 4, 129], BF16) for _ in range(2)]
        mst = [ph.sb([128, 4], F32) for _ in range(2)]
        b_C, b_Cb, b_m = bufs(2), bufs(2), bufs(2)
        for d in range(2):
            fw.op("pool", lambda e: e.memset(Cst[d][:, :, :], 0.0), writes=[b_C[d]])
            fw.op("pool", lambda e: e.memset(Cb[d][:, :, :], 0.0), writes=[b_Cb[d]])
            fw.op("pool", lambda e: e.memset(mst[d][:, :], NEG), writes=[b_m[d]])
        AB = ph.sb([128, 64], F32)
        R64 = ph.sb([64, 128], F32)
        X64 = ph.sb([64, 128], F32)
        Lall = ph.sb([64, 4, 128], F32)
        b_AB, b_R64, b_X64, b_Lall = bufs(4)
        fw.op("pool", lambda e: e.memset(AB[:, :], 0.0), writes=[b_AB])
        fw.op("pool", lambda e: e.memset(R64[:, :], 1.0), writes=[b_R64])
        fw.op("pool", lambda e: e.memset(X64[:, :], 1.0), writes=[b_X64])
        bsb = ph.sb([128, 4], F32)
        btot = ph.sb([128, 4], F32)
        bm = ph.sb([128, 4], F32)
        rowmax = ph.sb([128, 4], F32)
        mrow = ph.sb([128, 4], F32)
        E8 = ph.sb([128, 8], F32)
        small = ph.sb([128, 16], F32)
        mnew = ph.sb([128, 4], F32)
        ld = ph.sb([128, 4, 128], F32)
        Dm = ph.sb([128, 4, 128], F32)
        Wb = ph.sb([128, 4, 128], BF16)
        WT = ph.sb([128, 4, 128], BF16)
        kw = ph.sb([128, 4, 128], BF16)
        numt = ph.sb([128, 4, 129], F32)
        hout = [ph.sb([128, 4, 128], F32) for _ in range(2)]
        tmpC = ph.sb([128, 4, 129], F32)
        (b_bsb, b_btot, b_bm, b_rowmax, b_mrow, b_E8, b_small, b_mnew, b_ld, b_Dm, b_Wb, b_WT, b_kw, b_numt,
         b_tmpC) = bufs(15)
        b_hout = bufs(2)
        hcnt = [0]

        def v2(t):
            return t[:, 0:258].rearrange("p (h e) -> p h e", h=2)

        def step(d, ti, full, oc_tile):
            lf = LF[:, ti, d * 4:(d + 1) * 4]
            ic = IC[:, ti, d * 4:(d + 1) * 4]
            m = mst[d]

            def mm_b(e):
                e.matmul(pA[:, 0:4], lhsT=U[d][:, :], rhs=lf, start=True, stop=True)
                return e.matmul(pA[:, 4:8], lhsT=ones[:, :], rhs=lf, start=True, stop=True)
            fw.op("pe", mm_b, reads=[b_gate, b_c], writes=[b_pA])
            fw.op("dve", lambda e: e.tensor_copy(out=AB[:, 32:36], in_=pA[:, 0:4]), reads=[b_pA], writes=[b_AB, b_bsb])
            fw.op("dve", lambda e: e.tensor_tensor(out=AB[:, 0:4], in0=ic, in1=pA[:, 0:4], op=ALU.subtract),
                  reads=[b_pA, b_gate], writes=[b_AB])
            fw.op("act", lambda e: e.copy(out=btot[:, :], in_=pA[:, 4:8]), reads=[b_pA], writes=[b_btot])
            fw.op("dve", lambda e: e.tensor_tensor(out=bm[:, :], in0=AB[:, 32:36], in1=m[:, :], op=ALU.add),
                  reads=[b_AB, b_m[d]], writes=[b_bm])
            fw.op("pe", lambda e: e.transpose(pA[0:64, 16:144], AB[:, :], identF[:, :]), reads=[b_AB, b_idF], writes=[b_pA])
            fw.op("act", lambda e: e.copy(out=R64[0:32, :], in_=pA[0:32, 16:144]), reads=[b_pA], writes=[b_R64])
            fw.op("dve", lambda e: e.tensor_copy(out=X64[32:64, :], in_=pA[32:64, 16:144]), reads=[b_pA], writes=[b_X64])
            fw.op("dve", lambda e: e.tensor_tensor(out=Lall[:, :, :], in0=X64[:, :].unsqueeze(1).to_broadcast([64, 4, 128]),
                                                   in1=Bsel[:, :].unsqueeze(2).to_broadcast([64, 4, 128]), op=ALU.mult),
                  reads=[b_X64, b_c], writes=[b_Lall])

            def mm_ld(e):
                for h in range(4):
                    ins = e.matmul(pC[:, h * 128:(h + 1) * 128], lhsT=Lall[:, h, :], rhs=R64[:, :], start=True, stop=True)
                return ins
            fw.op("pe", mm_ld, reads=[b_Lall, b_R64], writes=[b_pC])
            fw.op("dve", lambda e: e.tensor_tensor(out=ld[:, :, :], in0=pC[:, :].rearrange("p (h s) -> p h s", h=4),
                                                   in1=NEGM[d][:, :].unsqueeze(1).to_broadcast([128, 4, 128]), op=ALU.add),
                  reads=[b_pC, b_c], writes=[b_ld])
            fw.op("dve", lambda e: e.tensor_reduce(out=rowmax[:, :], in_=ld[:, :, :], axis=AX.X, op=ALU.max),
                  reads=[b_ld], writes=[b_rowmax])
            if full:
                fw.op("dve", lambda e: e.tensor_tensor(out=mrow[:, :], in0=bm[:, :], in1=rowmax[:, :], op=ALU.max),
                      reads=[b_bm, b_rowmax], writes=[b_mrow])
                fw.op("dve", lambda e: e.tensor_tensor(out=ld[:, :, :], in0=ld[:, :, :],
                                                       in1=mrow[:, :].unsqueeze(2).to_broadcast([128, 4, 128]), op=ALU.subtract),
                      reads=[b_ld, b_mrow], writes=[b_ld])
                fw.op("dve", lambda e: e.tensor_scalar_max(out=ld[:, :, :], in0=ld[:, :, :], scalar1=-80.0), reads=[b_ld], writes=[b_ld])
                fw.op("act", lambda e: e.activation(out=Dm[:, :, :], in_=ld[:, :, :], func=AF.Exp), reads=[b_ld], writes=[b_Dm])
                oc0 = oc_tile * 128

                def mm_s(e):
                    for h in range(4):
                        ins = e.matmul(pD[:, h * 128:(h + 1) * 128], lhsT=qT[:, h, oc0:oc0 + 128], rhs=kT[:, h, oc0:oc0 + 128],
                                       start=True, stop=True)
                    return ins
                fw.op("pe", mm_s, reads=[b_qT, b_kT], writes=[b_pD])
                fw.op("dve", lambda e: e.tensor_tensor(out=Wb[:, :, :], in0=pD[:, :].rearrange("p (h s) -> p h s", h=4), in1=Dm[:, :, :],
                                                       op=ALU.mult), reads=[b_pD, b_Dm], writes=[b_Wb])

                def mm_t(e):
                    for h in range(4):
                        ins = e.transpose(pE[:, h * 128:(h + 1) * 128], Wb[:, h, :], identB[:, :])
                    return ins
                fw.op("pe", mm_t, reads=[b_Wb, b_idB], writes=[b_pE])
                fw.op("act", lambda e: e.copy(out=WT[:, :, :], in_=pE[:, :].rearrange("p (h j) -> p h j", h=4)), reads=[b_pE], writes=[b_WT])

                def mm_intra(e):
                    for h in range(4):
                        ins = e.matmul(v2(pF if h < 2 else pG)[:, h % 2, :], lhsT=WT[:, h, :], rhs=VA[:, ti, h, :], start=True, stop=True)
                    return ins
                fw.op("pe", mm_intra, reads=[b_WT, b_VA], writes=[b_pF, b_pG])

                def mm_inter(e):
                    for h in range(4):
                        ins = e.matmul(v2(pC if h < 2 else pD)[:, h % 2, :], lhsT=qT[:, h, oc0:oc0 + 128], rhs=Cb[d][:, h, :],
                                       start=True, stop=True)
                    return ins
                fw.op("pe", mm_inter, reads=[b_qT, b_Cb[d]], writes=[b_pC, b_pD])
                fw.op("dve", lambda e: e.tensor_tensor(out=E8[:, 0:4], in0=bm[:, :], in1=mrow[:, :], op=ALU.subtract),
                      reads=[b_bm, b_mrow], writes=[b_E8])
                fw.op("dve", lambda e: e.tensor_scalar(out=E8[:, 4:8], in0=mrow[:, :], scalar1=-1.0, scalar2=None, op0=ALU.mult),
                      reads=[b_mrow, b_E8], writes=[b_E8])
                fw.op("dve", lambda e: e.tensor_scalar_max(out=E8[:, :], in0=E8[:, :], scalar1=-80.0), reads=[b_E8], writes=[b_E8])
                fw.op("act", lambda e: e.activation(out=E8[:, :], in_=E8[:, :], func=AF.Exp), reads=[b_E8], writes=[b_E8])
                for hp, (pi_, pj_, bi_, bj_) in enumerate(((pC, pF, b_pC, b_pF), (pD, pG, b_pD, b_pG))):
                    hs = slice(hp * 2, hp * 2 + 2)
                    fw.op("dve", lambda e: e.tensor_tensor(out=numt[:, hs, :], in0=v2(pi_),
                                                           in1=E8[:, hs].unsqueeze(2).to_broadcast([128, 2, 129]), op=ALU.mult),
                          reads=[bi_, b_E8], writes=[b_numt])
                    fw.op("dve", lambda e: e.tensor_tensor(out=numt[:, hs, :], in0=numt[:, hs, :], in1=v2(pj_), op=ALU.add),
                          reads=[bj_, b_numt], writes=[b_numt])
                fw.op("dve", lambda e: e.tensor_scalar(out=small[:, 4:8], in0=numt[:, :, 128], scalar1=-1.0, scalar2=None, op0=ALU.mult),
                      reads=[b_numt], writes=[b_small])
                fw.op("dve", lambda e: e.tensor_tensor(out=small[:, 0:4], in0=numt[:, :, 128], in1=small[:, 4:8], op=ALU.max),
                      reads=[b_numt, b_small], writes=[b_small])
                fw.op("dve", lambda e: e.tensor_tensor(out=small[:, 0:4], in0=small[:, 0:4], in1=E8[:, 4:8], op=ALU.max),
                      reads=[b_small, b_E8], writes=[b_small])
                fw.op("dve", lambda e: e.reciprocal(out=small[:, 0:4], in_=small[:, 0:4]), reads=[b_small], writes=[b_small])
                hi = hcnt[0] % 2
                hcnt[0] += 1
                fw.op("dve", lambda e: e.tensor_tensor(out=hout[hi][:, :, :], in0=numt[:, :, 0:128],
                                                       in1=small[:, 0:4].unsqueeze(2).to_broadcast([128, 4, 128]), op=ALU.mult),
                      reads=[b_numt, b_small], writes=[b_hout[hi]])
                fw.dma("sp", S.HD[d, oc0:oc0 + 128, :], hout[hi][:, :, :].rearrange("p h e -> p (h e)"), reads=[b_hout[hi]])
            fw.op("pe", lambda e: e.matmul(pA[:, 8:12], lhsT=Sel[d][:, :], rhs=rowmax[:, :], start=True, stop=True),
                  reads=[b_rowmax, b_c], writes=[b_pA])
            fw.op("dve", lambda e: e.tensor_tensor(out=small[:, 8:12], in0=btot[:, :], in1=m[:, :], op=ALU.add),
                  reads=[b_btot, b_m[d]], writes=[b_small])
            fw.op("dve", lambda e: e.tensor_tensor(out=mnew[:, :], in0=small[:, 8:12], in1=pA[:, 8:12], op=ALU.max),
                  reads=[b_small, b_pA], writes=[b_mnew])
            fw.op("dve", lambda e: e.tensor_tensor(out=E8[:, 4:8], in0=small[:, 8:12], in1=mnew[:, :], op=ALU.subtract),
                  reads=[b_small, b_mnew, b_E8], writes=[b_E8])
            fw.op("dve", lambda e: e.tensor_tensor(out=E8[:, 0:4], in0=AB[:, 0:4], in1=btot[:, :], op=ALU.add),
                  reads=[b_AB, b_btot, b_E8], writes=[b_E8])
            fw.op("dve", lambda e: e.tensor_tensor(out=E8[:, 0:4], in0=E8[:, 0:4], in1=mnew[:, :], op=ALU.subtract),
                  reads=[b_E8, b_mnew], writes=[b_E8])
            fw.op("dve", lambda e: e.tensor_scalar_max(out=E8[:, :], in0=E8[:, :], scalar1=-80.0), reads=[b_E8], writes=[b_E8])
            fw.op("act", lambda e: e.activation(out=E8[:, :], in_=E8[:, :], func=AF.Exp), reads=[b_E8], writes=[b_E8])
            fw.op("dve", lambda e: e.tensor_tensor(out=kw[:, :, :], in0=Ktm[:, ti, :, :],
                                                   in1=E8[:, 0:4].unsqueeze(2).to_broadcast([128, 4, 128]), op=ALU.mult),
                  reads=[b_Ktm, b_E8], writes=[b_kw])

            def mm_dc(e):
                for h in range(4):
                    ins = e.matmul(v2(pF if h < 2 else pG)[:, h % 2, :], lhsT=kw[:, h, :], rhs=VA[:, ti, h, :], start=True, stop=True)
                return ins
            fw.op("pe", mm_dc, reads=[b_kw, b_VA], writes=[b_pF, b_pG])
            fw.op("dve", lambda e: e.tensor_tensor(out=tmpC[:, :, :], in0=Cst[d][:, :, :],
                                                   in1=E8[:, 4:8].unsqueeze(2).to_broadcast([128, 4, 129]), op=ALU.mult),
                  reads=[b_C[d], b_E8], writes=[b_tmpC])
            fw.op("dve", lambda e: e.tensor_tensor(out=Cst[d][:, 0:2, :], in0=tmpC[:, 0:2, :], in1=v2(pF), op=ALU.add),
                  reads=[b_tmpC, b_pF], writes=[b_C[d]])
            fw.op("dve", lambda e: e.tensor_tensor(out=Cst[d][:, 2:4, :], in0=tmpC[:, 2:4, :], in1=v2(pG), op=ALU.add),
                  reads=[b_tmpC, b_pG, b_C[d]], writes=[b_C[d]])
            fw.op("act", lambda e: e.copy(out=Cb[d][:, :, :], in_=Cst[d][:, :, :]), reads=[b_C[d]], writes=[b_Cb[d]])
            fw.op("dve", lambda e: e.tensor_copy(out=m[:, :], in_=mnew[:, :]), reads=[b_mnew], writes=[b_m[d]])

        n_own, n_ctx = cfg.nt_own, cfg.nt_ctx
        t_own0, t_oth0, t_ctx0 = 0, n_own, 2 * n_own
        near = [(t_ctx0 + i, need_ctx, n_own + i) for i in range(n_ctx)] + [(t_own0 + i, True, i) for i in range(n_own)]
        far = [(t_ctx0 + i, need_ctx, n_own + i) for i in reversed(range(n_ctx))]
        far += [(t_oth0 + i, False, None) for i in reversed(range(n_own))]
        far += [(t_own0 + i, True, i) for i in reversed(range(n_own))]
        for i in range(max(len(near), len(far))):
            if i < len(far):
                step(1, *far[i])
            if i < len(near):
                step(0, *near[i])


def small_inputs(inp, half):
    ig = np.asarray(inp["m_ig_b"]); fg = np.asarray(inp["m_fg_b"]); cv = np.asarray(inp["m_conv"])
    if half == 1:
        ig, fg, cv = ig[:, ::-1, :], fg[:, ::-1, :], cv[:, ::-1, :]
    out = {
        "m_ig_b": np.ascontiguousarray(ig.reshape(DEPTH, 8)), "m_fg_b": np.ascontiguousarray(fg.reshape(DEPTH, 8)),
        "m_conv": np.ascontiguousarray(cv.reshape(DEPTH, 3, 8, 128).transpose(0, 3, 2, 1).reshape(DEPTH, 128, 24)),
    }
    for k in ("a_qn_g", "a_kn_g", "c_sink", "m_norm_g", "norm1_g", "norm2_g", "b_rg", "b_re"):
        out[k] = np.ascontiguousarray(inp[k])
    out["final_g"] = np.ascontiguousarray(np.asarray(inp["final_g"])[None, :])
    return out


def _tiles_oc(cfg, x_own, xc, o_own, o_ctx, need_ctx):
    tl = [(t, t * 128, x_own[t * 128:(t + 1) * 128, :], 0, o_own[t * 128:(t + 1) * 128, :]) for t in range(cfg.nt_own)]
    if need_ctx:
        tl += [(cfg.nt_own + i, cfg.O_CTX + i * 128, xc[i * 128:(i + 1) * 128, :], 1, o_ctx[i * 128:(i + 1) * 128, :])
               for i in range(cfg.nt_ctx)]
    return tl


def phase_merge(fw, cfg, l, S, A, W, x_own, xc, o_own, o_ctx, need_ctx):
    nc = fw.nc
    To, Tc = cfg.T_OWN, cfg.T_CTX
    T_OC = To + Tc
    tiles = _tiles_oc(cfg, x_own, xc, o_own, o_ctx, need_ctx)
    with Phase(fw, "mg1") as ph:
        identB = ph.sb([128, 128], BF16)
        b_id = make_ident(fw, identB)
        gmn = ph.sb([128, 512], F32)
        b_gmn = Buf()
        load_bc(fw, "sp", gmn[:, :], A["m_norm_g"][l:l + 1, :], b_gmn)
        AT = ph.sb([128, 8, T_OC], BF16)
        CT = ph.sb([128, 4, T_OC], BF16)
        b_AT, b_CT = bufs(2)
        fw.dma("sp", AT[:, :, :], S.OT[0:1024, :].rearrange("(k p) t -> p k t", p=128), writes=[b_AT])
        fw.dma("sp", CT[:, :, :], S.OT[1536:2048, :].rearrange("(k p) t -> p k t", p=128), writes=[b_CT])
        Wa = ph.sb([128, 8, D], BF16)
        Wm = ph.sb([128, 4, D], BF16)
        Wc = ph.sb([128, 4, D], BF16)
        b_Wa, b_Wm, b_Wc = bufs(3)
        fw.dma("pool", Wa[:, :, :], W["w_br_a"].rearrange("(k p) c -> p k c", p=128), writes=[b_Wa])
        fw.dma("pool", Wm[:, :, :], W["w_br_m"].rearrange("(k p) c -> p k c", p=128), writes=[b_Wm])
        fw.dma("pool", Wc[:, :, :], W["w_br_c"].rearrange("(k p) c -> p k c", p=128), writes=[b_Wc])
        h0 = [ph.sb([128, 512], F32) for _ in range(2)]
        h1 = [ph.sb([128, 512], F32) for _ in range(2)]
        mo = [ph.sb([128, 512], BF16) for _ in range(2)]
        gg = [ph.sb([128, 3 * D], BF16) for _ in range(2)]
        b_h0, b_h1, b_mo, b_gg = bufs(2), bufs(2), bufs(2), bufs(2)
        sq = ph.sb([128, 512], F32)
        ss = ph.sb([128, 4], F32)
        sig = ph.sb([128, 512], F32)
        omb = ph.sb([128, 512], BF16)
        omT = [ph.sb([128, 4, 128], BF16) for _ in range(2)]
        z32 = ph.sb([128, 512], F32)
        t32 = ph.sb([128, 512], F32)
        zb = [ph.sb([128, D], BF16) for _ in range(2)]
        zTs = [ph.sb([128, NK, 128], BF16) for _ in range(2)]
        b_sq, b_ss, b_sig, b_omb, b_z32, b_t32 = bufs(6)
        b_omT, b_zb, b_zTs = bufs(2), bufs(2), bufs(2)
        ptr = [ph.ps([128, 512], BF16) for _ in range(2)]
        b_ptr = bufs(2)
        pa = [ph.ps([128, 512]) for _ in range(2)]
        pm = [ph.ps([128, 512]) for _ in range(2)]
        pc = [ph.ps([128, 512]) for _ in range(2)]
        b_pa, b_pm, b_pc = bufs(2), bufs(2), bufs(2)
        tp = 0
        pq = 0
        for n, (t, prow, xsrc, m, dst) in enumerate(tiles):
            s = n % 2
            tok = slice(t * 128, (t + 1) * 128)
            fw.dma("sp", h0[s][:, :], S.HD[0, t * 128:(t + 1) * 128, :], writes=[b_h0[s]])
            fw.dma("sp", h1[s][:, :], S.HD[1, t * 128:(t + 1) * 128, :], writes=[b_h1[s]])
            fw.dma("sp", mo[s][:, :], S.P[prow:prow + 128, C_MO:C_MO + 512], writes=[b_mo[s]])
            fw.dma("sp", gg[s][:, :], S.P[prow:prow + 128, C_G:C_G + 3 * D], writes=[b_gg[s]])
            fw.op("dve", lambda e: e.tensor_tensor(out=h0[s][:, :], in0=h0[s][:, :], in1=h1[s][:, :], op=ALU.add),
                  reads=[b_h0[s], b_h1[s]], writes=[b_h0[s]])
            fw.op("dve", lambda e: e.tensor_tensor(out=sq[:, :], in0=h0[s][:, :], in1=h0[s][:, :], op=ALU.mult),
                  reads=[b_h0[s]], writes=[b_sq])
            fw.op("dve", lambda e: e.tensor_reduce(out=ss[:, :], in_=sq[:, :].rearrange("p (h d) -> p h d", h=4), axis=AX.X, op=ALU.add),
                  reads=[b_sq], writes=[b_ss])
            _rstd(fw, ss[:, :], b_ss, 128)
            h3 = h0[s][:, :].rearrange("p (h d) -> p h d", h=4)
            fw.op("dve", lambda e: e.tensor_tensor(out=h3, in0=h3, in1=ss[:, :].unsqueeze(2).to_broadcast([128, 4, 128]), op=ALU.mult),
                  reads=[b_h0[s], b_ss], writes=[b_h0[s]])
            fw.op("dve", lambda e: e.tensor_tensor(out=h0[s][:, :], in0=h0[s][:, :], in1=gmn[:, :], op=ALU.mult),
                  reads=[b_h0[s], b_gmn], writes=[b_h0[s]])
            fw.op("act", lambda e: e.activation(out=sig[:, :], in_=mo[s][:, :], func=AF.Sigmoid), reads=[b_mo[s]], writes=[b_sig])
            fw.op("dve", lambda e: e.tensor_tensor(out=omb[:, :], in0=h0[s][:, :], in1=sig[:, :], op=ALU.mult),
                  reads=[b_h0[s], b_sig], writes=[b_omb])
            p = tp % 2
            tp += 1

            def tr(e):
                for k in range(4):
                    ins = e.transpose(ptr[p][:, k * 128:(k + 1) * 128], omb[:, k * 128:(k + 1) * 128], identB[:, :])
                return ins
            fw.op("pe", tr, reads=[b_omb, b_id], writes=[b_ptr[p]])
            fw.op("act", lambda e: e.copy(out=omT[s][:, :, :], in_=ptr[p][:, :].rearrange("p (k t) -> p k t", k=4)),
                  reads=[b_ptr[p]], writes=[b_omT[s]])
            fw.op("act", lambda e: e.activation(out=gg[s][:, :], in_=gg[s][:, :], func=AF.Sigmoid), reads=[b_gg[s]], writes=[b_gg[s]])
            for cb in range(4):
                q = pq % 2
                pq += 1
                cs = slice(cb * 512, (cb + 1) * 512)

                def mma(e):
                    for k in range(8):
                        ins = e.matmul(pa[q][:, :], lhsT=AT[:, k, tok], rhs=Wa[:, k, cs], start=(k == 0), stop=(k == 7))
                    return ins

                def mmm(e):
                    for k in range(4):
                        ins = e.matmul(pm[q][:, :], lhsT=omT[s][:, k, :], rhs=Wm[:, k, cs], start=(k == 0), stop=(k == 3))
                    return ins

                def mmc(e):
                    for k in range(4):
                        ins = e.matmul(pc[q][:, :], lhsT=CT[:, k, tok], rhs=Wc[:, k, cs], start=(k == 0), stop=(k == 3))
                    return ins
                fw.op("pe", mma, reads=[b_AT, b_Wa], writes=[b_pa[q]])
                fw.op("pe", mmm, reads=[b_omT[s], b_Wm], writes=[b_pm[q]])
                fw.op("pe", mmc, reads=[b_CT, b_Wc], writes=[b_pc[q]])
                fw.op("dve", lambda e: e.tensor_tensor(out=z32[:, :], in0=pa[q][:, :], in1=gg[s][:, cb * 512:(cb + 1) * 512], op=ALU.mult),
                      reads=[b_pa[q], b_gg[s]], writes=[b_z32])
                fw.op("dve", lambda e: e.tensor_tensor(out=t32[:, :], in0=pm[q][:, :], in1=gg[s][:, D + cb * 512:D + (cb + 1) * 512],
                                                       op=ALU.mult), reads=[b_pm[q], b_gg[s]], writes=[b_t32])
                fw.op("pool", lambda e: e.tensor_tensor(out=z32[:, :], in0=z32[:, :], in1=t32[:, :], op=ALU.add),
                      reads=[b_z32, b_t32], writes=[b_z32])
                fw.op("dve", lambda e: e.tensor_tensor(out=t32[:, :], in0=pc[q][:, :], in1=gg[s][:, 2 * D + cb * 512:2 * D + (cb + 1) * 512],
                                                       op=ALU.mult), reads=[b_pc[q], b_gg[s]], writes=[b_t32])
                fw.op("pool", lambda e: e.tensor_tensor(out=zb[s][:, cs], in0=z32[:, :], in1=t32[:, :], op=ALU.add),
                      reads=[b_z32, b_t32], writes=[b_zb[s]])
            for k4 in range(NK // 4):
                p = tp % 2
                tp += 1

                def tr2(e):
                    for kk in range(4):
                        k = k4 * 4 + kk
                        ins = e.transpose(ptr[p][:, kk * 128:(kk + 1) * 128], zb[s][:, k * 128:(k + 1) * 128], identB[:, :])
                    return ins
                fw.op("pe", tr2, reads=[b_zb[s], b_id], writes=[b_ptr[p]])
                fw.op("act", lambda e: e.copy(out=zTs[s][:, k4 * 4:(k4 + 1) * 4, :], in_=ptr[p][:, :].rearrange("p (k t) -> p k t", k=4)),
                      reads=[b_ptr[p]], writes=[b_zTs[s]])
            fw.dma("sp", S.ZT[:, tok].rearrange("(k p) t -> p k t", p=128), zTs[s][:, :, :], reads=[b_zTs[s]])
    with Phase(fw, "mg2") as ph:
        ZTs = ph.sb([128, NK, T_OC], BF16)
        Wo = ph.sb([128, NK, D], BF16)
        b_Z, b_Wo = bufs(2)
        fw.dma("sp", ZTs[:, :, :], S.ZT.rearrange("(k p) t -> p k t", p=128), writes=[b_Z])
        fw.dma("pool", Wo[:, :, :], W["w_out"].rearrange("(k p) c -> p k c", p=128), writes=[b_Wo])
        g1 = [ph.sb([128, D], F32) for _ in range(2)]
        b_g1 = bufs(2)
        for m in range(2):
            load_bc(fw, "sp", g1[m][:, :], S.mods[l][m:m + 1, 2 * D:3 * D], b_g1[m])
        xt = [ph.sb([128, D], F32) for _ in range(2)]
        xn = [ph.sb([128, D], F32) for _ in range(2)]
        b_xt, b_xn = bufs(2), bufs(2)
        py = [ph.ps([128, 512]) for _ in range(4)]
        b_py = bufs(4)
        pq = 0
        for n, (t, prow, xsrc, m, dst) in enumerate(tiles):
            s = n % 2
            tok = slice(t * 128, (t + 1) * 128)
            fw.dma("sp", xt[s][:, :], xsrc, writes=[b_xt[s]])
            for cb in range(4):
                q = pq % 4
                pq += 1
                cs = slice(cb * 512, (cb + 1) * 512)

                def mmo(e):
                    for k in range(NK):
                        ins = e.matmul(py[q][:, :], lhsT=ZTs[:, k, tok], rhs=Wo[:, k, cs], start=(k == 0), stop=(k == NK - 1))
                    return ins
                fw.op("pe", mmo, reads=[b_Z, b_Wo], writes=[b_py[q]])
                fw.op("dve", lambda e: e.tensor_tensor(out=xn[s][:, cs], in0=py[q][:, :], in1=g1[m][:, cs], op=ALU.mult),
                      reads=[b_py[q], b_g1[m]], writes=[b_xn[s]])
                fw.op("pool", lambda e: e.tensor_tensor(out=xn[s][:, cs], in0=xn[s][:, cs], in1=xt[s][:, cs], op=ALU.add),
                      reads=[b_xn[s], b_xt[s]], writes=[b_xn[s]])
            fw.dma("sp", dst, xn[s][:, :], reads=[b_xn[s]])


def phase_moe_router(fw, cfg, l, S, A, W, o_own, o_ctx, need_ctx):
    nc = fw.nc
    tiles = _tiles_oc(cfg, o_own, o_ctx, o_own, o_ctx, need_ctx)
    BIG = 1.0e4
    with Phase(fw, "mr") as ph:
        identB = ph.sb([128, 128], BF16)
        b_id = make_ident(fw, identB)
        gain = [ph.sb([128, D], F32) for _ in range(2)]
        shift = [ph.sb([128, D], F32) for _ in range(2)]
        tmpg = ph.sb([128, D], F32)
        b_gain, b_shift = bufs(2), bufs(2)
        b_tmp = Buf()
        load_bc(fw, "sp", tmpg[:, :], A["norm2_g"][l:l + 1, :], b_tmp)
        for m in range(2):
            load_bc(fw, "sp", shift[m][:, :], S.mods[l][m:m + 1, 3 * D:4 * D], b_shift[m])
            load_bc(fw, "sp", gain[m][:, :], S.mods[l][m:m + 1, 4 * D:5 * D], b_gain[m])
            fw.op("dve", lambda e: e.scalar_tensor_tensor(out=gain[m][:, :], in0=gain[m][:, :], scalar=1.0, in1=tmpg[:, :],
                                                          op0=ALU.add, op1=ALU.mult), reads=[b_gain[m], b_tmp], writes=[b_gain[m]])
        wr = ph.sb([128, NK, 20], F32)
        wrh = ph.sb([128, NK, 20], BF16)
        wrl = ph.sb([128, NK, 20], BF16)
        brb = ph.sb([128, 20], F32)
        b_wr, b_wrh, b_wrl, b_brb, b_brb2 = bufs(5)
        fw.dma("sp", wr[:, :, :], W["w_r"].rearrange("(k p) c -> p k c", p=128), writes=[b_wr])
        load_bc(fw, "sp", brb[:, 0:4], A["b_rg"][l:l + 1, :], b_brb)
        load_bc(fw, "sp", brb[:, 4:20], A["b_re"][l:l + 1, :], b_brb2)
        fw.op("act", lambda e: e.copy(out=wrh[:, :, :], in_=wr[:, :, :]), reads=[b_wr], writes=[b_wrh])
        fw.op("dve", lambda e: e.tensor_tensor(out=wrl[:, :, :], in0=wr[:, :, :], in1=wrh[:, :, :], op=ALU.subtract),
              reads=[b_wr, b_wrh], writes=[b_wrl])
        xt = [ph.sb([128, D], F32) for _ in range(2)]
        b_xt = bufs(2)
        junk = ph.sb([128, D], BF16)
        h32 = ph.sb([128, D], F32)
        hb = [ph.sb([128, D], BF16) for _ in range(2)]
        lb = [ph.sb([128, D], BF16) for _ in range(2)]
        hTt = [ph.sb([128, NK, 128], BF16) for _ in range(2)]
        lTt = [ph.sb([128, NK, 128], BF16) for _ in range(2)]
        ss = [ph.sb([128, 1], F32) for _ in range(2)]
        b_junk, b_h32 = bufs(2)
        b_hb, b_lb, b_hTt, b_lTt, b_ss = bufs(2), bufs(2), bufs(2), bufs(2), bufs(2)
        ptr = [ph.ps([128, 512], BF16) for _ in range(2)]
        b_ptr = bufs(2)
        plg = [ph.ps([128, 512]) for _ in range(2)]
        b_plg = bufs(2)
        NTM = cfg.nt_own + cfg.nt_ctx
        lgall = ph.sb([128, NTM, 20], F32)
        rs = ph.sb([128, 7, NTM], F32)
        r4 = ph.sb([128, 3, NTM, 4], F32)
        r16 = ph.sb([128, 4, NTM, 16], F32)
        b_lg, b_wk = bufs(2)
        tp = 0
        for n, (t, prow, xsrc, m, dst) in enumerate(tiles):
            s = n % 2
            tok = slice(t * 128, (t + 1) * 128)
            fw.dma("sp", xt[s][:, :], xsrc, writes=[b_xt[s]])
            fw.op("pool", lambda e: e.memset(ss[s][:, :], 0.0), writes=[b_ss[s]])
            fw.op("act", lambda e: e.activation(out=junk[:, :], in_=xt[s][:, :], func=AF.Square, accum_out=ss[s][:, 0:1]),
                  reads=[b_xt[s], b_ss[s]], writes=[b_junk, b_ss[s]])
            _rstd(fw, ss[s][:, :], b_ss[s], D)
            fw.op("dve", lambda e: e.scalar_tensor_tensor(out=h32[:, :], in0=xt[s][:, :], scalar=ss[s][:, 0:1], in1=gain[m][:, :],
                                                          op0=ALU.mult, op1=ALU.mult), reads=[b_xt[s], b_ss[s], b_gain[m]], writes=[b_h32])
            fw.op("pool", lambda e: e.tensor_tensor(out=h32[:, :], in0=h32[:, :], in1=shift[m][:, :], op=ALU.add),
                  reads=[b_h32, b_shift[m]], writes=[b_h32])
            fw.op("act", lambda e: e.copy(out=hb[s][:, :], in_=h32[:, :]), reads=[b_h32], writes=[b_hb[s]])
            fw.op("dve", lambda e: e.tensor_tensor(out=lb[s][:, :], in0=h32[:, :], in1=hb[s][:, :], op=ALU.subtract),
                  reads=[b_h32, b_hb[s]], writes=[b_lb[s]])
            for (srcb, b_src, dstT, b_dst) in ((hb[s], b_hb[s], hTt[s], b_hTt[s]), (lb[s], b_lb[s], lTt[s], b_lTt[s])):
                for k4 in range(NK // 4):
                    p = tp % 2
                    tp += 1

                    def tr(e):
                        for kk in range(4):
                            k = k4 * 4 + kk
                            ins = e.transpose(ptr[p][:, kk * 128:(kk + 1) * 128], srcb[:, k * 128:(k + 1) * 128], identB[:, :])
                        return ins
                    fw.op("pe", tr, reads=[b_src, b_id], writes=[b_ptr[p]])
                    fw.op("act", lambda e: e.copy(out=dstT[:, k4 * 4:(k4 + 1) * 4, :], in_=ptr[p][:, :].rearrange("p (k t) -> p k t", k=4)),
                          reads=[b_ptr[p]], writes=[b_dst])
            fw.dma("sp", S.HT2[:, tok].rearrange("(k p) t -> p k t", p=128), hTt[s][:, :, :], reads=[b_hTt[s]])

            def mml(e):
                i = 0
                for (L, R) in ((hTt[s], wrh), (lTt[s], wrh), (hTt[s], wrl)):
                    for k in range(NK):
                        ins = e.matmul(plg[s][:, 0:20], lhsT=L[:, k, :], rhs=R[:, k, :], start=(i == 0), stop=(i == 3 * NK - 1))
                        i += 1
                return ins
            fw.op("pe", mml, reads=[b_hTt[s], b_lTt[s], b_wrh, b_wrl], writes=[b_plg[s]])
            fw.op("dve", lambda e: e.tensor_tensor(out=lgall[:, n, :], in0=plg[s][:, 0:20], in1=brb[:, :], op=ALU.add),
                  reads=[b_plg[s], b_brb, b_brb2], writes=[b_lg])
        T = len(tiles)
        GL, EL = lgall[:, 0:T, 0:4], lgall[:, 0:T, 4:20]
        gmax, gsum, pg, m1, m2, w1, w2 = (rs[:, i, 0:T] for i in range(7))
        oh, ex, pen = r4[:, 0, 0:T, :], r4[:, 1, 0:T, :], r4[:, 2, 0:T, :]
        esel, mk1, mk2, e2 = r16[:, 0, 0:T, :], r16[:, 1, 0:T, :], r16[:, 2, 0:T, :], r16[:, 3, 0:T, :]

        def bc4(v):
            return v.unsqueeze(2).to_broadcast([128, T, 4])

        def bc16(v):
            return v.unsqueeze(2).to_broadcast([128, T, 16])
        seq = [
            ("dve", lambda e: e.tensor_reduce(out=gmax, in_=GL, axis=AX.X, op=ALU.max)),
            ("dve", lambda e: e.tensor_tensor(out=oh, in0=GL, in1=bc4(gmax), op=ALU.is_ge)),
            ("dve", lambda e: e.tensor_tensor(out=ex, in0=GL, in1=bc4(gmax), op=ALU.subtract)),
            ("act", lambda e: e.activation(out=ex, in_=ex, func=AF.Exp)),
            ("dve", lambda e: e.tensor_reduce(out=gsum, in_=ex, axis=AX.X, op=ALU.add)),
            ("dve", lambda e: e.reciprocal(out=pg, in_=gsum)),
            ("dve", lambda e: e.tensor_scalar(out=pen, in0=oh, scalar1=BIG, scalar2=-BIG, op0=ALU.mult, op1=ALU.add)),
            ("dve", lambda e: e.tensor_tensor(out=esel.rearrange("p t (g x) -> p t g x", g=4), in0=EL.rearrange("p t (g x) -> p t g x", g=4),
                                              in1=pen.unsqueeze(3).to_broadcast([128, T, 4, 4]), op=ALU.add)),
            ("dve", lambda e: e.tensor_reduce(out=m1, in_=esel, axis=AX.X, op=ALU.max)),
            ("dve", lambda e: e.tensor_tensor(out=mk1, in0=esel, in1=bc16(m1), op=ALU.is_ge)),
            ("dve", lambda e: e.scalar_tensor_tensor(out=e2, in0=mk1, scalar=-BIG, in1=esel, op0=ALU.mult, op1=ALU.add)),
            ("dve", lambda e: e.tensor_reduce(out=m2, in_=e2, axis=AX.X, op=ALU.max)),
            ("dve", lambda e: e.tensor_tensor(out=mk2, in0=e2, in1=bc16(m2), op=ALU.is_ge)),
            ("dve", lambda e: e.tensor_tensor(out=w2, in0=m2, in1=m1, op=ALU.subtract)),
            ("act", lambda e: e.activation(out=w2, in_=w2, func=AF.Exp)),
            ("dve", lambda e: e.tensor_scalar(out=w1, in0=w2, scalar1=1.0, scalar2=None, op0=ALU.add)),
            ("dve", lambda e: e.reciprocal(out=w1, in_=w1)),
            ("dve", lambda e: e.tensor_tensor(out=w2, in0=w2, in1=w1, op=ALU.mult)),
            ("dve", lambda e: e.tensor_tensor(out=w1, in0=w1, in1=pg, op=ALU.mult)),
            ("dve", lambda e: e.tensor_tensor(out=w2, in0=w2, in1=pg, op=ALU.mult)),
            ("dve", lambda e: e.tensor_tensor(out=mk1, in0=mk1, in1=bc16(w1), op=ALU.mult)),
            ("dve", lambda e: e.tensor_tensor(out=mk2, in0=mk2, in1=bc16(w2), op=ALU.mult)),
            ("dve", lambda e: e.tensor_tensor(out=esel, in0=mk1, in1=mk2, op=ALU.add)),
        ]
        for eng, f in seq:
            fw.op(eng, f, reads=[b_lg, b_wk], writes=[b_wk])
        fw.dma("sp", S.COMB[0:T * 128, :].rearrange("(t p) c -> p t c", p=128), esel, reads=[b_wk])


def phase_moe_experts(fw, cfg, l, S, A, W, o_own, o_ctx, x2_own, x2_ctx, need_ctx, final_out=None):
    nc = fw.nc
    To, Tc = cfg.T_OWN, cfg.T_CTX
    tiles = _tiles_oc(cfg, o_own, o_ctx, x2_own, x2_ctx, need_ctx)
    SB = 5 if need_ctx else 4
    with Phase(fw, "mx") as ph:
        identF = ph.sb([128, 128], F32)
        b_idF = make_ident(fw, identF)
        sel = ph.sb([16, 16, 128], BF16)
        b_sel = Buf()
        fw.op("pool", lambda e: e.memset(sel[:, :, :], 0.0), writes=[b_sel])
        fw.op("pool", lambda e: e.affine_select(out=sel[:, :, :], in_=sel[:, :, :], pattern=[[-1, 16], [0, 128]], compare_op=ALU.not_equal,
                                                fill=1.0, base=0, channel_multiplier=1), reads=[b_sel], writes=[b_sel])
        g2 = [ph.sb([128, D], F32) for _ in range(2)]
        b_g2 = bufs(2)
        for m in range(2):
            load_bc(fw, "sp", g2[m][:, :], S.mods[l][m:m + 1, 5 * D:6 * D], b_g2[m])
        b_fgb = Buf()
        if final_out is not None:
            fgb = ph.sb([128, D], F32)
            load_bc(fw, "sp", fgb[:, :], A["final_g"][0:1, :], b_fgb)
        acc = ph.sb([128, SB, D], F32)
        hT = ph.sb([128, NK, SB * 128], BF16)
        cm = ph.sb([128, SB, 16], F32)
        cmT = ph.sb([16, SB * 128], BF16)
        cbc = [ph.sb([128, SB * 128], F32) for _ in range(2)]
        Wg = [ph.sb([128, NK, 512], BF16) for _ in range(2)]
        Wu = [ph.sb([128, NK, 512], BF16) for _ in range(2)]
        Wd = [ph.sb([128, 4, D], BF16) for _ in range(2)]
        midT = [ph.sb([128, 4, SB * 128], BF16) for _ in range(2)]
        sa = ph.sb([128, 512], F32)
        ss = ph.sb([128, 1], F32)
        b_acc, b_hT, b_cm, b_cmT, b_sa, b_ss = bufs(6)
        b_cbc, b_Wg, b_Wu, b_Wd, b_midT = bufs(2), bufs(2), bufs(2), bufs(2), bufs(2)
        if SB == 5:
            xt = [ph.sb([128, D], F32)] * 2
            b_xt = [Buf()] * 2
        else:
            xt = [ph.sb([128, D], F32) for _ in range(2)]
            b_xt = bufs(2)
        junk = midT[0][:, :, :].rearrange("p f t -> p (f t)")[:, 0:D]
        b_junk = b_midT[0]
        pa = [ph.ps([128, 512]) for _ in range(2)]
        pu = [ph.ps([128, 512]) for _ in range(2)]
        po = [ph.ps([128, 512]) for _ in range(3)]
        pcb = ph.ps([128, 512])
        b_pa, b_pu, b_po = bufs(2), bufs(2), bufs(3)
        b_pcb = Buf()
        wi = 0
        pi = 0
        oi = 0
        for sb0 in range(0, len(tiles), SB):
            tl = tiles[sb0:sb0 + SB]
            nt = len(tl)
            ntok = nt * 128
            tok0 = tl[0][0] * 128
            fw.dma("sp", hT[:, :, 0:ntok], S.HT2[:, tok0:tok0 + ntok].rearrange("(k p) t -> p k t", p=128), writes=[b_hT])
            fw.dma("sp", cm[:, 0:nt, :], S.COMB[tok0:tok0 + ntok, :].rearrange("(t p) c -> p t c", p=128), writes=[b_cm])
            for i0 in range(0, nt, 4):
                ni = min(4, nt - i0)
                for i in range(ni):
                    fw.op("pe", lambda e: e.transpose(pcb[0:16, i * 128:(i + 1) * 128], cm[:, i0 + i, :], identF[:, :]),
                          reads=[b_cm, b_idF], writes=[b_pcb])
                fw.op("act", lambda e: e.copy(out=cmT[:, i0 * 128:(i0 + ni) * 128], in_=pcb[0:16, 0:ni * 128]),
                      reads=[b_pcb], writes=[b_cmT])
            for ex in range(16):
                w = wi % 2
                wi += 1
                fw.dma("pool", Wg[w][:, :, :], W["w_gate"][ex].rearrange("(k p) f -> p k f", p=128), writes=[b_Wg[w]])
                fw.dma("pool", Wu[w][:, :, :], W["w_up"][ex].rearrange("(k p) f -> p k f", p=128), writes=[b_Wu[w]])
                fw.dma("pool", Wd[w][:, :, :], W["w_down"][ex].rearrange("(k p) c -> p k c", p=128), writes=[b_Wd[w]])
                for tb in range(0, ntok, 512):
                    tw = min(512, ntok - tb)
                    fw.op("pe", lambda e: e.matmul(pcb[:, 0:tw], lhsT=sel[:, ex, :], rhs=cmT[:, tb:tb + tw], start=True, stop=True),
                          reads=[b_sel, b_cmT], writes=[b_pcb])
                    fw.op("act", lambda e: e.copy(out=cbc[w][:, tb:tb + tw], in_=pcb[:, 0:tw]), reads=[b_pcb], writes=[b_cbc[w]])
                for fc in range(4):
                    for tb in range(0, ntok, 512):
                        tw = min(512, ntok - tb)
                        p = pi % 2
                        pi += 1

                        def mg(e):
                            for k in range(NK):
                                ins = e.matmul(pa[p][:, 0:tw], lhsT=Wg[w][:, k, fc * 128:(fc + 1) * 128], rhs=hT[:, k, tb:tb + tw],
                                               start=(k == 0), stop=(k == NK - 1))
                            return ins

                        def mu(e):
                            for k in range(NK):
                                ins = e.matmul(pu[p][:, 0:tw], lhsT=Wu[w][:, k, fc * 128:(fc + 1) * 128], rhs=hT[:, k, tb:tb + tw],
                                               start=(k == 0), stop=(k == NK - 1))
                            return ins
                        fw.op("pe", mg, reads=[b_Wg[w], b_hT], writes=[b_pa[p]])
                        fw.op("pe", mu, reads=[b_Wu[w], b_hT], writes=[b_pu[p]])
                        fw.op("act", lambda e: e.activation(out=sa[:, 0:tw], in_=pa[p][:, 0:tw], func=AF.Silu), reads=[b_pa[p]], writes=[b_sa])
                        fw.op("dve", lambda e: e.tensor_tensor(out=sa[:, 0:tw], in0=sa[:, 0:tw], in1=pu[p][:, 0:tw], op=ALU.mult),
                              reads=[b_sa, b_pu[p]], writes=[b_sa])
                        fw.op("pool", lambda e: e.tensor_tensor(out=midT[w][:, fc, tb:tb + tw], in0=sa[:, 0:tw], in1=cbc[w][:, tb:tb + tw],
                                                                op=ALU.mult), reads=[b_sa, b_cbc[w]], writes=[b_midT[w]])
                for i in range(nt):
                    for dc in range(4):
                        o = oi % 3
                        oi += 1

                        def md(e):
                            for fc in range(4):
                                ins = e.matmul(po[o][:, :], lhsT=midT[w][:, fc, i * 128:(i + 1) * 128], rhs=Wd[w][:, fc, dc * 512:(dc + 1) * 512],
                                               start=(fc == 0), stop=(fc == 3))
                            return ins
                        fw.op("pe", md, reads=[b_midT[w], b_Wd[w]], writes=[b_po[o]])
                        if ex == 0:
                            fw.op("act", lambda e: e.copy(out=acc[:, i, dc * 512:(dc + 1) * 512], in_=po[o][:, :]),
                                  reads=[b_po[o]], writes=[b_acc])
                        else:
                            fw.op("dve", lambda e: e.tensor_tensor(out=acc[:, i, dc * 512:(dc + 1) * 512], in0=acc[:, i, dc * 512:(dc + 1) * 512],
                                                                   in1=po[o][:, :], op=ALU.add), reads=[b_po[o], b_acc], writes=[b_acc])
            for i, (t, prow, xsrc, m, dst) in enumerate(tl):
                s = i % 2
                fw.dma("sp", xt[s][:, :], xsrc, writes=[b_xt[s]])
                fw.op("dve", lambda e: e.tensor_tensor(out=acc[:, i, :], in0=acc[:, i, :], in1=g2[m][:, :], op=ALU.mult),
                      reads=[b_acc, b_g2[m]], writes=[b_acc])
                fw.op("pool", lambda e: e.tensor_tensor(out=xt[s][:, :], in0=xt[s][:, :], in1=acc[:, i, :], op=ALU.add),
                      reads=[b_acc, b_xt[s]], writes=[b_xt[s]])
                if final_out is not None and m == 0:
                    fw.op("pool", lambda e: e.memset(ss[:, :], 0.0), writes=[b_ss])
                    fw.op("act", lambda e: e.activation(out=junk[:, :], in_=xt[s][:, :], func=AF.Square, accum_out=ss[:, 0:1]),
                          reads=[b_xt[s], b_ss], writes=[b_junk, b_ss])
                    _rstd(fw, ss[:, :], b_ss, D)
                    fw.op("dve", lambda e: e.scalar_tensor_tensor(out=xt[s][:, :], in0=xt[s][:, :], scalar=ss[:, 0:1], in1=fgb[:, :],
                                                                  op0=ALU.mult, op1=ALU.mult), reads=[b_xt[s], b_ss, b_fgb], writes=[b_xt[s]])
                    fw.dma("sp", final_out[t * 128:(t + 1) * 128, :], xt[s][:, :], reads=[b_xt[s]])
                else:
                    fw.dma("sp", dst, xt[s][:, :], reads=[b_xt[s]])


WEIGHT_KEYS = ("w_mod", "bmod2", "w_in", "g1row", "w_br_a", "w_br_m", "w_br_c", "w_out", "w_r", "w_gate", "w_up", "w_down")


def layer_weights(inp, l, half):
    w = layer_inputs(inp, l, half)
    for k in ("w_br_a", "w_br_m", "w_br_c", "w_out", "w_gate", "w_up", "w_down"):
        w[k] = np.ascontiguousarray(inp[k][l])
    w["w_r"] = np.ascontiguousarray(np.concatenate([np.asarray(inp["w_rg"][l]), np.asarray(inp["w_re"][l])], axis=1))
    return w


def emit_layer(fw, cfg, S, A, l, last, x_own, x_oth, xc, x2o, x2c, sfx="", mid_hook=None):
    nc = fw.nc
    need_ctx = not last
    To, Tc = cfg.T_OWN, cfg.T_CTX
    x1o = nc.dram_tensor("x1o" + sfx, [To, D], F32, kind="Internal").ap()
    x1c = nc.dram_tensor("x1c" + sfx, [Tc, D], F32, kind="Internal").ap()
    W = {k: A[k + sfx] for k in WEIGHT_KEYS}
    phase_mods(fw, cfg, A["cvec"], W["w_mod"], W["bmod2"], S.mods[l])
    if mid_hook is None:
        phase_inproj(fw, cfg, l, x_own, x_oth, xc, S.mods[l], W["g1row"], W["w_in"], S)
    else:
        phase_inproj(fw, cfg, l, x_own, x_oth, xc, S.mods[l], W["g1row"], W["w_in"], S, which=("oc",))
        mid_hook()
        phase_inproj(fw, cfg, l, x_own, x_oth, xc, S.mods[l], W["g1row"], W["w_in"], S, which=("oth",))
    phase_attn(fw, cfg, l, S, A, "a", need_ctx)
    phase_attn(fw, cfg, l, S, A, "c", need_ctx)
    phase_mlstm(fw, cfg, l, S, A, need_ctx)
    phase_merge(fw, cfg, l, S, A, W, x_own, xc, x1o, x1c, need_ctx)
    phase_moe_router(fw, cfg, l, S, A, W, x1o, x1c, need_ctx)
    phase_moe_experts(fw, cfg, l, S, A, W, x1o, x1c, x2o, x2c, need_ctx, final_out=(x2o if last else None))


def phase_exchange(fw, cfg, A, x2o, xoth, part="ab", st=None):
    nc = fw.nc
    To = cfg.T_OWN
    nt = cfg.nt_own
    CH = 2
    nch = nt // CH
    if st is None:
        st = {}
    if "Z" not in st:
        st["Z"] = [nc.dram_tensor("xchgZ%d" % i, [2 * CH * 128, D], F32, kind="Internal").ap() for i in range(nch)]
        st["R"] = [nc.dram_tensor("xchgR%d" % i, [2 * CH * 128, D], F32, kind="Internal").ap() for i in range(nch)]
        st["b_R"] = bufs(nch)
    Z, R, b_R = st["Z"], st["R"], st["b_R"]
    if "a" in part:
        _exchange_a(fw, cfg, A, x2o, Z, R, b_R, CH, nch)
    if "b" in part:
        _exchange_b(fw, cfg, A, xoth, R, b_R, CH)


def _exchange_a(fw, cfg, A, x2o, Z, R, b_R, CH, nch):
    nt = cfg.nt_own
    with Phase(fw, "xa") as ph:
        sv = ph.sb([128, 2], F32)
        b_sv = Buf()
        fw.dma("sp", sv[:, :], A["selv"], writes=[b_sv])
        xt = [ph.sb([128, D], F32) for _ in range(2)]
        z0 = [ph.sb([128, D], F32) for _ in range(2)]
        z1 = [ph.sb([128, D], F32) for _ in range(2)]
        b_xt, b_z0, b_z1 = bufs(2), bufs(2), bufs(2)
        for i in range(nt):
            s = i % 2
            c, t = i // CH, i % CH
            fw.dma("sp", xt[s][:, :], x2o[i * 128:(i + 1) * 128, :], writes=[b_xt[s]])
            fw.op("dve", lambda e: e.tensor_scalar(out=z0[s][:, :], in0=xt[s][:, :], scalar1=sv[:, 0:1], scalar2=None, op0=ALU.mult),
                  reads=[b_xt[s], b_sv], writes=[b_z0[s]])
            fw.op("act", lambda e: e.activation(out=z1[s][:, :], in_=xt[s][:, :], func=AF.Copy, scale=sv[:, 1:2]),
                  reads=[b_xt[s], b_sv], writes=[b_z1[s]])
            fw.dma("sp", Z[c][t * 128:(t + 1) * 128, :], z0[s][:, :], reads=[b_z0[s]])
            fw.dma("sp", Z[c][CH * 128 + t * 128:CH * 128 + (t + 1) * 128, :], z1[s][:, :], reads=[b_z1[s]])
    for c in range(nch):
        fw.all_reduce(Z[c], R[c], [[0, 1], [2, 3], [4, 5], [6, 7]], writes=[b_R[c]])


def _exchange_b(fw, cfg, A, xoth, R, b_R, CH):
    nt = cfg.nt_own
    with Phase(fw, "xb") as ph:
        J = ph.sb([128, 2, 128], F32)
        b_J = Buf()
        fw.dma("sp", J[:, :, :], A["jsel"].rearrange("j r p -> r j p"), writes=[b_J])
        ra = [ph.sb([128, D], F32) for _ in range(2)]
        rb = [ph.sb([128, D], F32) for _ in range(2)]
        ot = [ph.sb([128, D], F32) for _ in range(2)]
        b_ra, b_rb, b_ot = bufs(2), bufs(2), bufs(2)
        pp = [ph.ps([128, 512]) for _ in range(4)]
        b_pp = bufs(4)
        pi = 0
        for i in range(nt):
            s = i % 2
            j = nt - 1 - i
            c, t = j // CH, j % CH
            fw.dma("sp", ra[s][:, :], R[c][t * 128:(t + 1) * 128, :], reads=[b_R[c]], writes=[b_ra[s]])
            fw.dma("sp", rb[s][:, :], R[c][CH * 128 + t * 128:CH * 128 + (t + 1) * 128, :], reads=[b_R[c]], writes=[b_rb[s]])
            for cb in range(4):
                p = pi % 4
                pi += 1
                cs = slice(cb * 512, (cb + 1) * 512)

                def mm(e):
                    e.matmul(pp[p][:, :], lhsT=J[:, 0, :], rhs=ra[s][:, cs], start=True, stop=False)
                    return e.matmul(pp[p][:, :], lhsT=J[:, 1, :], rhs=rb[s][:, cs], start=False, stop=True)
                fw.op("pe", mm, reads=[b_J, b_ra[s], b_rb[s]], writes=[b_pp[p]])
                if cb % 2 == 0:
                    fw.op("act", lambda e: e.copy(out=ot[s][:, cs], in_=pp[p][:, :]), reads=[b_pp[p]], writes=[b_ot[s]])
                else:
                    fw.op("dve", lambda e: e.tensor_copy(out=ot[s][:, cs], in_=pp[p][:, :]), reads=[b_pp[p]], writes=[b_ot[s]])
            fw.dma("sp", xoth[i * 128:(i + 1) * 128, :], ot[s][:, :], reads=[b_ot[s]])


def exchange_consts(half):
    selv = np.zeros((128, 2), np.float32)
    selv[:, half] = 1.0
    Jm = np.zeros((128, 128), np.float32)
    Jm[np.arange(128), 127 - np.arange(128)] = 1.0
    jsel = np.zeros((2, 128, 128), np.float32)
    jsel[1 - half] = Jm
    return {"selv": selv, "jsel": jsel}


def build_fused(cfg, shapes):
    nc = bass.Bass("TRN2", target_bir_lowering=False)
    fw = Fw(nc)
    A = {}
    for name, shp in shapes.items():
        A[name] = nc.dram_tensor(name, list(shp), F32, kind="ExternalInput").ap()
    S = Scratch(nc, cfg)
    To, Tc = cfg.T_OWN, cfg.T_CTX
    xm_o = nc.dram_tensor("xmid_o", [To, D], F32, kind="Internal").ap()
    xm_c = nc.dram_tensor("xmid_c", [Tc, D], F32, kind="Internal").ap()
    xm_oth = nc.dram_tensor("xmid_oth", [To, D], F32, kind="Internal").ap()
    out = nc.dram_tensor("out", [To, D], F32, kind="ExternalOutput").ap()
    dummy_c = nc.dram_tensor("xlast_c", [Tc, D], F32, kind="Internal").ap()
    emit_layer(fw, cfg, S, A, 0, False, A["x_own"], A["x_oth"], A["xc"], xm_o, xm_c, sfx="_0")
    xst = {}
    phase_exchange(fw, cfg, A, xm_o, xm_oth, part="a", st=xst)
    emit_layer(fw, cfg, S, A, 1, True, xm_o, xm_oth, xm_c, out, dummy_c, sfx="_1",
               mid_hook=lambda: phase_exchange(fw, cfg, A, xm_o, xm_oth, part="b", st=xst))
    fw.barrier()
    return nc


def build_layer(cfg, l, last, shapes):
    nc = bass.Bass("TRN2", target_bir_lowering=False)
    fw = Fw(nc)
    A = {}
    for name, shp in shapes.items():
        A[name] = nc.dram_tensor(name, list(shp), F32, kind="ExternalInput").ap()
    S = Scratch(nc, cfg)
    To, Tc = cfg.T_OWN, cfg.T_CTX
    x2o = nc.dram_tensor("x2o", [To, D], F32, kind="ExternalOutput").ap()
    x2c = nc.dram_tensor("x2c", [Tc, D], F32, kind="ExternalOutput").ap()
    emit_layer(fw, cfg, S, A, l, last, A["x_own"], A["x_oth"], A["xc"], x2o, x2c)
    fw.barrier()
    return nc


def run_fused(inp, cfg, cores):
    in_maps = []
    for (b, half) in cores:
        m = core_inputs(inp, cfg, b, half)
        m["rope"] = rope_table(cfg, half)
        m.update(small_inputs(inp, half))
        m.update(exchange_consts(half))
        for l in range(DEPTH):
            for k, v in layer_weights(inp, l, half).items():
                m[k + "_%d" % l] = v
        in_maps.append(m)
    shapes = {k: v.shape for k, v in in_maps[0].items()}
    nc = build_fused(cfg, shapes)
    res = run_bass_kernel_spmd(nc, in_maps, core_ids=list(range(len(cores))))
    To = cfg.T_OWN
    B = inp["x"].shape[0]
    x_new = np.zeros((B, 2 * To, D), np.float32)
    for (b, half), r in zip(cores, res.results):
        xo = np.asarray(r["out"], np.float32)
        if half == 0:
            x_new[b, :To] = xo
        else:
            x_new[b, To:] = xo[::-1]
    return x_new


def run_layer(inp, cfg, l, last, x_full, ctx_full, cores):
    in_maps = []
    cur = dict(inp)
    cur["x"] = x_full
    cur["ctx"] = ctx_full
    for (b, half) in cores:
        m = core_inputs(cur, cfg, b, half)
        m["rope"] = rope_table(cfg, half)
        m.update(small_inputs(inp, half))
        m.update(layer_weights(inp, l, half))
        in_maps.append(m)
    shapes = {k: v.shape for k, v in in_maps[0].items()}
    nc = build_layer(cfg, l, last, shapes)
    res = run_bass_kernel_spmd(nc, in_maps, core_ids=list(range(len(cores))))
    To = cfg.T_OWN
    B = x_full.shape[0]
    x_new = np.zeros((B, 2 * To, D), np.float32)
    c_new = np.zeros((B, cfg.T_CTX, D), np.float32)
    for (b, half), r in zip(cores, res.results):
        xo = np.asarray(r["x2o"], np.float32)
        if half == 0:
            x_new[b, :To] = xo
            c_new[b] = np.asarray(r["x2c"], np.float32)
        else:
            x_new[b, To:] = xo[::-1]
    return x_new, c_new


def kernel(**inputs):
    inp = {k: np.asarray(v) for k, v in inputs.items()}
    cfg = Cfg(nt_own=16, nt_ctx=2)
    cores = [(b, half) for b in range(4) for half in range(2)]
    return run_fused(inp, cfg, cores).astype(np.float32)
```

```python
from contextlib import ExitStack
import numpy as np
import concourse.bass as bass
import concourse.mybir as mybir
from concourse.bass_utils import run_bass_kernel_spmd

F32 = mybir.dt.float32
BF16 = mybir.dt.bfloat16
AF = mybir.ActivationFunctionType
ALU = mybir.AluOpType
AX = mybir.AxisListType

D = 2048
NK = D // 128
DEPTH = 2
EPS = 1e-6
NEG = -30000.0

ENGS = ("pe", "act", "dve", "pool", "sp")
RING = 12


class Buf:
    __slots__ = ("w", "r")

    def __init__(self):
        self.w = None
        self.r = {}


def bufs(n):
    return [Buf() for _ in range(n)]


class Fw:
    def __init__(self, nc):
        self.nc = nc
        self.e = dict(pe=nc.tensor, act=nc.scalar, dve=nc.vector, pool=nc.gpsimd, sp=nc.sync)
        self.semh = {}
        for k in ENGS:
            self.semh["e_" + k] = nc.alloc_semaphore("s_" + k)
        self.cnt = {k: 0 for k in ENGS}
        self.known = {k: {} for k in ENGS}
        self.rings = {}
        self.ring_n = {}
        self.ring_val = {}
        for q in ("sp", "pool", "act"):
            self.rings[q] = []
            self.ring_n[q] = 0
            for i in range(RING):
                key = "r_%s_%d" % (q, i)
                self.semh[key] = nc.alloc_semaphore(key)
                self.rings[q].append(key)
                self.ring_val[key] = 0

    def _wait(self, eng, dep):
        key, val, _ = dep
        if val <= 0 or self.known[eng].get(key, 0) >= val:
            return
        self.e[eng].wait_ge(self.semh[key], val)
        self.known[eng][key] = val

    def _sync(self, eng, issuer, reads, writes):
        for b in reads:
            if b.w is not None:
                self._wait(issuer, b.w)
        for b in writes:
            if b.w is not None and b.w[2] != eng:
                self._wait(issuer, b.w)
            for key, (val, pe) in b.r.items():
                if pe != eng:
                    self._wait(issuer, (key, val, pe))

    @staticmethod
    def _mark(dep, reads, writes):
        for b in reads:
            b.r[dep[0]] = (dep[1], dep[2])
        for b in writes:
            b.w = dep
            b.r = {}

    def op(self, eng, fn, reads=(), writes=()):
        self._sync(eng, eng, reads, writes)
        ins = fn(self.e[eng])
        self.cnt[eng] += 1
        ins.then_inc(self.semh["e_" + eng], 1)
        dep = ("e_" + eng, self.cnt[eng], eng)
        self._mark(dep, reads, writes)
        return dep

    def dma(self, q, out, in_, reads=(), writes=(), **kw):
        self._sync("dma", q, reads, writes)
        n = self.ring_n[q]
        self.ring_n[q] = n + 1
        key = self.rings[q][n % RING]
        prev = self.ring_val[key]
        self._wait(q, (key, prev, "dma"))
        self.e[q].dma_start(out=out, in_=in_, **kw).then_inc(self.semh[key], 16)
        self.ring_val[key] = prev + 16
        dep = (key, prev + 16, "dma")
        self._mark(dep, reads, writes)
        return dep

    def all_reduce(self, src, dst, groups, reads=(), writes=()):
        q = "pool"
        self._sync("dma", q, reads, writes)
        if "cc" not in self.semh:
            self.semh["cc"] = self.nc.alloc_semaphore("cc_sem")
            self.ring_val["cc"] = 0
        prev = self.ring_val["cc"]
        self._wait(q, ("cc", prev, "dma"))
        self.e[q].collective_compute("AllReduce", ALU.add, replica_groups=groups, ins=[src], outs=[dst]).then_inc(self.semh["cc"])
        self.ring_val["cc"] = prev + 1
        dep = ("cc", prev + 1, "dma")
        self._mark(dep, reads, writes)
        return dep

    def barrier(self):
        for k in ENGS:
            if k != "sp":
                self._wait("sp", ("e_" + k, self.cnt[k], k))
        for key, val in self.ring_val.items():
            self._wait("sp", (key, val, "dma"))
        self.e["sp"].sem_inc(self.semh["e_sp"], 1)
        self.cnt["sp"] += 1
        for k in ENGS:
            if k != "sp":
                self._wait(k, ("e_sp", self.cnt["sp"], "sp"))
        for k in ENGS:
            for kk in ENGS:
                self.known[k]["e_" + kk] = self.cnt[kk]
            for key, val in self.ring_val.items():
                self.known[k][key] = val


class Phase:
    def __init__(self, fw, name):
        self.fw = fw
        self.nc = fw.nc
        fw.nphase = getattr(fw, "nphase", 0) + 1
        self.name = "%s%d" % (name, fw.nphase)
        self.es = ExitStack()
        self.i = 0

    def __enter__(self):
        self.es.__enter__()
        return self

    def __exit__(self, *a):
        self.fw.barrier()
        return self.es.__exit__(*a)

    def sb(self, shape, dt):
        self.i += 1
        return self.es.enter_context(self.nc.sbuf_tensor("%s_s%d" % (self.name, self.i), list(shape), dt))

    def ps(self, shape, dt=F32):
        self.i += 1
        return self.es.enter_context(self.nc.psum_tensor("%s_p%d" % (self.name, self.i), list(shape), dt))


def make_ident(fw, t, n=128):
    b = Buf()
    fw.op("pool", lambda e: e.memset(t[:, :], 0.0), writes=[b])
    fw.op("pool", lambda e: e.affine_select(out=t[:, :], in_=t[:, :], pattern=[[-1, n]],
                                            compare_op=ALU.not_equal, fill=1.0, base=0,
                                            channel_multiplier=1), reads=[b], writes=[b])
    return b


C_AQ, C_AKV, C_MQ, C_MK, C_MV, C_MO, C_CQ, C_CKV, C_G, C_MG = 0, 1024, 1536, 2048, 2560, 3072, 3584, 4096, 4608, 10752
P_IN = 10768


class Cfg:
    def __init__(self, nt_own=16, nt_ctx=2):
        self.nt_own = nt_own
        self.nt_oth = nt_own
        self.nt_ctx = nt_ctx
        self.T_OWN = 128 * nt_own
        self.T_CTX = 128 * nt_ctx
        self.T_ALL = 2 * self.T_OWN + self.T_CTX
        self.O_OWN, self.O_OTH, self.O_CTX = 0, self.T_OWN, 2 * self.T_OWN


def phase_mods(fw, cfg, cvec, w_mod_l, bmod2_l, mods_l):
    nc = fw.nc
    with Phase(fw, "mod") as ph:
        cc = ph.sb([128, NK * 2], F32)
        S = ph.sb([128, NK * 2], BF16)
        bm = ph.sb([2, 6 * D], F32)
        out = ph.sb([2, 6 * D], F32)
        W = [ph.sb([128, NK, 512], BF16) for _ in range(2)]
        pp = [ph.ps([128, 512]) for _ in range(2)]
        b_cc, b_S, b_bm, b_out = bufs(4)
        b_W = bufs(2)
        b_pp = bufs(2)
        fw.dma("sp", cc[:, :], cvec, writes=[b_cc])
        fw.dma("sp", bm[:, :], bmod2_l, writes=[b_bm])
        fw.op("act", lambda e: e.activation(out=S[:, :], in_=cc[:, :], func=AF.Silu), reads=[b_cc], writes=[b_S])
        wv = w_mod_l.rearrange("(k p) c -> p k c", p=128)
        nblk = 6 * D // 512
        for j in range(nblk):
            s = j % 2
            fw.dma("pool", W[s][:, :, :], wv[:, :, j * 512:(j + 1) * 512], writes=[b_W[s]])

            def mm(e, s=s):
                for k in range(NK):
                    ins = e.matmul(pp[s][0:2, :], lhsT=S[:, 2 * k:2 * k + 2], rhs=W[s][:, k, :],
                                   start=(k == 0), stop=(k == NK - 1))
                return ins
            fw.op("pe", mm, reads=[b_S, b_W[s]], writes=[b_pp[s]])
            fw.op("dve", lambda e, s=s, j=j: e.tensor_tensor(out=out[:, j * 512:(j + 1) * 512], in0=pp[s][0:2, :],
                                                              in1=bm[:, j * 512:(j + 1) * 512], op=ALU.add),
                  reads=[b_pp[s], b_bm], writes=[b_out])
        fw.dma("sp", mods_l, out[:, :], reads=[b_out])


def mods_bg(fw, ph, cfg, cvec, w_mod_l, bmod2_l, mods_l):
    cc = ph.sb([128, NK * 2], F32)
    Sm = ph.sb([128, NK * 2], BF16)
    W = [ph.sb([128, NK, 512], BF16) for _ in range(2)]
    bsl = [ph.sb([2, 512], F32) for _ in range(2)]
    osl = [ph.sb([2, 512], F32) for _ in range(2)]
    pp = ph.ps([128, 512])
    b_cc, b_S, b_pp = bufs(3)
    b_W, b_bsl, b_osl = bufs(2), bufs(2), bufs(2)
    wv = w_mod_l.rearrange("(k p) c -> p k c", p=128)
    nblk = 6 * D // 512
    fw.dma("sp", cc[:, :], cvec, writes=[b_cc])
    fw.op("act", lambda e: e.activation(out=Sm[:, :], in_=cc[:, :], func=AF.Silu), reads=[b_cc], writes=[b_S])
    fw.dma("pool", W[0][:, :, :], wv[:, :, 0:512], writes=[b_W[0]])
    fw.dma("sp", bsl[0][:, :], bmod2_l[:, 0:512], writes=[b_bsl[0]])
    yield
    for j in range(nblk):
        s = j % 2
        if j + 1 < nblk:
            fw.dma("pool", W[1 - s][:, :, :], wv[:, :, (j + 1) * 512:(j + 2) * 512], writes=[b_W[1 - s]])
            fw.dma("sp", bsl[1 - s][:, :], bmod2_l[:, (j + 1) * 512:(j + 2) * 512], writes=[b_bsl[1 - s]])

        def mm(e):
            for k in range(NK):
                ins = e.matmul(pp[0:2, :], lhsT=Sm[:, 2 * k:2 * k + 2], rhs=W[s][:, k, :], start=(k == 0), stop=(k == NK - 1))
            return ins
        fw.op("pe", mm, reads=[b_S, b_W[s]], writes=[b_pp])
        fw.op("dve", lambda e: e.tensor_tensor(out=osl[s][:, :], in0=pp[0:2, :], in1=bsl[s][:, :], op=ALU.add),
              reads=[b_pp, b_bsl[s]], writes=[b_osl[s]])
        fw.dma("sp", mods_l[:, j * 512:(j + 1) * 512], osl[s][:, :], reads=[b_osl[s]])
        yield


def load_bc(fw, q, dst, src_row, wb):
    return fw.dma(q, dst, src_row.partition_broadcast(128), writes=[wb])


def phase_inproj(fw, cfg, l, x_own, x_oth, xc, mods_l, g1row, w_in_l, S, which=("oc", "oth")):
    nc = fw.nc
    T_OC = cfg.T_OWN + cfg.T_CTX
    passes = [
        ("oc", [(x_own, i, 0) for i in range(cfg.nt_own)] + [(xc, i, 1) for i in range(cfg.nt_ctx)]),
        ("oth", [(x_oth, i, 0) for i in range(cfg.nt_oth)]),
    ]
    with Phase(fw, "inp") as ph:
        ident = ph.sb([128, 128], BF16)
        b_id = make_ident(fw, ident)
        gain = [ph.sb([128, D], F32) for _ in range(2)]
        shift = [ph.sb([128, D], F32) for _ in range(2)]
        tmpg = ph.sb([128, D], F32)
        b_gain, b_shift = bufs(2), bufs(2)
        b_tmp = Buf()
        load_bc(fw, "sp", tmpg[:, :], g1row, b_tmp)
        for m in range(2):
            load_bc(fw, "sp", shift[m][:, :], mods_l[m:m + 1, 0:D], b_shift[m])
            load_bc(fw, "sp", gain[m][:, :], mods_l[m:m + 1, D:2 * D], b_gain[m])
            fw.op("dve", lambda e, m=m: e.scalar_tensor_tensor(out=gain[m][:, :], in0=gain[m][:, :], scalar=1.0,
                                                                in1=tmpg[:, :], op0=ALU.add, op1=ALU.mult),
                  reads=[b_gain[m], b_tmp], writes=[b_gain[m]])
        hT = ph.sb([128, NK, T_OC], BF16)
        xt = [ph.sb([128, D], F32)] * 2
        b_xt = [Buf()] * 2
        junk = ph.sb([128, D], BF16)
        b_junk = Buf()
        y32 = ph.sb([128, D], F32)
        b_y32 = Buf()
        hb = [ph.sb([128, D], BF16)] * 2
        b_hb = [Buf()] * 2
        ss = [ph.sb([128, 1], F32) for _ in range(2)]
        b_ss = bufs(2)
        ptr = [ph.ps([128, 512], BF16) for _ in range(2)]
        b_ptr = bufs(2)
        pmm = [ph.ps([128, 512]) for _ in range(4)]
        b_pmm = bufs(4)
        W = [ph.sb([128, NK, 512], BF16) for _ in range(2)]
        b_W = bufs(2)
        stage = [ph.sb([128, T_OC // 128, 512], BF16) for _ in range(2)]
        b_stage = bufs(2)
        stg32 = ph.sb([128, T_OC // 128, 16], F32)
        b_stg32 = Buf()
        wv = w_in_l.rearrange("(k p) c -> p k c", p=128)
        wi = 0
        ti = 0
        pi = 0
        for pname, tiles in passes:
            if pname not in which:
                continue
            nt = len(tiles)
            b_hT = bufs(nt)
            for t, (src, i, m) in enumerate(tiles):
                s = ti % 2
                ti += 1
                fw.dma("sp", xt[s][:, :], src[i * 128:(i + 1) * 128, :], writes=[b_xt[s]])
                fw.op("pool", lambda e, s=s: e.memset(ss[s][:, :], 0.0), writes=[b_ss[s]])
                fw.op("act", lambda e, s=s: e.activation(out=junk[:, :], in_=xt[s][:, :], func=AF.Square,
                                                         accum_out=ss[s][:, 0:1]),
                      reads=[b_xt[s], b_ss[s]], writes=[b_junk, b_ss[s]])
                fw.op("dve", lambda e, s=s: e.tensor_scalar(out=ss[s][:, :], in0=ss[s][:, :], scalar1=1.0 / D,
                                                            scalar2=EPS, op0=ALU.mult, op1=ALU.add),
                      reads=[b_ss[s]], writes=[b_ss[s]])
                fw.op("act", lambda e, s=s: e.sqrt(out=ss[s][:, :], in_=ss[s][:, :]), reads=[b_ss[s]], writes=[b_ss[s]])
                fw.op("dve", lambda e, s=s: e.reciprocal(out=ss[s][:, :], in_=ss[s][:, :]),
                      reads=[b_ss[s]], writes=[b_ss[s]])
                fw.op("dve", lambda e, s=s, m=m: e.scalar_tensor_tensor(out=y32[:, :], in0=xt[s][:, :],
                                                                        scalar=ss[s][:, 0:1], in1=gain[m][:, :],
                                                                        op0=ALU.mult, op1=ALU.mult),
                      reads=[b_xt[s], b_ss[s], b_gain[m]], writes=[b_y32])
                fw.op("pool", lambda e, s=s, m=m: e.tensor_tensor(out=hb[s][:, :], in0=y32[:, :], in1=shift[m][:, :],
                                                                  op=ALU.add),
                      reads=[b_y32, b_shift[m]], writes=[b_hb[s]])
                for k4 in range(NK // 4):
                    p = pi % 2
                    pi += 1

                    def tr(e, s=s, k4=k4, p=p):
                        for kk in range(4):
                            k = k4 * 4 + kk
                            ins = e.transpose(ptr[p][:, kk * 128:(kk + 1) * 128], hb[s][:, k * 128:(k + 1) * 128],
                                              ident[:, :])
                        return ins
                    fw.op("pe", tr, reads=[b_hb[s], b_id], writes=[b_ptr[p]])
                    fw.op("act", lambda e, k4=k4, p=p, t=t: e.copy(
                        out=hT[:, k4 * 4:(k4 + 1) * 4, t * 128:(t + 1) * 128],
                        in_=ptr[p][:, :].rearrange("p (k t) -> p k t", k=4)),
                        reads=[b_ptr[p]], writes=[b_hT[t]])
            if pname == "oc":
                blocks = [("tm", c0, 512) for c0 in range(0, C_MQ, 512)]
                blocks += [("fm", C_MQ, 512), ("fm", C_MK, 512)]
                blocks += [("tm", c0, 512) for c0 in range(C_MV, C_MG, 512)]
                blocks += [("tm32", C_MG, 16)]
                segs = [(0, cfg.nt_own, cfg.O_OWN), (cfg.nt_own, cfg.nt_ctx, cfg.O_CTX)]
            else:
                blocks = [("tm", C_AKV, 512), ("fm", C_MQ, 512), ("fm", C_MK, 512), ("tm", C_MV, 512),
                          ("tm", C_CKV, 512), ("tm32", C_MG, 16)]
                segs = [(0, cfg.nt_oth, cfg.O_OTH)]
            for kind, c0, cw in blocks:
                s = wi % 2
                wi += 1
                fw.dma("pool", W[s][:, :, 0:cw], wv[:, :, c0:c0 + cw], writes=[b_W[s]])
                if kind in ("tm", "tm32"):
                    st = stage[s] if kind == "tm" else stg32
                    bst = b_stage[s] if kind == "tm" else b_stg32
                    ntl_ = 1 if (pname == "oth" and c0 == C_CKV) else nt
                    for t in range(ntl_):
                        p = pi % 4
                        pi += 1

                        def mm(e, s=s, t=t, p=p, cw=cw):
                            for k in range(NK):
                                ins = e.matmul(pmm[p][:, 0:cw], lhsT=hT[:, k, t * 128:(t + 1) * 128],
                                               rhs=W[s][:, k, 0:cw], start=(k == 0), stop=(k == NK - 1))
                            return ins
                        fw.op("pe", mm, reads=[b_hT[t], b_W[s]], writes=[b_pmm[p]])
                        ev = "act" if t % 2 == 0 else "dve"
                        if ev == "act":
                            fw.op("act", lambda e, t=t, p=p, cw=cw, st=st: e.copy(out=st[:, t, 0:cw], in_=pmm[p][:, 0:cw]),
                                  reads=[b_pmm[p]], writes=[bst])
                        else:
                            fw.op("dve", lambda e, t=t, p=p, cw=cw, st=st: e.tensor_copy(out=st[:, t, 0:cw],
                                                                                         in_=pmm[p][:, 0:cw]),
                                  reads=[b_pmm[p]], writes=[bst])
                    dst = S.tm(c0, cw)
                    for (t0, ntl, o) in segs:
                        ntl = min(ntl, ntl_ - t0)
                        if ntl <= 0:
                            continue
                        fw.dma("sp", dst[o:o + ntl * 128, :].rearrange("(t p) c -> p t c", p=128),
                               st[:, t0:t0 + ntl, 0:cw], reads=[bst])
                else:
                    ntok = nt * 128 if (pname == "oc" or c0 == C_MK) else 128
                    stT = stage[s][:, :, :].rearrange("p t c -> p (t c)")
                    for cc in range(4):
                        for tb in range(0, ntok, 512):
                            tw = min(512, ntok - tb)
                            p = pi % 4
                            pi += 1

                            def mm(e, s=s, cc=cc, tb=tb, tw=tw, p=p):
                                for k in range(NK):
                                    ins = e.matmul(pmm[p][:, 0:tw], lhsT=W[s][:, k, cc * 128:(cc + 1) * 128],
                                                   rhs=hT[:, k, tb:tb + tw], start=(k == 0), stop=(k == NK - 1))
                                return ins
                            fw.op("pe", mm, reads=[b_hT[tt] for tt in range(tb // 128, (tb + tw) // 128)] + [b_W[s]],
                                  writes=[b_pmm[p]])
                            if (tb // 512) % 2 == 0:
                                fw.op("act", lambda e, tb=tb, tw=tw, p=p, cc=cc: e.copy(
                                    out=stT[:, cc * ntok + tb:cc * ntok + tb + tw], in_=pmm[p][:, 0:tw]),
                                    reads=[b_pmm[p]], writes=[b_stage[s]])
                            else:
                                fw.op("dve", lambda e, tb=tb, tw=tw, p=p, cc=cc: e.tensor_copy(
                                    out=stT[:, cc * ntok + tb:cc * ntok + tb + tw], in_=pmm[p][:, 0:tw]),
                                    reads=[b_pmm[p]], writes=[b_stage[s]])
                    dstT = S.fm(c0)
                    for (t0, ntl, o) in segs:
                        a0, a1 = t0 * 128, min((t0 + ntl) * 128, ntok)
                        if a1 <= a0:
                            continue
                        fw.dma("sp", dstT[:, o:o + a1 - a0].rearrange("(cc p) t -> p cc t", p=128),
                               stT[:, 0:4 * ntok].rearrange("p (cc t) -> p cc t", cc=4)[:, :, a0:a1],
                               reads=[b_stage[s]])


class Scratch:
    def __init__(self, nc, cfg, taps=()):
        self.nc = nc
        self.cfg = cfg
        T = cfg.T_ALL

        def dt(name, shape, dtype):
            kind = "ExternalOutput" if name in taps else "Internal"
            return nc.dram_tensor(name, list(shape), dtype, kind=kind).ap()
        self.mods = [dt("mods%d" % l, [2, 6 * D], F32) for l in range(DEPTH)]
        self.P = dt("P_tm", [T, C_MG], BF16)
        self.MG = dt("MG", [T, 16], F32)
        self.MQT = dt("MQT", [512, T], BF16)
        self.MKT = dt("MKT", [512, T], BF16)
        self.HT2 = dt("HT2", [2048, cfg.T_OWN + cfg.T_CTX], BF16)
        self.COMB = dt("COMB", [cfg.T_OWN + cfg.T_CTX, 16], F32)
        self.ZT = dt("ZT", [2048, cfg.T_OWN + cfg.T_CTX], BF16)
        self.HD = dt("HD", [2, cfg.T_OWN + cfg.T_CTX, 512], F32)
        self.OT = dt("OT", [2048, cfg.T_OWN + cfg.T_CTX], BF16)

    def tm(self, c0, cw):
        if c0 == C_MG:
            return self.MG
        return self.P[:, c0:c0 + cw]

    def fm(self, c0):
        return self.MQT if c0 == C_MQ else self.MKT


def _win_perm(half):
    mg = np.arange(3584, 3600).reshape(2, 2, 4)
    if half == 1:
        mg = mg[:, ::-1, :]
    return np.concatenate([np.arange(0, 3584), np.arange(3600, 10768), mg.reshape(-1)])


def core_inputs(inp, cfg, b, half):
    To = cfg.T_OWN
    seq = np.asarray(inp["x"][b][:2 * To])
    ctx = np.asarray(inp["ctx"][b][:cfg.T_CTX])
    if half == 1:
        seq = seq[::-1]
        ctx = ctx[::-1]
    cv = np.stack([np.asarray(inp["c"][b]).reshape(NK, 128).T, np.asarray(inp["c_ctx"]).reshape(NK, 128).T], axis=2)
    return {
        "x_own": np.ascontiguousarray(seq[:To]),
        "x_oth": np.ascontiguousarray(seq[To:]),
        "xc": np.ascontiguousarray(ctx),
        "cvec": np.ascontiguousarray(cv.reshape(128, 2 * NK)),
    }


def layer_inputs(inp, l, half):
    return {
        "w_mod": np.ascontiguousarray(inp["w_mod"][l]),
        "bmod2": np.ascontiguousarray(np.stack([inp["b_mod"][l], inp["b_mod"][l]], 0)),
        "w_in": np.ascontiguousarray(np.asarray(inp["w_in"][l])[:, _win_perm(half)]),
        "g1row": np.ascontiguousarray(np.asarray(inp["norm1_g"][l])[None, :]),
    }


def _rstd(fw, ss, b_ss, n):
    fw.op("dve", lambda e: e.tensor_scalar(out=ss, in0=ss, scalar1=1.0 / n, scalar2=EPS, op0=ALU.mult, op1=ALU.add),
          reads=[b_ss], writes=[b_ss])
    fw.op("act", lambda e: e.sqrt(out=ss, in_=ss), reads=[b_ss], writes=[b_ss])
    fw.op("dve", lambda e: e.reciprocal(out=ss, in_=ss), reads=[b_ss], writes=[b_ss])


def phase_attn(fw, cfg, l, S, A, kind, need_ctx, bg=None):
    nc = fw.nc
    To, Tc = cfg.T_OWN, cfg.T_CTX
    if kind == "a":
        Hq, Hkv, cq0, ckv0, orow = 8, 2, C_AQ, C_AKV, 0
    else:
        Hq, Hkv, cq0, ckv0, orow = 4, 2, C_CQ, C_CKV, 1536
    G = Hq // Hkv
    scale = 128.0 ** -0.5
    QB = min(512, To)
    nsub = QB // 128
    ktiles = [(cfg.O_OWN + i * 128, i * 128) for i in range(cfg.nt_own)]
    if kind == "a":
        ktiles += [(cfg.O_OTH + i * 128, To + i * 128) for i in range(cfg.nt_oth)]
    else:
        ktiles += [(cfg.O_OTH, To)]
    n_lat_k = len(ktiles)
    ktiles += [(cfg.O_CTX + i * 128, None) for i in range(cfg.nt_ctx)]
    nkt = len(ktiles)
    qtiles = [(cfg.O_OWN + i * 128, i * 128) for i in range(cfg.nt_own)]
    if need_ctx:
        qtiles += [(cfg.O_CTX + i * 128, None) for i in range(cfg.nt_ctx)]
    nqt = len(qtiles)
    with Phase(fw, "at" + kind) as ph:
        ident = ph.sb([128, 128], BF16)
        b_id = make_ident(fw, ident)
        KT = ph.sb([128, Hkv, nkt * 128], BF16)
        VA = ph.sb([128, nkt, Hkv, 129], BF16)
        QT = ph.sb([128, Hq, nqt * 128], BF16)
        b_KT, b_VA, b_QT = bufs(nkt), bufs(nkt), bufs(nqt)
        b_va1 = Buf()
        fw.op("pool", lambda e: e.memset(VA[:, :, :, 128:129], 1.0), writes=[b_va1])
        gq = ph.sb([128, 128], F32)
        gk = ph.sb([128, 128], F32)
        b_g = Buf()
        b_g2 = Buf()
        if kind == "a":
            load_bc(fw, "sp", gq[:, :], A["a_qn_g"][l:l + 1, :], b_g)
            load_bc(fw, "sp", gk[:, :], A["a_kn_g"][l:l + 1, :], b_g2)
            fw.op("dve", lambda e: e.tensor_scalar(out=gq[:, :], in0=gq[:, :], scalar1=scale, scalar2=None, op0=ALU.mult),
                  reads=[b_g, b_g2], writes=[b_g])
        esink = ph.sb([128, 4], F32)
        b_es = Buf()
        if kind == "c":
            load_bc(fw, "sp", esink[:, :], A["c_sink"][l:l + 1, :], b_es)
            fw.op("act", lambda e: e.activation(out=esink[:, :], in_=esink[:, :], func=AF.Exp), reads=[b_es], writes=[b_es])
        masks = {}
        b_mask = Buf()
        if kind == "c":
            mk_t = ph.sb([128, nsub + 2, QB], BF16)
            fw.op("pool", lambda e: e.memset(mk_t[:, :, :], 1.0), writes=[b_mask])
            for r in range(-1, nsub + 1):
                mv = mk_t[:, r + 1, :]
                fw.op("pool", lambda e, mv=mv, r=r: e.affine_select(out=mv, in_=mv, pattern=[[1, QB]], compare_op=ALU.is_ge,
                                                                    fill=0.0, base=128 - r * 128, channel_multiplier=-1),
                      reads=[b_mask], writes=[b_mask])
                fw.op("pool", lambda e, mv=mv, r=r: e.affine_select(out=mv, in_=mv, pattern=[[-1, QB]], compare_op=ALU.is_ge,
                                                                    fill=0.0, base=128 + r * 128, channel_multiplier=1),
                      reads=[b_mask], writes=[b_mask])
                masks[r] = mv
        HM = max(Hq, 2 * Hkv)
        ld = [ph.sb([128, HM * 128], BF16) for _ in range(2)]
        b_ld = bufs(2)
        rp = [ph.sb([128, 128], F32) for _ in range(2)]
        b_rp = bufs(2)
        x32 = ph.sb([128, Hq * 128], F32)
        sq = ph.sb([128, Hq * 128], F32)
        t1 = ph.sb([128, Hq * 64], F32)
        t2 = ph.sb([128, Hq * 64], F32)
        xr = [ph.sb([128, Hq * 128], BF16) for _ in range(2)]
        b_x32, b_sq, b_t1, b_t2 = bufs(4)
        b_xr = bufs(2)
        ssn = ph.sb([128, Hq], F32)
        b_ssn = Buf()
        ptr = [ph.ps([128, 512], BF16) for _ in range(2)]
        b_ptr = bufs(2)
        cnt = {"ld": 0, "tr": 0, "xr": 0}

        def prep(row, rrow, c0, H, gt, dests, vdest=None):
            s = cnt["ld"] % 2
            cnt["ld"] += 1
            w = H * 128 + (Hkv * 128 if vdest is not None else 0)
            fw.dma("sp", ld[s][:, 0:w], S.P[row:row + 128, c0:c0 + w], writes=[b_ld[s]])
            if rrow is not None:
                fw.dma("sp", rp[s][:, :], A["rope"][rrow:rrow + 128, :], writes=[b_rp[s]])
            if vdest is not None:
                fw.op("pool", lambda e: e.tensor_copy(out=vdest[0], in_=ld[s][:, H * 128:w].rearrange("p (h d) -> p h d", h=Hkv)),
                      reads=[b_ld[s], b_va1], writes=[vdest[1]])
            xs = x32[:, 0:H * 128]
            if gt is not None:
                fw.op("act", lambda e: e.copy(out=xs, in_=ld[s][:, 0:H * 128]), reads=[b_ld[s]], writes=[b_x32])
                fw.op("dve", lambda e: e.tensor_tensor(out=sq[:, 0:H * 128], in0=xs, in1=xs, op=ALU.mult),
                      reads=[b_x32], writes=[b_sq])
                fw.op("dve", lambda e: e.tensor_reduce(out=ssn[:, 0:H], in_=sq[:, 0:H * 128].rearrange("p (h d) -> p h d", h=H),
                                                       axis=AX.X, op=ALU.add), reads=[b_sq], writes=[b_ssn])
                _rstd(fw, ssn[:, 0:H], b_ssn, 128)
                x3 = xs.rearrange("p (h d) -> p h d", h=H)
                fw.op("dve", lambda e: e.tensor_tensor(out=x3, in0=x3, in1=ssn[:, 0:H].unsqueeze(2).to_broadcast([128, H, 128]),
                                                       op=ALU.mult), reads=[b_x32, b_ssn], writes=[b_x32])
                fw.op("dve", lambda e: e.tensor_tensor(out=x3, in0=x3, in1=gt[:, :].unsqueeze(1).to_broadcast([128, H, 128]),
                                                       op=ALU.mult), reads=[b_x32, b_g], writes=[b_x32])
            else:
                sc = scale if dests[0][2] == "q" else 1.0
                fw.op("act", lambda e: e.activation(out=xs, in_=ld[s][:, 0:H * 128], func=AF.Copy, scale=sc),
                      reads=[b_ld[s]], writes=[b_x32])
            xi = cnt["xr"] % 2
            cnt["xr"] += 1
            xo = xr[xi][:, 0:H * 128]
            if rrow is not None:
                x5 = xs.rearrange("p (h a b f) -> p h a b f", h=H, a=2, b=2)
                o5 = xo.rearrange("p (h a b f) -> p h a b f", h=H, a=2, b=2)
                cs = rp[s][:, 0:64].rearrange("p (a f) -> p a f", a=2).unsqueeze(1).to_broadcast([128, H, 2, 32])
                sn = rp[s][:, 64:128].rearrange("p (a f) -> p a f", a=2).unsqueeze(1).to_broadcast([128, H, 2, 32])
                x1, x2 = x5[:, :, :, 0, :], x5[:, :, :, 1, :]
                u1 = t1[:, 0:H * 64].rearrange("p (h a f) -> p h a f", h=H, a=2)
                u2 = t2[:, 0:H * 64].rearrange("p (h a f) -> p h a f", h=H, a=2)
                fw.op("dve", lambda e: e.tensor_tensor(out=u1, in0=x1, in1=cs, op=ALU.mult), reads=[b_x32, b_rp[s]], writes=[b_t1])
                fw.op("dve", lambda e: e.tensor_tensor(out=u2, in0=x2, in1=sn, op=ALU.mult), reads=[b_x32, b_rp[s]], writes=[b_t2])
                fw.op("dve", lambda e: e.tensor_tensor(out=o5[:, :, :, 0, :], in0=u1, in1=u2, op=ALU.subtract),
                      reads=[b_t1, b_t2], writes=[b_xr[xi]])
                fw.op("dve", lambda e: e.tensor_tensor(out=u1, in0=x2, in1=cs, op=ALU.mult), reads=[b_x32, b_rp[s]], writes=[b_t1])
                fw.op("dve", lambda e: e.tensor_tensor(out=u2, in0=x1, in1=sn, op=ALU.mult), reads=[b_x32, b_rp[s]], writes=[b_t2])
                fw.op("dve", lambda e: e.tensor_tensor(out=o5[:, :, :, 1, :], in0=u1, in1=u2, op=ALU.add),
                      reads=[b_t1, b_t2], writes=[b_xr[xi]])
            else:
                fw.op("dve", lambda e: e.tensor_copy(out=xo, in_=xs), reads=[b_x32], writes=[b_xr[xi]])
            for h0 in range(0, H, 4):
                hn = min(4, H - h0)
                p = cnt["tr"] % 2
                cnt["tr"] += 1

                def tr(e):
                    for hh in range(hn):
                        ins = e.transpose(ptr[p][:, hh * 128:(hh + 1) * 128], xo[:, (h0 + hh) * 128:(h0 + hh + 1) * 128], ident[:, :])
                    return ins
                fw.op("pe", tr, reads=[b_xr[xi], b_id], writes=[b_ptr[p]])
                fw.op("act", lambda e: e.copy(out=dests[0][0][:, h0:h0 + hn, dests[0][3]:dests[0][3] + 128],
                                              in_=ptr[p][:, 0:hn * 128].rearrange("p (h t) -> p h t", h=hn)),
                      reads=[b_ptr[p]], writes=[dests[0][1]])

        for ki, (row, rrow) in enumerate(ktiles):
            prep(row, rrow, ckv0, Hkv, gk if kind == "a" else None, [(KT, b_KT[ki], "k", ki * 128)],
                 vdest=(VA[:, ki, :, 0:128], b_VA[ki]))
        for qi, (row, rrow) in enumerate(qtiles):
            prep(row, rrow, cq0, Hq, gq if kind == "a" else None, [(QT, b_QT[qi], "q", qi * 128)])

        if getattr(S, "dbg", None) and kind in S.dbg:
            dq = nc.dram_tensor("dbgQT" + kind, [128, Hq, nqt * 128], BF16, kind="ExternalOutput").ap()
            dk = nc.dram_tensor("dbgKT" + kind, [128, Hkv, nkt * 128], BF16, kind="ExternalOutput").ap()
            dv = nc.dram_tensor("dbgVA" + kind, [128, nkt, Hkv, 129], BF16, kind="ExternalOutput").ap()
            fw.dma("sp", dq, QT[:, :, :], reads=b_QT)
            fw.dma("sp", dk, KT[:, :, :], reads=b_KT)
            fw.dma("sp", dv, VA[:, :, :, :], reads=b_VA + [b_va1])
        pst = [ph.ps([128, 512]) for _ in range(2)]
        b_pst = bufs(2)
        po = [ph.ps([128, 512]) for _ in range(4)]
        b_po = bufs(4)
        PT = [ph.sb([128, 512], BF16) for _ in range(3)]
        b_PT = bufs(3)
        rden = ph.sb([128, 4], F32)
        b_rden = Buf()
        on = [ph.sb([128, 4, 128], BF16) for _ in range(2)]
        b_on = bufs(2)
        ost = [ph.sb([128, 512], BF16) for _ in range(2)]
        b_ost = bufs(2)
        it = {"s": 0, "p": 0, "o": 0}
        qblocks = []
        for q0 in range(0, To, QB):
            i0 = q0 // 128
            if kind == "a":
                kl = [(ki, None) for ki in range(nkt)]
            else:
                kl = []
                for r in range(-1, nsub + 1):
                    kt = i0 + r
                    if 0 <= kt <= cfg.nt_own:
                        kl.append((kt, r))
                kl += [(n_lat_k + i, None) for i in range(cfg.nt_ctx)]
            qblocks.append((q0, nsub, kl, q0))
        if need_ctx:
            qblocks.append((To, cfg.nt_ctx, [(n_lat_k + i, None) for i in range(cfg.nt_ctx)], To))
        bgen = bg(ph) if bg is not None else None
        for h in range(Hq):
            g = h // G
            for (q0, ns, kl, oc0) in qblocks:
                qw = ns * 128
                if bgen is not None:
                    next(bgen, None)
                def emit_s(ii):
                    ki_, r_ = kl[ii]
                    sp__ = it["s"] % 2
                    it["s"] += 1
                    fw.op("pe", lambda e: e.matmul(pst[sp__][:, 0:qw], lhsT=KT[:, g, ki_ * 128:(ki_ + 1) * 128],
                                                   rhs=QT[:, h, q0:q0 + qw], start=True, stop=True),
                          reads=[b_KT[ki_]] + [b_QT[q0 // 128 + j] for j in range(ns)], writes=[b_pst[sp__]])
                    return sp__
                sq_ = [emit_s(0)]
                for idx, (ki, r) in enumerate(kl):
                    if idx + 1 < len(kl):
                        sq_.append(emit_s(idx + 1))
                    sp_ = sq_[idx]
                    pi_ = it["p"] % 3
                    it["p"] += 1
                    fw.op("act", lambda e: e.activation(out=PT[pi_][:, 0:qw], in_=pst[sp_][:, 0:qw], func=AF.Exp),
                          reads=[b_pst[sp_]], writes=[b_PT[pi_]])
                    if r is not None:
                        fw.op("pool", lambda e: e.tensor_tensor(out=PT[pi_][:, 0:qw], in0=PT[pi_][:, 0:qw], in1=masks[r][:, 0:qw],
                                                                op=ALU.mult), reads=[b_PT[pi_], b_mask], writes=[b_PT[pi_]])

                    def pv(e):
                        ins = None
                        for j in range(ns):
                            if r is not None and abs(r - j) > 1:
                                continue
                            first = (idx == 0) if r is None else (ki == max(0, q0 // 128 + j - 1))
                            ins = e.matmul(po[j][:, 0:129], lhsT=PT[pi_][:, j * 128:(j + 1) * 128], rhs=VA[:, ki, g, :],
                                           start=first, stop=(idx == len(kl) - 1))
                        return ins
                    fw.op("pe", pv, reads=[b_PT[pi_], b_VA[ki], b_va1], writes=b_po[0:ns])
                oi = it["o"] % 2
                it["o"] += 1
                for j in range(ns):
                    if kind == "c":
                        fw.op("dve", lambda e: e.tensor_scalar(out=rden[:, j:j + 1], in0=po[j][:, 128:129],
                                                               scalar1=esink[:, h:h + 1], scalar2=None, op0=ALU.add),
                              reads=[b_po[j], b_es], writes=[b_rden])
                        fw.op("dve", lambda e: e.reciprocal(out=rden[:, j:j + 1], in_=rden[:, j:j + 1]),
                              reads=[b_rden], writes=[b_rden])
                    else:
                        fw.op("dve", lambda e: e.reciprocal(out=rden[:, j:j + 1], in_=po[j][:, 128:129]),
                              reads=[b_po[j]], writes=[b_rden])
                    fw.op("dve", lambda e: e.tensor_scalar(out=on[oi][:, j, :], in0=po[j][:, 0:128], scalar1=rden[:, j:j + 1],
                                                           scalar2=None, op0=ALU.mult),
                          reads=[b_po[j], b_rden], writes=[b_on[oi]])
                p = cnt["tr"] % 2
                cnt["tr"] += 1

                def tr2(e):
                    for j in range(ns):
                        ins = e.transpose(ptr[p][:, j * 128:(j + 1) * 128], on[oi][:, j, :], ident[:, :])
                    return ins
                fw.op("pe", tr2, reads=[b_on[oi], b_id], writes=[b_ptr[p]])
                fw.op("act", lambda e: e.copy(out=ost[oi][:, 0:qw], in_=ptr[p][:, 0:qw]), reads=[b_ptr[p]], writes=[b_ost[oi]])
                fw.dma("sp", S.OT[orow + h * 128:orow + (h + 1) * 128, oc0:oc0 + qw], ost[oi][:, 0:qw], reads=[b_ost[oi]])
        if bgen is not None:
            for _ in bgen:
                pass


def rope_table(cfg, half):
    n = 2 * cfg.T_OWN
    t = np.arange(n)
    if half == 1:
        t = n - 1 - t
    rows = (t // 64).astype(np.float32)
    cols = (t % 64).astype(np.float32)
    inv = (10000.0 ** (-np.arange(0, 64, 2, dtype=np.float32) / 64.0)).astype(np.float32)
    ang = np.concatenate([rows[:, None] * inv, cols[:, None] * inv], axis=-1).astype(np.float32)
    return np.ascontiguousarray(np.concatenate([np.cos(ang), np.sin(ang)], axis=1).astype(np.float32))


def phase_mlstm(fw, cfg, l, S, A, need_ctx):
    nc = fw.nc
    To, Tc, TA = cfg.T_OWN, cfg.T_CTX, cfg.T_ALL
    NT = TA // 128
    T_OC = To + Tc
    kscale = 128.0 ** -0.5
    with Phase(fw, "ml") as ph:
        identB = ph.sb([128, 128], BF16)
        b_idB = make_ident(fw, identB)
        identF = ph.sb([128, 128], F32)
        b_idF = make_ident(fw, identF)
        ones = ph.sb([128, 128], F32)
        U = [ph.sb([128, 128], F32) for _ in range(2)]
        NEGM = [ph.sb([128, 128], F32) for _ in range(2)]
        Sel = [ph.sb([128, 128], F32) for _ in range(2)]
        Bsel = ph.sb([64, 4], F32)
        b_c = Buf()
        fw.op("pool", lambda e: e.memset(ones[:, :], 1.0), writes=[b_c])
        for d in range(2):
            fw.op("pool", lambda e: e.memset(U[d][:, :], 1.0), writes=[b_c])
            fw.op("pool", lambda e: e.memset(NEGM[d][:, :], 0.0), writes=[b_c])
            fw.op("pool", lambda e: e.memset(Sel[d][:, :], 1.0), writes=[b_c])
        fw.op("pool", lambda e: e.memset(Bsel[:, :], 0.0), writes=[b_c])
        sg = [1, -1]
        for d in range(2):
            fw.op("pool", lambda e: e.affine_select(out=U[d][:, :], in_=U[d][:, :], pattern=[[sg[d], 128]], compare_op=ALU.is_ge,
                                                    fill=0.0, base=0, channel_multiplier=-sg[d]), reads=[b_c], writes=[b_c])
            fw.op("pool", lambda e: e.affine_select(out=NEGM[d][:, :], in_=NEGM[d][:, :], pattern=[[-sg[d], 128]],
                                                    compare_op=ALU.is_ge, fill=NEG, base=0, channel_multiplier=sg[d]),
                  reads=[b_c], writes=[b_c])
            last = 127 if d == 0 else 0
            fw.op("pool", lambda e: e.affine_select(out=Sel[d][:, :], in_=Sel[d][:, :], pattern=[[0, 128]], compare_op=ALU.is_equal,
                                                    fill=0.0, base=-last, channel_multiplier=1), reads=[b_c], writes=[b_c])
        for base in (0, -32):
            fw.op("pool", lambda e: e.affine_select(out=Bsel[:, :], in_=Bsel[:, :], pattern=[[-1, 4]], compare_op=ALU.not_equal,
                                                    fill=1.0, base=base, channel_multiplier=1), reads=[b_c], writes=[b_c])
        G = ph.sb([128, NT, 16], F32)
        IC = ph.sb([128, NT, 8], F32)
        LF = ph.sb([128, NT, 8], F32)
        gb = ph.sb([128, 16], F32)
        b_G, b_gate, b_gb, b_gb2 = bufs(4)
        fw.dma("sp", G[:, :, :], S.MG.rearrange("(t p) c -> p t c", p=128), writes=[b_G])
        load_bc(fw, "sp", gb[:, 0:8], A["m_ig_b"][l:l + 1, :], b_gb)
        load_bc(fw, "sp", gb[:, 8:16], A["m_fg_b"][l:l + 1, :], b_gb2)
        fw.op("dve", lambda e: e.tensor_tensor(out=IC[:, :, :], in0=G[:, :, 0:8], in1=gb[:, 0:8].unsqueeze(1).to_broadcast([128, NT, 8]),
                                               op=ALU.add), reads=[b_G, b_gb], writes=[b_gate])
        fw.op("dve", lambda e: e.tensor_tensor(out=LF[:, :, :], in0=G[:, :, 8:16], in1=gb[:, 8:16].unsqueeze(1).to_broadcast([128, NT, 8]),
                                               op=ALU.add), reads=[b_G, b_gb2, b_gate], writes=[b_gate])
        fw.op("act", lambda e: e.activation(out=LF[:, :, :], in_=LF[:, :, :], func=AF.Exp, scale=-1.0), reads=[b_gate], writes=[b_gate])
        fw.op("act", lambda e: e.activation(out=LF[:, :, :], in_=LF[:, :, :], func=AF.Ln, bias=1.0), reads=[b_gate], writes=[b_gate])
        fw.op("dve", lambda e: e.tensor_scalar(out=LF[:, :, :], in0=LF[:, :, :], scalar1=-1.0, scalar2=None, op0=ALU.mult),
              reads=[b_gate], writes=[b_gate])
        qT = ph.sb([128, 4, T_OC], BF16)
        kT = ph.sb([128, 4, T_OC], BF16)
        Ktm = ph.sb([128, NT, 4, 128], BF16)
        VA = ph.sb([128, NT, 4, 129], BF16)
        b_qT, b_kT, b_Ktm, b_VA = bufs(4)
        fw.op("pool", lambda e: e.memset(VA[:, :, :, 128:129], 1.0), writes=[b_VA])
        for h in range(4):
            fw.dma("sp", VA[:, :, h, 0:128], S.P[:, C_MV + h * 128:C_MV + (h + 1) * 128].rearrange("(t p) d -> p t d", p=128),
                   reads=[b_VA], writes=[b_VA])
        cw = ph.sb([128, 8, 3], F32)
        b_cw = Buf()
        fw.dma("sp", cw[:, :, :].rearrange("p c k -> p (c k)"), A["m_conv"][l], writes=[b_cw])
        with Phase(fw, "mlc") as pc:
            X = [pc.sb([128, TA], BF16) for _ in range(2)]
            acc = [pc.sb([128, TA], F32) for _ in range(2)]
            ktmp = pc.sb([128, TA], BF16)
            b_X, b_acc = bufs(2), bufs(2)
            b_ktmp = Buf()
            ptr = [pc.ps([128, 512], BF16) for _ in range(2)]
            b_ptr = bufs(2)
            n = 0
            tp = 0
            for qk in range(2):
                src = S.MQT if qk == 0 else S.MKT
                for hc in range(4):
                    s = n % 2
                    n += 1
                    if qk == 0:
                        fw.dma("sp", X[s][:, 0:To + 128], src[hc * 128:(hc + 1) * 128, 0:To + 128], writes=[b_X[s]])
                        fw.dma("sp", X[s][:, 2 * To:TA], src[hc * 128:(hc + 1) * 128, 2 * To:TA], reads=[b_X[s]], writes=[b_X[s]])
                    else:
                        fw.dma("sp", X[s][:, :], src[hc * 128:(hc + 1) * 128, :], writes=[b_X[s]])
                    w = cw[:, qk * 4 + hc, :]
                    for (a, b) in (((0, To + 128) if qk == 0 else (0, 2 * To)), (2 * To, TA)):
                        fw.op("dve", lambda e: e.tensor_scalar(out=acc[s][:, a:b], in0=X[s][:, a:b], scalar1=w[:, 1:2], scalar2=None,
                                                               op0=ALU.mult), reads=[b_X[s], b_cw], writes=[b_acc[s]])
                        fw.op("dve", lambda e: e.scalar_tensor_tensor(out=acc[s][:, a + 1:b], in0=X[s][:, a:b - 1], scalar=w[:, 0:1],
                                                                      in1=acc[s][:, a + 1:b], op0=ALU.mult, op1=ALU.add),
                              reads=[b_X[s], b_cw, b_acc[s]], writes=[b_acc[s]])
                        fw.op("dve", lambda e: e.scalar_tensor_tensor(out=acc[s][:, a:b - 1], in0=X[s][:, a + 1:b], scalar=w[:, 2:3],
                                                                      in1=acc[s][:, a:b - 1], op0=ALU.mult, op1=ALU.add),
                              reads=[b_X[s], b_cw, b_acc[s]], writes=[b_acc[s]])
                    if qk == 0:
                        fw.op("act", lambda e: e.activation(out=qT[:, hc, 0:To], in_=acc[s][:, 0:To], func=AF.Silu),
                              reads=[b_acc[s]], writes=[b_qT])
                        fw.op("act", lambda e: e.activation(out=qT[:, hc, To:T_OC], in_=acc[s][:, 2 * To:TA], func=AF.Silu),
                              reads=[b_acc[s]], writes=[b_qT])
                    else:
                        fw.op("act", lambda e: e.activation(out=acc[s][:, :], in_=acc[s][:, :], func=AF.Silu),
                              reads=[b_acc[s]], writes=[b_acc[s]])
                        fw.op("dve", lambda e: e.tensor_scalar(out=ktmp[:, :], in0=acc[s][:, :], scalar1=kscale, scalar2=None,
                                                               op0=ALU.mult), reads=[b_acc[s]], writes=[b_ktmp])
                        fw.op("pool", lambda e: e.tensor_copy(out=kT[:, hc, 0:To], in_=ktmp[:, 0:To]), reads=[b_ktmp], writes=[b_kT])
                        fw.op("pool", lambda e: e.tensor_copy(out=kT[:, hc, To:T_OC], in_=ktmp[:, 2 * To:TA]), reads=[b_ktmp], writes=[b_kT])
                        for t0 in range(0, NT, 4):
                            tn = min(4, NT - t0)
                            p = tp % 2
                            tp += 1

                            def tr(e):
                                for tt in range(tn):
                                    ins = e.transpose(ptr[p][:, tt * 128:(tt + 1) * 128], ktmp[:, (t0 + tt) * 128:(t0 + tt + 1) * 128],
                                                      identB[:, :])
                                return ins
                            fw.op("pe", tr, reads=[b_ktmp, b_idB], writes=[b_ptr[p]])
                            fw.op("act", lambda e: e.copy(out=Ktm[:, t0:t0 + tn, hc, :],
                                                          in_=ptr[p][:, 0:tn * 128].rearrange("p (t d) -> p t d", t=tn)),
                                  reads=[b_ptr[p]], writes=[b_Ktm])
        pA = ph.ps([128, 512])
        pC = ph.ps([128, 512])
        pD = ph.ps([128, 512])
        pE = ph.ps([128, 512], BF16)
        pF = ph.ps([128, 512])
        pG = ph.ps([128, 512])
        b_pA, b_pC, b_pD, b_pE, b_pF, b_pG = bufs(6)
        Cst = [ph.sb([128, 4, 129], F32) for _ in range(2)]
        Cb = [ph.sb([128, 4, 129], BF16) for _ in range(2)]
        mst = [ph.sb([128, 4], F32) for _ in range(2)]
        b_C, b_Cb, b_m = bufs(2), bufs(2), bufs(2)
        for d in range(2):
            fw.op("pool", lambda e: e.memset(Cst[d][:, :, :], 0.0), writes=[b_C[d]])
            fw.op("pool", lambda e: e.memset(Cb[d][:, :, :], 0.0), writes=[b_Cb[d]])
            fw.op("pool", lambda e: e.memset(mst[d][:, :], NEG), writes=[b_m[d]])
        AB = ph.sb([128, 64], F32)
        R64 = ph.sb([64, 128], F32)
        X64 = ph.sb([64, 128], F32)
        Lall = ph.sb([64, 4, 128], F32)
        b_AB, b_R64, b_X64, b_Lall = bufs(4)
        fw.op("pool", lambda e: e.memset(AB[:, :], 0.0), writes=[b_AB])
        fw.op("pool", lambda e: e.memset(R64[:, :], 1.0), writes=[b_R64])
        fw.op("pool", lambda e: e.memset(X64[:, :], 1.0), writes=[b_X64])
        bsb = ph.sb([128, 4], F32)
        btot = ph.sb([128, 4], F32)
        bm = ph.sb([128, 4], F32)
        rowmax = ph.sb([128, 4], F32)
        mrow = ph.sb([128, 4], F32)
        E8 = ph.sb([128, 8], F32)
        small = ph.sb([128, 16], F32)
        mnew = ph.sb([128, 4], F32)
        ld = ph.sb([128, 4, 128], F32)
        Dm = ph.sb([128, 4, 128], F32)
        Wb = ph.sb([128, 4, 128], BF16)
        WT = ph.sb([128, 4, 128], BF16)
        kw = ph.sb([128, 4, 128], BF16)
        numt = ph.sb([128, 4, 129], F32)
        hout = [ph.sb([128, 4, 128], F32) for _ in range(2)]
        tmpC = ph.sb([128, 4, 129], F32)
        (b_bsb, b_btot, b_bm, b_rowmax, b_mrow, b_E8, b_small, b_mnew, b_ld, b_Dm, b_Wb, b_WT, b_kw, b_numt,
         b_tmpC) = bufs(15)
        b_hout = bufs(2)
        hcnt = [0]

        def v2(t):
            return t[:, 0:258].rearrange("p (h e) -> p h e", h=2)

        def step(d, ti, full, oc_tile):
            lf = LF[:, ti, d * 4:(d + 1) * 4]
            ic = IC[:, ti, d * 4:(d + 1) * 4]
            m = mst[d]

            def mm_b(e):
                e.matmul(pA[:, 0:4], lhsT=U[d][:, :], rhs=lf, start=True, stop=True)
                return e.matmul(pA[:, 4:8], lhsT=ones[:, :], rhs=lf, start=True, stop=True)
            fw.op("pe", mm_b, reads=[b_gate, b_c], writes=[b_pA])
            fw.op("dve", lambda e: e.tensor_copy(out=AB[:, 32:36], in_=pA[:, 0:4]), reads=[b_pA], writes=[b_AB, b_bsb])
            fw.op("dve", lambda e: e.tensor_tensor(out=AB[:, 0:4], in0=ic, in1=pA[:, 0:4], op=ALU.subtract),
                  reads=[b_pA, b_gate], writes=[b_AB])
            fw.op("act", lambda e: e.copy(out=btot[:, :], in_=pA[:, 4:8]), reads=[b_pA], writes=[b_btot])
            fw.op("dve", lambda e: e.tensor_tensor(out=bm[:, :], in0=AB[:, 32:36], in1=m[:, :], op=ALU.add),
                  reads=[b_AB, b_m[d]], writes=[b_bm])
            fw.op("pe", lambda e: e.transpose(pA[0:64, 16:144], AB[:, :], identF[:, :]), reads=[b_AB, b_idF], writes=[b_pA])
            fw.op("act", lambda e: e.copy(out=R64[0:32, :], in_=pA[0:32, 16:144]), reads=[b_pA], writes=[b_R64])
            fw.op("dve", lambda e: e.tensor_copy(out=X64[32:64, :], in_=pA[32:64, 16:144]), reads=[b_pA], writes=[b_X64])
            fw.op("dve", lambda e: e.tensor_tensor(out=Lall[:, :, :], in0=X64[:, :].unsqueeze(1).to_broadcast([64, 4, 128]),
                                                   in1=Bsel[:, :].unsqueeze(2).to_broadcast([64, 4, 128]), op=ALU.mult),
                  reads=[b_X64, b_c], writes=[b_Lall])

            def mm_ld(e):
                for h in range(4):
                    ins = e.matmul(pC[:, h * 128:(h + 1) * 128], lhsT=Lall[:, h, :], rhs=R64[:, :], start=True, stop=True)
                return ins
            fw.op("pe", mm_ld, reads=[b_Lall, b_R64], writes=[b_pC])
            fw.op("dve", lambda e: e.tensor_tensor(out=ld[:, :, :], in0=pC[:, :].rearrange("p (h s) -> p h s", h=4),
                                                   in1=NEGM[d][:, :].unsqueeze(1).to_broadcast([128, 4, 128]), op=ALU.add),
                  reads=[b_pC, b_c], writes=[b_ld])
            fw.op("dve", lambda e: e.tensor_reduce(out=rowmax[:, :], in_=ld[:, :, :], axis=AX.X, op=ALU.max),
                  reads=[b_ld], writes=[b_rowmax])
            if full:
                fw.op("dve", lambda e: e.tensor_tensor(out=mrow[:, :], in0=bm[:, :], in1=rowmax[:, :], op=ALU.max),
                      reads=[b_bm, b_rowmax], writes=[b_mrow])
                fw.op("dve", lambda e: e.tensor_tensor(out=ld[:, :, :], in0=ld[:, :, :],
                                                       in1=mrow[:, :].unsqueeze(2).to_broadcast([128, 4, 128]), op=ALU.subtract),
                      reads=[b_ld, b_mrow], writes=[b_ld])
                fw.op("dve", lambda e: e.tensor_scalar_max(out=ld[:, :, :], in0=ld[:, :, :], scalar1=-80.0), reads=[b_ld], writes=[b_ld])
                fw.op("act", lambda e: e.activation(out=Dm[:, :, :], in_=ld[:, :, :], func=AF.Exp), reads=[b_ld], writes=[b_Dm])
                oc0 = oc_tile * 128

                def mm_s(e):
                    for h in range(4):
                        ins = e.matmul(pD[:, h * 128:(h + 1) * 128], lhsT=qT[:, h, oc0:oc0 + 128], rhs=kT[:, h, oc0:oc0 + 128],
                                       start=True, stop=True)
                    return ins
                fw.op("pe", mm_s, reads=[b_qT, b_kT], writes=[b_pD])
                fw.op("dve", lambda e: e.tensor_tensor(out=Wb[:, :, :], in0=pD[:, :].rearrange("p (h s) -> p h s", h=4), in1=Dm[:, :, :],
                                                       op=ALU.mult), reads=[b_pD, b_Dm], writes=[b_Wb])

                def mm_t(e):
                    for h in range(4):
                        ins = e.transpose(pE[:, h * 128:(h + 1) * 128], Wb[:, h, :], identB[:, :])
                    return ins
                fw.op("pe", mm_t, reads=[b_Wb, b_idB], writes=[b_pE])
                fw.op("act", lambda e: e.copy(out=WT[:, :, :], in_=pE[:, :].rearrange("p (h j) -> p h j", h=4)), reads=[b_pE], writes=[b_WT])

                def mm_intra(e):
                    for h in range(4):
                        ins = e.matmul(v2(pF if h < 2 else pG)[:, h % 2, :], lhsT=WT[:, h, :], rhs=VA[:, ti, h, :], start=True, stop=True)
                    return ins
                fw.op("pe", mm_intra, reads=[b_WT, b_VA], writes=[b_pF, b_pG])

                def mm_inter(e):
                    for h in range(4):
                        ins = e.matmul(v2(pC if h < 2 else pD)[:, h % 2, :], lhsT=qT[:, h, oc0:oc0 + 128], rhs=Cb[d][:, h, :],
                                       start=True, stop=True)
                    return ins
                fw.op("pe", mm_inter, reads=[b_qT, b_Cb[d]], writes=[b_pC, b_pD])
                fw.op("dve", lambda e: e.tensor_tensor(out=E8[:, 0:4], in0=bm[:, :], in1=mrow[:, :], op=ALU.subtract),
                      reads=[b_bm, b_mrow], writes=[b_E8])
                fw.op("dve", lambda e: e.tensor_scalar(out=E8[:, 4:8], in0=mrow[:, :], scalar1=-1.0, scalar2=None, op0=ALU.mult),
                      reads=[b_mrow, b_E8], writes=[b_E8])
                fw.op("dve", lambda e: e.tensor_scalar_max(out=E8[:, :], in0=E8[:, :], scalar1=-80.0), reads=[b_E8], writes=[b_E8])
                fw.op("act", lambda e: e.activation(out=E8[:, :], in_=E8[:, :], func=AF.Exp), reads=[b_E8], writes=[b_E8])
                for hp, (pi_, pj_, bi_, bj_) in enumerate(((pC, pF, b_pC, b_pF), (pD, pG, b_pD, b_pG))):
                    hs = slice(hp * 2, hp * 2 + 2)
                    fw.op("dve", lambda e: e.tensor_tensor(out=numt[:, hs, :], in0=v2(pi_),
                                                           in1=E8[:, hs].unsqueeze(2).to_broadcast([128, 2, 129]), op=ALU.mult),
                          reads=[bi_, b_E8], writes=[b_numt])
                    fw.op("dve", lambda e: e.tensor_tensor(out=numt[:, hs, :], in0=numt[:, hs, :], in1=v2(pj_), op=ALU.add),
                          reads=[bj_, b_numt], writes=[b_numt])
                fw.op("dve", lambda e: e.tensor_scalar(out=small[:, 4:8], in0=numt[:, :, 128], scalar1=-1.0, scalar2=None, op0=ALU.mult),
                      reads=[b_numt], writes=[b_small])
                fw.op("dve", lambda e: e.tensor_tensor(out=small[:, 0:4], in0=numt[:, :, 128], in1=small[:, 4:8], op=ALU.max),
                      reads=[b_numt, b_small], writes=[b_small])
                fw.op("dve", lambda e: e.tensor_tensor(out=small[:, 0:4], in0=small[:, 0:4], in1=E8[:, 4:8], op=ALU.max),
                      reads=[b_small, b_E8], writes=[b_small])
                fw.op("dve", lambda e: e.reciprocal(out=small[:, 0:4], in_=small[:, 0:4]), reads=[b_small], writes=[b_small])
                hi = hcnt[0] % 2
                hcnt[0] += 1
                fw.op("dve", lambda e: e.tensor_tensor(out=hout[hi][:, :, :], in0=numt[:, :, 0:128],
                                                       in1=small[:, 0:4].unsqueeze(2).to_broadcast([128, 4, 128]), op=ALU.mult),
                      reads=[b_numt, b_small], writes=[b_hout[hi]])
                fw.dma("sp", S.HD[d, oc0:oc0 + 128, :], hout[hi][:, :, :].rearrange("p h e -> p (h e)"), reads=[b_hout[hi]])
            fw.op("pe", lambda e: e.matmul(pA[:, 8:12], lhsT=Sel[d][:, :], rhs=rowmax[:, :], start=True, stop=True),
                  reads=[b_rowmax, b_c], writes=[b_pA])
            fw.op("dve", lambda e: e.tensor_tensor(out=small[:, 8:12], in0=btot[:, :], in1=m[:, :], op=ALU.add),
                  reads=[b_btot, b_m[d]], writes=[b_small])
            fw.op("dve", lambda e: e.tensor_tensor(out=mnew[:, :], in0=small[:, 8:12], in1=pA[:, 8:12], op=ALU.max),
                  reads=[b_small, b_pA], writes=[b_mnew])
            fw.op("dve", lambda e: e.tensor_tensor(out=E8[:, 4:8], in0=small[:, 8:12], in1=mnew[:, :], op=ALU.subtract),
                  reads=[b_small, b_mnew, b_E8], writes=[b_E8])
            fw.op("dve", lambda e: e.tensor_tensor(out=E8[:, 0:4], in0=AB[:, 0:4], in1=btot[:, :], op=ALU.add),
                  reads=[b_AB, b_btot, b_E8], writes=[b_E8])
            fw.op("dve", lambda e: e.tensor_tensor(out=E8[:, 0:4], in0=E8[:, 0:4], in1=mnew[:, :], op=ALU.subtract),
                  reads=[b_E8, b_mnew], writes=[b_E8])
            fw.op("dve", lambda e: e.tensor_scalar_max(out=E8[:, :], in0=E8[:, :], scalar1=-80.0), reads=[b_E8], writes=[b_E8])
            fw.op("act", lambda e: e.activation(out=E8[:, :], in_=E8[:, :], func=AF.Exp), reads=[b_E8], writes=[b_E8])
            fw.op("dve", lambda e: e.tensor_tensor(out=kw[:, :, :], in0=Ktm[:, ti, :, :],
                                                   in1=E8[:, 0:4].unsqueeze(2).to_broadcast([128, 4, 128]), op=ALU.mult),
                  reads=[b_Ktm, b_E8], writes=[b_kw])

            def mm_dc(e):
                for h in range(4):
                    ins = e.matmul(v2(pF if h < 2 else pG)[:, h % 2, :], lhsT=kw[:, h, :], rhs=VA[:, ti, h, :], start=True, stop=True)
                return ins
            fw.op("pe", mm_dc, reads=[b_kw, b_VA], writes=[b_pF, b_pG])
            fw.op("dve", lambda e: e.tensor_tensor(out=tmpC[:, :, :], in0=Cst[d][:, :, :],
                                                   in1=E8[:, 4:8].unsqueeze(2).to_broadcast([128, 4, 129]), op=ALU.mult),
                  reads=[b_C[d], b_E8], writes=[b_tmpC])
            fw.op("dve", lambda e: e.tensor_tensor(out=Cst[d][:, 0:2, :], in0=tmpC[:, 0:2, :], in1=v2(pF), op=ALU.add),
                  reads=[b_tmpC, b_pF], writes=[b_C[d]])
            fw.op("dve", lambda e: e.tensor_tensor(out=Cst[d][:, 2:4, :], in0=tmpC[:, 2:4, :], in1=v2(pG), op=ALU.add),
                  reads=[b_tmpC, b_pG, b_C[d]], writes=[b_C[d]])
            fw.op("act", lambda e: e.copy(out=Cb[d][:, :, :], in_=Cst[d][:, :, :]), reads=[b_C[d]], writes=[b_Cb[d]])
            fw.op("dve", lambda e: e.tensor_copy(out=m[:, :], in_=mnew[:, :]), reads=[b_mnew], writes=[b_m[d]])

        n_own, n_ctx = cfg.nt_own, cfg.nt_ctx
        t_own0, t_oth0, t_ctx0 = 0, n_own, 2 * n_own
        near = [(t_ctx0 + i, need_ctx, n_own + i) for i in range(n_ctx)] + [(t_own0 + i, True, i) for i in range(n_own)]
        far = [(t_ctx0 + i, need_ctx, n_own + i) for i in reversed(range(n_ctx))]
        far += [(t_oth0 + i, False, None) for i in reversed(range(n_own))]
        far += [(t_own0 + i, True, i) for i in reversed(range(n_own))]
        for i in range(max(len(near), len(far))):
            if i < len(far):
                step(1, *far[i])
            if i < len(near):
                step(0, *near[i])


def small_inputs(inp, half):
    ig = np.asarray(inp["m_ig_b"]); fg = np.asarray(inp["m_fg_b"]); cv = np.asarray(inp["m_conv"])
    if half == 1:
        ig, fg, cv = ig[:, ::-1, :], fg[:, ::-1, :], cv[:, ::-1, :]
    out = {
        "m_ig_b": np.ascontiguousarray(ig.reshape(DEPTH, 8)), "m_fg_b": np.ascontiguousarray(fg.reshape(DEPTH, 8)),
        "m_conv": np.ascontiguousarray(cv.reshape(DEPTH, 3, 8, 128).transpose(0, 3, 2, 1).reshape(DEPTH, 128, 24)),
    }
    for k in ("a_qn_g", "a_kn_g", "c_sink", "m_norm_g", "norm1_g", "norm2_g", "b_rg", "b_re"):
        out[k] = np.ascontiguousarray(inp[k])
    out["final_g"] = np.ascontiguousarray(np.asarray(inp["final_g"])[None, :])
    return out


def _tiles_oc(cfg, x_own, xc, o_own, o_ctx, need_ctx):
    tl = [(t, t * 128, x_own[t * 128:(t + 1) * 128, :], 0, o_own[t * 128:(t + 1) * 128, :]) for t in range(cfg.nt_own)]
    if need_ctx:
        tl += [(cfg.nt_own + i, cfg.O_CTX + i * 128, xc[i * 128:(i + 1) * 128, :], 1, o_ctx[i * 128:(i + 1) * 128, :])
               for i in range(cfg.nt_ctx)]
    return tl


def phase_merge(fw, cfg, l, S, A, W, x_own, xc, o_own, o_ctx, need_ctx):
    nc = fw.nc
    To, Tc = cfg.T_OWN, cfg.T_CTX
    T_OC = To + Tc
    tiles = _tiles_oc(cfg, x_own, xc, o_own, o_ctx, need_ctx)
    with Phase(fw, "mg1") as ph:
        identB = ph.sb([128, 128], BF16)
        b_id = make_ident(fw, identB)
        gmn = ph.sb([128, 512], F32)
        b_gmn = Buf()
        load_bc(fw, "sp", gmn[:, :], A["m_norm_g"][l:l + 1, :], b_gmn)
        AT = ph.sb([128, 8, T_OC], BF16)
        CT = ph.sb([128, 4, T_OC], BF16)
        b_AT, b_CT = bufs(2)
        fw.dma("sp", AT[:, :, :], S.OT[0:1024, :].rearrange("(k p) t -> p k t", p=128), writes=[b_AT])
        fw.dma("sp", CT[:, :, :], S.OT[1536:2048, :].rearrange("(k p) t -> p k t", p=128), writes=[b_CT])
        Wa = ph.sb([128, 8, D], BF16)
        Wm = ph.sb([128, 4, D], BF16)
        Wc = ph.sb([128, 4, D], BF16)
        b_Wa, b_Wm, b_Wc = bufs(3)
        fw.dma("pool", Wa[:, :, :], W["w_br_a"].rearrange("(k p) c -> p k c", p=128), writes=[b_Wa])
        fw.dma("pool", Wm[:, :, :], W["w_br_m"].rearrange("(k p) c -> p k c", p=128), writes=[b_Wm])
        fw.dma("pool", Wc[:, :, :], W["w_br_c"].rearrange("(k p) c -> p k c", p=128), writes=[b_Wc])
        h0 = [ph.sb([128, 512], F32) for _ in range(2)]
        h1 = [ph.sb([128, 512], F32) for _ in range(2)]
        mo = [ph.sb([128, 512], BF16) for _ in range(2)]
        gg = [ph.sb([128, 3 * D], BF16) for _ in range(2)]
        b_h0, b_h1, b_mo, b_gg = bufs(2), bufs(2), bufs(2), bufs(2)
        sq = ph.sb([128, 512], F32)
        ss = ph.sb([128, 4], F32)
        sig = ph.sb([128, 512], F32)
        omb = ph.sb([128, 512], BF16)
        omT = [ph.sb([128, 4, 128], BF16) for _ in range(2)]
        z32 = ph.sb([128, 512], F32)
        t32 = ph.sb([128, 512], F32)
        zb = [ph.sb([128, D], BF16) for _ in range(2)]
        zTs = [ph.sb([128, NK, 128], BF16) for _ in range(2)]
        b_sq, b_ss, b_sig, b_omb, b_z32, b_t32 = bufs(6)
        b_omT, b_zb, b_zTs = bufs(2), bufs(2), bufs(2)
        ptr = [ph.ps([128, 512], BF16) for _ in range(2)]
        b_ptr = bufs(2)
        pa = [ph.ps([128, 512]) for _ in range(2)]
        pm = [ph.ps([128, 512]) for _ in range(2)]
        pc = [ph.ps([128, 512]) for _ in range(2)]
        b_pa, b_pm, b_pc = bufs(2), bufs(2), bufs(2)
        tp = 0
        pq = 0
        for n, (t, prow, xsrc, m, dst) in enumerate(tiles):
            s = n % 2
            tok = slice(t * 128, (t + 1) * 128)
            fw.dma("sp", h0[s][:, :], S.HD[0, t * 128:(t + 1) * 128, :], writes=[b_h0[s]])
            fw.dma("sp", h1[s][:, :], S.HD[1, t * 128:(t + 1) * 128, :], writes=[b_h1[s]])
            fw.dma("sp", mo[s][:, :], S.P[prow:prow + 128, C_MO:C_MO + 512], writes=[b_mo[s]])
            fw.dma("sp", gg[s][:, :], S.P[prow:prow + 128, C_G:C_G + 3 * D], writes=[b_gg[s]])
            fw.op("dve", lambda e: e.tensor_tensor(out=h0[s][:, :], in0=h0[s][:, :], in1=h1[s][:, :], op=ALU.add),
                  reads=[b_h0[s], b_h1[s]], writes=[b_h0[s]])
            fw.op("dve", lambda e: e.tensor_tensor(out=sq[:, :], in0=h0[s][:, :], in1=h0[s][:, :], op=ALU.mult),
                  reads=[b_h0[s]], writes=[b_sq])
            fw.op("dve", lambda e: e.tensor_reduce(out=ss[:, :], in_=sq[:, :].rearrange("p (h d) -> p h d", h=4), axis=AX.X, op=ALU.add),
                  reads=[b_sq], writes=[b_ss])
            _rstd(fw, ss[:, :], b_ss, 128)
            h3 = h0[s][:, :].rearrange("p (h d) -> p h d", h=4)
            fw.op("dve", lambda e: e.tensor_tensor(out=h3, in0=h3, in1=ss[:, :].unsqueeze(2).to_broadcast([128, 4, 128]), op=ALU.mult),
                  reads=[b_h0[s], b_ss], writes=[b_h0[s]])
            fw.op("dve", lambda e: e.tensor_tensor(out=h0[s][:, :], in0=h0[s][:, :], in1=gmn[:, :], op=ALU.mult),
                  reads=[b_h0[s], b_gmn], writes=[b_h0[s]])
            fw.op("act", lambda e: e.activation(out=sig[:, :], in_=mo[s][:, :], func=AF.Sigmoid), reads=[b_mo[s]], writes=[b_sig])
            fw.op("dve", lambda e: e.tensor_tensor(out=omb[:, :], in0=h0[s][:, :], in1=sig[:, :], op=ALU.mult),
                  reads=[b_h0[s], b_sig], writes=[b_omb])
            p = tp % 2
            tp += 1

            def tr(e):
                for k in range(4):
                    ins = e.transpose(ptr[p][:, k * 128:(k + 1) * 128], omb[:, k * 128:(k + 1) * 128], identB[:, :])
                return ins
            fw.op("pe", tr, reads=[b_omb, b_id], writes=[b_ptr[p]])
            fw.op("act", lambda e: e.copy(out=omT[s][:, :, :], in_=ptr[p][:, :].rearrange("p (k t) -> p k t", k=4)),
                  reads=[b_ptr[p]], writes=[b_omT[s]])
            fw.op("act", lambda e: e.activation(out=gg[s][:, :], in_=gg[s][:, :], func=AF.Sigmoid), reads=[b_gg[s]], writes=[b_gg[s]])
            for cb in range(4):
                q = pq % 2
                pq += 1
                cs = slice(cb * 512, (cb + 1) * 512)

                def mma(e):
                    for k in range(8):
                        ins = e.matmul(pa[q][:, :], lhsT=AT[:, k, tok], rhs=Wa[:, k, cs], start=(k == 0), stop=(k == 7))
                    return ins

                def mmm(e):
                    for k in range(4):
                        ins = e.matmul(pm[q][:, :], lhsT=omT[s][:, k, :], rhs=Wm[:, k, cs], start=(k == 0), stop=(k == 3))
                    return ins

                def mmc(e):
                    for k in range(4):
                        ins = e.matmul(pc[q][:, :], lhsT=CT[:, k, tok], rhs=Wc[:, k, cs], start=(k == 0), stop=(k == 3))
                    return ins
                fw.op("pe", mma, reads=[b_AT, b_Wa], writes=[b_pa[q]])
                fw.op("pe", mmm, reads=[b_omT[s], b_Wm], writes=[b_pm[q]])
                fw.op("pe", mmc, reads=[b_CT, b_Wc], writes=[b_pc[q]])
                fw.op("dve", lambda e: e.tensor_tensor(out=z32[:, :], in0=pa[q][:, :], in1=gg[s][:, cb * 512:(cb + 1) * 512], op=ALU.mult),
                      reads=[b_pa[q], b_gg[s]], writes=[b_z32])
                fw.op("dve", lambda e: e.tensor_tensor(out=t32[:, :], in0=pm[q][:, :], in1=gg[s][:, D + cb * 512:D + (cb + 1) * 512],
                                                       op=ALU.mult), reads=[b_pm[q], b_gg[s]], writes=[b_t32])
                fw.op("pool", lambda e: e.tensor_tensor(out=z32[:, :], in0=z32[:, :], in1=t32[:, :], op=ALU.add),
                      reads=[b_z32, b_t32], writes=[b_z32])
                fw.op("dve", lambda e: e.tensor_tensor(out=t32[:, :], in0=pc[q][:, :], in1=gg[s][:, 2 * D + cb * 512:2 * D + (cb + 1) * 512],
                                                       op=ALU.mult), reads=[b_pc[q], b_gg[s]], writes=[b_t32])
                fw.op("pool", lambda e: e.tensor_tensor(out=zb[s][:, cs], in0=z32[:, :], in1=t32[:, :], op=ALU.add),
                      reads=[b_z32, b_t32], writes=[b_zb[s]])
            for k4 in range(NK // 4):
                p = tp % 2
                tp += 1

                def tr2(e):
                    for kk in range(4):
                        k = k4 * 4 + kk
                        ins = e.transpose(ptr[p][:, kk * 128:(kk + 1) * 128], zb[s][:, k * 128:(k + 1) * 128], identB[:, :])
                    return ins
                fw.op("pe", tr2, reads=[b_zb[s], b_id], writes=[b_ptr[p]])
                fw.op("act", lambda e: e.copy(out=zTs[s][:, k4 * 4:(k4 + 1) * 4, :], in_=ptr[p][:, :].rearrange("p (k t) -> p k t", k=4)),
                      reads=[b_ptr[p]], writes=[b_zTs[s]])
            fw.dma("sp", S.ZT[:, tok].rearrange("(k p) t -> p k t", p=128), zTs[s][:, :, :], reads=[b_zTs[s]])
    with Phase(fw, "mg2") as ph:
        ZTs = ph.sb([128, NK, T_OC], BF16)
        Wo = ph.sb([128, NK, D], BF16)
        b_Z, b_Wo = bufs(2)
        fw.dma("sp", ZTs[:, :, :], S.ZT.rearrange("(k p) t -> p k t", p=128), writes=[b_Z])
        fw.dma("pool", Wo[:, :, :], W["w_out"].rearrange("(k p) c -> p k c", p=128), writes=[b_Wo])
        g1 = [ph.sb([128, D], F32) for _ in range(2)]
        b_g1 = bufs(2)
        for m in range(2):
            load_bc(fw, "sp", g1[m][:, :], S.mods[l][m:m + 1, 2 * D:3 * D], b_g1[m])
        xt = [ph.sb([128, D], F32) for _ in range(2)]
        xn = [ph.sb([128, D], F32) for _ in range(2)]
        b_xt, b_xn = bufs(2), bufs(2)
        py = [ph.ps([128, 512]) for _ in range(4)]
        b_py = bufs(4)
        pq = 0
        for n, (t, prow, xsrc, m, dst) in enumerate(tiles):
            s = n % 2
            tok = slice(t * 128, (t + 1) * 128)
            fw.dma("sp", xt[s][:, :], xsrc, writes=[b_xt[s]])
            for cb in range(4):
                q = pq % 4
                pq += 1
                cs = slice(cb * 512, (cb + 1) * 512)

                def mmo(e):
                    for k in range(NK):
                        ins = e.matmul(py[q][:, :], lhsT=ZTs[:, k, tok], rhs=Wo[:, k, cs], start=(k == 0), stop=(k == NK - 1))
                    return ins
                fw.op("pe", mmo, reads=[b_Z, b_Wo], writes=[b_py[q]])
                fw.op("dve", lambda e: e.tensor_tensor(out=xn[s][:, cs], in0=py[q][:, :], in1=g1[m][:, cs], op=ALU.mult),
                      reads=[b_py[q], b_g1[m]], writes=[b_xn[s]])
                fw.op("pool", lambda e: e.tensor_tensor(out=xn[s][:, cs], in0=xn[s][:, cs], in1=xt[s][:, cs], op=ALU.add),
                      reads=[b_xn[s], b_xt[s]], writes=[b_xn[s]])
            fw.dma("sp", dst, xn[s][:, :], reads=[b_xn[s]])


def phase_moe_router(fw, cfg, l, S, A, W, o_own, o_ctx, need_ctx):
    nc = fw.nc
    tiles = _tiles_oc(cfg, o_own, o_ctx, o_own, o_ctx, need_ctx)
    BIG = 1.0e4
    with Phase(fw, "mr") as ph:
        identB = ph.sb([128, 128], BF16)
        b_id = make_ident(fw, identB)
        gain = [ph.sb([128, D], F32) for _ in range(2)]
        shift = [ph.sb([128, D], F32) for _ in range(2)]
        tmpg = ph.sb([128, D], F32)
        b_gain, b_shift = bufs(2), bufs(2)
        b_tmp = Buf()
        load_bc(fw, "sp", tmpg[:, :], A["norm2_g"][l:l + 1, :], b_tmp)
        for m in range(2):
            load_bc(fw, "sp", shift[m][:, :], S.mods[l][m:m + 1, 3 * D:4 * D], b_shift[m])
            load_bc(fw, "sp", gain[m][:, :], S.mods[l][m:m + 1, 4 * D:5 * D], b_gain[m])
            fw.op("dve", lambda e: e.scalar_tensor_tensor(out=gain[m][:, :], in0=gain[m][:, :], scalar=1.0, in1=tmpg[:, :],
                                                          op0=ALU.add, op1=ALU.mult), reads=[b_gain[m], b_tmp], writes=[b_gain[m]])
        wr = ph.sb([128, NK, 20], F32)
        wrh = ph.sb([128, NK, 20], BF16)
        wrl = ph.sb([128, NK, 20], BF16)
        brb = ph.sb([128, 20], F32)
        b_wr, b_wrh, b_wrl, b_brb, b_brb2 = bufs(5)
        fw.dma("sp", wr[:, :, :], W["w_r"].rearrange("(k p) c -> p k c", p=128), writes=[b_wr])
        load_bc(fw, "sp", brb[:, 0:4], A["b_rg"][l:l + 1, :], b_brb)
        load_bc(fw, "sp", brb[:, 4:20], A["b_re"][l:l + 1, :], b_brb2)
        fw.op("act", lambda e: e.copy(out=wrh[:, :, :], in_=wr[:, :, :]), reads=[b_wr], writes=[b_wrh])
        fw.op("dve", lambda e: e.tensor_tensor(out=wrl[:, :, :], in0=wr[:, :, :], in1=wrh[:, :, :], op=ALU.subtract),
              reads=[b_wr, b_wrh], writes=[b_wrl])
        xt = [ph.sb([128, D], F32) for _ in range(2)]
        b_xt = bufs(2)
        junk = ph.sb([128, D], BF16)
        h32 = ph.sb([128, D], F32)
        hb = [ph.sb([128, D], BF16) for _ in range(2)]
        lb = [ph.sb([128, D], BF16) for _ in range(2)]
        hTt = [ph.sb([128, NK, 128], BF16) for _ in range(2)]
        lTt = [ph.sb([128, NK, 128], BF16) for _ in range(2)]
        ss = [ph.sb([128, 1], F32) for _ in range(2)]
        b_junk, b_h32 = bufs(2)
        b_hb, b_lb, b_hTt, b_lTt, b_ss = bufs(2), bufs(2), bufs(2), bufs(2), bufs(2)
        ptr = [ph.ps([128, 512], BF16) for _ in range(2)]
        b_ptr = bufs(2)
        plg = [ph.ps([128, 512]) for _ in range(2)]
        b_plg = bufs(2)
        NTM = cfg.nt_own + cfg.nt_ctx
        lgall = ph.sb([128, NTM, 20], F32)
        rs = ph.sb([128, 7, NTM], F32)
        r4 = ph.sb([128, 3, NTM, 4], F32)
        r16 = ph.sb([128, 4, NTM, 16], F32)
        b_lg, b_wk = bufs(2)
        tp = 0
        for n, (t, prow, xsrc, m, dst) in enumerate(tiles):
            s = n % 2
            tok = slice(t * 128, (t + 1) * 128)
            fw.dma("sp", xt[s][:, :], xsrc, writes=[b_xt[s]])
            fw.op("pool", lambda e: e.memset(ss[s][:, :], 0.0), writes=[b_ss[s]])
            fw.op("act", lambda e: e.activation(out=junk[:, :], in_=xt[s][:, :], func=AF.Square, accum_out=ss[s][:, 0:1]),
                  reads=[b_xt[s], b_ss[s]], writes=[b_junk, b_ss[s]])
            _rstd(fw, ss[s][:, :], b_ss[s], D)
            fw.op("dve", lambda e: e.scalar_tensor_tensor(out=h32[:, :], in0=xt[s][:, :], scalar=ss[s][:, 0:1], in1=gain[m][:, :],
                                                          op0=ALU.mult, op1=ALU.mult), reads=[b_xt[s], b_ss[s], b_gain[m]], writes=[b_h32])
            fw.op("pool", lambda e: e.tensor_tensor(out=h32[:, :], in0=h32[:, :], in1=shift[m][:, :], op=ALU.add),
                  reads=[b_h32, b_shift[m]], writes=[b_h32])
            fw.op("act", lambda e: e.copy(out=hb[s][:, :], in_=h32[:, :]), reads=[b_h32], writes=[b_hb[s]])
            fw.op("dve", lambda e: e.tensor_tensor(out=lb[s][:, :], in0=h32[:, :], in1=hb[s][:, :], op=ALU.subtract),
                  reads=[b_h32, b_hb[s]], writes=[b_lb[s]])
            for (srcb, b_src, dstT, b_dst) in ((hb[s], b_hb[s], hTt[s], b_hTt[s]), (lb[s], b_lb[s], lTt[s], b_lTt[s])):
                for k4 in range(NK // 4):
                    p = tp % 2
                    tp += 1

                    def tr(e):
                        for kk in range(4):
                            k = k4 * 4 + kk
                            ins = e.transpose(ptr[p][:, kk * 128:(kk + 1) * 128], srcb[:, k * 128:(k + 1) * 128], identB[:, :])
                        return ins
                    fw.op("pe", tr, reads=[b_src, b_id], writes=[b_ptr[p]])
                    fw.op("act", lambda e: e.copy(out=dstT[:, k4 * 4:(k4 + 1) * 4, :], in_=ptr[p][:, :].rearrange("p (k t) -> p k t", k=4)),
                          reads=[b_ptr[p]], writes=[b_dst])
            fw.dma("sp", S.HT2[:, tok].rearrange("(k p) t -> p k t", p=128), hTt[s][:, :, :], reads=[b_hTt[s]])

            def mml(e):
                i = 0
                for (L, R) in ((hTt[s], wrh), (lTt[s], wrh), (hTt[s], wrl)):
                    for k in range(NK):
                        ins = e.matmul(plg[s][:, 0:20], lhsT=L[:, k, :], rhs=R[:, k, :], start=(i == 0), stop=(i == 3 * NK - 1))
                        i += 1
                return ins
            fw.op("pe", mml, reads=[b_hTt[s], b_lTt[s], b_wrh, b_wrl], writes=[b_plg[s]])
            fw.op("dve", lambda e: e.tensor_tensor(out=lgall[:, n, :], in0=plg[s][:, 0:20], in1=brb[:, :], op=ALU.add),
                  reads=[b_plg[s], b_brb, b_brb2], writes=[b_lg])
        T = len(tiles)
        GL, EL = lgall[:, 0:T, 0:4], lgall[:, 0:T, 4:20]
        gmax, gsum, pg, m1, m2, w1, w2 = (rs[:, i, 0:T] for i in range(7))
        oh, ex, pen = r4[:, 0, 0:T, :], r4[:, 1, 0:T, :], r4[:, 2, 0:T, :]
        esel, mk1, mk2, e2 = r16[:, 0, 0:T, :], r16[:, 1, 0:T, :], r16[:, 2, 0:T, :], r16[:, 3, 0:T, :]

        def bc4(v):
            return v.unsqueeze(2).to_broadcast([128, T, 4])

        def bc16(v):
            return v.unsqueeze(2).to_broadcast([128, T, 16])
        seq = [
            ("dve", lambda e: e.tensor_reduce(out=gmax, in_=GL, axis=AX.X, op=ALU.max)),
            ("dve", lambda e: e.tensor_tensor(out=oh, in0=GL, in1=bc4(gmax), op=ALU.is_ge)),
            ("dve", lambda e: e.tensor_tensor(out=ex, in0=GL, in1=bc4(gmax), op=ALU.subtract)),
            ("act", lambda e: e.activation(out=ex, in_=ex, func=AF.Exp)),
            ("dve", lambda e: e.tensor_reduce(out=gsum, in_=ex, axis=AX.X, op=ALU.add)),
            ("dve", lambda e: e.reciprocal(out=pg, in_=gsum)),
            ("dve", lambda e: e.tensor_scalar(out=pen, in0=oh, scalar1=BIG, scalar2=-BIG, op0=ALU.mult, op1=ALU.add)),
            ("dve", lambda e: e.tensor_tensor(out=esel.rearrange("p t (g x) -> p t g x", g=4), in0=EL.rearrange("p t (g x) -> p t g x", g=4),
                                              in1=pen.unsqueeze(3).to_broadcast([128, T, 4, 4]), op=ALU.add)),
            ("dve", lambda e: e.tensor_reduce(out=m1, in_=esel, axis=AX.X, op=ALU.max)),
            ("dve", lambda e: e.tensor_tensor(out=mk1, in0=esel, in1=bc16(m1), op=ALU.is_ge)),
            ("dve", lambda e: e.scalar_tensor_tensor(out=e2, in0=mk1, scalar=-BIG, in1=esel, op0=ALU.mult, op1=ALU.add)),
            ("dve", lambda e: e.tensor_reduce(out=m2, in_=e2, axis=AX.X, op=ALU.max)),
            ("dve", lambda e: e.tensor_tensor(out=mk2, in0=e2, in1=bc16(m2), op=ALU.is_ge)),
            ("dve", lambda e: e.tensor_tensor(out=w2, in0=m2, in1=m1, op=ALU.subtract)),
            ("act", lambda e: e.activation(out=w2, in_=w2, func=AF.Exp)),
            ("dve", lambda e: e.tensor_scalar(out=w1, in0=w2, scalar1=1.0, scalar2=None, op0=ALU.add)),
            ("dve", lambda e: e.reciprocal(out=w1, in_=w1)),
            ("dve", lambda e: e.tensor_tensor(out=w2, in0=w2, in1=w1, op=ALU.mult)),
            ("dve", lambda e: e.tensor_tensor(out=w1, in0=w1, in1=pg, op=ALU.mult)),
            ("dve", lambda e: e.tensor_tensor(out=w2, in0=w2, in1=pg, op=ALU.mult)),
            ("dve", lambda e: e.tensor_tensor(out=mk1, in0=mk1, in1=bc16(w1), op=ALU.mult)),
            ("dve", lambda e: e.tensor_tensor(out=mk2, in0=mk2, in1=bc16(w2), op=ALU.mult)),
            ("dve", lambda e: e.tensor_tensor(out=esel, in0=mk1, in1=mk2, op=ALU.add)),
        ]
        for eng, f in seq:
            fw.op(eng, f, reads=[b_lg, b_wk], writes=[b_wk])
        fw.dma("sp", S.COMB[0:T * 128, :].rearrange("(t p) c -> p t c", p=128), esel, reads=[b_wk])


def phase_moe_experts(fw, cfg, l, S, A, W, o_own, o_ctx, x2_own, x2_ctx, need_ctx, final_out=None):
    nc = fw.nc
    To, Tc = cfg.T_OWN, cfg.T_CTX
    tiles = _tiles_oc(cfg, o_own, o_ctx, x2_own, x2_ctx, need_ctx)
    SB = 6
    nblk = -(-len(tiles) // SB)
    sizes = [len(tiles) // nblk + (1 if i < len(tiles) % nblk else 0) for i in range(nblk)]
    with Phase(fw, "mx") as ph:
        identF = ph.sb([128, 128], F32)
        b_idF = make_ident(fw, identF)
        sel = ph.sb([16, 16, 128], BF16)
        b_sel = Buf()
        fw.op("pool", lambda e: e.memset(sel[:, :, :], 0.0), writes=[b_sel])
        fw.op("pool", lambda e: e.affine_select(out=sel[:, :, :], in_=sel[:, :, :], pattern=[[-1, 16], [0, 128]], compare_op=ALU.not_equal,
                                                fill=1.0, base=0, channel_multiplier=1), reads=[b_sel], writes=[b_sel])
        g2 = [None, None]
        b_g2 = bufs(2)
        for m in range(2 if need_ctx else 1):
            g2[m] = ph.sb([128, D], F32)
            load_bc(fw, "sp", g2[m][:, :], S.mods[l][m:m + 1, 5 * D:6 * D], b_g2[m])
        b_fgb = Buf()
        if final_out is not None:
            fgb = ph.sb([128, D], F32)
            load_bc(fw, "sp", fgb[:, :], A["final_g"][0:1, :], b_fgb)
        acc = ph.sb([128, SB, D], F32)
        hT = ph.sb([128, NK, SB * 128], BF16)
        cm = ph.sb([128, SB, 16], F32)
        cmT = ph.sb([16, SB * 128], BF16)
        cbc = [ph.sb([128, SB * 128], F32) for _ in range(2)]
        Wg = [ph.sb([128, NK, 512], BF16) for _ in range(2)]
        Wu = [ph.sb([128, NK, 512], BF16) for _ in range(2)]
        if SB == 6:
            Wd = [ph.sb([128, 4, D], BF16)] * 2
        else:
            Wd = [ph.sb([128, 4, D], BF16) for _ in range(2)]
        midT = [ph.sb([128, 4, SB * 128], BF16) for _ in range(2)]
        sa = ph.sb([128, 512], F32)
        ss = ph.sb([128, 1], F32)
        b_acc, b_hT, b_cm, b_cmT, b_sa, b_ss = bufs(6)
        b_cbc, b_Wg, b_Wu, b_Wd, b_midT = bufs(2), bufs(2), bufs(2), bufs(2), bufs(2)
        if SB == 6:
            b_Wd = [b_Wd[0]] * 2
        if SB >= 5:
            xt = [ph.sb([128, D], F32)] * 2
            b_xt = [Buf()] * 2
        else:
            xt = [ph.sb([128, D], F32) for _ in range(2)]
            b_xt = bufs(2)
        junk = midT[0][:, :, :].rearrange("p f t -> p (f t)")[:, 0:D]
        b_junk = b_midT[0]
        pa = [ph.ps([128, 512]) for _ in range(2)]
        pu = [ph.ps([128, 512]) for _ in range(2)]
        po = [ph.ps([128, 512]) for _ in range(3)]
        pcb = ph.ps([128, 512])
        b_pa, b_pu, b_po = bufs(2), bufs(2), bufs(3)
        b_pcb = Buf()
        wi = 0
        pi = 0
        oi = 0
        if sizes is None:
            starts = [(i, min(SB, len(tiles) - i)) for i in range(0, len(tiles), SB)]
        else:
            starts, o_ = [], 0
            for z_ in sizes:
                starts.append((o_, z_))
                o_ += z_
        for sb0, sbn in starts:
            tl = tiles[sb0:sb0 + sbn]
            nt = len(tl)
            ntok = nt * 128
            tok0 = tl[0][0] * 128
            fw.dma("sp", hT[:, :, 0:ntok], S.HT2[:, tok0:tok0 + ntok].rearrange("(k p) t -> p k t", p=128), writes=[b_hT])
            fw.dma("sp", cm[:, 0:nt, :], S.COMB[tok0:tok0 + ntok, :].rearrange("(t p) c -> p t c", p=128), writes=[b_cm])
            for i0 in range(0, nt, 4):
                ni = min(4, nt - i0)
                for i in range(ni):
                    fw.op("pe", lambda e: e.transpose(pcb[0:16, i * 128:(i + 1) * 128], cm[:, i0 + i, :], identF[:, :]),
                          reads=[b_cm, b_idF], writes=[b_pcb])
                fw.op("act", lambda e: e.copy(out=cmT[:, i0 * 128:(i0 + ni) * 128], in_=pcb[0:16, 0:ni * 128]),
                      reads=[b_pcb], writes=[b_cmT])
            for ex in range(16):
                w = wi % 2
                wi += 1
                fw.dma("pool", Wg[w][:, :, :], W["w_gate"][ex].rearrange("(k p) f -> p k f", p=128), writes=[b_Wg[w]])
                fw.dma("pool", Wu[w][:, :, :], W["w_up"][ex].rearrange("(k p) f -> p k f", p=128), writes=[b_Wu[w]])
                fw.dma("pool", Wd[w][:, :, :], W["w_down"][ex].rearrange("(k p) c -> p k c", p=128), writes=[b_Wd[w]])
                for tb in range(0, ntok, 512):
                    tw = min(512, ntok - tb)
                    fw.op("pe", lambda e: e.matmul(pcb[:, 0:tw], lhsT=sel[:, ex, :], rhs=cmT[:, tb:tb + tw], start=True, stop=True),
                          reads=[b_sel, b_cmT], writes=[b_pcb])
                    fw.op("act", lambda e: e.copy(out=cbc[w][:, tb:tb + tw], in_=pcb[:, 0:tw]), reads=[b_pcb], writes=[b_cbc[w]])
                for fc in range(4):
                    for tb in range(0, ntok, 512):
                        tw = min(512, ntok - tb)
                        p = pi % 2
                        pi += 1

                        def mg(e):
                            for k in range(NK):
                                ins = e.matmul(pa[p][:, 0:tw], lhsT=Wg[w][:, k, fc * 128:(fc + 1) * 128], rhs=hT[:, k, tb:tb + tw],
                                               start=(k == 0), stop=(k == NK - 1))
                            return ins

                        def mu(e):
                            for k in range(NK):
                                ins = e.matmul(pu[p][:, 0:tw], lhsT=Wu[w][:, k, fc * 128:(fc + 1) * 128], rhs=hT[:, k, tb:tb + tw],
                                               start=(k == 0), stop=(k == NK - 1))
                            return ins
                        fw.op("pe", mg, reads=[b_Wg[w], b_hT], writes=[b_pa[p]])
                        fw.op("pe", mu, reads=[b_Wu[w], b_hT], writes=[b_pu[p]])
                        fw.op("act", lambda e: e.activation(out=sa[:, 0:tw], in_=pa[p][:, 0:tw], func=AF.Silu), reads=[b_pa[p]], writes=[b_sa])
                        fw.op("dve", lambda e: e.tensor_tensor(out=sa[:, 0:tw], in0=sa[:, 0:tw], in1=pu[p][:, 0:tw], op=ALU.mult),
                              reads=[b_sa, b_pu[p]], writes=[b_sa])
                        fw.op("pool", lambda e: e.tensor_tensor(out=midT[w][:, fc, tb:tb + tw], in0=sa[:, 0:tw], in1=cbc[w][:, tb:tb + tw],
                                                                op=ALU.mult), reads=[b_sa, b_cbc[w]], writes=[b_midT[w]])
                for i in range(nt):
                    for dc in range(4):
                        o = oi % 3
                        oi += 1

                        def md(e):
                            for fc in range(4):
                                ins = e.matmul(po[o][:, :], lhsT=midT[w][:, fc, i * 128:(i + 1) * 128], rhs=Wd[w][:, fc, dc * 512:(dc + 1) * 512],
                                               start=(fc == 0), stop=(fc == 3))
                            return ins
                        fw.op("pe", md, reads=[b_midT[w], b_Wd[w]], writes=[b_po[o]])
                        if ex == 0:
                            fw.op("act", lambda e: e.copy(out=acc[:, i, dc * 512:(dc + 1) * 512], in_=po[o][:, :]),
                                  reads=[b_po[o]], writes=[b_acc])
                        else:
                            fw.op("dve", lambda e: e.tensor_tensor(out=acc[:, i, dc * 512:(dc + 1) * 512], in0=acc[:, i, dc * 512:(dc + 1) * 512],
                                                                   in1=po[o][:, :], op=ALU.add), reads=[b_po[o], b_acc], writes=[b_acc])
            for i, (t, prow, xsrc, m, dst) in enumerate(tl):
                s = i % 2
                fw.dma("sp", xt[s][:, :], xsrc, writes=[b_xt[s]])
                fw.op("dve", lambda e: e.tensor_tensor(out=acc[:, i, :], in0=acc[:, i, :], in1=g2[m][:, :], op=ALU.mult),
                      reads=[b_acc, b_g2[m]], writes=[b_acc])
                fw.op("pool", lambda e: e.tensor_tensor(out=xt[s][:, :], in0=xt[s][:, :], in1=acc[:, i, :], op=ALU.add),
                      reads=[b_acc, b_xt[s]], writes=[b_xt[s]])
                if final_out is not None and m == 0:
                    fw.op("pool", lambda e: e.memset(ss[:, :], 0.0), writes=[b_ss])
                    fw.op("act", lambda e: e.activation(out=junk[:, :], in_=xt[s][:, :], func=AF.Square, accum_out=ss[:, 0:1]),
                          reads=[b_xt[s], b_ss], writes=[b_junk, b_ss])
                    _rstd(fw, ss[:, :], b_ss, D)
                    fw.op("dve", lambda e: e.scalar_tensor_tensor(out=xt[s][:, :], in0=xt[s][:, :], scalar=ss[:, 0:1], in1=fgb[:, :],
                                                                  op0=ALU.mult, op1=ALU.mult), reads=[b_xt[s], b_ss, b_fgb], writes=[b_xt[s]])
                    fw.dma("sp", final_out[t * 128:(t + 1) * 128, :], xt[s][:, :], reads=[b_xt[s]])
                else:
                    fw.dma("sp", dst, xt[s][:, :], reads=[b_xt[s]])


WEIGHT_KEYS = ("w_mod", "bmod2", "w_in", "g1row", "w_br_a", "w_br_m", "w_br_c", "w_out", "w_r", "w_gate", "w_up", "w_down")


def layer_weights(inp, l, half):
    w = layer_inputs(inp, l, half)
    for k in ("w_br_a", "w_br_m", "w_br_c", "w_out", "w_gate", "w_up", "w_down"):
        w[k] = np.ascontiguousarray(inp[k][l])
    w["w_r"] = np.ascontiguousarray(np.concatenate([np.asarray(inp["w_rg"][l]), np.asarray(inp["w_re"][l])], axis=1))
    return w


def emit_layer(fw, cfg, S, A, l, last, x_own, x_oth, xc, x2o, x2c, sfx="", mid_hook=None, skip_mods=False, attn_bg=None):
    nc = fw.nc
    need_ctx = not last
    To, Tc = cfg.T_OWN, cfg.T_CTX
    x1o = nc.dram_tensor("x1o" + sfx, [To, D], F32, kind="Internal").ap()
    x1c = nc.dram_tensor("x1c" + sfx, [Tc, D], F32, kind="Internal").ap()
    W = {k: A[k + sfx] for k in WEIGHT_KEYS}
    if not skip_mods:
        phase_mods(fw, cfg, A["cvec"], W["w_mod"], W["bmod2"], S.mods[l])
    if mid_hook is None:
        phase_inproj(fw, cfg, l, x_own, x_oth, xc, S.mods[l], W["g1row"], W["w_in"], S)
    else:
        phase_inproj(fw, cfg, l, x_own, x_oth, xc, S.mods[l], W["g1row"], W["w_in"], S, which=("oc",))
        mid_hook()
        phase_inproj(fw, cfg, l, x_own, x_oth, xc, S.mods[l], W["g1row"], W["w_in"], S, which=("oth",))
    phase_attn(fw, cfg, l, S, A, "a", need_ctx, bg=attn_bg)
    phase_attn(fw, cfg, l, S, A, "c", need_ctx)
    phase_mlstm(fw, cfg, l, S, A, need_ctx)
    phase_merge(fw, cfg, l, S, A, W, x_own, xc, x1o, x1c, need_ctx)
    phase_moe_router(fw, cfg, l, S, A, W, x1o, x1c, need_ctx)
    phase_moe_experts(fw, cfg, l, S, A, W, x1o, x1c, x2o, x2c, need_ctx, final_out=(x2o if last else None))


def phase_exchange(fw, cfg, A, x2o, xoth, part="ab", st=None):
    nc = fw.nc
    To = cfg.T_OWN
    nt = cfg.nt_own
    CH = 2
    nch = nt // CH
    if st is None:
        st = {}
    if "Z" not in st:
        st["Z"] = [nc.dram_tensor("xchgZ%d" % i, [2 * CH * 128, D], F32, kind="Internal").ap() for i in range(nch)]
        st["R"] = [nc.dram_tensor("xchgR%d" % i, [2 * CH * 128, D], F32, kind="Internal").ap() for i in range(nch)]
        st["b_R"] = bufs(nch)
    Z, R, b_R = st["Z"], st["R"], st["b_R"]
    if "a" in part:
        _exchange_a(fw, cfg, A, x2o, Z, R, b_R, CH, nch)
    if "b" in part:
        _exchange_b(fw, cfg, A, xoth, R, b_R, CH)


def _exchange_a(fw, cfg, A, x2o, Z, R, b_R, CH, nch):
    nt = cfg.nt_own
    with Phase(fw, "xa") as ph:
        sv = ph.sb([128, 2], F32)
        b_sv = Buf()
        fw.dma("sp", sv[:, :], A["selv"], writes=[b_sv])
        xt = [ph.sb([128, D], F32) for _ in range(2)]
        z0 = [ph.sb([128, D], F32) for _ in range(2)]
        z1 = [ph.sb([128, D], F32) for _ in range(2)]
        b_xt, b_z0, b_z1 = bufs(2), bufs(2), bufs(2)
        for i in range(nt):
            s = i % 2
            c, t = i // CH, i % CH
            fw.dma("sp", xt[s][:, :], x2o[i * 128:(i + 1) * 128, :], writes=[b_xt[s]])
            fw.op("dve", lambda e: e.tensor_scalar(out=z0[s][:, :], in0=xt[s][:, :], scalar1=sv[:, 0:1], scalar2=None, op0=ALU.mult),
                  reads=[b_xt[s], b_sv], writes=[b_z0[s]])
            fw.op("act", lambda e: e.activation(out=z1[s][:, :], in_=xt[s][:, :], func=AF.Copy, scale=sv[:, 1:2]),
                  reads=[b_xt[s], b_sv], writes=[b_z1[s]])
            fw.dma("sp", Z[c][t * 128:(t + 1) * 128, :], z0[s][:, :], reads=[b_z0[s]])
            fw.dma("sp", Z[c][CH * 128 + t * 128:CH * 128 + (t + 1) * 128, :], z1[s][:, :], reads=[b_z1[s]])
    for c in range(nch):
        fw.all_reduce(Z[c], R[c], [[0, 1], [2, 3], [4, 5], [6, 7]], writes=[b_R[c]])


def _exchange_b(fw, cfg, A, xoth, R, b_R, CH):
    nt = cfg.nt_own
    with Phase(fw, "xb") as ph:
        J = ph.sb([128, 2, 128], F32)
        b_J = Buf()
        fw.dma("sp", J[:, :, :], A["jsel"].rearrange("j r p -> r j p"), writes=[b_J])
        ra = [ph.sb([128, D], F32) for _ in range(2)]
        rb = [ph.sb([128, D], F32) for _ in range(2)]
        ot = [ph.sb([128, D], F32) for _ in range(2)]
        b_ra, b_rb, b_ot = bufs(2), bufs(2), bufs(2)
        pp = [ph.ps([128, 512]) for _ in range(4)]
        b_pp = bufs(4)
        pi = 0
        for i in range(nt):
            s = i % 2
            j = nt - 1 - i
            c, t = j // CH, j % CH
            fw.dma("sp", ra[s][:, :], R[c][t * 128:(t + 1) * 128, :], reads=[b_R[c]], writes=[b_ra[s]])
            fw.dma("sp", rb[s][:, :], R[c][CH * 128 + t * 128:CH * 128 + (t + 1) * 128, :], reads=[b_R[c]], writes=[b_rb[s]])
            for cb in range(4):
                p = pi % 4
                pi += 1
                cs = slice(cb * 512, (cb + 1) * 512)

                def mm(e):
                    e.matmul(pp[p][:, :], lhsT=J[:, 0, :], rhs=ra[s][:, cs], start=True, stop=False)
                    return e.matmul(pp[p][:, :], lhsT=J[:, 1, :], rhs=rb[s][:, cs], start=False, stop=True)
                fw.op("pe", mm, reads=[b_J, b_ra[s], b_rb[s]], writes=[b_pp[p]])
                if cb % 2 == 0:
                    fw.op("act", lambda e: e.copy(out=ot[s][:, cs], in_=pp[p][:, :]), reads=[b_pp[p]], writes=[b_ot[s]])
                else:
                    fw.op("dve", lambda e: e.tensor_copy(out=ot[s][:, cs], in_=pp[p][:, :]), reads=[b_pp[p]], writes=[b_ot[s]])
            fw.dma("sp", xoth[i * 128:(i + 1) * 128, :], ot[s][:, :], reads=[b_ot[s]])


def exchange_consts(half):
    selv = np.zeros((128, 2), np.float32)
    selv[:, half] = 1.0
    Jm = np.zeros((128, 128), np.float32)
    Jm[np.arange(128), 127 - np.arange(128)] = 1.0
    jsel = np.zeros((2, 128, 128), np.float32)
    jsel[1 - half] = Jm
    return {"selv": selv, "jsel": jsel}


def build_fused(cfg, shapes):
    nc = bass.Bass("TRN2", target_bir_lowering=False)
    fw = Fw(nc)
    A = {}
    for name, shp in shapes.items():
        A[name] = nc.dram_tensor(name, list(shp), F32, kind="ExternalInput").ap()
    S = Scratch(nc, cfg)
    To, Tc = cfg.T_OWN, cfg.T_CTX
    xm_o = nc.dram_tensor("xmid_o", [To, D], F32, kind="Internal").ap()
    xm_c = nc.dram_tensor("xmid_c", [Tc, D], F32, kind="Internal").ap()
    xm_oth = nc.dram_tensor("xmid_oth", [To, D], F32, kind="Internal").ap()
    out = nc.dram_tensor("out", [To, D], F32, kind="ExternalOutput").ap()
    dummy_c = nc.dram_tensor("xlast_c", [Tc, D], F32, kind="Internal").ap()
    emit_layer(fw, cfg, S, A, 0, False, A["x_own"], A["x_oth"], A["xc"], xm_o, xm_c, sfx="_0")
    xst = {}
    phase_exchange(fw, cfg, A, xm_o, xm_oth, part="a", st=xst)
    emit_layer(fw, cfg, S, A, 1, True, xm_o, xm_oth, xm_c, out, dummy_c, sfx="_1",
               mid_hook=lambda: phase_exchange(fw, cfg, A, xm_o, xm_oth, part="b", st=xst))
    fw.barrier()
    return nc


def build_layer(cfg, l, last, shapes):
    nc = bass.Bass("TRN2", target_bir_lowering=False)
    fw = Fw(nc)
    A = {}
    for name, shp in shapes.items():
        A[name] = nc.dram_tensor(name, list(shp), F32, kind="ExternalInput").ap()
    S = Scratch(nc, cfg)
    To, Tc = cfg.T_OWN, cfg.T_CTX
    x2o = nc.dram_tensor("x2o", [To, D], F32, kind="ExternalOutput").ap()
    x2c = nc.dram_tensor("x2c", [Tc, D], F32, kind="ExternalOutput").ap()
    emit_layer(fw, cfg, S, A, l, last, A["x_own"], A["x_oth"], A["xc"], x2o, x2c)
    fw.barrier()
    return nc


def run_fused(inp, cfg, cores):
    in_maps = []
    for (b, half) in cores:
        m = core_inputs(inp, cfg, b, half)
        m["rope"] = rope_table(cfg, half)
        m.update(small_inputs(inp, half))
        m.update(exchange_consts(half))
        for l in range(DEPTH):
            for k, v in layer_weights(inp, l, half).items():
                m[k + "_%d" % l] = v
        in_maps.append(m)
    shapes = {k: v.shape for k, v in in_maps[0].items()}
    nc = build_fused(cfg, shapes)
    res = run_bass_kernel_spmd(nc, in_maps, core_ids=list(range(len(cores))))
    To = cfg.T_OWN
    B = inp["x"].shape[0]
    x_new = np.zeros((B, 2 * To, D), np.float32)
    for (b, half), r in zip(cores, res.results):
        xo = np.asarray(r["out"], np.float32)
        if half == 0:
            x_new[b, :To] = xo
        else:
            x_new[b, To:] = xo[::-1]
    return x_new


def run_layer(inp, cfg, l, last, x_full, ctx_full, cores):
    in_maps = []
    cur = dict(inp)
    cur["x"] = x_full
    cur["ctx"] = ctx_full
    for (b, half) in cores:
        m = core_inputs(cur, cfg, b, half)
        m["rope"] = rope_table(cfg, half)
        m.update(small_inputs(inp, half))
        m.update(layer_weights(inp, l, half))
        in_maps.append(m)
    shapes = {k: v.shape for k, v in in_maps[0].items()}
    nc = build_layer(cfg, l, last, shapes)
    res = run_bass_kernel_spmd(nc, in_maps, core_ids=list(range(len(cores))))
    To = cfg.T_OWN
    B = x_full.shape[0]
    x_new = np.zeros((B, 2 * To, D), np.float32)
    c_new = np.zeros((B, cfg.T_CTX, D), np.float32)
    for (b, half), r in zip(cores, res.results):
        xo = np.asarray(r["x2o"], np.float32)
        if half == 0:
            x_new[b, :To] = xo
            c_new[b] = np.asarray(r["x2c"], np.float32)
        else:
            x_new[b, To:] = xo[::-1]
    return x_new, c_new


def kernel(**inputs):
    inp = {k: np.asarray(v) for k, v in inputs.items()}
    cfg = Cfg(nt_own=16, nt_ctx=2)
    cores = [(b, half) for b in range(4) for half in range(2)]
    return run_fused(inp, cfg, cores).astype(np.float32)
```

```python
from contextlib import ExitStack
import numpy as np
import concourse.bass as bass
import concourse.mybir as mybir
from concourse.bass_utils import run_bass_kernel_spmd

F32 = mybir.dt.float32
BF16 = mybir.dt.bfloat16
AF = mybir.ActivationFunctionType
ALU = mybir.AluOpType
AX = mybir.AxisListType

D = 2048
NK = D // 128
DEPTH = 2
EPS = 1e-6
NEG = -30000.0

ENGS = ("pe", "act", "dve", "pool", "sp")
RING = 12


class Buf:
    __slots__ = ("w", "r")

    def __init__(self):
        self.w = None
        self.r = {}


def bufs(n):
    return [Buf() for _ in range(n)]


class Fw:
    def __init__(self, nc):
        self.nc = nc
        self.e = dict(pe=nc.tensor, act=nc.scalar, dve=nc.vector, pool=nc.gpsimd, sp=nc.sync)
        self.semh = {}
        for k in ENGS:
            self.semh["e_" + k] = nc.alloc_semaphore("s_" + k)
        self.cnt = {k: 0 for k in ENGS}
        self.known = {k: {} for k in ENGS}
        self.rings = {}
        self.ring_n = {}
        self.ring_val = {}
        for q in ("sp", "pool", "act"):
            self.rings[q] = []
            self.ring_n[q] = 0
            for i in range(RING):
                key = "r_%s_%d" % (q, i)
                self.semh[key] = nc.alloc_semaphore(key)
                self.rings[q].append(key)
                self.ring_val[key] = 0

    def _wait(self, eng, dep):
        key, val, _ = dep
        if val <= 0 or self.known[eng].get(key, 0) >= val:
            return
        self.e[eng].wait_ge(self.semh[key], val)
        self.known[eng][key] = val

    def _sync(self, eng, issuer, reads, writes):
        for b in reads:
            if b.w is not None:
                self._wait(issuer, b.w)
        for b in writes:
            if b.w is not None and b.w[2] != eng:
                self._wait(issuer, b.w)
            for key, (val, pe) in b.r.items():
                if pe != eng:
                    self._wait(issuer, (key, val, pe))

    @staticmethod
    def _mark(dep, reads, writes):
        for b in reads:
            b.r[dep[0]] = (dep[1], dep[2])
        for b in writes:
            b.w = dep
            b.r = {}

    def op(self, eng, fn, reads=(), writes=()):
        self._sync(eng, eng, reads, writes)
        ins = fn(self.e[eng])
        self.cnt[eng] += 1
        ins.then_inc(self.semh["e_" + eng], 1)
        dep = ("e_" + eng, self.cnt[eng], eng)
        self._mark(dep, reads, writes)
        return dep

    def dma(self, q, out, in_, reads=(), writes=(), **kw):
        self._sync("dma", q, reads, writes)
        n = self.ring_n[q]
        self.ring_n[q] = n + 1
        key = self.rings[q][n % RING]
        prev = self.ring_val[key]
        self._wait(q, (key, prev, "dma"))
        self.e[q].dma_start(out=out, in_=in_, **kw).then_inc(self.semh[key], 16)
        self.ring_val[key] = prev + 16
        dep = (key, prev + 16, "dma")
        self._mark(dep, reads, writes)
        return dep

    def all_reduce(self, src, dst, groups, reads=(), writes=()):
        q = "pool"
        self._sync("dma", q, reads, writes)
        if "cc" not in self.semh:
            self.semh["cc"] = self.nc.alloc_semaphore("cc_sem")
            self.ring_val["cc"] = 0
        prev = self.ring_val["cc"]
        self._wait(q, ("cc", prev, "dma"))
        self.e[q].collective_compute("AllReduce", ALU.add, replica_groups=groups, ins=[src], outs=[dst]).then_inc(self.semh["cc"])
        self.ring_val["cc"] = prev + 1
        dep = ("cc", prev + 1, "dma")
        self._mark(dep, reads, writes)
        return dep

    def barrier(self):
        for k in ENGS:
            if k != "sp":
                self._wait("sp", ("e_" + k, self.cnt[k], k))
        for key, val in self.ring_val.items():
            self._wait("sp", (key, val, "dma"))
        self.e["sp"].sem_inc(self.semh["e_sp"], 1)
        self.cnt["sp"] += 1
        for k in ENGS:
            if k != "sp":
                self._wait(k, ("e_sp", self.cnt["sp"], "sp"))
        for k in ENGS:
            for kk in ENGS:
                self.known[k]["e_" + kk] = self.cnt[kk]
            for key, val in self.ring_val.items():
                self.known[k][key] = val


class Phase:
    def __init__(self, fw, name):
        self.fw = fw
        self.nc = fw.nc
        fw.nphase = getattr(fw, "nphase", 0) + 1
        self.name = "%s%d" % (name, fw.nphase)
        self.es = ExitStack()
        self.i = 0

    def __enter__(self):
        self.es.__enter__()
        return self

    def __exit__(self, *a):
        self.fw.barrier()
        return self.es.__exit__(*a)

    def sb(self, shape, dt):
        self.i += 1
        return self.es.enter_context(self.nc.sbuf_tensor("%s_s%d" % (self.name, self.i), list(shape), dt))

    def ps(self, shape, dt=F32):
        self.i += 1
        return self.es.enter_context(self.nc.psum_tensor("%s_p%d" % (self.name, self.i), list(shape), dt))


def make_ident(fw, t, n=128):
    b = Buf()
    fw.op("pool", lambda e: e.memset(t[:, :], 0.0), writes=[b])
    fw.op("pool", lambda e: e.affine_select(out=t[:, :], in_=t[:, :], pattern=[[-1, n]],
                                            compare_op=ALU.not_equal, fill=1.0, base=0,
                                            channel_multiplier=1), reads=[b], writes=[b])
    return b


C_AQ, C_AKV, C_MQ, C_MK, C_MV, C_MO, C_CQ, C_CKV, C_G, C_MG = 0, 1024, 1536, 2048, 2560, 3072, 3584, 4096, 4608, 10752
P_IN = 10768


class Cfg:
    def __init__(self, nt_own=16, nt_ctx=2):
        self.nt_own = nt_own
        self.nt_oth = nt_own
        self.nt_ctx = nt_ctx
        self.T_OWN = 128 * nt_own
        self.T_CTX = 128 * nt_ctx
        self.T_ALL = 2 * self.T_OWN + self.T_CTX
        self.O_OWN, self.O_OTH, self.O_CTX = 0, self.T_OWN, 2 * self.T_OWN


def phase_mods(fw, cfg, cvec, w_mod_l, bmod2_l, mods_l):
    nc = fw.nc
    with Phase(fw, "mod") as ph:
        cc = ph.sb([128, NK * 2], F32)
        S = ph.sb([128, NK * 2], BF16)
        bm = ph.sb([2, 6 * D], F32)
        out = ph.sb([2, 6 * D], F32)
        W = [ph.sb([128, NK, 512], BF16) for _ in range(2)]
        pp = [ph.ps([128, 512]) for _ in range(2)]
        b_cc, b_S, b_bm, b_out = bufs(4)
        b_W = bufs(2)
        b_pp = bufs(2)
        fw.dma("sp", cc[:, :], cvec, writes=[b_cc])
        fw.dma("sp", bm[:, :], bmod2_l, writes=[b_bm])
        fw.op("act", lambda e: e.activation(out=S[:, :], in_=cc[:, :], func=AF.Silu), reads=[b_cc], writes=[b_S])
        wv = w_mod_l.rearrange("(k p) c -> p k c", p=128)
        nblk = 6 * D // 512
        for j in range(nblk):
            s = j % 2
            fw.dma("pool", W[s][:, :, :], wv[:, :, j * 512:(j + 1) * 512], writes=[b_W[s]])

            def mm(e, s=s):
                for k in range(NK):
                    ins = e.matmul(pp[s][0:2, :], lhsT=S[:, 2 * k:2 * k + 2], rhs=W[s][:, k, :],
                                   start=(k == 0), stop=(k == NK - 1))
                return ins
            fw.op("pe", mm, reads=[b_S, b_W[s]], writes=[b_pp[s]])
            fw.op("dve", lambda e, s=s, j=j: e.tensor_tensor(out=out[:, j * 512:(j + 1) * 512], in0=pp[s][0:2, :],
                                                              in1=bm[:, j * 512:(j + 1) * 512], op=ALU.add),
                  reads=[b_pp[s], b_bm], writes=[b_out])
        fw.dma("sp", mods_l, out[:, :], reads=[b_out])


def mods_bg(fw, ph, cfg, cvec, w_mod_l, bmod2_l, mods_l):
    cc = ph.sb([128, NK * 2], F32)
    Sm = ph.sb([128, NK * 2], BF16)
    W = [ph.sb([128, NK, 512], BF16) for _ in range(2)]
    bsl = [ph.sb([2, 512], F32) for _ in range(2)]
    osl = [ph.sb([2, 512], F32) for _ in range(2)]
    pp = ph.ps([128, 512])
    b_cc, b_S, b_pp = bufs(3)
    b_W, b_bsl, b_osl = bufs(2), bufs(2), bufs(2)
    wv = w_mod_l.rearrange("(k p) c -> p k c", p=128)
    nblk = 6 * D // 512
    fw.dma("sp", cc[:, :], cvec, writes=[b_cc])
    fw.op("act", lambda e: e.activation(out=Sm[:, :], in_=cc[:, :], func=AF.Silu), reads=[b_cc], writes=[b_S])
    fw.dma("pool", W[0][:, :, :], wv[:, :, 0:512], writes=[b_W[0]])
    fw.dma("sp", bsl[0][:, :], bmod2_l[:, 0:512], writes=[b_bsl[0]])
    yield
    for j in range(nblk):
        s = j % 2
        if j + 1 < nblk:
            fw.dma("pool", W[1 - s][:, :, :], wv[:, :, (j + 1) * 512:(j + 2) * 512], writes=[b_W[1 - s]])
            fw.dma("sp", bsl[1 - s][:, :], bmod2_l[:, (j + 1) * 512:(j + 2) * 512], writes=[b_bsl[1 - s]])

        def mm(e):
            for k in range(NK):
                ins = e.matmul(pp[0:2, :], lhsT=Sm[:, 2 * k:2 * k + 2], rhs=W[s][:, k, :], start=(k == 0), stop=(k == NK - 1))
            return ins
        fw.op("pe", mm, reads=[b_S, b_W[s]], writes=[b_pp])
        fw.op("dve", lambda e: e.tensor_tensor(out=osl[s][:, :], in0=pp[0:2, :], in1=bsl[s][:, :], op=ALU.add),
              reads=[b_pp, b_bsl[s]], writes=[b_osl[s]])
        fw.dma("sp", mods_l[:, j * 512:(j + 1) * 512], osl[s][:, :], reads=[b_osl[s]])
        yield


def load_bc(fw, q, dst, src_row, wb):
    return fw.dma(q, dst, src_row.partition_broadcast(128), writes=[wb])


def phase_inproj(fw, cfg, l, x_own, x_oth, xc, mods_l, g1row, w_in_l, S, which=("oc", "oth")):
    nc = fw.nc
    T_OC = cfg.T_OWN + cfg.T_CTX
    passes = [
        ("oc", [(x_own, i, 0) for i in range(cfg.nt_own)] + [(xc, i, 1) for i in range(cfg.nt_ctx)]),
        ("oth", [(x_oth, i, 0) for i in range(cfg.nt_oth)]),
    ]
    with Phase(fw, "inp") as ph:
        ident = ph.sb([128, 128], BF16)
        b_id = make_ident(fw, ident)
        gain = [ph.sb([128, D], F32) for _ in range(2)]
        shift = [ph.sb([128, D], F32) for _ in range(2)]
        tmpg = ph.sb([128, D], F32)
        b_gain, b_shift = bufs(2), bufs(2)
        b_tmp = Buf()
        load_bc(fw, "sp", tmpg[:, :], g1row, b_tmp)
        for m in range(2):
            load_bc(fw, "sp", shift[m][:, :], mods_l[m:m + 1, 0:D], b_shift[m])
            load_bc(fw, "sp", gain[m][:, :], mods_l[m:m + 1, D:2 * D], b_gain[m])
            fw.op("dve", lambda e, m=m: e.scalar_tensor_tensor(out=gain[m][:, :], in0=gain[m][:, :], scalar=1.0,
                                                                in1=tmpg[:, :], op0=ALU.add, op1=ALU.mult),
                  reads=[b_gain[m], b_tmp], writes=[b_gain[m]])
        hT = ph.sb([128, NK, T_OC], BF16)
        xt = [ph.sb([128, D], F32)] * 2
        b_xt = [Buf()] * 2
        junk = ph.sb([128, D], BF16)
        b_junk = Buf()
        y32 = ph.sb([128, D], F32)
        b_y32 = Buf()
        hb = [ph.sb([128, D], BF16)] * 2
        b_hb = [Buf()] * 2
        ss = [ph.sb([128, 1], F32) for _ in range(2)]
        b_ss = bufs(2)
        ptr = [ph.ps([128, 512], BF16) for _ in range(2)]
        b_ptr = bufs(2)
        pmm = [ph.ps([128, 512]) for _ in range(4)]
        b_pmm = bufs(4)
        W = [ph.sb([128, NK, 512], BF16) for _ in range(2)]
        b_W = bufs(2)
        stage = [ph.sb([128, T_OC // 128, 512], BF16) for _ in range(2)]
        b_stage = bufs(2)
        stg32 = ph.sb([128, T_OC // 128, 16], F32)
        b_stg32 = Buf()
        wv = w_in_l.rearrange("(k p) c -> p k c", p=128)
        wi = 0
        ti = 0
        pi = 0
        for pname, tiles in passes:
            if pname not in which:
                continue
            nt = len(tiles)
            b_hT = bufs(nt)
            for t, (src, i, m) in enumerate(tiles):
                s = ti % 2
                ti += 1
                fw.dma("sp", xt[s][:, :], src[i * 128:(i + 1) * 128, :], writes=[b_xt[s]])
                fw.op("pool", lambda e, s=s: e.memset(ss[s][:, :], 0.0), writes=[b_ss[s]])
                fw.op("act", lambda e, s=s: e.activation(out=junk[:, :], in_=xt[s][:, :], func=AF.Square,
                                                         accum_out=ss[s][:, 0:1]),
                      reads=[b_xt[s], b_ss[s]], writes=[b_junk, b_ss[s]])
                fw.op("dve", lambda e, s=s: e.tensor_scalar(out=ss[s][:, :], in0=ss[s][:, :], scalar1=1.0 / D,
                                                            scalar2=EPS, op0=ALU.mult, op1=ALU.add),
                      reads=[b_ss[s]], writes=[b_ss[s]])
                fw.op("act", lambda e, s=s: e.sqrt(out=ss[s][:, :], in_=ss[s][:, :]), reads=[b_ss[s]], writes=[b_ss[s]])
                fw.op("dve", lambda e, s=s: e.reciprocal(out=ss[s][:, :], in_=ss[s][:, :]),
                      reads=[b_ss[s]], writes=[b_ss[s]])
                fw.op("dve", lambda e, s=s, m=m: e.scalar_tensor_tensor(out=y32[:, :], in0=xt[s][:, :],
                                                                        scalar=ss[s][:, 0:1], in1=gain[m][:, :],
                                                                        op0=ALU.mult, op1=ALU.mult),
                      reads=[b_xt[s], b_ss[s], b_gain[m]], writes=[b_y32])
                fw.op("pool", lambda e, s=s, m=m: e.tensor_tensor(out=hb[s][:, :], in0=y32[:, :], in1=shift[m][:, :],
                                                                  op=ALU.add),
                      reads=[b_y32, b_shift[m]], writes=[b_hb[s]])
                for k4 in range(NK // 4):
                    p = pi % 2
                    pi += 1

                    def tr(e, s=s, k4=k4, p=p):
                        for kk in range(4):
                            k = k4 * 4 + kk
                            ins = e.transpose(ptr[p][:, kk * 128:(kk + 1) * 128], hb[s][:, k * 128:(k + 1) * 128],
                                              ident[:, :])
                        return ins
                    fw.op("pe", tr, reads=[b_hb[s], b_id], writes=[b_ptr[p]])
                    fw.op("act", lambda e, k4=k4, p=p, t=t: e.copy(
                        out=hT[:, k4 * 4:(k4 + 1) * 4, t * 128:(t + 1) * 128],
                        in_=ptr[p][:, :].rearrange("p (k t) -> p k t", k=4)),
                        reads=[b_ptr[p]], writes=[b_hT[t]])
            if pname == "oc":
                blocks = [("tm", c0, 512) for c0 in range(0, C_MQ, 512)]
                blocks += [("fm", C_MQ, 512), ("fm", C_MK, 512)]
                blocks += [("tm", c0, 512) for c0 in range(C_MV, C_MG, 512)]
                blocks += [("tm32", C_MG, 16)]
                segs = [(0, cfg.nt_own, cfg.O_OWN), (cfg.nt_own, cfg.nt_ctx, cfg.O_CTX)]
            else:
                blocks = [("tm", C_AKV, 512), ("fm", C_MQ, 512), ("fm", C_MK, 512), ("tm", C_MV, 512),
                          ("tm", C_CKV, 512), ("tm32", C_MG, 16)]
                segs = [(0, cfg.nt_oth, cfg.O_OTH)]
            for kind, c0, cw in blocks:
                s = wi % 2
                wi += 1
                fw.dma("pool", W[s][:, :, 0:cw], wv[:, :, c0:c0 + cw], writes=[b_W[s]])
                if kind in ("tm", "tm32"):
                    st = stage[s] if kind == "tm" else stg32
                    bst = b_stage[s] if kind == "tm" else b_stg32
                    ntl_ = 1 if (pname == "oth" and c0 == C_CKV) else nt
                    for t in range(ntl_):
                        p = pi % 4
                        pi += 1

                        def mm(e, s=s, t=t, p=p, cw=cw):
                            for k in range(NK):
                                ins = e.matmul(pmm[p][:, 0:cw], lhsT=hT[:, k, t * 128:(t + 1) * 128],
                                               rhs=W[s][:, k, 0:cw], start=(k == 0), stop=(k == NK - 1))
                            return ins
                        fw.op("pe", mm, reads=[b_hT[t], b_W[s]], writes=[b_pmm[p]])
                        ev = "act" if t % 2 == 0 else "dve"
                        if ev == "act":
                            fw.op("act", lambda e, t=t, p=p, cw=cw, st=st: e.copy(out=st[:, t, 0:cw], in_=pmm[p][:, 0:cw]),
                                  reads=[b_pmm[p]], writes=[bst])
                        else:
                            fw.op("dve", lambda e, t=t, p=p, cw=cw, st=st: e.tensor_copy(out=st[:, t, 0:cw],
                                                                                         in_=pmm[p][:, 0:cw]),
                                  reads=[b_pmm[p]], writes=[bst])
                    dst = S.tm(c0, cw)
                    for (t0, ntl, o) in segs:
                        ntl = min(ntl, ntl_ - t0)
                        if ntl <= 0:
                            continue
                        fw.dma("sp", dst[o:o + ntl * 128, :].rearrange("(t p) c -> p t c", p=128),
                               st[:, t0:t0 + ntl, 0:cw], reads=[bst])
                else:
                    ntok = nt * 128 if (pname == "oc" or c0 == C_MK) else 128
                    stT = stage[s][:, :, :].rearrange("p t c -> p (t c)")
                    for cc in range(4):
                        for tb in range(0, ntok, 512):
                            tw = min(512, ntok - tb)
                            p = pi % 4
                            pi += 1

                            def mm(e, s=s, cc=cc, tb=tb, tw=tw, p=p):
                                for k in range(NK):
                                    ins = e.matmul(pmm[p][:, 0:tw], lhsT=W[s][:, k, cc * 128:(cc + 1) * 128],
                                                   rhs=hT[:, k, tb:tb + tw], start=(k == 0), stop=(k == NK - 1))
                                return ins
                            fw.op("pe", mm, reads=[b_hT[tt] for tt in range(tb // 128, (tb + tw) // 128)] + [b_W[s]],
                                  writes=[b_pmm[p]])
                            if (tb // 512) % 2 == 0:
                                fw.op("act", lambda e, tb=tb, tw=tw, p=p, cc=cc: e.copy(
                                    out=stT[:, cc * ntok + tb:cc * ntok + tb + tw], in_=pmm[p][:, 0:tw]),
                                    reads=[b_pmm[p]], writes=[b_stage[s]])
                            else:
                                fw.op("dve", lambda e, tb=tb, tw=tw, p=p, cc=cc: e.tensor_copy(
                                    out=stT[:, cc * ntok + tb:cc * ntok + tb + tw], in_=pmm[p][:, 0:tw]),
                                    reads=[b_pmm[p]], writes=[b_stage[s]])
                    dstT = S.fm(c0)
                    for (t0, ntl, o) in segs:
                        a0, a1 = t0 * 128, min((t0 + ntl) * 128, ntok)
                        if a1 <= a0:
                            continue
                        fw.dma("sp", dstT[:, o:o + a1 - a0].rearrange("(cc p) t -> p cc t", p=128),
                               stT[:, 0:4 * ntok].rearrange("p (cc t) -> p cc t", cc=4)[:, :, a0:a1],
                               reads=[b_stage[s]])


class Scratch:
    def __init__(self, nc, cfg, taps=()):
        self.nc = nc
        self.cfg = cfg
        T = cfg.T_ALL

        def dt(name, shape, dtype):
            kind = "ExternalOutput" if name in taps else "Internal"
            return nc.dram_tensor(name, list(shape), dtype, kind=kind).ap()
        self.mods = [dt("mods%d" % l, [2, 6 * D], F32) for l in range(DEPTH)]
        self.P = dt("P_tm", [T, C_MG], BF16)
        self.MG = dt("MG", [T, 16], F32)
        self.MQT = dt("MQT", [512, T], BF16)
        self.MKT = dt("MKT", [512, T], BF16)
        self.HT2 = dt("HT2", [2048, cfg.T_OWN + cfg.T_CTX], BF16)
        self.COMB = dt("COMB", [cfg.T_OWN + cfg.T_CTX, 16], F32)
        self.ZT = dt("ZT", [2048, cfg.T_OWN + cfg.T_CTX], BF16)
        self.HD = dt("HD", [2, cfg.T_OWN + cfg.T_CTX, 512], F32)
        self.OT = dt("OT", [2048, cfg.T_OWN + cfg.T_CTX], BF16)

    def tm(self, c0, cw):
        if c0 == C_MG:
            return self.MG
        return self.P[:, c0:c0 + cw]

    def fm(self, c0):
        return self.MQT if c0 == C_MQ else self.MKT


def _win_perm(half):
    mg = np.arange(3584, 3600).reshape(2, 2, 4)
    if half == 1:
        mg = mg[:, ::-1, :]
    return np.concatenate([np.arange(0, 3584), np.arange(3600, 10768), mg.reshape(-1)])


def core_inputs(inp, cfg, b, half):
    To = cfg.T_OWN
    seq = np.asarray(inp["x"][b][:2 * To])
    ctx = np.asarray(inp["ctx"][b][:cfg.T_CTX])
    if half == 1:
        seq = seq[::-1]
        ctx = ctx[::-1]
    cv = np.stack([np.asarray(inp["c"][b]).reshape(NK, 128).T, np.asarray(inp["c_ctx"]).reshape(NK, 128).T], axis=2)
    return {
        "x_own": np.ascontiguousarray(seq[:To]),
        "x_oth": np.ascontiguousarray(seq[To:]),
        "xc": np.ascontiguousarray(ctx),
        "cvec": np.ascontiguousarray(cv.reshape(128, 2 * NK)),
    }


def layer_inputs(inp, l, half):
    return {
        "w_mod": np.ascontiguousarray(inp["w_mod"][l]),
        "bmod2": np.ascontiguousarray(np.stack([inp["b_mod"][l], inp["b_mod"][l]], 0)),
        "w_in": np.ascontiguousarray(np.asarray(inp["w_in"][l])[:, _win_perm(half)]),
        "g1row": np.ascontiguousarray(np.asarray(inp["norm1_g"][l])[None, :]),
    }


def _rstd(fw, ss, b_ss, n):
    fw.op("dve", lambda e: e.tensor_scalar(out=ss, in0=ss, scalar1=1.0 / n, scalar2=EPS, op0=ALU.mult, op1=ALU.add),
          reads=[b_ss], writes=[b_ss])
    fw.op("act", lambda e: e.sqrt(out=ss, in_=ss), reads=[b_ss], writes=[b_ss])
    fw.op("dve", lambda e: e.reciprocal(out=ss, in_=ss), reads=[b_ss], writes=[b_ss])


def phase_attn(fw, cfg, l, S, A, kind, need_ctx, bg=None):
    nc = fw.nc
    To, Tc = cfg.T_OWN, cfg.T_CTX
    if kind == "a":
        Hq, Hkv, cq0, ckv0, orow = 8, 2, C_AQ, C_AKV, 0
    else:
        Hq, Hkv, cq0, ckv0, orow = 4, 2, C_CQ, C_CKV, 1536
    G = Hq // Hkv
    scale = 128.0 ** -0.5
    QB = min(512, To)
    nsub = QB // 128
    ktiles = [(cfg.O_OWN + i * 128, i * 128) for i in range(cfg.nt_own)]
    if kind == "a":
        ktiles += [(cfg.O_OTH + i * 128, To + i * 128) for i in range(cfg.nt_oth)]
    else:
        ktiles += [(cfg.O_OTH, To)]
    n_lat_k = len(ktiles)
    ktiles += [(cfg.O_CTX + i * 128, None) for i in range(cfg.nt_ctx)]
    nkt = len(ktiles)
    qtiles = [(cfg.O_OWN + i * 128, i * 128) for i in range(cfg.nt_own)]
    if need_ctx:
        qtiles += [(cfg.O_CTX + i * 128, None) for i in range(cfg.nt_ctx)]
    nqt = len(qtiles)
    with Phase(fw, "at" + kind) as ph:
        ident = ph.sb([128, 128], BF16)
        b_id = make_ident(fw, ident)
        KT = ph.sb([128, Hkv, nkt * 128], BF16)
        VA = ph.sb([128, nkt, Hkv, 129], BF16)
        QT = ph.sb([128, Hq, nqt * 128], BF16)
        b_KT, b_VA, b_QT = bufs(nkt), bufs(nkt), bufs(nqt)
        b_va1 = Buf()
        fw.op("pool", lambda e: e.memset(VA[:, :, :, 128:129], 1.0), writes=[b_va1])
        gq = ph.sb([128, 128], F32)
        gk = ph.sb([128, 128], F32)
        b_g = Buf()
        b_g2 = Buf()
        if kind == "a":
            load_bc(fw, "sp", gq[:, :], A["a_qn_g"][l:l + 1, :], b_g)
            load_bc(fw, "sp", gk[:, :], A["a_kn_g"][l:l + 1, :], b_g2)
            fw.op("dve", lambda e: e.tensor_scalar(out=gq[:, :], in0=gq[:, :], scalar1=scale, scalar2=None, op0=ALU.mult),
                  reads=[b_g, b_g2], writes=[b_g])
        esink = ph.sb([128, 4], F32)
        b_es = Buf()
        if kind == "c":
            load_bc(fw, "sp", esink[:, :], A["c_sink"][l:l + 1, :], b_es)
            fw.op("act", lambda e: e.activation(out=esink[:, :], in_=esink[:, :], func=AF.Exp), reads=[b_es], writes=[b_es])
        masks = {}
        b_mask = Buf()
        if kind == "c":
            mk_t = ph.sb([128, nsub + 2, QB], BF16)
            fw.op("pool", lambda e: e.memset(mk_t[:, :, :], 1.0), writes=[b_mask])
            for r in range(-1, nsub + 1):
                mv = mk_t[:, r + 1, :]
                fw.op("pool", lambda e, mv=mv, r=r: e.affine_select(out=mv, in_=mv, pattern=[[1, QB]], compare_op=ALU.is_ge,
                                                                    fill=0.0, base=128 - r * 128, channel_multiplier=-1),
                      reads=[b_mask], writes=[b_mask])
                fw.op("pool", lambda e, mv=mv, r=r: e.affine_select(out=mv, in_=mv, pattern=[[-1, QB]], compare_op=ALU.is_ge,
                                                                    fill=0.0, base=128 + r * 128, channel_multiplier=1),
                      reads=[b_mask], writes=[b_mask])
                masks[r] = mv
        HM = max(Hq, 2 * Hkv)
        ld = [ph.sb([128, HM * 128], BF16) for _ in range(2)]
        b_ld = bufs(2)
        rp = [ph.sb([128, 128], F32) for _ in range(2)]
        b_rp = bufs(2)
        x32 = ph.sb([128, Hq * 128], F32)
        sq = ph.sb([128, Hq * 128], F32)
        t1 = ph.sb([128, Hq * 64], F32)
        t2 = ph.sb([128, Hq * 64], F32)
        xr = [ph.sb([128, Hq * 128], BF16) for _ in range(2)]
        b_x32, b_sq, b_t1, b_t2 = bufs(4)
        b_xr = bufs(2)
        ssn = ph.sb([128, Hq], F32)
        b_ssn = Buf()
        ptr = [ph.ps([128, 512], BF16) for _ in range(2)]
        b_ptr = bufs(2)
        cnt = {"ld": 0, "tr": 0, "xr": 0}

        def prep(row, rrow, c0, H, gt, dests, vdest=None):
            s = cnt["ld"] % 2
            cnt["ld"] += 1
            w = H * 128 + (Hkv * 128 if vdest is not None else 0)
            fw.dma("sp", ld[s][:, 0:w], S.P[row:row + 128, c0:c0 + w], writes=[b_ld[s]])
            if rrow is not None:
                fw.dma("sp", rp[s][:, :], A["rope"][rrow:rrow + 128, :], writes=[b_rp[s]])
            if vdest is not None:
                fw.op("pool", lambda e: e.tensor_copy(out=vdest[0], in_=ld[s][:, H * 128:w].rearrange("p (h d) -> p h d", h=Hkv)),
                      reads=[b_ld[s], b_va1], writes=[vdest[1]])
            xs = x32[:, 0:H * 128]
            if gt is not None:
                fw.op("act", lambda e: e.copy(out=xs, in_=ld[s][:, 0:H * 128]), reads=[b_ld[s]], writes=[b_x32])
                fw.op("dve", lambda e: e.tensor_tensor(out=sq[:, 0:H * 128], in0=xs, in1=xs, op=ALU.mult),
                      reads=[b_x32], writes=[b_sq])
                fw.op("dve", lambda e: e.tensor_reduce(out=ssn[:, 0:H], in_=sq[:, 0:H * 128].rearrange("p (h d) -> p h d", h=H),
                                                       axis=AX.X, op=ALU.add), reads=[b_sq], writes=[b_ssn])
                _rstd(fw, ssn[:, 0:H], b_ssn, 128)
                x3 = xs.rearrange("p (h d) -> p h d", h=H)
                fw.op("dve", lambda e: e.tensor_tensor(out=x3, in0=x3, in1=ssn[:, 0:H].unsqueeze(2).to_broadcast([128, H, 128]),
                                                       op=ALU.mult), reads=[b_x32, b_ssn], writes=[b_x32])
                fw.op("dve", lambda e: e.tensor_tensor(out=x3, in0=x3, in1=gt[:, :].unsqueeze(1).to_broadcast([128, H, 128]),
                                                       op=ALU.mult), reads=[b_x32, b_g], writes=[b_x32])
            else:
                sc = scale if dests[0][2] == "q" else 1.0
                fw.op("act", lambda e: e.activation(out=xs, in_=ld[s][:, 0:H * 128], func=AF.Copy, scale=sc),
                      reads=[b_ld[s]], writes=[b_x32])
            xi = cnt["xr"] % 2
            cnt["xr"] += 1
            xo = xr[xi][:, 0:H * 128]
            if rrow is not None:
                x5 = xs.rearrange("p (h a b f) -> p h a b f", h=H, a=2, b=2)
                o5 = xo.rearrange("p (h a b f) -> p h a b f", h=H, a=2, b=2)
                cs = rp[s][:, 0:64].rearrange("p (a f) -> p a f", a=2).unsqueeze(1).to_broadcast([128, H, 2, 32])
                sn = rp[s][:, 64:128].rearrange("p (a f) -> p a f", a=2).unsqueeze(1).to_broadcast([128, H, 2, 32])
                x1, x2 = x5[:, :, :, 0, :], x5[:, :, :, 1, :]
                u1 = t1[:, 0:H * 64].rearrange("p (h a f) -> p h a f", h=H, a=2)
                u2 = t2[:, 0:H * 64].rearrange("p (h a f) -> p h a f", h=H, a=2)
                fw.op("dve", lambda e: e.tensor_tensor(out=u1, in0=x1, in1=cs, op=ALU.mult), reads=[b_x32, b_rp[s]], writes=[b_t1])
                fw.op("dve", lambda e: e.tensor_tensor(out=u2, in0=x2, in1=sn, op=ALU.mult), reads=[b_x32, b_rp[s]], writes=[b_t2])
                fw.op("dve", lambda e: e.tensor_tensor(out=o5[:, :, :, 0, :], in0=u1, in1=u2, op=ALU.subtract),
                      reads=[b_t1, b_t2], writes=[b_xr[xi]])
                fw.op("dve", lambda e: e.tensor_tensor(out=u1, in0=x2, in1=cs, op=ALU.mult), reads=[b_x32, b_rp[s]], writes=[b_t1])
                fw.op("dve", lambda e: e.tensor_tensor(out=u2, in0=x1, in1=sn, op=ALU.mult), reads=[b_x32, b_rp[s]], writes=[b_t2])
                fw.op("dve", lambda e: e.tensor_tensor(out=o5[:, :, :, 1, :], in0=u1, in1=u2, op=ALU.add),
                      reads=[b_t1, b_t2], writes=[b_xr[xi]])
            else:
                fw.op("dve", lambda e: e.tensor_copy(out=xo, in_=xs), reads=[b_x32], writes=[b_xr[xi]])
            for h0 in range(0, H, 4):
                hn = min(4, H - h0)
                p = cnt["tr"] % 2
                cnt["tr"] += 1

                def tr(e):
                    for hh in range(hn):
                        ins = e.transpose(ptr[p][:, hh * 128:(hh + 1) * 128], xo[:, (h0 + hh) * 128:(h0 + hh + 1) * 128], ident[:, :])
                    return ins
                fw.op("pe", tr, reads=[b_xr[xi], b_id], writes=[b_ptr[p]])
                fw.op("act", lambda e: e.copy(out=dests[0][0][:, h0:h0 + hn, dests[0][3]:dests[0][3] + 128],
                                              in_=ptr[p][:, 0:hn * 128].rearrange("p (h t) -> p h t", h=hn)),
                      reads=[b_ptr[p]], writes=[dests[0][1]])

        for ki, (row, rrow) in enumerate(ktiles):
            prep(row, rrow, ckv0, Hkv, gk if kind == "a" else None, [(KT, b_KT[ki], "k", ki * 128)],
                 vdest=(VA[:, ki, :, 0:128], b_VA[ki]))
        for qi, (row, rrow) in enumerate(qtiles):
            prep(row, rrow, cq0, Hq, gq if kind == "a" else None, [(QT, b_QT[qi], "q", qi * 128)])

        if getattr(S, "dbg", None) and kind in S.dbg:
            dq = nc.dram_tensor("dbgQT" + kind, [128, Hq, nqt * 128], BF16, kind="ExternalOutput").ap()
            dk = nc.dram_tensor("dbgKT" + kind, [128, Hkv, nkt * 128], BF16, kind="ExternalOutput").ap()
            dv = nc.dram_tensor("dbgVA" + kind, [128, nkt, Hkv, 129], BF16, kind="ExternalOutput").ap()
            fw.dma("sp", dq, QT[:, :, :], reads=b_QT)
            fw.dma("sp", dk, KT[:, :, :], reads=b_KT)
            fw.dma("sp", dv, VA[:, :, :, :], reads=b_VA + [b_va1])
        pst = [ph.ps([128, 512]) for _ in range(2)]
        b_pst = bufs(2)
        po = [ph.ps([128, 512]) for _ in range(4)]
        b_po = bufs(4)
        PT = [ph.sb([128, 512], BF16) for _ in range(3)]
        b_PT = bufs(3)
        rden = ph.sb([128, 4], F32)
        b_rden = Buf()
        on = [ph.sb([128, 4, 128], BF16) for _ in range(2)]
        b_on = bufs(2)
        ost = [ph.sb([128, 512], BF16) for _ in range(2)]
        b_ost = bufs(2)
        it = {"s": 0, "p": 0, "o": 0}
        qblocks = []
        for q0 in range(0, To, QB):
            i0 = q0 // 128
            if kind == "a":
                kl = [(ki, None) for ki in range(nkt)]
            else:
                kl = []
                for r in range(-1, nsub + 1):
                    kt = i0 + r
                    if 0 <= kt <= cfg.nt_own:
                        kl.append((kt, r))
                kl += [(n_lat_k + i, None) for i in range(cfg.nt_ctx)]
            qblocks.append((q0, nsub, kl, q0))
        if need_ctx:
            qblocks.append((To, cfg.nt_ctx, [(n_lat_k + i, None) for i in range(cfg.nt_ctx)], To))
        bgen = bg(ph) if bg is not None else None
        for h in range(Hq):
            g = h // G
            for (q0, ns, kl, oc0) in qblocks:
                qw = ns * 128
                if bgen is not None:
                    next(bgen, None)
                def emit_s(ii):
                    ki_, r_ = kl[ii]
                    sp__ = it["s"] % 2
                    it["s"] += 1
                    fw.op("pe", lambda e: e.matmul(pst[sp__][:, 0:qw], lhsT=KT[:, g, ki_ * 128:(ki_ + 1) * 128],
                                                   rhs=QT[:, h, q0:q0 + qw], start=True, stop=True),
                          reads=[b_KT[ki_]] + [b_QT[q0 // 128 + j] for j in range(ns)], writes=[b_pst[sp__]])
                    return sp__
                sq_ = [emit_s(0)]
                for idx, (ki, r) in enumerate(kl):
                    if idx + 1 < len(kl):
                        sq_.append(emit_s(idx + 1))
                    sp_ = sq_[idx]
                    pi_ = it["p"] % 3
                    it["p"] += 1
                    fw.op("act", lambda e: e.activation(out=PT[pi_][:, 0:qw], in_=pst[sp_][:, 0:qw], func=AF.Exp),
                          reads=[b_pst[sp_]], writes=[b_PT[pi_]])
                    if r is not None:
                        fw.op("pool", lambda e: e.tensor_tensor(out=PT[pi_][:, 0:qw], in0=PT[pi_][:, 0:qw], in1=masks[r][:, 0:qw],
                                                                op=ALU.mult), reads=[b_PT[pi_], b_mask], writes=[b_PT[pi_]])

                    def pv(e):
                        ins = None
                        for j in range(ns):
                            if r is not None and abs(r - j) > 1:
                                continue
                            first = (idx == 0) if r is None else (ki == max(0, q0 // 128 + j - 1))
                            ins = e.matmul(po[j][:, 0:129], lhsT=PT[pi_][:, j * 128:(j + 1) * 128], rhs=VA[:, ki, g, :],
                                           start=first, stop=(idx == len(kl) - 1))
                        return ins
                    fw.op("pe", pv, reads=[b_PT[pi_], b_VA[ki], b_va1], writes=b_po[0:ns])
                oi = it["o"] % 2
                it["o"] += 1
                for j in range(ns):
                    if kind == "c":
                        fw.op("dve", lambda e: e.tensor_scalar(out=rden[:, j:j + 1], in0=po[j][:, 128:129],
                                                               scalar1=esink[:, h:h + 1], scalar2=None, op0=ALU.add),
                              reads=[b_po[j], b_es], writes=[b_rden])
                        fw.op("dve", lambda e: e.reciprocal(out=rden[:, j:j + 1], in_=rden[:, j:j + 1]),
                              reads=[b_rden], writes=[b_rden])
                    else:
                        fw.op("dve", lambda e: e.reciprocal(out=rden[:, j:j + 1], in_=po[j][:, 128:129]),
                              reads=[b_po[j]], writes=[b_rden])
                    fw.op("dve", lambda e: e.tensor_scalar(out=on[oi][:, j, :], in0=po[j][:, 0:128], scalar1=rden[:, j:j + 1],
                                                           scalar2=None, op0=ALU.mult),
                          reads=[b_po[j], b_rden], writes=[b_on[oi]])
                p = cnt["tr"] % 2
                cnt["tr"] += 1

                def tr2(e):
                    for j in range(ns):
                        ins = e.transpose(ptr[p][:, j * 128:(j + 1) * 128], on[oi][:, j, :], ident[:, :])
                    return ins
                fw.op("pe", tr2, reads=[b_on[oi], b_id], writes=[b_ptr[p]])
                fw.op("act", lambda e: e.copy(out=ost[oi][:, 0:qw], in_=ptr[p][:, 0:qw]), reads=[b_ptr[p]], writes=[b_ost[oi]])
                fw.dma("sp", S.OT[orow + h * 128:orow + (h + 1) * 128, oc0:oc0 + qw], ost[oi][:, 0:qw], reads=[b_ost[oi]])
        if bgen is not None:
            for _ in bgen:
                pass


def rope_table(cfg, half):
    n = 2 * cfg.T_OWN
    t = np.arange(n)
    if half == 1:
        t = n - 1 - t
    rows = (t // 64).astype(np.float32)
    cols = (t % 64).astype(np.float32)
    inv = (10000.0 ** (-np.arange(0, 64, 2, dtype=np.float32) / 64.0)).astype(np.float32)
    ang = np.concatenate([rows[:, None] * inv, cols[:, None] * inv], axis=-1).astype(np.float32)
    return np.ascontiguousarray(np.concatenate([np.cos(ang), np.sin(ang)], axis=1).astype(np.float32))


def phase_mlstm(fw, cfg, l, S, A, need_ctx, bg=None):
    nc = fw.nc
    To, Tc, TA = cfg.T_OWN, cfg.T_CTX, cfg.T_ALL
    NT = TA // 128
    T_OC = To + Tc
    kscale = 128.0 ** -0.5
    with Phase(fw, "ml") as ph:
        identB = ph.sb([128, 128], BF16)
        b_idB = make_ident(fw, identB)
        identF = ph.sb([128, 128], F32)
        b_idF = make_ident(fw, identF)
        ones = ph.sb([128, 128], F32)
        U = [ph.sb([128, 128], F32) for _ in range(2)]
        NEGM = [ph.sb([128, 128], F32) for _ in range(2)]
        Sel = [ph.sb([128, 128], F32) for _ in range(2)]
        Bsel = ph.sb([64, 4], F32)
        b_c = Buf()
        fw.op("pool", lambda e: e.memset(ones[:, :], 1.0), writes=[b_c])
        for d in range(2):
            fw.op("pool", lambda e: e.memset(U[d][:, :], 1.0), writes=[b_c])
            fw.op("pool", lambda e: e.memset(NEGM[d][:, :], 0.0), writes=[b_c])
            fw.op("pool", lambda e: e.memset(Sel[d][:, :], 1.0), writes=[b_c])
        fw.op("pool", lambda e: e.memset(Bsel[:, :], 0.0), writes=[b_c])
        sg = [1, -1]
        for d in range(2):
            fw.op("pool", lambda e: e.affine_select(out=U[d][:, :], in_=U[d][:, :], pattern=[[sg[d], 128]], compare_op=ALU.is_ge,
                                                    fill=0.0, base=0, channel_multiplier=-sg[d]), reads=[b_c], writes=[b_c])
            fw.op("pool", lambda e: e.affine_select(out=NEGM[d][:, :], in_=NEGM[d][:, :], pattern=[[-sg[d], 128]],
                                                    compare_op=ALU.is_ge, fill=NEG, base=0, channel_multiplier=sg[d]),
                  reads=[b_c], writes=[b_c])
            last = 127 if d == 0 else 0
            fw.op("pool", lambda e: e.affine_select(out=Sel[d][:, :], in_=Sel[d][:, :], pattern=[[0, 128]], compare_op=ALU.is_equal,
                                                    fill=0.0, base=-last, channel_multiplier=1), reads=[b_c], writes=[b_c])
        for base in (0, -32):
            fw.op("pool", lambda e: e.affine_select(out=Bsel[:, :], in_=Bsel[:, :], pattern=[[-1, 4]], compare_op=ALU.not_equal,
                                                    fill=1.0, base=base, channel_multiplier=1), reads=[b_c], writes=[b_c])
        G = ph.sb([128, NT, 16], F32)
        IC = ph.sb([128, NT, 8], F32)
        LF = ph.sb([128, NT, 8], F32)
        gb = ph.sb([128, 16], F32)
        b_G, b_gate, b_gb, b_gb2 = bufs(4)
        fw.dma("sp", G[:, :, :], S.MG.rearrange("(t p) c -> p t c", p=128), writes=[b_G])
        load_bc(fw, "sp", gb[:, 0:8], A["m_ig_b"][l:l + 1, :], b_gb)
        load_bc(fw, "sp", gb[:, 8:16], A["m_fg_b"][l:l + 1, :], b_gb2)
        fw.op("dve", lambda e: e.tensor_tensor(out=IC[:, :, :], in0=G[:, :, 0:8], in1=gb[:, 0:8].unsqueeze(1).to_broadcast([128, NT, 8]),
                                               op=ALU.add), reads=[b_G, b_gb], writes=[b_gate])
        fw.op("dve", lambda e: e.tensor_tensor(out=LF[:, :, :], in0=G[:, :, 8:16], in1=gb[:, 8:16].unsqueeze(1).to_broadcast([128, NT, 8]),
                                               op=ALU.add), reads=[b_G, b_gb2, b_gate], writes=[b_gate])
        fw.op("act", lambda e: e.activation(out=LF[:, :, :], in_=LF[:, :, :], func=AF.Exp, scale=-1.0), reads=[b_gate], writes=[b_gate])
        fw.op("act", lambda e: e.activation(out=LF[:, :, :], in_=LF[:, :, :], func=AF.Ln, bias=1.0), reads=[b_gate], writes=[b_gate])
        fw.op("dve", lambda e: e.tensor_scalar(out=LF[:, :, :], in0=LF[:, :, :], scalar1=-1.0, scalar2=None, op0=ALU.mult),
              reads=[b_gate], writes=[b_gate])
        qT = ph.sb([128, 4, T_OC], BF16)
        kT = ph.sb([128, 4, T_OC], BF16)
        Ktm = ph.sb([128, NT, 4, 128], BF16)
        VA = ph.sb([128, NT, 4, 129], BF16)
        b_qT, b_kT, b_Ktm, b_VA = bufs(4)
        fw.op("pool", lambda e: e.memset(VA[:, :, :, 128:129], 1.0), writes=[b_VA])
        for h in range(4):
            fw.dma("sp", VA[:, :, h, 0:128], S.P[:, C_MV + h * 128:C_MV + (h + 1) * 128].rearrange("(t p) d -> p t d", p=128),
                   reads=[b_VA], writes=[b_VA])
        cw = ph.sb([128, 8, 3], F32)
        b_cw = Buf()
        fw.dma("sp", cw[:, :, :].rearrange("p c k -> p (c k)"), A["m_conv"][l], writes=[b_cw])
        with Phase(fw, "mlc") as pc:
            X = [pc.sb([128, TA], BF16) for _ in range(2)]
            acc = [pc.sb([128, TA], F32) for _ in range(2)]
            ktmp = pc.sb([128, TA], BF16)
            b_X, b_acc = bufs(2), bufs(2)
            b_ktmp = Buf()
            ptr = [pc.ps([128, 512], BF16) for _ in range(2)]
            b_ptr = bufs(2)
            n = 0
            tp = 0
            for qk in range(2):
                src = S.MQT if qk == 0 else S.MKT
                for hc in range(4):
                    s = n % 2
                    n += 1
                    if qk == 0:
                        fw.dma("sp", X[s][:, 0:To + 128], src[hc * 128:(hc + 1) * 128, 0:To + 128], writes=[b_X[s]])
                        fw.dma("sp", X[s][:, 2 * To:TA], src[hc * 128:(hc + 1) * 128, 2 * To:TA], reads=[b_X[s]], writes=[b_X[s]])
                    else:
                        fw.dma("sp", X[s][:, :], src[hc * 128:(hc + 1) * 128, :], writes=[b_X[s]])
                    w = cw[:, qk * 4 + hc, :]
                    for (a, b) in (((0, To + 128) if qk == 0 else (0, 2 * To)), (2 * To, TA)):
                        fw.op("dve", lambda e: e.tensor_scalar(out=acc[s][:, a:b], in0=X[s][:, a:b], scalar1=w[:, 1:2], scalar2=None,
                                                               op0=ALU.mult), reads=[b_X[s], b_cw], writes=[b_acc[s]])
                        fw.op("dve", lambda e: e.scalar_tensor_tensor(out=acc[s][:, a + 1:b], in0=X[s][:, a:b - 1], scalar=w[:, 0:1],
                                                                      in1=acc[s][:, a + 1:b], op0=ALU.mult, op1=ALU.add),
                              reads=[b_X[s], b_cw, b_acc[s]], writes=[b_acc[s]])
                        fw.op("dve", lambda e: e.scalar_tensor_tensor(out=acc[s][:, a:b - 1], in0=X[s][:, a + 1:b], scalar=w[:, 2:3],
                                                                      in1=acc[s][:, a:b - 1], op0=ALU.mult, op1=ALU.add),
                              reads=[b_X[s], b_cw, b_acc[s]], writes=[b_acc[s]])
                    if qk == 0:
                        fw.op("act", lambda e: e.activation(out=qT[:, hc, 0:To], in_=acc[s][:, 0:To], func=AF.Silu),
                              reads=[b_acc[s]], writes=[b_qT])
                        fw.op("act", lambda e: e.activation(out=qT[:, hc, To:T_OC], in_=acc[s][:, 2 * To:TA], func=AF.Silu),
                              reads=[b_acc[s]], writes=[b_qT])
                    else:
                        fw.op("act", lambda e: e.activation(out=acc[s][:, :], in_=acc[s][:, :], func=AF.Silu),
                              reads=[b_acc[s]], writes=[b_acc[s]])
                        fw.op("dve", lambda e: e.tensor_scalar(out=ktmp[:, :], in0=acc[s][:, :], scalar1=kscale, scalar2=None,
                                                               op0=ALU.mult), reads=[b_acc[s]], writes=[b_ktmp])
                        fw.op("pool", lambda e: e.tensor_copy(out=kT[:, hc, 0:To], in_=ktmp[:, 0:To]), reads=[b_ktmp], writes=[b_kT])
                        fw.op("pool", lambda e: e.tensor_copy(out=kT[:, hc, To:T_OC], in_=ktmp[:, 2 * To:TA]), reads=[b_ktmp], writes=[b_kT])
                        for t0 in range(0, NT, 4):
                            tn = min(4, NT - t0)
                            p = tp % 2
                            tp += 1

                            def tr(e):
                                for tt in range(tn):
                                    ins = e.transpose(ptr[p][:, tt * 128:(tt + 1) * 128], ktmp[:, (t0 + tt) * 128:(t0 + tt + 1) * 128],
                                                      identB[:, :])
                                return ins
                            fw.op("pe", tr, reads=[b_ktmp, b_idB], writes=[b_ptr[p]])
                            fw.op("act", lambda e: e.copy(out=Ktm[:, t0:t0 + tn, hc, :],
                                                          in_=ptr[p][:, 0:tn * 128].rearrange("p (t d) -> p t d", t=tn)),
                                  reads=[b_ptr[p]], writes=[b_Ktm])
        pA = ph.ps([128, 512])
        pC = ph.ps([128, 512])
        pD = ph.ps([128, 512])
        pE = ph.ps([128, 512], BF16)
        pF = ph.ps([128, 512])
        pG = ph.ps([128, 512])
        b_pA, b_pC, b_pD, b_pE, b_pF, b_pG = bufs(6)
        Cst = [ph.sb([128, 4, 129], F32) for _ in range(2)]
        Cb = [ph.sb([128, 4, 129], BF16) for _ in range(2)]
        mst = [ph.sb([128, 4], F32) for _ in range(2)]
        b_C, b_Cb, b_m = bufs(2), bufs(2), bufs(2)
        for d in range(2):
            fw.op("pool", lambda e: e.memset(Cst[d][:, :, :], 0.0), writes=[b_C[d]])
            fw.op("pool", lambda e: e.memset(Cb[d][:, :, :], 0.0), writes=[b_Cb[d]])
            fw.op("pool", lambda e: e.memset(mst[d][:, :], NEG), writes=[b_m[d]])
        AB = ph.sb([128, 64], F32)
        R64 = ph.sb([64, 128], F32)
        X64 = ph.sb([64, 128], F32)
        Lall = ph.sb([64, 4, 128], F32)
        b_AB, b_R64, b_X64, b_Lall = bufs(4)
        fw.op("pool", lambda e: e.memset(AB[:, :], 0.0), writes=[b_AB])
        fw.op("pool", lambda e: e.memset(R64[:, :], 1.0), writes=[b_R64])
        fw.op("pool", lambda e: e.memset(X64[:, :], 1.0), writes=[b_X64])
        bsb = ph.sb([128, 4], F32)
        btot = ph.sb([128, 4], F32)
        bm = ph.sb([128, 4], F32)
        rowmax = ph.sb([128, 4], F32)
        mrow = ph.sb([128, 4], F32)
        E8 = ph.sb([128, 8], F32)
        small = ph.sb([128, 16], F32)
        mnew = ph.sb([128, 4], F32)
        ld = ph.sb([128, 4, 128], F32)
        Dm = ph.sb([128, 4, 128], F32)
        Wb = ph.sb([128, 4, 128], BF16)
        WT = ph.sb([128, 4, 128], BF16)
        kw = ph.sb([128, 4, 128], BF16)
        numt = ph.sb([128, 4, 129], F32)
        hout = [ph.sb([128, 4, 128], F32) for _ in range(2)]
        tmpC = ph.sb([128, 4, 129], F32)
        (b_bsb, b_btot, b_bm, b_rowmax, b_mrow, b_E8, b_small, b_mnew, b_ld, b_Dm, b_Wb, b_WT, b_kw, b_numt,
         b_tmpC) = bufs(15)
        b_hout = bufs(2)
        hcnt = [0]

        def v2(t):
            return t[:, 0:258].rearrange("p (h e) -> p h e", h=2)

        def step(d, ti, full, oc_tile):
            lf = LF[:, ti, d * 4:(d + 1) * 4]
            ic = IC[:, ti, d * 4:(d + 1) * 4]
            m = mst[d]

            def mm_b(e):
                e.matmul(pA[:, 0:4], lhsT=U[d][:, :], rhs=lf, start=True, stop=True)
                return e.matmul(pA[:, 4:8], lhsT=ones[:, :], rhs=lf, start=True, stop=True)
            fw.op("pe", mm_b, reads=[b_gate, b_c], writes=[b_pA])
            fw.op("dve", lambda e: e.tensor_copy(out=AB[:, 32:36], in_=pA[:, 0:4]), reads=[b_pA], writes=[b_AB, b_bsb])
            fw.op("dve", lambda e: e.tensor_tensor(out=AB[:, 0:4], in0=ic, in1=pA[:, 0:4], op=ALU.subtract),
                  reads=[b_pA, b_gate], writes=[b_AB])
            fw.op("act", lambda e: e.copy(out=btot[:, :], in_=pA[:, 4:8]), reads=[b_pA], writes=[b_btot])
            fw.op("dve", lambda e: e.tensor_tensor(out=bm[:, :], in0=AB[:, 32:36], in1=m[:, :], op=ALU.add),
                  reads=[b_AB, b_m[d]], writes=[b_bm])
            fw.op("pe", lambda e: e.transpose(pA[0:64, 16:144], AB[:, :], identF[:, :]), reads=[b_AB, b_idF], writes=[b_pA])
            fw.op("act", lambda e: e.copy(out=R64[0:32, :], in_=pA[0:32, 16:144]), reads=[b_pA], writes=[b_R64])
            fw.op("dve", lambda e: e.tensor_copy(out=X64[32:64, :], in_=pA[32:64, 16:144]), reads=[b_pA], writes=[b_X64])
            fw.op("dve", lambda e: e.tensor_tensor(out=Lall[:, :, :], in0=X64[:, :].unsqueeze(1).to_broadcast([64, 4, 128]),
                                                   in1=Bsel[:, :].unsqueeze(2).to_broadcast([64, 4, 128]), op=ALU.mult),
                  reads=[b_X64, b_c], writes=[b_Lall])

            def mm_ld(e):
                for h in range(4):
                    ins = e.matmul(pC[:, h * 128:(h + 1) * 128], lhsT=Lall[:, h, :], rhs=R64[:, :], start=True, stop=True)
                return ins
            fw.op("pe", mm_ld, reads=[b_Lall, b_R64], writes=[b_pC])
            fw.op("dve", lambda e: e.tensor_tensor(out=ld[:, :, :], in0=pC[:, :].rearrange("p (h s) -> p h s", h=4),
                                                   in1=NEGM[d][:, :].unsqueeze(1).to_broadcast([128, 4, 128]), op=ALU.add),
                  reads=[b_pC, b_c], writes=[b_ld])
            fw.op("dve", lambda e: e.tensor_reduce(out=rowmax[:, :], in_=ld[:, :, :], axis=AX.X, op=ALU.max),
                  reads=[b_ld], writes=[b_rowmax])
            if full:
                fw.op("dve", lambda e: e.tensor_tensor(out=mrow[:, :], in0=bm[:, :], in1=rowmax[:, :], op=ALU.max),
                      reads=[b_bm, b_rowmax], writes=[b_mrow])
                fw.op("dve", lambda e: e.tensor_tensor(out=ld[:, :, :], in0=ld[:, :, :],
                                                       in1=mrow[:, :].unsqueeze(2).to_broadcast([128, 4, 128]), op=ALU.subtract),
                      reads=[b_ld, b_mrow], writes=[b_ld])
                fw.op("dve", lambda e: e.tensor_scalar_max(out=ld[:, :, :], in0=ld[:, :, :], scalar1=-80.0), reads=[b_ld], writes=[b_ld])
                fw.op("act", lambda e: e.activation(out=Dm[:, :, :], in_=ld[:, :, :], func=AF.Exp), reads=[b_ld], writes=[b_Dm])
                oc0 = oc_tile * 128

                def mm_s(e):
                    for h in range(4):
                        ins = e.matmul(pD[:, h * 128:(h + 1) * 128], lhsT=qT[:, h, oc0:oc0 + 128], rhs=kT[:, h, oc0:oc0 + 128],
                                       start=True, stop=True)
                    return ins
                fw.op("pe", mm_s, reads=[b_qT, b_kT], writes=[b_pD])
                fw.op("dve", lambda e: e.tensor_tensor(out=Wb[:, :, :], in0=pD[:, :].rearrange("p (h s) -> p h s", h=4), in1=Dm[:, :, :],
                                                       op=ALU.mult), reads=[b_pD, b_Dm], writes=[b_Wb])

                def mm_t(e):
                    for h in range(4):
                        ins = e.transpose(pE[:, h * 128:(h + 1) * 128], Wb[:, h, :], identB[:, :])
                    return ins
                fw.op("pe", mm_t, reads=[b_Wb, b_idB], writes=[b_pE])
                fw.op("act", lambda e: e.copy(out=WT[:, :, :], in_=pE[:, :].rearrange("p (h j) -> p h j", h=4)), reads=[b_pE], writes=[b_WT])

                def mm_intra(e):
                    for h in range(4):
                        ins = e.matmul(v2(pF if h < 2 else pG)[:, h % 2, :], lhsT=WT[:, h, :], rhs=VA[:, ti, h, :], start=True, stop=True)
                    return ins
                fw.op("pe", mm_intra, reads=[b_WT, b_VA], writes=[b_pF, b_pG])

                def mm_inter(e):
                    for h in range(4):
                        ins = e.matmul(v2(pC if h < 2 else pD)[:, h % 2, :], lhsT=qT[:, h, oc0:oc0 + 128], rhs=Cb[d][:, h, :],
                                       start=True, stop=True)
                    return ins
                fw.op("pe", mm_inter, reads=[b_qT, b_Cb[d]], writes=[b_pC, b_pD])
                fw.op("dve", lambda e: e.tensor_tensor(out=E8[:, 0:4], in0=bm[:, :], in1=mrow[:, :], op=ALU.subtract),
                      reads=[b_bm, b_mrow], writes=[b_E8])
                fw.op("dve", lambda e: e.tensor_scalar(out=E8[:, 4:8], in0=mrow[:, :], scalar1=-1.0, scalar2=None, op0=ALU.mult),
                      reads=[b_mrow, b_E8], writes=[b_E8])
                fw.op("dve", lambda e: e.tensor_scalar_max(out=E8[:, :], in0=E8[:, :], scalar1=-80.0), reads=[b_E8], writes=[b_E8])
                fw.op("act", lambda e: e.activation(out=E8[:, :], in_=E8[:, :], func=AF.Exp), reads=[b_E8], writes=[b_E8])
                for hp, (pi_, pj_, bi_, bj_) in enumerate(((pC, pF, b_pC, b_pF), (pD, pG, b_pD, b_pG))):
                    hs = slice(hp * 2, hp * 2 + 2)
                    fw.op("dve", lambda e: e.tensor_tensor(out=numt[:, hs, :], in0=v2(pi_),
                                                           in1=E8[:, hs].unsqueeze(2).to_broadcast([128, 2, 129]), op=ALU.mult),
                          reads=[bi_, b_E8], writes=[b_numt])
                    fw.op("dve", lambda e: e.tensor_tensor(out=numt[:, hs, :], in0=numt[:, hs, :], in1=v2(pj_), op=ALU.add),
                          reads=[bj_, b_numt], writes=[b_numt])
                fw.op("dve", lambda e: e.tensor_scalar(out=small[:, 4:8], in0=numt[:, :, 128], scalar1=-1.0, scalar2=None, op0=ALU.mult),
                      reads=[b_numt], writes=[b_small])
                fw.op("dve", lambda e: e.tensor_tensor(out=small[:, 0:4], in0=numt[:, :, 128], in1=small[:, 4:8], op=ALU.max),
                      reads=[b_numt, b_small], writes=[b_small])
                fw.op("dve", lambda e: e.tensor_tensor(out=small[:, 0:4], in0=small[:, 0:4], in1=E8[:, 4:8], op=ALU.max),
                      reads=[b_small, b_E8], writes=[b_small])
                fw.op("dve", lambda e: e.reciprocal(out=small[:, 0:4], in_=small[:, 0:4]), reads=[b_small], writes=[b_small])
                hi = hcnt[0] % 2
                hcnt[0] += 1
                fw.op("dve", lambda e: e.tensor_tensor(out=hout[hi][:, :, :], in0=numt[:, :, 0:128],
                                                       in1=small[:, 0:4].unsqueeze(2).to_broadcast([128, 4, 128]), op=ALU.mult),
                      reads=[b_numt, b_small], writes=[b_hout[hi]])
                fw.dma("sp", S.HD[d, oc0:oc0 + 128, :], hout[hi][:, :, :].rearrange("p h e -> p (h e)"), reads=[b_hout[hi]])
            fw.op("pe", lambda e: e.matmul(pA[:, 8:12], lhsT=Sel[d][:, :], rhs=rowmax[:, :], start=True, stop=True),
                  reads=[b_rowmax, b_c], writes=[b_pA])
            fw.op("dve", lambda e: e.tensor_tensor(out=small[:, 8:12], in0=btot[:, :], in1=m[:, :], op=ALU.add),
                  reads=[b_btot, b_m[d]], writes=[b_small])
            fw.op("dve", lambda e: e.tensor_tensor(out=mnew[:, :], in0=small[:, 8:12], in1=pA[:, 8:12], op=ALU.max),
                  reads=[b_small, b_pA], writes=[b_mnew])
            fw.op("dve", lambda e: e.tensor_tensor(out=E8[:, 4:8], in0=small[:, 8:12], in1=mnew[:, :], op=ALU.subtract),
                  reads=[b_small, b_mnew, b_E8], writes=[b_E8])
            fw.op("dve", lambda e: e.tensor_tensor(out=E8[:, 0:4], in0=AB[:, 0:4], in1=btot[:, :], op=ALU.add),
                  reads=[b_AB, b_btot, b_E8], writes=[b_E8])
            fw.op("dve", lambda e: e.tensor_tensor(out=E8[:, 0:4], in0=E8[:, 0:4], in1=mnew[:, :], op=ALU.subtract),
                  reads=[b_E8, b_mnew], writes=[b_E8])
            fw.op("dve", lambda e: e.tensor_scalar_max(out=E8[:, :], in0=E8[:, :], scalar1=-80.0), reads=[b_E8], writes=[b_E8])
            fw.op("act", lambda e: e.activation(out=E8[:, :], in_=E8[:, :], func=AF.Exp), reads=[b_E8], writes=[b_E8])
            fw.op("dve", lambda e: e.tensor_tensor(out=kw[:, :, :], in0=Ktm[:, ti, :, :],
                                                   in1=E8[:, 0:4].unsqueeze(2).to_broadcast([128, 4, 128]), op=ALU.mult),
                  reads=[b_Ktm, b_E8], writes=[b_kw])

            def mm_dc(e):
                for h in range(4):
                    ins = e.matmul(v2(pF if h < 2 else pG)[:, h % 2, :], lhsT=kw[:, h, :], rhs=VA[:, ti, h, :], start=True, stop=True)
                return ins
            fw.op("pe", mm_dc, reads=[b_kw, b_VA], writes=[b_pF, b_pG])
            fw.op("dve", lambda e: e.tensor_tensor(out=tmpC[:, :, :], in0=Cst[d][:, :, :],
                                                   in1=E8[:, 4:8].unsqueeze(2).to_broadcast([128, 4, 129]), op=ALU.mult),
                  reads=[b_C[d], b_E8], writes=[b_tmpC])
            fw.op("dve", lambda e: e.tensor_tensor(out=Cst[d][:, 0:2, :], in0=tmpC[:, 0:2, :], in1=v2(pF), op=ALU.add),
                  reads=[b_tmpC, b_pF], writes=[b_C[d]])
            fw.op("dve", lambda e: e.tensor_tensor(out=Cst[d][:, 2:4, :], in0=tmpC[:, 2:4, :], in1=v2(pG), op=ALU.add),
                  reads=[b_tmpC, b_pG, b_C[d]], writes=[b_C[d]])
            fw.op("act", lambda e: e.copy(out=Cb[d][:, :, :], in_=Cst[d][:, :, :]), reads=[b_C[d]], writes=[b_Cb[d]])
            fw.op("dve", lambda e: e.tensor_copy(out=m[:, :], in_=mnew[:, :]), reads=[b_mnew], writes=[b_m[d]])

        n_own, n_ctx = cfg.nt_own, cfg.nt_ctx
        t_own0, t_oth0, t_ctx0 = 0, n_own, 2 * n_own
        near = [(t_ctx0 + i, need_ctx, n_own + i) for i in range(n_ctx)] + [(t_own0 + i, True, i) for i in range(n_own)]
        far = [(t_ctx0 + i, need_ctx, n_own + i) for i in reversed(range(n_ctx))]
        far += [(t_oth0 + i, False, None) for i in reversed(range(n_own))]
        far += [(t_own0 + i, True, i) for i in reversed(range(n_own))]
        bgen = bg(ph) if bg is not None else None
        for i in range(max(len(near), len(far))):
            if bgen is not None:
                next(bgen, None)
            if i < len(far):
                step(1, *far[i])
            if i < len(near):
                step(0, *near[i])
        if bgen is not None:
            for _ in bgen:
                pass


def small_inputs(inp, half):
    ig = np.asarray(inp["m_ig_b"]); fg = np.asarray(inp["m_fg_b"]); cv = np.asarray(inp["m_conv"])
    if half == 1:
        ig, fg, cv = ig[:, ::-1, :], fg[:, ::-1, :], cv[:, ::-1, :]
    out = {
        "m_ig_b": np.ascontiguousarray(ig.reshape(DEPTH, 8)), "m_fg_b": np.ascontiguousarray(fg.reshape(DEPTH, 8)),
        "m_conv": np.ascontiguousarray(cv.reshape(DEPTH, 3, 8, 128).transpose(0, 3, 2, 1).reshape(DEPTH, 128, 24)),
    }
    for k in ("a_qn_g", "a_kn_g", "c_sink", "m_norm_g", "norm1_g", "norm2_g", "b_rg", "b_re"):
        out[k] = np.ascontiguousarray(inp[k])
    out["final_g"] = np.ascontiguousarray(np.asarray(inp["final_g"])[None, :])
    return out


def _tiles_oc(cfg, x_own, xc, o_own, o_ctx, need_ctx):
    tl = [(t, t * 128, x_own[t * 128:(t + 1) * 128, :], 0, o_own[t * 128:(t + 1) * 128, :]) for t in range(cfg.nt_own)]
    if need_ctx:
        tl += [(cfg.nt_own + i, cfg.O_CTX + i * 128, xc[i * 128:(i + 1) * 128, :], 1, o_ctx[i * 128:(i + 1) * 128, :])
               for i in range(cfg.nt_ctx)]
    return tl


def phase_merge(fw, cfg, l, S, A, W, x_own, xc, o_own, o_ctx, need_ctx):
    nc = fw.nc
    To, Tc = cfg.T_OWN, cfg.T_CTX
    T_OC = To + Tc
    tiles = _tiles_oc(cfg, x_own, xc, o_own, o_ctx, need_ctx)
    with Phase(fw, "mg1") as ph:
        identB = ph.sb([128, 128], BF16)
        b_id = make_ident(fw, identB)
        gmn = ph.sb([128, 512], F32)
        b_gmn = Buf()
        load_bc(fw, "sp", gmn[:, :], A["m_norm_g"][l:l + 1, :], b_gmn)
        AT = ph.sb([128, 8, T_OC], BF16)
        CT = ph.sb([128, 4, T_OC], BF16)
        b_AT, b_CT = bufs(2)
        fw.dma("sp", AT[:, :, :], S.OT[0:1024, :].rearrange("(k p) t -> p k t", p=128), writes=[b_AT])
        fw.dma("sp", CT[:, :, :], S.OT[1536:2048, :].rearrange("(k p) t -> p k t", p=128), writes=[b_CT])
        Wa = ph.sb([128, 8, D], BF16)
        Wm = ph.sb([128, 4, D], BF16)
        Wc = ph.sb([128, 4, D], BF16)
        b_Wa, b_Wm, b_Wc = bufs(3)
        fw.dma("pool", Wa[:, :, :], W["w_br_a"].rearrange("(k p) c -> p k c", p=128), writes=[b_Wa])
        fw.dma("pool", Wm[:, :, :], W["w_br_m"].rearrange("(k p) c -> p k c", p=128), writes=[b_Wm])
        fw.dma("pool", Wc[:, :, :], W["w_br_c"].rearrange("(k p) c -> p k c", p=128), writes=[b_Wc])
        h0 = [ph.sb([128, 512], F32) for _ in range(2)]
        h1 = [ph.sb([128, 512], F32) for _ in range(2)]
        mo = [ph.sb([128, 512], BF16) for _ in range(2)]
        gg = [ph.sb([128, 3 * D], BF16) for _ in range(2)]
        b_h0, b_h1, b_mo, b_gg = bufs(2), bufs(2), bufs(2), bufs(2)
        sq = ph.sb([128, 512], F32)
        ss = ph.sb([128, 4], F32)
        sig = ph.sb([128, 512], F32)
        omb = ph.sb([128, 512], BF16)
        omT = [ph.sb([128, 4, 128], BF16) for _ in range(2)]
        z32 = ph.sb([128, 512], F32)
        t32 = ph.sb([128, 512], F32)
        zb = [ph.sb([128, D], BF16) for _ in range(2)]
        zTs = [ph.sb([128, NK, 128], BF16) for _ in range(2)]
        b_sq, b_ss, b_sig, b_omb, b_z32, b_t32 = bufs(6)
        b_omT, b_zb, b_zTs = bufs(2), bufs(2), bufs(2)
        ptr = [ph.ps([128, 512], BF16) for _ in range(2)]
        b_ptr = bufs(2)
        pa = [ph.ps([128, 512]) for _ in range(2)]
        pm = [ph.ps([128, 512]) for _ in range(2)]
        pc = [ph.ps([128, 512]) for _ in range(2)]
        b_pa, b_pm, b_pc = bufs(2), bufs(2), bufs(2)
        tp = 0
        pq = 0
        for n, (t, prow, xsrc, m, dst) in enumerate(tiles):
            s = n % 2
            tok = slice(t * 128, (t + 1) * 128)
            fw.dma("sp", h0[s][:, :], S.HD[0, t * 128:(t + 1) * 128, :], writes=[b_h0[s]])
            fw.dma("sp", h1[s][:, :], S.HD[1, t * 128:(t + 1) * 128, :], writes=[b_h1[s]])
            fw.dma("sp", mo[s][:, :], S.P[prow:prow + 128, C_MO:C_MO + 512], writes=[b_mo[s]])
            fw.dma("sp", gg[s][:, :], S.P[prow:prow + 128, C_G:C_G + 3 * D], writes=[b_gg[s]])
            fw.op("dve", lambda e: e.tensor_tensor(out=h0[s][:, :], in0=h0[s][:, :], in1=h1[s][:, :], op=ALU.add),
                  reads=[b_h0[s], b_h1[s]], writes=[b_h0[s]])
            fw.op("dve", lambda e: e.tensor_tensor(out=sq[:, :], in0=h0[s][:, :], in1=h0[s][:, :], op=ALU.mult),
                  reads=[b_h0[s]], writes=[b_sq])
            fw.op("dve", lambda e: e.tensor_reduce(out=ss[:, :], in_=sq[:, :].rearrange("p (h d) -> p h d", h=4), axis=AX.X, op=ALU.add),
                  reads=[b_sq], writes=[b_ss])
            _rstd(fw, ss[:, :], b_ss, 128)
            h3 = h0[s][:, :].rearrange("p (h d) -> p h d", h=4)
            fw.op("dve", lambda e: e.tensor_tensor(out=h3, in0=h3, in1=ss[:, :].unsqueeze(2).to_broadcast([128, 4, 128]), op=ALU.mult),
                  reads=[b_h0[s], b_ss], writes=[b_h0[s]])
            fw.op("dve", lambda e: e.tensor_tensor(out=h0[s][:, :], in0=h0[s][:, :], in1=gmn[:, :], op=ALU.mult),
                  reads=[b_h0[s], b_gmn], writes=[b_h0[s]])
            fw.op("act", lambda e: e.activation(out=sig[:, :], in_=mo[s][:, :], func=AF.Sigmoid), reads=[b_mo[s]], writes=[b_sig])
            fw.op("dve", lambda e: e.tensor_tensor(out=omb[:, :], in0=h0[s][:, :], in1=sig[:, :], op=ALU.mult),
                  reads=[b_h0[s], b_sig], writes=[b_omb])
            p = tp % 2
            tp += 1

            def tr(e):
                for k in range(4):
                    ins = e.transpose(ptr[p][:, k * 128:(k + 1) * 128], omb[:, k * 128:(k + 1) * 128], identB[:, :])
                return ins
            fw.op("pe", tr, reads=[b_omb, b_id], writes=[b_ptr[p]])
            fw.op("act", lambda e: e.copy(out=omT[s][:, :, :], in_=ptr[p][:, :].rearrange("p (k t) -> p k t", k=4)),
                  reads=[b_ptr[p]], writes=[b_omT[s]])
            fw.op("act", lambda e: e.activation(out=gg[s][:, :], in_=gg[s][:, :], func=AF.Sigmoid), reads=[b_gg[s]], writes=[b_gg[s]])
            for cb in range(4):
                q = pq % 2
                pq += 1
                cs = slice(cb * 512, (cb + 1) * 512)

                def mma(e):
                    for k in range(8):
                        ins = e.matmul(pa[q][:, :], lhsT=AT[:, k, tok], rhs=Wa[:, k, cs], start=(k == 0), stop=(k == 7))
                    return ins

                def mmm(e):
                    for k in range(4):
                        ins = e.matmul(pm[q][:, :], lhsT=omT[s][:, k, :], rhs=Wm[:, k, cs], start=(k == 0), stop=(k == 3))
                    return ins

                def mmc(e):
                    for k in range(4):
                        ins = e.matmul(pc[q][:, :], lhsT=CT[:, k, tok], rhs=Wc[:, k, cs], start=(k == 0), stop=(k == 3))
                    return ins
                fw.op("pe", mma, reads=[b_AT, b_Wa], writes=[b_pa[q]])
                fw.op("pe", mmm, reads=[b_omT[s], b_Wm], writes=[b_pm[q]])
                fw.op("pe", mmc, reads=[b_CT, b_Wc], writes=[b_pc[q]])
                fw.op("dve", lambda e: e.tensor_tensor(out=z32[:, :], in0=pa[q][:, :], in1=gg[s][:, cb * 512:(cb + 1) * 512], op=ALU.mult),
                      reads=[b_pa[q], b_gg[s]], writes=[b_z32])
                fw.op("dve", lambda e: e.tensor_tensor(out=t32[:, :], in0=pm[q][:, :], in1=gg[s][:, D + cb * 512:D + (cb + 1) * 512],
                                                       op=ALU.mult), reads=[b_pm[q], b_gg[s]], writes=[b_t32])
                fw.op("pool", lambda e: e.tensor_tensor(out=z32[:, :], in0=z32[:, :], in1=t32[:, :], op=ALU.add),
                      reads=[b_z32, b_t32], writes=[b_z32])
                fw.op("dve", lambda e: e.tensor_tensor(out=t32[:, :], in0=pc[q][:, :], in1=gg[s][:, 2 * D + cb * 512:2 * D + (cb + 1) * 512],
                                                       op=ALU.mult), reads=[b_pc[q], b_gg[s]], writes=[b_t32])
                fw.op("pool", lambda e: e.tensor_tensor(out=zb[s][:, cs], in0=z32[:, :], in1=t32[:, :], op=ALU.add),
                      reads=[b_z32, b_t32], writes=[b_zb[s]])
            for k4 in range(NK // 4):
                p = tp % 2
                tp += 1

                def tr2(e):
                    for kk in range(4):
                        k = k4 * 4 + kk
                        ins = e.transpose(ptr[p][:, kk * 128:(kk + 1) * 128], zb[s][:, k * 128:(k + 1) * 128], identB[:, :])
                    return ins
                fw.op("pe", tr2, reads=[b_zb[s], b_id], writes=[b_ptr[p]])
                fw.op("act", lambda e: e.copy(out=zTs[s][:, k4 * 4:(k4 + 1) * 4, :], in_=ptr[p][:, :].rearrange("p (k t) -> p k t", k=4)),
                      reads=[b_ptr[p]], writes=[b_zTs[s]])
            fw.dma("sp", S.ZT[:, tok].rearrange("(k p) t -> p k t", p=128), zTs[s][:, :, :], reads=[b_zTs[s]])
    with Phase(fw, "mg2") as ph:
        ZTs = ph.sb([128, NK, T_OC], BF16)
        Wo = ph.sb([128, NK, D], BF16)
        b_Z, b_Wo = bufs(2)
        fw.dma("sp", ZTs[:, :, :], S.ZT.rearrange("(k p) t -> p k t", p=128), writes=[b_Z])
        fw.dma("pool", Wo[:, :, :], W["w_out"].rearrange("(k p) c -> p k c", p=128), writes=[b_Wo])
        g1 = [ph.sb([128, D], F32) for _ in range(2)]
        b_g1 = bufs(2)
        for m in range(2):
            load_bc(fw, "sp", g1[m][:, :], S.mods[l][m:m + 1, 2 * D:3 * D], b_g1[m])
        xt = [ph.sb([128, D], F32) for _ in range(2)]
        xn = [ph.sb([128, D], F32) for _ in range(2)]
        b_xt, b_xn = bufs(2), bufs(2)
        py = [ph.ps([128, 512]) for _ in range(4)]
        b_py = bufs(4)
        pq = 0
        for n, (t, prow, xsrc, m, dst) in enumerate(tiles):
            s = n % 2
            tok = slice(t * 128, (t + 1) * 128)
            fw.dma("sp", xt[s][:, :], xsrc, writes=[b_xt[s]])
            for cb in range(4):
                q = pq % 4
                pq += 1
                cs = slice(cb * 512, (cb + 1) * 512)

                def mmo(e):
                    for k in range(NK):
                        ins = e.matmul(py[q][:, :], lhsT=ZTs[:, k, tok], rhs=Wo[:, k, cs], start=(k == 0), stop=(k == NK - 1))
                    return ins
                fw.op("pe", mmo, reads=[b_Z, b_Wo], writes=[b_py[q]])
                fw.op("dve", lambda e: e.tensor_tensor(out=xn[s][:, cs], in0=py[q][:, :], in1=g1[m][:, cs], op=ALU.mult),
                      reads=[b_py[q], b_g1[m]], writes=[b_xn[s]])
                fw.op("pool", lambda e: e.tensor_tensor(out=xn[s][:, cs], in0=xn[s][:, cs], in1=xt[s][:, cs], op=ALU.add),
                      reads=[b_xn[s], b_xt[s]], writes=[b_xn[s]])
            fw.dma("sp", dst, xn[s][:, :], reads=[b_xn[s]])


def phase_moe_router(fw, cfg, l, S, A, W, o_own, o_ctx, need_ctx):
    nc = fw.nc
    tiles = _tiles_oc(cfg, o_own, o_ctx, o_own, o_ctx, need_ctx)
    BIG = 1.0e4
    with Phase(fw, "mr") as ph:
        identB = ph.sb([128, 128], BF16)
        b_id = make_ident(fw, identB)
        gain = [ph.sb([128, D], F32) for _ in range(2)]
        shift = [ph.sb([128, D], F32) for _ in range(2)]
        tmpg = ph.sb([128, D], F32)
        b_gain, b_shift = bufs(2), bufs(2)
        b_tmp = Buf()
        load_bc(fw, "sp", tmpg[:, :], A["norm2_g"][l:l + 1, :], b_tmp)
        for m in range(2):
            load_bc(fw, "sp", shift[m][:, :], S.mods[l][m:m + 1, 3 * D:4 * D], b_shift[m])
            load_bc(fw, "sp", gain[m][:, :], S.mods[l][m:m + 1, 4 * D:5 * D], b_gain[m])
            fw.op("dve", lambda e: e.scalar_tensor_tensor(out=gain[m][:, :], in0=gain[m][:, :], scalar=1.0, in1=tmpg[:, :],
                                                          op0=ALU.add, op1=ALU.mult), reads=[b_gain[m], b_tmp], writes=[b_gain[m]])
        wr = ph.sb([128, NK, 20], F32)
        wrh = ph.sb([128, NK, 20], BF16)
        wrl = ph.sb([128, NK, 20], BF16)
        brb = ph.sb([128, 20], F32)
        b_wr, b_wrh, b_wrl, b_brb, b_brb2 = bufs(5)
        fw.dma("sp", wr[:, :, :], W["w_r"].rearrange("(k p) c -> p k c", p=128), writes=[b_wr])
        load_bc(fw, "sp", brb[:, 0:4], A["b_rg"][l:l + 1, :], b_brb)
        load_bc(fw, "sp", brb[:, 4:20], A["b_re"][l:l + 1, :], b_brb2)
        fw.op("act", lambda e: e.copy(out=wrh[:, :, :], in_=wr[:, :, :]), reads=[b_wr], writes=[b_wrh])
        fw.op("dve", lambda e: e.tensor_tensor(out=wrl[:, :, :], in0=wr[:, :, :], in1=wrh[:, :, :], op=ALU.subtract),
              reads=[b_wr, b_wrh], writes=[b_wrl])
        xt = [ph.sb([128, D], F32) for _ in range(2)]
        b_xt = bufs(2)
        junk = ph.sb([128, D], BF16)
        h32 = ph.sb([128, D], F32)
        hb = [ph.sb([128, D], BF16) for _ in range(2)]
        lb = [ph.sb([128, D], BF16) for _ in range(2)]
        hTt = [ph.sb([128, NK, 128], BF16) for _ in range(2)]
        lTt = [ph.sb([128, NK, 128], BF16) for _ in range(2)]
        ss = [ph.sb([128, 1], F32) for _ in range(2)]
        b_junk, b_h32 = bufs(2)
        b_hb, b_lb, b_hTt, b_lTt, b_ss = bufs(2), bufs(2), bufs(2), bufs(2), bufs(2)
        ptr = [ph.ps([128, 512], BF16) for _ in range(2)]
        b_ptr = bufs(2)
        plg = [ph.ps([128, 512]) for _ in range(2)]
        b_plg = bufs(2)
        NTM = cfg.nt_own + cfg.nt_ctx
        lgall = ph.sb([128, NTM, 20], F32)
        rs = ph.sb([128, 7, NTM], F32)
        r4 = ph.sb([128, 3, NTM, 4], F32)
        r16 = ph.sb([128, 4, NTM, 16], F32)
        b_lg, b_wk = bufs(2)
        tp = 0
        for n, (t, prow, xsrc, m, dst) in enumerate(tiles):
            s = n % 2
            tok = slice(t * 128, (t + 1) * 128)
            fw.dma("sp", xt[s][:, :], xsrc, writes=[b_xt[s]])
            fw.op("pool", lambda e: e.memset(ss[s][:, :], 0.0), writes=[b_ss[s]])
            fw.op("act", lambda e: e.activation(out=junk[:, :], in_=xt[s][:, :], func=AF.Square, accum_out=ss[s][:, 0:1]),
                  reads=[b_xt[s], b_ss[s]], writes=[b_junk, b_ss[s]])
            _rstd(fw, ss[s][:, :], b_ss[s], D)
            fw.op("dve", lambda e: e.scalar_tensor_tensor(out=h32[:, :], in0=xt[s][:, :], scalar=ss[s][:, 0:1], in1=gain[m][:, :],
                                                          op0=ALU.mult, op1=ALU.mult), reads=[b_xt[s], b_ss[s], b_gain[m]], writes=[b_h32])
            fw.op("pool", lambda e: e.tensor_tensor(out=h32[:, :], in0=h32[:, :], in1=shift[m][:, :], op=ALU.add),
                  reads=[b_h32, b_shift[m]], writes=[b_h32])
            fw.op("act", lambda e: e.copy(out=hb[s][:, :], in_=h32[:, :]), reads=[b_h32], writes=[b_hb[s]])
            fw.op("dve", lambda e: e.tensor_tensor(out=lb[s][:, :], in0=h32[:, :], in1=hb[s][:, :], op=ALU.subtract),
                  reads=[b_h32, b_hb[s]], writes=[b_lb[s]])
            for (srcb, b_src, dstT, b_dst) in ((hb[s], b_hb[s], hTt[s], b_hTt[s]), (lb[s], b_lb[s], lTt[s], b_lTt[s])):
                for k4 in range(NK // 4):
                    p = tp % 2
                    tp += 1

                    def tr(e):
                        for kk in range(4):
                            k = k4 * 4 + kk
                            ins = e.transpose(ptr[p][:, kk * 128:(kk + 1) * 128], srcb[:, k * 128:(k + 1) * 128], identB[:, :])
                        return ins
                    fw.op("pe", tr, reads=[b_src, b_id], writes=[b_ptr[p]])
                    fw.op("act", lambda e: e.copy(out=dstT[:, k4 * 4:(k4 + 1) * 4, :], in_=ptr[p][:, :].rearrange("p (k t) -> p k t", k=4)),
                          reads=[b_ptr[p]], writes=[b_dst])
            fw.dma("sp", S.HT2[:, tok].rearrange("(k p) t -> p k t", p=128), hTt[s][:, :, :], reads=[b_hTt[s]])

            def mml(e):
                i = 0
                for (L, R) in ((hTt[s], wrh), (lTt[s], wrh), (hTt[s], wrl)):
                    for k in range(NK):
                        ins = e.matmul(plg[s][:, 0:20], lhsT=L[:, k, :], rhs=R[:, k, :], start=(i == 0), stop=(i == 3 * NK - 1))
                        i += 1
                return ins
            fw.op("pe", mml, reads=[b_hTt[s], b_lTt[s], b_wrh, b_wrl], writes=[b_plg[s]])
            fw.op("dve", lambda e: e.tensor_tensor(out=lgall[:, n, :], in0=plg[s][:, 0:20], in1=brb[:, :], op=ALU.add),
                  reads=[b_plg[s], b_brb, b_brb2], writes=[b_lg])
        T = len(tiles)
        GL, EL = lgall[:, 0:T, 0:4], lgall[:, 0:T, 4:20]
        gmax, gsum, pg, m1, m2, w1, w2 = (rs[:, i, 0:T] for i in range(7))
        oh, ex, pen = r4[:, 0, 0:T, :], r4[:, 1, 0:T, :], r4[:, 2, 0:T, :]
        esel, mk1, mk2, e2 = r16[:, 0, 0:T, :], r16[:, 1, 0:T, :], r16[:, 2, 0:T, :], r16[:, 3, 0:T, :]

        def bc4(v):
            return v.unsqueeze(2).to_broadcast([128, T, 4])

        def bc16(v):
            return v.unsqueeze(2).to_broadcast([128, T, 16])
        seq = [
            ("dve", lambda e: e.tensor_reduce(out=gmax, in_=GL, axis=AX.X, op=ALU.max)),
            ("dve", lambda e: e.tensor_tensor(out=oh, in0=GL, in1=bc4(gmax), op=ALU.is_ge)),
            ("dve", lambda e: e.tensor_tensor(out=ex, in0=GL, in1=bc4(gmax), op=ALU.subtract)),
            ("act", lambda e: e.activation(out=ex, in_=ex, func=AF.Exp)),
            ("dve", lambda e: e.tensor_reduce(out=gsum, in_=ex, axis=AX.X, op=ALU.add)),
            ("dve", lambda e: e.reciprocal(out=pg, in_=gsum)),
            ("dve", lambda e: e.tensor_scalar(out=pen, in0=oh, scalar1=BIG, scalar2=-BIG, op0=ALU.mult, op1=ALU.add)),
            ("dve", lambda e: e.tensor_tensor(out=esel.rearrange("p t (g x) -> p t g x", g=4), in0=EL.rearrange("p t (g x) -> p t g x", g=4),
                                              in1=pen.unsqueeze(3).to_broadcast([128, T, 4, 4]), op=ALU.add)),
            ("dve", lambda e: e.tensor_reduce(out=m1, in_=esel, axis=AX.X, op=ALU.max)),
            ("dve", lambda e: e.tensor_tensor(out=mk1, in0=esel, in1=bc16(m1), op=ALU.is_ge)),
            ("dve", lambda e: e.scalar_tensor_tensor(out=e2, in0=mk1, scalar=-BIG, in1=esel, op0=ALU.mult, op1=ALU.add)),
            ("dve", lambda e: e.tensor_reduce(out=m2, in_=e2, axis=AX.X, op=ALU.max)),
            ("dve", lambda e: e.tensor_tensor(out=mk2, in0=e2, in1=bc16(m2), op=ALU.is_ge)),
            ("dve", lambda e: e.tensor_tensor(out=w2, in0=m2, in1=m1, op=ALU.subtract)),
            ("act", lambda e: e.activation(out=w2, in_=w2, func=AF.Exp)),
            ("dve", lambda e: e.tensor_scalar(out=w1, in0=w2, scalar1=1.0, scalar2=None, op0=ALU.add)),
            ("dve", lambda e: e.reciprocal(out=w1, in_=w1)),
            ("dve", lambda e: e.tensor_tensor(out=w2, in0=w2, in1=w1, op=ALU.mult)),
            ("dve", lambda e: e.tensor_tensor(out=w1, in0=w1, in1=pg, op=ALU.mult)),
            ("dve", lambda e: e.tensor_tensor(out=w2, in0=w2, in1=pg, op=ALU.mult)),
            ("dve", lambda e: e.tensor_tensor(out=mk1, in0=mk1, in1=bc16(w1), op=ALU.mult)),
            ("dve", lambda e: e.tensor_tensor(out=mk2, in0=mk2, in1=bc16(w2), op=ALU.mult)),
            ("dve", lambda e: e.tensor_tensor(out=esel, in0=mk1, in1=mk2, op=ALU.add)),
        ]
        for eng, f in seq:
            fw.op(eng, f, reads=[b_lg, b_wk], writes=[b_wk])
        fw.dma("sp", S.COMB[0:T * 128, :].rearrange("(t p) c -> p t c", p=128), esel, reads=[b_wk])


def phase_moe_experts(fw, cfg, l, S, A, W, o_own, o_ctx, x2_own, x2_ctx, need_ctx, final_out=None):
    nc = fw.nc
    To, Tc = cfg.T_OWN, cfg.T_CTX
    tiles = _tiles_oc(cfg, o_own, o_ctx, x2_own, x2_ctx, need_ctx)
    SB = 6
    nblk = -(-len(tiles) // SB)
    sizes = [len(tiles) // nblk + (1 if i < len(tiles) % nblk else 0) for i in range(nblk)]
    with Phase(fw, "mx") as ph:
        identF = ph.sb([128, 128], F32)
        b_idF = make_ident(fw, identF)
        sel = ph.sb([16, 16, 128], BF16)
        b_sel = Buf()
        fw.op("pool", lambda e: e.memset(sel[:, :, :], 0.0), writes=[b_sel])
        fw.op("pool", lambda e: e.affine_select(out=sel[:, :, :], in_=sel[:, :, :], pattern=[[-1, 16], [0, 128]], compare_op=ALU.not_equal,
                                                fill=1.0, base=0, channel_multiplier=1), reads=[b_sel], writes=[b_sel])
        g2 = [None, None]
        b_g2 = bufs(2)
        for m in range(2 if need_ctx else 1):
            g2[m] = ph.sb([128, D], F32)
            load_bc(fw, "sp", g2[m][:, :], S.mods[l][m:m + 1, 5 * D:6 * D], b_g2[m])
        b_fgb = Buf()
        if final_out is not None:
            fgb = ph.sb([128, D], F32)
            load_bc(fw, "sp", fgb[:, :], A["final_g"][0:1, :], b_fgb)
        acc = ph.sb([128, SB, D], F32)
        hT = ph.sb([128, NK, SB * 128], BF16)
        cm = ph.sb([128, SB, 16], F32)
        cmT = ph.sb([16, SB * 128], BF16)
        cbc = [ph.sb([128, SB * 128], F32) for _ in range(2)]
        Wg = [ph.sb([128, NK, 512], BF16) for _ in range(2)]
        Wu = [ph.sb([128, NK, 512], BF16) for _ in range(2)]
        if SB == 6:
            Wd = [ph.sb([128, 4, D], BF16)] * 2
        else:
            Wd = [ph.sb([128, 4, D], BF16) for _ in range(2)]
        midT = [ph.sb([128, 4, SB * 128], BF16) for _ in range(2)]
        sa = ph.sb([128, 512], F32)
        ss = ph.sb([128, 1], F32)
        b_acc, b_hT, b_cm, b_cmT, b_sa, b_ss = bufs(6)
        b_cbc, b_Wg, b_Wu, b_Wd, b_midT = bufs(2), bufs(2), bufs(2), bufs(2), bufs(2)
        if SB == 6:
            b_Wd = [b_Wd[0]] * 2
        if SB >= 5:
            xt = [ph.sb([128, D], F32)] * 2
            b_xt = [Buf()] * 2
        else:
            xt = [ph.sb([128, D], F32) for _ in range(2)]
            b_xt = bufs(2)
        junk = midT[0][:, :, :].rearrange("p f t -> p (f t)")[:, 0:D]
        b_junk = b_midT[0]
        pa = [ph.ps([128, 512]) for _ in range(2)]
        pu = [ph.ps([128, 512]) for _ in range(2)]
        po = [ph.ps([128, 512]) for _ in range(3)]
        pcb = ph.ps([128, 512])
        b_pa, b_pu, b_po = bufs(2), bufs(2), bufs(3)
        b_pcb = Buf()
        wi = 0
        pi = 0
        oi = 0
        if sizes is None:
            starts = [(i, min(SB, len(tiles) - i)) for i in range(0, len(tiles), SB)]
        else:
            starts, o_ = [], 0
            for z_ in sizes:
                starts.append((o_, z_))
                o_ += z_
        for sb0, sbn in starts:
            tl = tiles[sb0:sb0 + sbn]
            nt = len(tl)
            ntok = nt * 128
            tok0 = tl[0][0] * 128
            fw.dma("sp", hT[:, :, 0:ntok], S.HT2[:, tok0:tok0 + ntok].rearrange("(k p) t -> p k t", p=128), writes=[b_hT])
            fw.dma("sp", cm[:, 0:nt, :], S.COMB[tok0:tok0 + ntok, :].rearrange("(t p) c -> p t c", p=128), writes=[b_cm])
            for i0 in range(0, nt, 4):
                ni = min(4, nt - i0)
                for i in range(ni):
                    fw.op("pe", lambda e: e.transpose(pcb[0:16, i * 128:(i + 1) * 128], cm[:, i0 + i, :], identF[:, :]),
                          reads=[b_cm, b_idF], writes=[b_pcb])
                fw.op("act", lambda e: e.copy(out=cmT[:, i0 * 128:(i0 + ni) * 128], in_=pcb[0:16, 0:ni * 128]),
                      reads=[b_pcb], writes=[b_cmT])
            for ex in range(16):
                w = wi % 2
                wi += 1
                fw.dma("pool", Wg[w][:, :, :], W["w_gate"][ex].rearrange("(k p) f -> p k f", p=128), writes=[b_Wg[w]])
                fw.dma("pool", Wu[w][:, :, :], W["w_up"][ex].rearrange("(k p) f -> p k f", p=128), writes=[b_Wu[w]])
                fw.dma("pool", Wd[w][:, :, :], W["w_down"][ex].rearrange("(k p) c -> p k c", p=128), writes=[b_Wd[w]])
                for tb in range(0, ntok, 512):
                    tw = min(512, ntok - tb)
                    fw.op("pe", lambda e: e.matmul(pcb[:, 0:tw], lhsT=sel[:, ex, :], rhs=cmT[:, tb:tb + tw], start=True, stop=True),
                          reads=[b_sel, b_cmT], writes=[b_pcb])
                    fw.op("act", lambda e: e.copy(out=cbc[w][:, tb:tb + tw], in_=pcb[:, 0:tw]), reads=[b_pcb], writes=[b_cbc[w]])
                for fc in range(4):
                    for tb in range(0, ntok, 512):
                        tw = min(512, ntok - tb)
                        p = pi % 2
                        pi += 1

                        def mg(e):
                            for k in range(NK):
                                ins = e.matmul(pa[p][:, 0:tw], lhsT=Wg[w][:, k, fc * 128:(fc + 1) * 128], rhs=hT[:, k, tb:tb + tw],
                                               start=(k == 0), stop=(k == NK - 1))
                            return ins

                        def mu(e):
                            for k in range(NK):
                                ins = e.matmul(pu[p][:, 0:tw], lhsT=Wu[w][:, k, fc * 128:(fc + 1) * 128], rhs=hT[:, k, tb:tb + tw],
                                               start=(k == 0), stop=(k == NK - 1))
                            return ins
                        fw.op("pe", mg, reads=[b_Wg[w], b_hT], writes=[b_pa[p]])
                        fw.op("pe", mu, reads=[b_Wu[w], b_hT], writes=[b_pu[p]])
                        fw.op("act", lambda e: e.activation(out=sa[:, 0:tw], in_=pa[p][:, 0:tw], func=AF.Silu), reads=[b_pa[p]], writes=[b_sa])
                        fw.op("dve", lambda e: e.tensor_tensor(out=sa[:, 0:tw], in0=sa[:, 0:tw], in1=pu[p][:, 0:tw], op=ALU.mult),
                              reads=[b_sa, b_pu[p]], writes=[b_sa])
                        fw.op("pool", lambda e: e.tensor_tensor(out=midT[w][:, fc, tb:tb + tw], in0=sa[:, 0:tw], in1=cbc[w][:, tb:tb + tw],
                                                                op=ALU.mult), reads=[b_sa, b_cbc[w]], writes=[b_midT[w]])
                for i in range(nt):
                    for dc in range(4):
                        o = oi % 3
                        oi += 1

                        def md(e):
                            for fc in range(4):
                                ins = e.matmul(po[o][:, :], lhsT=midT[w][:, fc, i * 128:(i + 1) * 128], rhs=Wd[w][:, fc, dc * 512:(dc + 1) * 512],
                                               start=(fc == 0), stop=(fc == 3))
                            return ins
                        fw.op("pe", md, reads=[b_midT[w], b_Wd[w]], writes=[b_po[o]])
                        if ex == 0:
                            fw.op("act", lambda e: e.copy(out=acc[:, i, dc * 512:(dc + 1) * 512], in_=po[o][:, :]),
                                  reads=[b_po[o]], writes=[b_acc])
                        else:
                            fw.op("dve", lambda e: e.tensor_tensor(out=acc[:, i, dc * 512:(dc + 1) * 512], in0=acc[:, i, dc * 512:(dc + 1) * 512],
                                                                   in1=po[o][:, :], op=ALU.add), reads=[b_po[o], b_acc], writes=[b_acc])
            for i, (t, prow, xsrc, m, dst) in enumerate(tl):
                s = i % 2
                fw.dma("sp", xt[s][:, :], xsrc, writes=[b_xt[s]])
                fw.op("dve", lambda e: e.tensor_tensor(out=acc[:, i, :], in0=acc[:, i, :], in1=g2[m][:, :], op=ALU.mult),
                      reads=[b_acc, b_g2[m]], writes=[b_acc])
                fw.op("pool", lambda e: e.tensor_tensor(out=xt[s][:, :], in0=xt[s][:, :], in1=acc[:, i, :], op=ALU.add),
                      reads=[b_acc, b_xt[s]], writes=[b_xt[s]])
                if final_out is not None and m == 0:
                    fw.op("pool", lambda e: e.memset(ss[:, :], 0.0), writes=[b_ss])
                    fw.op("act", lambda e: e.activation(out=junk[:, :], in_=xt[s][:, :], func=AF.Square, accum_out=ss[:, 0:1]),
                          reads=[b_xt[s], b_ss], writes=[b_junk, b_ss])
                    _rstd(fw, ss[:, :], b_ss, D)
                    fw.op("dve", lambda e: e.scalar_tensor_tensor(out=xt[s][:, :], in0=xt[s][:, :], scalar=ss[:, 0:1], in1=fgb[:, :],
                                                                  op0=ALU.mult, op1=ALU.mult), reads=[b_xt[s], b_ss, b_fgb], writes=[b_xt[s]])
                    fw.dma("sp", final_out[t * 128:(t + 1) * 128, :], xt[s][:, :], reads=[b_xt[s]])
                else:
                    fw.dma("sp", dst, xt[s][:, :], reads=[b_xt[s]])


WEIGHT_KEYS = ("w_mod", "bmod2", "w_in", "g1row", "w_br_a", "w_br_m", "w_br_c", "w_out", "w_r", "w_gate", "w_up", "w_down")


def layer_weights(inp, l, half):
    w = layer_inputs(inp, l, half)
    for k in ("w_br_a", "w_br_m", "w_br_c", "w_out", "w_gate", "w_up", "w_down"):
        w[k] = np.ascontiguousarray(inp[k][l])
    w["w_r"] = np.ascontiguousarray(np.concatenate([np.asarray(inp["w_rg"][l]), np.asarray(inp["w_re"][l])], axis=1))
    return w


def emit_layer(fw, cfg, S, A, l, last, x_own, x_oth, xc, x2o, x2c, sfx="", mid_hook=None, skip_mods=False, attn_bg=None):
    nc = fw.nc
    need_ctx = not last
    To, Tc = cfg.T_OWN, cfg.T_CTX
    x1o = nc.dram_tensor("x1o" + sfx, [To, D], F32, kind="Internal").ap()
    x1c = nc.dram_tensor("x1c" + sfx, [Tc, D], F32, kind="Internal").ap()
    W = {k: A[k + sfx] for k in WEIGHT_KEYS}
    if not skip_mods:
        phase_mods(fw, cfg, A["cvec"], W["w_mod"], W["bmod2"], S.mods[l])
    if mid_hook is None:
        phase_inproj(fw, cfg, l, x_own, x_oth, xc, S.mods[l], W["g1row"], W["w_in"], S)
    else:
        phase_inproj(fw, cfg, l, x_own, x_oth, xc, S.mods[l], W["g1row"], W["w_in"], S, which=("oc",))
        mid_hook()
        phase_inproj(fw, cfg, l, x_own, x_oth, xc, S.mods[l], W["g1row"], W["w_in"], S, which=("oth",))
    phase_attn(fw, cfg, l, S, A, "a", need_ctx)
    phase_attn(fw, cfg, l, S, A, "c", need_ctx)
    phase_mlstm(fw, cfg, l, S, A, need_ctx, bg=attn_bg)
    phase_merge(fw, cfg, l, S, A, W, x_own, xc, x1o, x1c, need_ctx)
    phase_moe_router(fw, cfg, l, S, A, W, x1o, x1c, need_ctx)
    phase_moe_experts(fw, cfg, l, S, A, W, x1o, x1c, x2o, x2c, need_ctx, final_out=(x2o if last else None))


def phase_exchange(fw, cfg, A, x2o, xoth, part="ab", st=None):
    nc = fw.nc
    To = cfg.T_OWN
    nt = cfg.nt_own
    CH = 2
    nch = nt // CH
    if st is None:
        st = {}
    if "Z" not in st:
        st["Z"] = [nc.dram_tensor("xchgZ%d" % i, [2 * CH * 128, D], F32, kind="Internal").ap() for i in range(nch)]
        st["R"] = [nc.dram_tensor("xchgR%d" % i, [2 * CH * 128, D], F32, kind="Internal").ap() for i in range(nch)]
        st["b_R"] = bufs(nch)
    Z, R, b_R = st["Z"], st["R"], st["b_R"]
    if "a" in part:
        _exchange_a(fw, cfg, A, x2o, Z, R, b_R, CH, nch)
    if "b" in part:
        _exchange_b(fw, cfg, A, xoth, R, b_R, CH)


def _exchange_a(fw, cfg, A, x2o, Z, R, b_R, CH, nch):
    nt = cfg.nt_own
    with Phase(fw, "xa") as ph:
        sv = ph.sb([128, 2], F32)
        b_sv = Buf()
        fw.dma("sp", sv[:, :], A["selv"], writes=[b_sv])
        xt = [ph.sb([128, D], F32) for _ in range(2)]
        z0 = [ph.sb([128, D], F32) for _ in range(2)]
        z1 = [ph.sb([128, D], F32) for _ in range(2)]
        b_xt, b_z0, b_z1 = bufs(2), bufs(2), bufs(2)
        for i in range(nt):
            s = i % 2
            c, t = i // CH, i % CH
            fw.dma("sp", xt[s][:, :], x2o[i * 128:(i + 1) * 128, :], writes=[b_xt[s]])
            fw.op("dve", lambda e: e.tensor_scalar(out=z0[s][:, :], in0=xt[s][:, :], scalar1=sv[:, 0:1], scalar2=None, op0=ALU.mult),
                  reads=[b_xt[s], b_sv], writes=[b_z0[s]])
            fw.op("act", lambda e: e.activation(out=z1[s][:, :], in_=xt[s][:, :], func=AF.Copy, scale=sv[:, 1:2]),
                  reads=[b_xt[s], b_sv], writes=[b_z1[s]])
            fw.dma("sp", Z[c][t * 128:(t + 1) * 128, :], z0[s][:, :], reads=[b_z0[s]])
            fw.dma("sp", Z[c][CH * 128 + t * 128:CH * 128 + (t + 1) * 128, :], z1[s][:, :], reads=[b_z1[s]])
    for c in range(nch):
        fw.all_reduce(Z[c], R[c], [[0, 1], [2, 3], [4, 5], [6, 7]], writes=[b_R[c]])


def _exchange_b(fw, cfg, A, xoth, R, b_R, CH):
    nt = cfg.nt_own
    with Phase(fw, "xb") as ph:
        J = ph.sb([128, 2, 128], F32)
        b_J = Buf()
        fw.dma("sp", J[:, :, :], A["jsel"].rearrange("j r p -> r j p"), writes=[b_J])
        ra = [ph.sb([128, D], F32) for _ in range(2)]
        rb = [ph.sb([128, D], F32) for _ in range(2)]
        ot = [ph.sb([128, D], F32) for _ in range(2)]
        b_ra, b_rb, b_ot = bufs(2), bufs(2), bufs(2)
        pp = [ph.ps([128, 512]) for _ in range(4)]
        b_pp = bufs(4)
        pi = 0
        for i in range(nt):
            s = i % 2
            j = nt - 1 - i
            c, t = j // CH, j % CH
            fw.dma("sp", ra[s][:, :], R[c][t * 128:(t + 1) * 128, :], reads=[b_R[c]], writes=[b_ra[s]])
            fw.dma("sp", rb[s][:, :], R[c][CH * 128 + t * 128:CH * 128 + (t + 1) * 128, :], reads=[b_R[c]], writes=[b_rb[s]])
            for cb in range(4):
                p = pi % 4
                pi += 1
                cs = slice(cb * 512, (cb + 1) * 512)

                def mm(e):
                    e.matmul(pp[p][:, :], lhsT=J[:, 0, :], rhs=ra[s][:, cs], start=True, stop=False)
                    return e.matmul(pp[p][:, :], lhsT=J[:, 1, :], rhs=rb[s][:, cs], start=False, stop=True)
                fw.op("pe", mm, reads=[b_J, b_ra[s], b_rb[s]], writes=[b_pp[p]])
                if cb % 2 == 0:
                    fw.op("act", lambda e: e.copy(out=ot[s][:, cs], in_=pp[p][:, :]), reads=[b_pp[p]], writes=[b_ot[s]])
                else:
                    fw.op("dve", lambda e: e.tensor_copy(out=ot[s][:, cs], in_=pp[p][:, :]), reads=[b_pp[p]], writes=[b_ot[s]])
            fw.dma("sp", xoth[i * 128:(i + 1) * 128, :], ot[s][:, :], reads=[b_ot[s]])


def exchange_consts(half):
    selv = np.zeros((128, 2), np.float32)
    selv[:, half] = 1.0
    Jm = np.zeros((128, 128), np.float32)
    Jm[np.arange(128), 127 - np.arange(128)] = 1.0
    jsel = np.zeros((2, 128, 128), np.float32)
    jsel[1 - half] = Jm
    return {"selv": selv, "jsel": jsel}


def build_fused(cfg, shapes):
    nc = bass.Bass("TRN2", target_bir_lowering=False)
    fw = Fw(nc)
    A = {}
    for name, shp in shapes.items():
        A[name] = nc.dram_tensor(name, list(shp), F32, kind="ExternalInput").ap()
    S = Scratch(nc, cfg)
    To, Tc = cfg.T_OWN, cfg.T_CTX
    xm_o = nc.dram_tensor("xmid_o", [To, D], F32, kind="Internal").ap()
    xm_c = nc.dram_tensor("xmid_c", [Tc, D], F32, kind="Internal").ap()
    xm_oth = nc.dram_tensor("xmid_oth", [To, D], F32, kind="Internal").ap()
    out = nc.dram_tensor("out", [To, D], F32, kind="ExternalOutput").ap()
    dummy_c = nc.dram_tensor("xlast_c", [Tc, D], F32, kind="Internal").ap()
    emit_layer(fw, cfg, S, A, 0, False, A["x_own"], A["x_oth"], A["xc"], xm_o, xm_c, sfx="_0",
               attn_bg=lambda ph: mods_bg(fw, ph, cfg, A["cvec"], A["w_mod_1"], A["bmod2_1"], S.mods[1]))
    xst = {}
    phase_exchange(fw, cfg, A, xm_o, xm_oth, part="a", st=xst)
    emit_layer(fw, cfg, S, A, 1, True, xm_o, xm_oth, xm_c, out, dummy_c, sfx="_1", skip_mods=True,
               mid_hook=lambda: phase_exchange(fw, cfg, A, xm_o, xm_oth, part="b", st=xst))
    fw.barrier()
    return nc


def build_layer(cfg, l, last, shapes):
    nc = bass.Bass("TRN2", target_bir_lowering=False)
    fw = Fw(nc)
    A = {}
    for name, shp in shapes.items():
        A[name] = nc.dram_tensor(name, list(shp), F32, kind="ExternalInput").ap()
    S = Scratch(nc, cfg)
    To, Tc = cfg.T_OWN, cfg.T_CTX
    x2o = nc.dram_tensor("x2o", [To, D], F32, kind="ExternalOutput").ap()
    x2c = nc.dram_tensor("x2c", [Tc, D], F32, kind="ExternalOutput").ap()
    emit_layer(fw, cfg, S, A, l, last, A["x_own"], A["x_oth"], A["xc"], x2o, x2c)
    fw.barrier()
    return nc


def run_fused(inp, cfg, cores):
    in_maps = []
    for (b, half) in cores:
        m = core_inputs(inp, cfg, b, half)
        m["rope"] = rope_table(cfg, half)
        m.update(small_inputs(inp, half))
        m.update(exchange_consts(half))
        for l in range(DEPTH):
            for k, v in layer_weights(inp, l, half).items():
                m[k + "_%d" % l] = v
        in_maps.append(m)
    shapes = {k: v.shape for k, v in in_maps[0].items()}
    nc = build_fused(cfg, shapes)
    res = run_bass_kernel_spmd(nc, in_maps, core_ids=list(range(len(cores))))
    To = cfg.T_OWN
    B = inp["x"].shape[0]
    x_new = np.zeros((B, 2 * To, D), np.float32)
    for (b, half), r in zip(cores, res.results):
        xo = np.asarray(r["out"], np.float32)
        if half == 0:
            x_new[b, :To] = xo
        else:
            x_new[b, To:] = xo[::-1]
    return x_new


def run_layer(inp, cfg, l, last, x_full, ctx_full, cores):
    in_maps = []
    cur = dict(inp)
    cur["x"] = x_full
    cur["ctx"] = ctx_full
    for (b, half) in cores:
        m = core_inputs(cur, cfg, b, half)
        m["rope"] = rope_table(cfg, half)
        m.update(small_inputs(inp, half))
        m.update(layer_weights(inp, l, half))
        in_maps.append(m)
    shapes = {k: v.shape for k, v in in_maps[0].items()}
    nc = build_layer(cfg, l, last, shapes)
    res = run_bass_kernel_spmd(nc, in_maps, core_ids=list(range(len(cores))))
    To = cfg.T_OWN
    B = x_full.shape[0]
    x_new = np.zeros((B, 2 * To, D), np.float32)
    c_new = np.zeros((B, cfg.T_CTX, D), np.float32)
    for (b, half), r in zip(cores, res.results):
        xo = np.asarray(r["x2o"], np.float32)
        if half == 0:
            x_new[b, :To] = xo
            c_new[b] = np.asarray(r["x2c"], np.float32)
        else:
            x_new[b, To:] = xo[::-1]
    return x_new, c_new


def kernel(**inputs):
    inp = {k: np.asarray(v) for k, v in inputs.items()}
    cfg = Cfg(nt_own=16, nt_ctx=2)
    cores = [(b, half) for b in range(4) for half in range(2)]
    return run_fused(inp, cfg, cores).astype(np.float32)
```
